# Optimizing a Trainium2 kernel written in Bass

```python
import jax, jax.numpy as jnp
from jax import lax
import numpy as np

D_MODEL = 4096
BATCH = 2
SEQ = 8192
DEPTH = 2
DEC_BATCH = 1
DEC_SEQ = 8192
PAST_LEN = 128

HEAD_DIM = 128
MIX_HEADS = D_MODEL // (4 * HEAD_DIM)
MIX_WIDTH = MIX_HEADS * HEAD_DIM
D_MIX = 4 * MIX_WIDTH
ATTN_HEADS = MIX_HEADS
ATTN_KV_HEADS = MIX_HEADS // 4
ATTN_BLOCK = 128
ROPE_THETA = 10000.0
GRID_W = 64
HGRN_HEADS = MIX_HEADS
HGRN_DK = HEAD_DIM
HGRN_DV = HEAD_DIM
HGRN_CHUNK = 16
GMLP_GROUPS = MIX_HEADS
GMLP_GROUP_DIM = HEAD_DIM
GMLP_CHUNK = 128
RET_HEADS = MIX_HEADS
RET_DK = HEAD_DIM
RET_DV = HEAD_DIM
RET_CHUNK = 128
D_FF = 5632
N_EXPERTS = 8
TOP_K = 2
D_FF_EXPERT = 1024
N_DENSE = (DEPTH + 1) // 2
N_MOE = DEPTH // 2
DEEPNORM_ALPHA = (2 * DEPTH) ** 0.25
DEEPNORM_BETA = (8 * DEPTH) ** -0.25
NORM_EPS = 1e-6
LN_EPS = 1e-5
IN_SPLIT = (MIX_WIDTH, ATTN_KV_HEADS * HEAD_DIM, ATTN_KV_HEADS * HEAD_DIM,
            MIX_WIDTH, MIX_WIDTH, MIX_WIDTH, MIX_WIDTH, MIX_WIDTH,
            MIX_WIDTH, MIX_WIDTH,
            MIX_WIDTH, MIX_WIDTH, MIX_WIDTH, MIX_WIDTH)
D_IN = sum(IN_SPLIT)

kernel_name = "hybrid_bidir_encoder_hgrn2_gmlp_retention_gqa"


def _layer_norm(x, g, b):
    xf = x.astype(jnp.float32)
    xc = xf - jnp.mean(xf, axis=-1, keepdims=True)
    var = jnp.mean(xc * xc, axis=-1, keepdims=True)
    return (xc * lax.rsqrt(var + LN_EPS) * g + b).astype(x.dtype)


def _rms_norm(x, g):
    xf = x.astype(jnp.float32)
    return xf * lax.rsqrt(jnp.mean(xf * xf, axis=-1, keepdims=True) + NORM_EPS) * g


def _flip(a):
    return a[:, ::-1]


def _axial_rope_tables(n_tokens):
    n_rows = n_tokens // GRID_W
    rows = jnp.repeat(jnp.arange(n_rows, dtype=jnp.float32), GRID_W)
    cols = jnp.tile(jnp.arange(GRID_W, dtype=jnp.float32), n_rows)
    pairs_per_axis = HEAD_DIM // 4
    inv_freq = ROPE_THETA ** (-jnp.arange(pairs_per_axis, dtype=jnp.float32) / pairs_per_axis)
    ang = jnp.concatenate([rows[:, None] * inv_freq, cols[:, None] * inv_freq], axis=-1)
    return jnp.cos(ang), jnp.sin(ang)


def _apply_rope(x, cos, sin):
    xf = x.astype(jnp.float32).reshape(*x.shape[:-1], x.shape[-1] // 2, 2)
    x0, x1 = xf[..., 0], xf[..., 1]
    c = cos[None, :, None, :]
    s = sin[None, :, None, :]
    return jnp.stack([x0 * c - x1 * s, x0 * s + x1 * c], axis=-1).reshape(x.shape)


def _block_attention(q, k, v):
    B, T, H, D = q.shape
    kvh = k.shape[2]
    grp = H // kvh
    qb = q.reshape(B, T // ATTN_BLOCK, ATTN_BLOCK, kvh, grp, D).transpose(1, 0, 2, 3, 4, 5)
    scale = D ** -0.5

    def one_block(q_blk):
        s = jnp.einsum('bqkgd,bskd->bkgqs', q_blk, k) * scale
        p = jax.nn.softmax(s, axis=-1)
        return jnp.einsum('bkgqs,bskd->bqkgd', p, v)

    o = lax.map(one_block, qb)
    return o.transpose(1, 0, 2, 3, 4, 5).reshape(B, T, H * D)


def _hgrn2_gates(z, lb):
    z = z.astype(jnp.float32).reshape(*z.shape[:-1], HGRN_HEADS, HGRN_DK)
    log_f = jnp.logaddexp(jnp.log(lb), jnp.log1p(-lb) + jax.nn.log_sigmoid(z))
    k = (1.0 - lb) * jax.nn.sigmoid(-z)
    return k, log_f


def _hgrn2_scan(q, k, v, log_f):
    B, T, H, DK = q.shape
    DV = v.shape[-1]
    C = HGRN_CHUNK
    n = T // C
    causal = jnp.tril(jnp.ones((C, C), dtype=bool))[None, None, :, :, None]

    def chunks(a):
        return a.reshape(B, n, C, H, a.shape[-1]).transpose(1, 0, 3, 2, 4)

    def step(S, inp):
        qi, ki, vi, gi = inp
        b = jnp.cumsum(gi, axis=2)
        b_end = b[:, :, -1, :]
        rel = jnp.exp(jnp.where(causal, b[:, :, :, None, :] - b[:, :, None, :, :], -jnp.inf))
        scores = jnp.einsum('bhtsk,bhsk->bhts', rel * qi[:, :, :, None, :], ki)
        o = jnp.einsum('bhts,bhsv->bhtv', scores, vi) + jnp.einsum('bhtk,bhkv->bhtv', qi * jnp.exp(b), S)
        S = S * jnp.exp(b_end)[..., None] + jnp.einsum('bhsk,bhsv->bhkv', ki * jnp.exp(b_end[:, :, None, :] - b), vi)
        return S, o

    S0 = jnp.zeros((B, H, DK, DV), jnp.float32)
    _, o = lax.scan(step, S0, (chunks(q), chunks(k), chunks(v), chunks(log_f)))
    return o.transpose(1, 0, 3, 2, 4).reshape(B, T, H, DV)


def _retention_scan(q, k, v, log_gamma, include_diag):
    B, T, H, DK = q.shape
    DV = v.shape[-1]
    C = RET_CHUNK
    n = T // C
    pos = jnp.arange(C, dtype=jnp.float32)
    diff = pos[:, None] - pos[None, :]
    valid = (diff >= 0) if include_diag else (diff > 0)
    decay = jnp.where(valid[None], jnp.exp(diff[None] * log_gamma[:, None, None]), 0.0)
    q_decay = jnp.exp((pos + 1.0)[None, :] * log_gamma[:, None])[None, :, :, None]
    k_decay = jnp.exp((C - 1.0 - pos)[None, :] * log_gamma[:, None])[None, :, :, None]
    chunk_decay = jnp.exp(C * log_gamma)[None, :, None, None]

    def chunks(a):
        return a.reshape(B, n, C, H, a.shape[-1]).transpose(1, 0, 3, 2, 4)

    def step(R, inp):
        qi, ki, vi = inp
        scores = jnp.einsum('bhtk,bhsk->bhts', qi, ki) * decay[None]
        o = jnp.einsum('bhts,bhsv->bhtv', scores, vi) + jnp.einsum('bhtk,bhkv->bhtv', qi * q_decay, R)
        R = R * chunk_decay + jnp.einsum('bhsk,bhsv->bhkv', ki * k_decay, vi)
        return R, o

    R0 = jnp.zeros((B, H, DK, DV), jnp.float32)
    _, o = lax.scan(step, R0, (chunks(q), chunks(k), chunks(v)))
    return o.transpose(1, 0, 3, 2, 4).reshape(B, T, H, DV)


def _token_mixer(x, cos, sin, lower_bound, log_gamma, w_in, q_norm, k_norm, hgrn_norm,
                 v_norm_g, v_norm_b, w_s, b_s, ret_norm, merge_scale, w_o):
    B, T, _ = x.shape
    f32 = jnp.float32
    offsets = np.cumsum(IN_SPLIT)[:-1].tolist()
    (aq, ak, av, hq, hf_fwd, hf_bwd, hi, hg, gu, gv, rq, rk, rv, rg) = jnp.split(x @ w_in, offsets, axis=-1)

    a_q = _apply_rope(_rms_norm(aq.reshape(B, T, ATTN_HEADS, HEAD_DIM), q_norm), cos, sin)
    a_k = _apply_rope(_rms_norm(ak.reshape(B, T, ATTN_KV_HEADS, HEAD_DIM), k_norm), cos, sin)
    a_v = av.astype(f32).reshape(B, T, ATTN_KV_HEADS, HEAD_DIM)
    out_a = _block_attention(a_q, a_k, a_v)

    h_q = jax.nn.silu(hq.astype(f32)).reshape(B, T, HGRN_HEADS, HGRN_DK) * HGRN_DK ** -0.5
    h_i = hi.astype(f32).reshape(B, T, HGRN_HEADS, HGRN_DV)
    lb = lower_bound.reshape(HGRN_HEADS, HGRN_DK)
    k_fw, logf_fw = _hgrn2_gates(hf_fwd, lb)
    k_bw, logf_bw = _hgrn2_gates(hf_bwd, lb)
    o_b = _hgrn2_scan(h_q, k_fw, h_i, logf_fw) + _flip(
        _hgrn2_scan(_flip(h_q), _flip(k_bw), _flip(h_i), _flip(logf_bw)))
    out_b = (_rms_norm(o_b, hgrn_norm.reshape(HGRN_HEADS, HGRN_DV))
             * jax.nn.silu(hg.astype(f32)).reshape(B, T, HGRN_HEADS, HGRN_DV)).reshape(B, T, MIX_WIDTH)

    g_u = jax.nn.gelu(gu.astype(f32), approximate=False)
    g_v = _layer_norm(jax.nn.gelu(gv.astype(f32), approximate=False), v_norm_g, v_norm_b)
    g_v = g_v.reshape(B, T // GMLP_CHUNK, GMLP_CHUNK, GMLP_GROUPS, GMLP_GROUP_DIM)
    g_v = jnp.einsum('gps,bnsgc->bnpgc', w_s.astype(f32), g_v) + b_s.T[None, None, :, :, None]
    out_c = g_u * g_v.reshape(B, T, MIX_WIDTH)

    r_q = _apply_rope(rq.reshape(B, T, RET_HEADS, RET_DK), cos, sin)
    r_k = _apply_rope(rk.reshape(B, T, RET_HEADS, RET_DK), cos, sin) * RET_DK ** -0.5
    r_v = rv.astype(f32).reshape(B, T, RET_HEADS, RET_DV)
    o_d = _retention_scan(r_q, r_k, r_v, log_gamma, True) + _flip(
        _retention_scan(_flip(r_q), _flip(r_k), _flip(r_v), log_gamma, False))
    out_d = (_rms_norm(o_d, ret_norm.reshape(RET_HEADS, RET_DV))
             * jax.nn.silu(rg.astype(f32)).reshape(B, T, RET_HEADS, RET_DV)).reshape(B, T, MIX_WIDTH)

    merged = jnp.concatenate([out_a, out_b, out_c, out_d], axis=-1) * merge_scale
    return merged.astype(x.dtype) @ w_o


def _swiglu(x, w_gate, w_up, w_down):
    return (jax.nn.silu(x @ w_gate) * (x @ w_up)) @ w_down


def _moe(x, w_router, w_gate, w_up, w_down):
    B, T, D = x.shape
    xf = x.reshape(B * T, D)
    logits = (xf @ w_router).astype(jnp.float32)
    top_v, top_i = lax.top_k(logits, TOP_K)
    top_w = jax.nn.softmax(top_v, axis=-1)
    gates = jnp.sum(jax.nn.one_hot(top_i, N_EXPERTS, dtype=jnp.float32) * top_w[..., None], axis=1)
    h = jax.nn.silu(jnp.einsum('nd,edf->nef', xf, w_gate)) * jnp.einsum('nd,edf->nef', xf, w_up)
    h = h * gates[:, :, None].astype(h.dtype)
    out = jnp.einsum('nef,efd->nd', h, w_down)
    return out.reshape(B, T, D)


def setup_inputs(seed: int = 0) -> dict:
    key = jax.random.key(seed)
    ks = jax.random.split(key, 32)
    f32 = jnp.float32

    def nrm(k, shape, scale):
        return jax.random.normal(k, shape, f32) * scale

    return {
        "x_prompt": nrm(ks[0], (BATCH, SEQ, D_MODEL), 1.0),
        "x_sample": nrm(ks[1], (DEC_BATCH, DEC_SEQ, D_MODEL), 1.0),
        "w_in": nrm(ks[2], (DEPTH, D_MODEL, D_IN), D_MODEL ** -0.5),
        "attn_q_norm": 1.0 + nrm(ks[3], (DEPTH, HEAD_DIM), 0.02),
        "attn_k_norm": 1.0 + nrm(ks[4], (DEPTH, HEAD_DIM), 0.02),
        "hgrn_lower_bound": nrm(ks[5], (DEPTH, MIX_WIDTH), 0.5),
        "hgrn_out_norm": 1.0 + nrm(ks[6], (DEPTH, MIX_WIDTH), 0.02),
        "gmlp_v_norm_g": 1.0 + nrm(ks[7], (DEPTH, MIX_WIDTH), 0.02),
        "gmlp_v_norm_b": nrm(ks[8], (DEPTH, MIX_WIDTH), 0.02),
        "gmlp_w_s": nrm(ks[9], (DEPTH, GMLP_GROUPS, GMLP_CHUNK, GMLP_CHUNK), GMLP_CHUNK ** -0.5),
        "gmlp_b_s": 1.0 + nrm(ks[10], (DEPTH, GMLP_GROUPS, GMLP_CHUNK), 0.02),
        "ret_out_norm": 1.0 + nrm(ks[11], (DEPTH, MIX_WIDTH), 0.02),
        "merge_scale": 1.0 + nrm(ks[12], (DEPTH, D_MIX), 0.02),
        "w_o": nrm(ks[13], (DEPTH, D_MIX, D_MODEL), D_MIX ** -0.5 * DEEPNORM_BETA),
        "ln1_g": 1.0 + nrm(ks[14], (DEPTH, D_MODEL), 0.02),
        "ln1_b": nrm(ks[15], (DEPTH, D_MODEL), 0.02),
        "ffn_w_gate": nrm(ks[16], (N_DENSE, D_MODEL, D_FF), D_MODEL ** -0.5),
        "ffn_w_up": nrm(ks[17], (N_DENSE, D_MODEL, D_FF), D_MODEL ** -0.5),
        "ffn_w_down": nrm(ks[18], (N_DENSE, D_FF, D_MODEL), D_FF ** -0.5 * DEEPNORM_BETA),
        "moe_router": nrm(ks[19], (N_MOE, D_MODEL, N_EXPERTS), D_MODEL ** -0.5),
        "moe_w_gate": nrm(ks[20], (N_MOE, N_EXPERTS, D_MODEL, D_FF_EXPERT), D_MODEL ** -0.5),
        "moe_w_up": nrm(ks[21], (N_MOE, N_EXPERTS, D_MODEL, D_FF_EXPERT), D_MODEL ** -0.5),
        "moe_w_down": nrm(ks[22], (N_MOE, N_EXPERTS, D_FF_EXPERT, D_MODEL), D_FF_EXPERT ** -0.5 * DEEPNORM_BETA),
        "ln2_g": 1.0 + nrm(ks[23], (DEPTH, D_MODEL), 0.02),
        "ln2_b": nrm(ks[24], (DEPTH, D_MODEL), 0.02),
    }


def reference(x_prompt, x_sample, w_in, attn_q_norm, attn_k_norm, hgrn_lower_bound, hgrn_out_norm,
              gmlp_v_norm_g, gmlp_v_norm_b, gmlp_w_s, gmlp_b_s, ret_out_norm, merge_scale, w_o,
              ln1_g, ln1_b, ffn_w_gate, ffn_w_up, ffn_w_down, moe_router, moe_w_gate, moe_w_up,
              moe_w_down, ln2_g, ln2_b):
    lb_all = jnp.cumsum(jax.nn.softmax(hgrn_lower_bound.astype(jnp.float32), axis=0), axis=0)
    lb_all = lb_all - lb_all[0]
    log_gamma = jnp.log1p(-jnp.exp2(-5.0 - jnp.arange(RET_HEADS, dtype=jnp.float32)))

    def trunk(x):
        cos, sin = _axial_rope_tables(x.shape[1])
        for l in range(DEPTH):
            h = _token_mixer(x, cos, sin, lb_all[l], log_gamma, w_in[l], attn_q_norm[l], attn_k_norm[l],
                             hgrn_out_norm[l], gmlp_v_norm_g[l], gmlp_v_norm_b[l], gmlp_w_s[l], gmlp_b_s[l],
                             ret_out_norm[l], merge_scale[l], w_o[l])
            x = _layer_norm(DEEPNORM_ALPHA * x + h, ln1_g[l], ln1_b[l])
            if l % 2 == 0:
                f = _swiglu(x, ffn_w_gate[l // 2], ffn_w_up[l // 2], ffn_w_down[l // 2])
            else:
                f = _moe(x, moe_router[l // 2], moe_w_gate[l // 2], moe_w_up[l // 2], moe_w_down[l // 2])
            x = _layer_norm(DEEPNORM_ALPHA * x + f, ln2_g[l], ln2_b[l])
        return x

    y_prompt = trunk(x_prompt)
    y_sample = trunk(x_sample)
    return (y_prompt, y_sample)
```

```python
import contextlib
import numpy as np
import concourse.bass as bass
import concourse.mybir as mybir
from concourse.bass_utils import run_bass_kernel_spmd

F32 = mybir.dt.float32
BF16 = mybir.dt.bfloat16
AF = mybir.ActivationFunctionType
ALU = mybir.AluOpType
AX = mybir.AxisListType

D = 4096
DIN = 12800
DFF = 5632
NE = 8
DFE = 1024
HD = 128
OFF = dict(aq=0, ak=1024, av=1280, hq=1536, hff=2560, hfb=3584, hi=4608, hg=5632,
           gu=6656, gv=7680, rq=8704, rk=9728, rv=10752, rg=11776)
ALPHA = 4.0 ** 0.25
LN_EPS = 1e-5
NORM_EPS = 1e-6
NCORES = 3


class Tk:
    __slots__ = ("w", "r")

    def __init__(self):
        self.w = None
        self.r = {}


class Sched:
    NDS = 16

    def __init__(self, nc, es):
        self.nc = nc
        self.eng = {"pe": nc.tensor, "act": nc.scalar, "dve": nc.vector, "pool": nc.gpsimd, "sp": nc.sync}
        self.sem = {k: es.enter_context(nc.semaphore("s_" + k)) for k in self.eng}
        self.cnt = {k: 0 for k in self.eng}
        self.seen = {k: {} for k in self.eng}
        self.dsem = {}
        self.dval = {}
        self.dnext = {}
        for q in ("sp", "pool", "act"):
            self.dsem[q] = [es.enter_context(nc.semaphore("d_%s%d" % (q, i))) for i in range(self.NDS)]
            self.dval[q] = [0] * self.NDS
            self.dnext[q] = 0
        self.ninst = 0

    def _semobj(self, k):
        if isinstance(k, tuple):
            return self.dsem[k[0]][k[1]]
        return self.sem[k]

    def _wait(self, e, deps):
        eng = self.eng[e]
        seen = self.seen[e]
        for k, v in deps.items():
            if k == e and e in ("pe", "sp"):
                continue
            if seen.get(k, 0) >= v:
                continue
            eng.wait_ge(self._semobj(k), v)
            seen[k] = v

    @staticmethod
    def _deps(reads, writes):
        deps = {}
        for t in reads:
            if t.w is not None:
                k, v = t.w
                if deps.get(k, 0) < v:
                    deps[k] = v
        for t in writes:
            if t.w is not None:
                k, v = t.w
                if deps.get(k, 0) < v:
                    deps[k] = v
            for k, v in t.r.items():
                if deps.get(k, 0) < v:
                    deps[k] = v
        return deps

    @staticmethod
    def _mark(tok, reads, writes):
        k, v = tok
        for t in reads:
            t.r[k] = v
        for t in writes:
            t.w = tok
            t.r = {}

    def op(self, e, fn, reads=(), writes=()):
        self._wait(e, self._deps(reads, writes))
        ins = fn(self.eng[e])
        self.cnt[e] += 1
        ins.then_inc(self.sem[e], 1)
        self._mark((e, self.cnt[e]), reads, writes)
        self.ninst += 1
        return ins

    def dma(self, q, out, in_, reads=(), writes=()):
        deps = self._deps(reads, writes)
        i = self.dnext[q]
        self.dnext[q] = (i + 1) % self.NDS
        key = (q, i)
        if self.dval[q][i] > 0 and deps.get(key, 0) < self.dval[q][i]:
            deps[key] = self.dval[q][i]
        self._wait(q, deps)
        ins = self.eng[q].dma_start(out=out, in_=in_)
        self.dval[q][i] += 16
        ins.then_inc(self.dsem[q][i], 16)
        self._mark((key, self.dval[q][i]), reads, writes)
        self.ninst += 1
        return ins

    def barrier(self):
        deps = {k: self.cnt[k] for k in self.eng if self.cnt[k] > 0}
        for q in self.dsem:
            for i in range(self.NDS):
                if self.dval[q][i] > 0:
                    deps[(q, i)] = self.dval[q][i]
        for e in self.eng:
            self._wait(e, deps)


class Prog:
    def __init__(self, T, dbg=(), layers=2, phases=None):
        self.T = T
        self.dbg = set(dbg)
        self.layers = layers
        self.phases = phases
        self.nc = nc = bass.Bass("TRN2", target_bir_lowering=False)
        self.es = es = contextlib.ExitStack()
        self.S = Sched(nc, es)
        self.rr = 0
        self.build()
        es.close()

    def din(self, name, shape, dt=F32):
        return self.nc.dram_tensor(name, list(shape), dt, kind="ExternalInput").ap()

    def dscr(self, name, shape, dt, out=False):
        kind = "ExternalOutput" if (out or name in self.dbg) else "Internal"
        return self.nc.dram_tensor(name, list(shape), dt, kind=kind).ap()

    def sb(self, st, name, shape, dt):
        self.uid = getattr(self, "uid", 0) + 1
        return st.enter_context(self.nc.sbuf_tensor("%s_%d" % (name, self.uid), list(shape), dt))

    def any2(self):
        self.rr += 1
        return ("act", "dve")[self.rr % 2]

    def any3(self):
        self.rr += 1
        return ("act", "dve", "pool")[self.rr % 3]

    @staticmethod
    def copy(e, eng, out, in_):
        if e == "act":
            return eng.activation(out=out, in_=in_, func=AF.Copy)
        return eng.tensor_copy(out=out, in_=in_)

    def convert(self, src, R, C, dst_fn, piece=2048):
        S = self.S
        with contextlib.ExitStack() as st:
            NB = 3
            stg = [self.sb(st, "cv_f%d" % i, [128, piece], F32) for i in range(NB)]
            stb = [self.sb(st, "cv_b%d" % i, [128, piece], BF16) for i in range(NB)]
            tf = [Tk() for _ in range(NB)]
            tb = [Tk() for _ in range(NB)]
            i = 0
            for rt in range(R // 128):
                for c0 in range(0, C, piece):
                    n = min(piece, C - c0)
                    b = i % NB
                    i += 1
                    S.dma("sp", stg[b][:, :n], src[rt * 128:(rt + 1) * 128, c0:c0 + n], writes=[tf[b]])
                    e = self.any3()
                    S.op(e, lambda eng, b=b, n=n, e=e: self.copy(e, eng, stb[b][:, :n], stg[b][:, :n]),
                         reads=[tf[b]], writes=[tb[b]])
                    dst, view = dst_fn(rt, c0, n)
                    S.dma("pool", dst, view(stb[b][:, :n]), reads=[tb[b]], writes=[Tk()])
        S.barrier()

    def conv_blocked(self, src, K, N, Wb, CB, off=0, w=None, cb_base=0):
        w = w or CB

        def dst_fn(rt, c0, n):
            nb = n // w
            cb0 = cb_base + c0 // w
            dst = Wb[cb0:cb0 + nb, :, rt, off:off + w].rearrange("cb p c -> p cb c")
            return dst, (lambda v: v.rearrange("p (cb c) -> p cb c", c=w))
        piece = 2048 if N % 2048 == 0 or N > 2048 else N
        self.convert(src, K, N, dst_fn, piece=piece)

    def conv_plain(self, src, R, C, dst):
        def dst_fn(rt, c0, n):
            return dst[rt * 128:(rt + 1) * 128, c0:c0 + n], (lambda v: v)
        self.convert(src, R, C, dst_fn, piece=min(2048, C))

    def gemm(self, actT, KC, Wb, NCB, CB, form, epi, TT, banks, name):
        S = self.S
        T = self.T
        TT = min(TT, T)
        TS = min(512, TT)
        nb = len(banks)
        bi = 0
        with contextlib.ExitStack() as st:
            act = self.sb(st, name + "_act", [128, KC, TT], BF16)
            G = 8
            ngr = (KC + G - 1) // G
            t_act = [Tk() for _ in range(ngr)]
            wbuf = [self.sb(st, name + "_w%d" % i, [128, KC, CB], BF16) for i in range(2)]
            t_w = [Tk(), Tk()]
            aT = actT.rearrange("(kc p) t -> p kc t", p=128)
            wi = 0
            for st_i in range(T // TT):
                for g in range(ngr):
                    k0, k1 = g * G, min(KC, (g + 1) * G)
                    S.dma("sp", act[:, k0:k1, :], aT[:, k0:k1, st_i * TT:(st_i + 1) * TT], writes=[t_act[g]])
                for cb in range(NCB):
                    wb = wi % 2
                    wi += 1
                    S.dma("sp", wbuf[wb][:], Wb[cb], writes=[t_w[wb]])
                    if form == "A":
                        for ts in range(TT // 128):
                            bank, tb = banks[bi % nb]
                            bi += 1
                            for kc in range(KC):
                                S.op("pe", lambda e, kc=kc, ts=ts, wb=wb, bank=bank: e.matmul(
                                    bank[:, :CB], act[:, kc, ts * 128:(ts + 1) * 128], wbuf[wb][:, kc, :],
                                    start=(kc == 0), stop=(kc == KC - 1)),
                                    reads=[t_act[kc // G], t_w[wb]], writes=[tb])
                            epi((bank, tb), st_i * TT + ts * 128, cb)
                    else:
                        for ts in range(TT // TS):
                            subs = []
                            for sub in range(CB // 128):
                                bank, tb = banks[bi % nb]
                                bi += 1
                                for kc in range(KC):
                                    S.op("pe", lambda e, kc=kc, ts=ts, wb=wb, bank=bank, sub=sub: e.matmul(
                                        bank[:, :TS], wbuf[wb][:, kc, sub * 128:(sub + 1) * 128],
                                        act[:, kc, ts * TS:(ts + 1) * TS],
                                        start=(kc == 0), stop=(kc == KC - 1)),
                                        reads=[t_act[kc // G], t_w[wb]], writes=[tb])
                                subs.append((bank, tb))
                            epi(subs, st_i * TT + ts * TS, cb)
        S.barrier()


    def build(self):
        nc, S, T = self.nc, self.S, self.T
        es = self.es
        L = self.layers
        ph = self.phases
        NPP, NRP = 400, 4352
        self.xT = self.din("xT", [D, T])
        self.w_in = self.din("w_in", [2, D, DIN])
        self.w_o = self.din("w_o", [2, D, D])
        self.ffn_g = self.din("ffn_g", [D, DFF])
        self.ffn_u = self.din("ffn_u", [D, DFF])
        self.ffn_d = self.din("ffn_d", [DFF, D])
        self.moe_g = self.din("moe_g", [NE, D, DFE])
        self.moe_u = self.din("moe_u", [NE, D, DFE])
        self.moe_d = self.din("moe_d", [NE * DFE, D])
        self.router = self.din("router", [128, 32, NE])
        self.pp_in = self.din("pp", [128, NPP])
        self.rp_in = self.din("rp", [2, 128, NRP])
        self.wsT_in = self.din("wsT", [2, 128, 8, 128])
        self.bsT_in = self.din("bsT", [2, 128, 8])
        self.cos_in = self.din("cos16", [T, 1024])
        self.sin_in = self.din("sin16", [T, 1024])
        self.dsym_in = self.din("dsymT", [128, 8, 128])
        self.rdec_in = self.din("rdec", [128, 32])
        self.mbd_in = self.din("maskBD", [128, 2, 128])
        self.sel_in = self.din("sel", [8, 8, 128])
        self.yT = self.dscr("yT", [D, T], F32, out=True)
        self.xTb = self.dscr("xTb", [D, T], BF16)
        self.winA = [self.dscr("winA%d" % l, [17, 128, 32, 512], BF16) for l in range(2)]
        self.winB = [self.dscr("winB%d" % l, [16, 128, 32, 256], BF16) for l in range(2)]
        self.wob = [self.dscr("wob%d" % l, [16, 128, 32, 256], BF16) for l in range(2)]
        self.wgub = self.dscr("wgub", [44, 128, 32, 256], BF16)
        self.wdb = self.dscr("wdb", [16, 128, 44, 256], BF16)
        self.wmgub = self.dscr("wmgub", [64, 128, 32, 256], BF16)
        self.wmdb = self.dscr("wmdb", [16, 128, 64, 256], BF16)
        self.Y_a = self.dscr("Y_a", [T, 1536], F32)
        self.Y_hi = self.dscr("Y_hi", [T, 1024], F32)
        self.Y_g = self.dscr("Y_g", [T, 2048], F32)
        self.Y_r = self.dscr("Y_r", [T, 4096], F32)
        self.YT = self.dscr("YT", [D, T], F32)
        self.aqkT = self.dscr("aqkT", [10, 128, T], BF16)
        self.rqkT = self.dscr("rqkT", [16, 128, T], BF16)
        self.RK = self.dscr("RK", [3, T, 1024], BF16)
        self.mergedT = self.dscr("mergedT", [D, T], BF16)
        self.zT = self.dscr("zT", [D, T], F32)
        self.x1T = self.dscr("x1T", [D, T], F32)
        self.x1Tb = self.dscr("x1Tb", [D, T], BF16)
        self.x2T = self.dscr("x2T", [D, T], F32)
        self.x2Tb = self.dscr("x2Tb", [D, T], BF16)
        self.hT = self.dscr("hT", [NE * DFE, T], BF16)
        self.Grep = self.dscr("Grep", [NE, 128, T], F32)
        self.banks = []
        for i in range(6):
            p = es.enter_context(nc.psum_tensor("ps%d" % i, [128, 512], F32))
            self.banks.append((p, Tk()))
        self.bbanks = []
        for i in range(2):
            p = es.enter_context(nc.psum_tensor("pb%d" % i, [128, 1024], BF16))
            self.bbanks.append((p, Tk()))
        self.pp = self.sb(es, "pp", [128, NPP], F32)
        self.t_pp = Tk()
        self.idf = self.sb(es, "idf", [128, 128], F32)
        self.idb = self.sb(es, "idb", [128, 128], BF16)
        self.onesb = self.sb(es, "onesb", [128, 128], BF16)
        self.epsln = self.sb(es, "epsln", [128, 2], F32)
        self.t_c = Tk()
        S.dma("sp", self.pp[:], self.pp_in, writes=[self.t_pp])
        S.op("pool", lambda e: e.memset(self.idf[:], 0.0), writes=[self.t_c])
        S.op("pool", lambda e: e.affine_select(out=self.idf[:], in_=self.idf[:], pattern=[[-1, 128]],
                                               compare_op=ALU.not_equal, fill=1.0, base=0, channel_multiplier=1),
             writes=[self.t_c])
        S.op("pool", lambda e: e.tensor_copy(out=self.idb[:], in_=self.idf[:]), writes=[self.t_c])
        S.op("pool", lambda e: e.memset(self.onesb[:], 1.0), writes=[self.t_c])
        S.op("pool", lambda e: e.memset(self.epsln[:, 0:1], LN_EPS), writes=[self.t_c])
        S.op("pool", lambda e: e.memset(self.epsln[:, 1:2], NORM_EPS), writes=[self.t_c])
        S.barrier()

        def on(p):
            return ph is None or p in ph
        if on("conv"):
            self.conv_plain(self.xT, D, T, self.xTb)
            for l in range(L):
                w = self.w_in[l]
                self.conv_blocked(w[:, 0:1536], D, 1536, self.winA[l], 512, cb_base=0)
                self.conv_blocked(w[:, 4608:5632], D, 1024, self.winA[l], 512, cb_base=3)
                self.conv_blocked(w[:, 6656:12800], D, 6144, self.winA[l], 512, cb_base=5)
                self.conv_blocked(w[:, 1536:4608], D, 3072, self.winB[l], 256, cb_base=0)
                self.conv_blocked(w[:, 5632:6656], D, 1024, self.winB[l], 256, cb_base=12)
                self.conv_blocked(self.w_o[l], D, D, self.wob[l], 256)
            if on("ffn"):
                self.conv_blocked(self.ffn_g, D, DFF, self.wgub, 256, off=0, w=128)
                self.conv_blocked(self.ffn_u, D, DFF, self.wgub, 256, off=128, w=128)
                self.conv_blocked(self.ffn_d, DFF, D, self.wdb, 256)
            if L > 1 and on("moe"):
                for e_ in range(NE):
                    self.conv_blocked(self.moe_g[e_], D, DFE, self.wmgub, 256, off=0, w=128, cb_base=e_ * 8)
                    self.conv_blocked(self.moe_u[e_], D, DFE, self.wmgub, 256, off=128, w=128, cb_base=e_ * 8)
                self.conv_blocked(self.moe_d, NE * DFE, D, self.wmdb, 256)
        xF, xB = self.xT, self.xTb
        for l in range(L):
            if on("inproj"):
                self.in_proj(l, xB)
            if on("gmlp"):
                self.mix_gmlp(l)
            if on("attn"):
                self.mix_attn(l)
            if on("ret"):
                self.mix_ret(l)
            if on("hgrn"):
                self.mix_hgrn(l)
            if on("wo"):
                self.resid_gemm(self.mergedT, 32, self.wob[l], xF, 1024, "wo")
                self.ln_pass(self.zT, l * 200 + 0, l * 200 + 32, self.x1T, self.x1Tb)
            if l == 0:
                if on("ffn"):
                    self.ffn_up(self.x1Tb, self.wgub, DFF // 128, None)
                    self.resid_gemm(self.hT, DFF // 128, self.wdb, self.x1T, 1024, "fd")
            else:
                if on("moe"):
                    self.router_pass(self.x1T)
                    self.ffn_up(self.x1Tb, self.wmgub, NE * DFE // 128, self.Grep)
                    self.resid_gemm(self.hT, NE * DFE // 128, self.wmdb, self.x1T, 512, "md")
            if on("ffn") or on("moe"):
                last = (l == L - 1)
                self.ln_pass(self.zT, l * 200 + 64, l * 200 + 96, self.yT if last else self.x2T,
                             None if last else self.x2Tb)
            xF, xB = self.x2T, self.x2Tb
        S.barrier()

    def in_proj(self, l, actT):
        S = self.S
        colA = ([(self.Y_a, 512 * i) for i in range(3)] + [(self.Y_hi, 512 * i) for i in range(2)]
                + [(self.Y_g, 512 * i) for i in range(4)] + [(self.Y_r, 512 * i) for i in range(8)])
        with contextlib.ExitStack() as st:
            NSTG = 4
            stg = [self.sb(st, "p1_s%d" % i, [128, 512], F32) for i in range(NSTG)]
            ts = [Tk() for _ in range(NSTG)]
            cnt = [0]

            def epi(bt, tok0, cb):
                bank, tb = bt
                i = cnt[0] % NSTG
                cnt[0] += 1
                e = self.any2()
                S.op(e, lambda eng: self.copy(e, eng, stg[i][:], bank[:]), reads=[tb], writes=[ts[i]])
                yt_, yc_ = colA[cb]
                S.dma("pool", yt_[tok0:tok0 + 128, yc_:yc_ + 512], stg[i][:], reads=[ts[i]], writes=[Tk()])
            self.gemm(actT, 32, self.winA[l], 17, 512, "A", epi, 1024, self.banks[:4], "p1a")
        with contextlib.ExitStack() as st:
            NSTG = 4
            TS = min(512, self.T)
            stg = [self.sb(st, "p1b_s%d" % i, [128, TS], F32) for i in range(NSTG)]
            ts = [Tk() for _ in range(NSTG)]
            cnt = [0]

            def epi(subs, tok0, cb):
                for sub, (bank, tb) in enumerate(subs):
                    i = cnt[0] % NSTG
                    cnt[0] += 1
                    e = self.any2()
                    S.op(e, lambda eng, i=i, bank=bank, e=e: self.copy(e, eng, stg[i][:], bank[:, :TS]), reads=[tb], writes=[ts[i]])
                    r0 = (cb * 2 + sub) * 128
                    S.dma("pool", self.YT[r0:r0 + 128, tok0:tok0 + TS], stg[i][:], reads=[ts[i]], writes=[Tk()])
            self.gemm(actT, 32, self.winB[l], 16, 256, "B", epi, 1024, self.banks[:4], "p1b")

    def resid_gemm(self, actT, KC, Wb, xresT, TT, name):
        S = self.S
        TS = min(512, self.T)
        with contextlib.ExitStack() as st:
            NSTG = 3
            xr = [self.sb(st, name + "_x%d" % i, [128, TS], F32) for i in range(NSTG)]
            zt = [self.sb(st, name + "_z%d" % i, [128, TS], F32) for i in range(NSTG)]
            tx = [Tk() for _ in range(NSTG)]
            tz = [Tk() for _ in range(NSTG)]
            cnt = [0]

            def epi(subs, tok0, cb):
                for sub, (bank, tb) in enumerate(subs):
                    i = cnt[0] % NSTG
                    cnt[0] += 1
                    r0 = (cb * 2 + sub) * 128
                    S.dma("sp", xr[i][:], xresT[r0:r0 + 128, tok0:tok0 + TS], writes=[tx[i]])
                    S.op("dve", lambda e, i=i, bank=bank: e.scalar_tensor_tensor(
                        out=zt[i][:], in0=xr[i][:], scalar=ALPHA, in1=bank[:, :TS], op0=ALU.mult, op1=ALU.add),
                        reads=[tx[i], tb], writes=[tz[i]])
                    S.dma("pool", self.zT[r0:r0 + 128, tok0:tok0 + TS], zt[i][:], reads=[tz[i]], writes=[Tk()])
            self.gemm(actT, KC, Wb, 16, 256, "B", epi, TT, self.banks[:4], name)

    def ffn_up(self, actT, Wb, NCB, grep):
        S = self.S
        TS = min(512, self.T)
        with contextlib.ExitStack() as st:
            NSTG = 3
            sg = [self.sb(st, "fu_s%d" % i, [128, TS], F32) for i in range(NSTG)]
            h1 = [self.sb(st, "fu_h%d" % i, [128, TS], F32) for i in range(NSTG)]
            gr = [self.sb(st, "fu_g%d" % i, [128, TS], F32) for i in range(NSTG)]
            hb = [self.sb(st, "fu_b%d" % i, [128, TS], BF16) for i in range(NSTG)]
            tsg = [Tk() for _ in range(NSTG)]
            th1 = [Tk() for _ in range(NSTG)]
            tgr = [Tk() for _ in range(NSTG)]
            thb = [Tk() for _ in range(NSTG)]
            cnt = [0]

            def epi(subs, tok0, cb):
                (bg, tg), (bu, tu) = subs
                i = cnt[0] % NSTG
                cnt[0] += 1
                S.op("act", lambda e: e.activation(out=sg[i][:], in_=bg[:, :TS], func=AF.Silu), reads=[tg], writes=[tsg[i]])
                if grep is None:
                    S.op("dve", lambda e: e.tensor_tensor(out=hb[i][:], in0=sg[i][:], in1=bu[:, :TS], op=ALU.mult),
                         reads=[tsg[i], tu], writes=[thb[i]])
                else:
                    S.dma("sp", gr[i][:], grep[cb // 8, :, tok0:tok0 + TS], writes=[tgr[i]])
                    S.op("dve", lambda e: e.tensor_tensor(out=h1[i][:], in0=sg[i][:], in1=bu[:, :TS], op=ALU.mult),
                         reads=[tsg[i], tu], writes=[th1[i]])
                    S.op("pool", lambda e: e.tensor_tensor(out=hb[i][:], in0=h1[i][:], in1=gr[i][:], op=ALU.mult),
                         reads=[th1[i], tgr[i]], writes=[thb[i]])
                S.dma("pool", self.hT[cb * 128:(cb + 1) * 128, tok0:tok0 + TS], hb[i][:], reads=[thb[i]], writes=[Tk()])
            self.gemm(actT, 32, Wb, NCB, 256, "B", epi, 1024, self.banks[:4], "fu")

    def ln_pass(self, zT, gcol, bcol, outF, outB):
        S = self.S
        T = self.T
        TS = min(512, T)
        bs_, bq_ = self.banks[4], self.banks[5]
        zTr = zT.rearrange("(c p) t -> p c t", p=128)
        with contextlib.ExitStack() as st:
            z = [self.sb(st, "ln_z%d" % i, [128, 32, TS], F32) for i in range(2)]
            tz = [[Tk() for _ in range(4)] for _ in range(2)]
            NR = 3
            zb = [self.sb(st, "ln_zb%d" % i, [128, TS], BF16) for i in range(NR)]
            zq = [self.sb(st, "ln_zq%d" % i, [128, TS], BF16) for i in range(NR)]
            tzb = [Tk() for _ in range(NR)]
            tzq = [Tk() for _ in range(NR)]
            mean = self.sb(st, "ln_mean", [128, TS], F32)
            msq = self.sb(st, "ln_msq", [128, TS], F32)
            rstd = self.sb(st, "ln_rstd", [128, TS], F32)
            tst = Tk()
            t1 = [self.sb(st, "ln_t%d" % i, [128, TS], F32) for i in range(NR)]
            of = [self.sb(st, "ln_of%d" % i, [128, TS], F32) for i in range(NR)]
            ob = [self.sb(st, "ln_ob%d" % i, [128, TS], BF16) for i in range(NR)]
            tt1 = [Tk() for _ in range(NR)]
            tof = [Tk() for _ in range(NR)]
            tob = [Tk() for _ in range(NR)]
            k = 0
            for tt in range(T // TS):
                zi = tt % 2
                tok = slice(tt * TS, (tt + 1) * TS)
                for g in range(4):
                    S.dma("sp", z[zi][:, g * 8:(g + 1) * 8, :], zTr[:, g * 8:(g + 1) * 8, tok], writes=[tz[zi][g]])
                for c in range(32):
                    i = k % NR
                    k += 1
                    S.op("act", lambda e: e.activation(out=zb[i][:], in_=z[zi][:, c, :], func=AF.Copy),
                         reads=[tz[zi][c // 8]], writes=[tzb[i]])
                    S.op("pool", lambda e: e.tensor_tensor(out=zq[i][:], in0=z[zi][:, c, :], in1=z[zi][:, c, :], op=ALU.mult),
                         reads=[tz[zi][c // 8]], writes=[tzq[i]])
                    S.op("pe", lambda e: e.matmul(bs_[0][:, :TS], self.onesb[:], zb[i][:], start=(c == 0), stop=(c == 31)),
                         reads=[tzb[i], self.t_c], writes=[bs_[1]])
                    S.op("pe", lambda e: e.matmul(bq_[0][:, :TS], self.onesb[:], zq[i][:], start=(c == 0), stop=(c == 31)),
                         reads=[tzq[i], self.t_c], writes=[bq_[1]])
                S.op("dve", lambda e: e.tensor_scalar(out=mean[:], in0=bs_[0][:, :TS], scalar1=1.0 / D, scalar2=1.0, op0=ALU.mult, op1=ALU.mult),
                     reads=[bs_[1]], writes=[tst])
                S.op("dve", lambda e: e.tensor_tensor(out=msq[:], in0=mean[:], in1=mean[:], op=ALU.mult), reads=[tst], writes=[tst])
                S.op("dve", lambda e: e.scalar_tensor_tensor(out=msq[:], in0=bq_[0][:, :TS], scalar=1.0 / D, in1=msq[:],
                                                            op0=ALU.mult, op1=ALU.subtract), reads=[bq_[1], tst], writes=[tst])
                S.op("act", lambda e: e.activation(out=rstd[:], in_=msq[:], func=AF.Sqrt, bias=self.epsln[:, 0:1], scale=1.0),
                     reads=[tst, self.t_c], writes=[tst])
                S.op("dve", lambda e: e.reciprocal(out=rstd[:], in_=rstd[:]), reads=[tst], writes=[tst])
                for c in range(32):
                    i = k % NR
                    k += 1
                    S.op("dve", lambda e: e.tensor_tensor(out=t1[i][:], in0=z[zi][:, c, :], in1=mean[:], op=ALU.subtract),
                         reads=[tz[zi][c // 8], tst], writes=[tt1[i]])
                    S.op("pool", lambda e: e.tensor_tensor(out=t1[i][:], in0=t1[i][:], in1=rstd[:], op=ALU.mult),
                         reads=[tst], writes=[tt1[i]])
                    S.op("act", lambda e: e.activation(out=of[i][:], in_=t1[i][:], func=AF.Identity,
                                                       bias=self.pp[:, bcol + c:bcol + c + 1], scale=self.pp[:, gcol + c:gcol + c + 1]),
                         reads=[tt1[i], self.t_pp], writes=[tof[i]])
                    S.dma("pool", outF[c * 128:(c + 1) * 128, tok], of[i][:], reads=[tof[i]], writes=[Tk()])
                    if outB is not None:
                        S.op("dve", lambda e: e.tensor_copy(out=ob[i][:], in_=of[i][:]), reads=[tof[i]], writes=[tob[i]])
                        S.dma("pool", outB[c * 128:(c + 1) * 128, tok], ob[i][:], reads=[tob[i]], writes=[Tk()])
        S.barrier()

    def router_pass(self, x1T):
        S = self.S
        T = self.T
        TS = min(512, T)
        xr_ = x1T.rearrange("(c p) t -> p c t", p=128)
        bl, bt, br = self.banks[0], self.banks[1], self.banks[2]
        with contextlib.ExitStack() as st:
            wr = self.sb(st, "rt_w", [128, 32, NE], F32)
            sel = self.sb(st, "rt_sel", [8, 8, 128], F32)
            tw = Tk()
            S.dma("sp", wr[:], self.router, writes=[tw])
            S.dma("sp", sel[:], self.sel_in, writes=[tw])
            xr = [self.sb(st, "rt_x%d" % i, [128, 32, 128], F32) for i in range(2)]
            tx = [Tk(), Tk()]
            sm = self.sb(st, "rt_sm", [128, 64], F32)
            tsm = Tk()
            gT = self.sb(st, "rt_gT", [8, TS], F32)
            tgT = Tk()
            gr = [self.sb(st, "rt_gr%d" % i, [128, TS], F32) for i in range(2)]
            tgr = [Tk(), Tk()]
            lg, eq1, l2, eq2, g1, gt = (sm[:, 0:8], sm[:, 8:16], sm[:, 16:24], sm[:, 24:32], sm[:, 32:40], sm[:, 40:48])
            m1, m2, dl, w1, w2 = (sm[:, 48:49], sm[:, 49:50], sm[:, 50:51], sm[:, 51:52], sm[:, 52:53])
            npg = TS // 128
            k = 0
            for n in range(T // 128):
                i = n % 2
                S.dma("sp", xr[i][:], xr_[:, :, n * 128:(n + 1) * 128], writes=[tx[i]])
                for c in range(32):
                    S.op("pe", lambda e: e.matmul(bl[0][:, 0:NE], xr[i][:, c, :], wr[:, c, :], start=(c == 0), stop=(c == 31)),
                         reads=[tx[i], tw], writes=[bl[1]])
                V = "dve"
                S.op(V, lambda e: e.tensor_copy(out=lg, in_=bl[0][:, 0:NE]), reads=[bl[1]], writes=[tsm])
                S.op(V, lambda e: e.tensor_reduce(out=m1, in_=lg, axis=AX.X, op=ALU.max), reads=[tsm], writes=[tsm])
                S.op(V, lambda e: e.tensor_scalar(out=eq1, in0=lg, scalar1=m1, scalar2=1.0, op0=ALU.is_equal, op1=ALU.mult), reads=[tsm], writes=[tsm])
                S.op(V, lambda e: e.scalar_tensor_tensor(out=l2, in0=eq1, scalar=-1e30, in1=lg, op0=ALU.mult, op1=ALU.add), reads=[tsm], writes=[tsm])
                S.op(V, lambda e: e.tensor_reduce(out=m2, in_=l2, axis=AX.X, op=ALU.max), reads=[tsm], writes=[tsm])
                S.op(V, lambda e: e.tensor_scalar(out=eq2, in0=l2, scalar1=m2, scalar2=1.0, op0=ALU.is_equal, op1=ALU.mult), reads=[tsm], writes=[tsm])
                S.op(V, lambda e: e.tensor_tensor(out=dl, in0=m1, in1=m2, op=ALU.subtract), reads=[tsm], writes=[tsm])
                S.op("act", lambda e: e.activation(out=w1, in_=dl, func=AF.Sigmoid), reads=[tsm], writes=[tsm])
                S.op("act", lambda e: e.activation(out=w2, in_=dl, func=AF.Sigmoid, scale=-1.0), reads=[tsm], writes=[tsm])
                S.op(V, lambda e: e.tensor_scalar(out=g1, in0=eq1, scalar1=w1, scalar2=1.0, op0=ALU.mult, op1=ALU.mult), reads=[tsm], writes=[tsm])
                S.op(V, lambda e: e.scalar_tensor_tensor(out=gt, in0=eq2, scalar=w2, in1=g1, op0=ALU.mult, op1=ALU.add), reads=[tsm], writes=[tsm])
                j = n % npg
                S.op("pe", lambda e: e.transpose(bt[0][0:8, j * 128:(j + 1) * 128], gt, self.idf[:]),
                     reads=[tsm, self.t_c], writes=[bt[1]])
                if j == npg - 1:
                    tok0 = (n - j) * 128
                    S.op("act", lambda e: e.activation(out=gT[:], in_=bt[0][0:8, :TS], func=AF.Copy), reads=[bt[1]], writes=[tgT])
                    for ex in range(NE):
                        S.op("pe", lambda e: e.matmul(br[0][:, :TS], sel[:, ex, :], gT[:], start=True, stop=True),
                             reads=[tgT, tw], writes=[br[1]])
                        b = k % 2
                        k += 1
                        en = self.any2()
                        S.op(en, lambda eng: self.copy(en, eng, gr[b][:], br[0][:, :TS]), reads=[br[1]], writes=[tgr[b]])
                        S.dma("pool", self.Grep[ex, :, tok0:tok0 + TS], gr[b][:], reads=[tgr[b]], writes=[Tk()])
        S.barrier()

    def rope(self, S, xin, rb, cs, sn, W, tin, tcs, tout, tmp, ttmp):
        xv = xin.rearrange("p (j two) -> p j two", two=2)
        ov = rb.rearrange("p (j two) -> p j two", two=2)
        x0, x1 = xv[:, :, 0], xv[:, :, 1]
        t1, t2, t3, t4 = tmp
        S.op("dve", lambda e: e.tensor_tensor(out=t1, in0=x0, in1=cs, op=ALU.mult), reads=[tin, tcs], writes=[ttmp[0]])
        S.op("pool", lambda e: e.tensor_tensor(out=t2, in0=x1, in1=sn, op=ALU.mult), reads=[tin, tcs], writes=[ttmp[1]])
        S.op("pool", lambda e: e.tensor_tensor(out=t3, in0=x0, in1=sn, op=ALU.mult), reads=[tin, tcs], writes=[ttmp[2]])
        S.op("dve", lambda e: e.tensor_tensor(out=t4, in0=x1, in1=cs, op=ALU.mult), reads=[tin, tcs], writes=[ttmp[3]])
        S.op("dve", lambda e: e.tensor_tensor(out=ov[:, :, 0], in0=t1, in1=t2, op=ALU.subtract),
             reads=[ttmp[0], ttmp[1]], writes=[tout])
        S.op("pool", lambda e: e.tensor_tensor(out=ov[:, :, 1], in0=t3, in1=t4, op=ALU.add),
             reads=[ttmp[2], ttmp[3], tout], writes=[tout])

    def mix_gmlp(self, l):
        S = self.S
        T = self.T
        bA, bB = self.banks[0], self.banks[1]
        with contextlib.ExitStack() as st:
            grep = self.sb(st, "gm_g", [128, 1024], F32)
            brep = self.sb(st, "gm_b", [128, 1024], F32)
            msrep = self.sb(st, "gm_ms", [128, 1024], F32)
            wsf = self.sb(st, "gm_wsf", [128, 8, 128], F32)
            wsb = self.sb(st, "gm_wsb", [128, 8, 128], BF16)
            bs = self.sb(st, "gm_bs", [128, 8], F32)
            tc_ = Tk()
            S.dma("sp", grep[:], self.rp_in[l, :, 1280:2304], writes=[tc_])
            S.dma("sp", brep[:], self.rp_in[l, :, 2304:3328], writes=[tc_])
            S.dma("sp", msrep[:], self.rp_in[l, :, 3328:4352], writes=[tc_])
            S.dma("sp", wsf[:], self.wsT_in[l], writes=[tc_])
            S.dma("sp", bs[:], self.bsT_in[l], writes=[tc_])
            S.op("dve", lambda e: e.tensor_copy(out=wsb[:], in_=wsf[:]), reads=[tc_], writes=[tc_])
            NB = 2
            gv = [self.sb(st, "gm_gv%d" % i, [128, 1024], F32) for i in range(NB)]
            gu = [self.sb(st, "gm_gu%d" % i, [128, 1024], F32) for i in range(NB)]
            a = [self.sb(st, "gm_a%d" % i, [128, 1024], F32) for i in range(NB)]
            sq = [self.sb(st, "gm_sq%d" % i, [128, 1024], F32) for i in range(NB)]
            vnb = [self.sb(st, "gm_vn%d" % i, [128, 1024], BF16) for i in range(NB)]
            oc = [self.sb(st, "gm_oc%d" % i, [128, 1024], F32) for i in range(NB)]
            ocb = [self.sb(st, "gm_ob%d" % i, [128, 1024], BF16) for i in range(NB)]
            mT = [self.sb(st, "gm_mT%d" % i, [128, 1024], BF16) for i in range(NB)]
            sm = [self.sb(st, "gm_sm%d" % i, [128, 8], F32) for i in range(NB)]
            tgv = [Tk() for _ in range(NB)]
            tgu = [Tk() for _ in range(NB)]
            ta = [Tk() for _ in range(NB)]
            tsq = [Tk() for _ in range(NB)]
            tvn = [Tk() for _ in range(NB)]
            toc = [Tk() for _ in range(NB)]
            tob = [Tk() for _ in range(NB)]
            tmT = [Tk() for _ in range(NB)]
            tsm = [Tk() for _ in range(NB)]
            mdst = self.mergedT[2048:3072, :].rearrange("(g p) t -> p g t", p=128)
            for n in range(T // 128):
                i = n % NB
                rows = slice(n * 128, (n + 1) * 128)
                S.dma("sp", gv[i][:], self.Y_g[rows, 1024:2048], writes=[tgv[i]])
                S.dma("sp", gu[i][:], self.Y_g[rows, 0:1024], writes=[tgu[i]])
                s1, nm, s2, rs = sm[i][:, 0:1], sm[i][:, 1:2], sm[i][:, 2:3], sm[i][:, 3:4]
                S.op("act", lambda e: e.activation(out=a[i][:], in_=gv[i][:], func=AF.Gelu), reads=[tgv[i]], writes=[ta[i]])
                S.op("dve", lambda e: e.tensor_reduce(out=s1, in_=a[i][:], axis=AX.X, op=ALU.add), reads=[ta[i]], writes=[tsm[i]])
                S.op("dve", lambda e: e.tensor_scalar(out=nm, in0=s1, scalar1=-1.0 / 1024, scalar2=1.0, op0=ALU.mult, op1=ALU.mult), reads=[tsm[i]], writes=[tsm[i]])
                S.op("dve", lambda e: e.tensor_scalar(out=a[i][:], in0=a[i][:], scalar1=nm, scalar2=0.0, op0=ALU.add, op1=ALU.add), reads=[tsm[i]], writes=[ta[i]])
                S.op("pool", lambda e: e.tensor_tensor(out=sq[i][:], in0=a[i][:], in1=a[i][:], op=ALU.mult), reads=[ta[i]], writes=[tsq[i]])
                S.op("dve", lambda e: e.tensor_reduce(out=s2, in_=sq[i][:], axis=AX.X, op=ALU.add), reads=[tsq[i]], writes=[tsm[i]])
                S.op("act", lambda e: e.activation(out=rs, in_=s2, func=AF.Sqrt, bias=self.epsln[:, 0:1], scale=1.0 / 1024),
                     reads=[tsm[i], self.t_c], writes=[tsm[i]])
                S.op("dve", lambda e: e.reciprocal(out=rs, in_=rs), reads=[tsm[i]], writes=[tsm[i]])
                S.op("dve", lambda e: e.scalar_tensor_tensor(out=sq[i][:], in0=a[i][:], scalar=rs, in1=grep[:], op0=ALU.mult, op1=ALU.mult),
                     reads=[ta[i], tsm[i], tc_], writes=[tsq[i]])
                S.op("pool", lambda e: e.tensor_tensor(out=vnb[i][:], in0=sq[i][:], in1=brep[:], op=ALU.add), reads=[tsq[i], tc_], writes=[tvn[i]])
                for g in range(8):
                    bank = bA if g < 4 else bB
                    c0 = (g % 4) * 128
                    S.op("pe", lambda e: e.matmul(bank[0][:, c0:c0 + 128], wsb[:, g, :], vnb[i][:, g * 128:(g + 1) * 128], start=True, stop=True),
                         reads=[tvn[i], tc_], writes=[bank[1]])
                S.op("act", lambda e: e.activation(out=gu[i][:], in_=gu[i][:], func=AF.Gelu), reads=[tgu[i]], writes=[tgu[i]])
                for g in range(8):
                    bank = bA if g < 4 else bB
                    c0 = (g % 4) * 128
                    S.op("dve", lambda e: e.scalar_tensor_tensor(out=oc[i][:, g * 128:(g + 1) * 128], in0=bank[0][:, c0:c0 + 128],
                                                                scalar=bs[:, g:g + 1], in1=gu[i][:, g * 128:(g + 1) * 128],
                                                                op0=ALU.add, op1=ALU.mult),
                         reads=[bank[1], tgu[i], tc_], writes=[toc[i]])
                S.op("pool", lambda e: e.tensor_tensor(out=ocb[i][:], in0=oc[i][:], in1=msrep[:], op=ALU.mult), reads=[toc[i], tc_], writes=[tob[i]])
                pb, tpb = self.bbanks[n % 2]
                for g in range(8):
                    S.op("pe", lambda e: e.transpose(pb[:, g * 128:(g + 1) * 128], ocb[i][:, g * 128:(g + 1) * 128], self.idb[:]),
                         reads=[tob[i], self.t_c], writes=[tpb])
                S.op("act", lambda e: e.activation(out=mT[i][:], in_=pb[:], func=AF.Copy), reads=[tpb], writes=[tmT[i]])
                S.dma("pool", mdst[:, :, rows], mT[i][:].rearrange("p (g t) -> p g t", t=128), reads=[tmT[i]], writes=[Tk()])
        S.barrier()

    def mix_attn(self, l):
        S = self.S
        T = self.T
        NCH = T // 128
        QT = min(512, T)
        with contextlib.ExitStack() as st:
            gain = self.sb(st, "at_gain", [128, 1280], F32)
            tcst = Tk()
            S.dma("sp", gain[:], self.rp_in[l, :, 0:1280], writes=[tcst])
            NB = 2
            qk = [self.sb(st, "at_qk%d" % i, [128, 1280], F32) for i in range(NB)]
            sq = [self.sb(st, "at_sq%d" % i, [128, 1280], F32) for i in range(NB)]
            cs = [self.sb(st, "at_cs%d" % i, [128, 640], F32) for i in range(NB)]
            sn = [self.sb(st, "at_sn%d" % i, [128, 640], F32) for i in range(NB)]
            tmp = [[self.sb(st, "at_t%d_%d" % (i, j), [128, 640], F32) for j in range(4)] for i in range(NB)]
            rb = [self.sb(st, "at_rb%d" % i, [128, 1280], BF16) for i in range(NB)]
            qkT = [self.sb(st, "at_qkT%d" % i, [128, 1280], BF16) for i in range(NB)]
            sm = [self.sb(st, "at_sm%d" % i, [128, 16], F32) for i in range(NB)]
            tqk = [Tk() for _ in range(NB)]
            tsq = [Tk() for _ in range(NB)]
            tcs = [Tk() for _ in range(NB)]
            ttmp = [[Tk() for _ in range(4)] for _ in range(NB)]
            trb = [Tk() for _ in range(NB)]
            tqT = [Tk() for _ in range(NB)]
            tsm = [Tk() for _ in range(NB)]
            dst = self.aqkT.rearrange("h d t -> d h t")
            for n in range(NCH):
                i = n % NB
                rows = slice(n * 128, (n + 1) * 128)
                S.dma("sp", qk[i][:], self.Y_a[rows, 0:1280], writes=[tqk[i]])
                S.dma("sp", cs[i][:], self.cos_in[rows, 0:640], writes=[tcs[i]])
                S.dma("sp", sn[i][:], self.sin_in[rows, 0:640], writes=[tcs[i]])
                S.op("pool", lambda e: e.tensor_tensor(out=sq[i][:], in0=qk[i][:], in1=qk[i][:], op=ALU.mult), reads=[tqk[i]], writes=[tsq[i]])
                ss = sm[i][:, 0:10]
                S.op("dve", lambda e: e.tensor_reduce(out=ss, in_=sq[i][:].rearrange("p (h d) -> p h d", d=128), axis=AX.X, op=ALU.add),
                     reads=[tsq[i]], writes=[tsm[i]])
                S.op("act", lambda e: e.activation(out=ss, in_=ss, func=AF.Sqrt, bias=self.epsln[:, 1:2], scale=1.0 / 128),
                     reads=[tsm[i], self.t_c], writes=[tsm[i]])
                S.op("dve", lambda e: e.reciprocal(out=ss, in_=ss), reads=[tsm[i]], writes=[tsm[i]])
                S.op("dve", lambda e: e.tensor_tensor(out=sq[i][:].rearrange("p (h d) -> p h d", d=128),
                                                     in0=qk[i][:].rearrange("p (h d) -> p h d", d=128),
                                                     in1=ss.unsqueeze(2).to_broadcast([128, 10, 128]), op=ALU.mult),
                     reads=[tqk[i], tsm[i]], writes=[tsq[i]])
                S.op("pool", lambda e: e.tensor_tensor(out=sq[i][:], in0=sq[i][:], in1=gain[:], op=ALU.mult), reads=[tcst], writes=[tsq[i]])
                self.rope(S, sq[i][:], rb[i][:], cs[i][:], sn[i][:], 1280, tsq[i], tcs[i], trb[i],
                          [t[:] for t in tmp[i]], ttmp[i])
                for h in range(10):
                    pb, tpb = self.bbanks[0] if h < 8 else self.bbanks[1]
                    c0 = (h % 8) * 128
                    S.op("pe", lambda e: e.transpose(pb[:, c0:c0 + 128], rb[i][:, h * 128:(h + 1) * 128], self.idb[:]),
                         reads=[trb[i], self.t_c], writes=[tpb])
                S.op("act", lambda e: e.activation(out=qkT[i][:, 0:1024], in_=self.bbanks[0][0][:], func=AF.Copy),
                     reads=[self.bbanks[0][1]], writes=[tqT[i]])
                S.op("dve", lambda e: e.tensor_copy(out=qkT[i][:, 1024:1280], in_=self.bbanks[1][0][:, 0:256]),
                     reads=[self.bbanks[1][1]], writes=[tqT[i]])
                S.dma("pool", dst[:, :, rows], qkT[i][:].rearrange("p (h t) -> p h t", t=128), reads=[tqT[i]], writes=[Tk()])
        S.barrier()
        scale = 128.0 ** -0.5
        with contextlib.ExitStack() as st:
            kT = self.sb(st, "ac_kT", [128, T], BF16)
            vf = self.sb(st, "ac_vf", [128, NCH, 128], F32)
            vb = self.sb(st, "ac_vb", [128, NCH, 128], BF16)
            tk_, tvf, tvb = Tk(), Tk(), Tk()
            qT = [self.sb(st, "ac_qT%d" % i, [128, QT], BF16) for i in range(2)]
            tq = [Tk(), Tk()]
            NP = 3
            pT = [self.sb(st, "ac_pT%d" % i, [128, QT], BF16) for i in range(NP)]
            tp = [Tk() for _ in range(NP)]
            rec = self.sb(st, "ac_rec", [128, QT], F32)
            of = self.sb(st, "ac_of", [128, QT], F32)
            ob = [self.sb(st, "ac_ob%d" % i, [128, QT], BF16) for i in range(2)]
            trec, tof = Tk(), Tk()
            tob = [Tk(), Tk()]
            sbank = [self.banks[0], self.banks[1]]
            accs = [(self.banks[2], self.banks[3]), (self.banks[4], self.banks[5])]
            it = 0
            pi = 0
            for g in range(2):
                S.dma("sp", kT[:], self.aqkT[8 + g], writes=[tk_])
                self.dma_chunks("sp", vf[:], self.Y_a[:, 1280 + g * 128:1280 + (g + 1) * 128].rearrange("(n p) d -> p n d", p=128), NCH, tvf)
                S.op("pool", lambda e: e.tensor_copy(out=vb[:], in_=vf[:]), reads=[tvf], writes=[tvb])
                for h in range(4 * g, 4 * g + 4):
                    for qt in range(T // QT):
                        qi = it % 2
                        oacc, dacc = accs[it % 2]
                        it += 1
                        S.dma("sp", qT[qi][:], self.aqkT[h, :, qt * QT:(qt + 1) * QT], writes=[tq[qi]])
                        for kc in range(NCH):
                            sb_, tsb = sbank[kc % 2]
                            S.op("pe", lambda e: e.matmul(sb_[:, :QT], kT[:, kc * 128:(kc + 1) * 128], qT[qi][:], start=True, stop=True),
                                 reads=[tk_, tq[qi]], writes=[tsb])
                            p = pi % NP
                            pi += 1
                            S.op("act", lambda e: e.activation(out=pT[p][:], in_=sb_[:, :QT], func=AF.Exp, scale=scale),
                                 reads=[tsb], writes=[tp[p]])
                            S.op("pe", lambda e: e.matmul(oacc[0][:, :QT], vb[:, kc, :], pT[p][:], start=(kc == 0), stop=(kc == NCH - 1)),
                                 reads=[tvb, tp[p]], writes=[oacc[1]])
                            S.op("pe", lambda e: e.matmul(dacc[0][:, :QT], self.onesb[:], pT[p][:], start=(kc == 0), stop=(kc == NCH - 1)),
                                 reads=[tp[p], self.t_c], writes=[dacc[1]])
                        S.op("dve", lambda e: e.reciprocal(out=rec[:], in_=dacc[0][:, :QT]), reads=[dacc[1]], writes=[trec])
                        S.op("dve", lambda e: e.tensor_tensor(out=of[:], in0=oacc[0][:, :QT], in1=rec[:], op=ALU.mult),
                             reads=[oacc[1], trec], writes=[tof])
                        mc = l * 200 + 128 + h
                        S.op("pool", lambda e: e.tensor_scalar(out=ob[qi][:], in0=of[:], scalar1=self.pp[:, mc:mc + 1], scalar2=1.0,
                                                               op0=ALU.mult, op1=ALU.mult), reads=[tof, self.t_pp], writes=[tob[qi]])
                        S.dma("pool", self.mergedT[h * 128:(h + 1) * 128, qt * QT:(qt + 1) * QT], ob[qi][:], reads=[tob[qi]], writes=[Tk()])
        S.barrier()

    def mix_ret(self, l):
        S = self.S
        T = self.T
        NCH = T // 128
        gam = [1.0 - 2.0 ** (-5.0 - h) for h in range(8)]
        gC = [float(np.float64(g) ** 128) for g in gam]
        with contextlib.ExitStack() as st:
            rdec = self.sb(st, "rt_rdec", [128, 32], F32)
            tcst = Tk()
            S.dma("sp", rdec[:], self.rdec_in, writes=[tcst])
            NB = 2
            qk = [self.sb(st, "rp_qk%d" % i, [128, 2048], F32) for i in range(NB)]
            rv = [self.sb(st, "rp_rv%d" % i, [128, 1024], F32) for i in range(NB)]
            cs = [self.sb(st, "rp_cs%d" % i, [128, 1024], F32) for i in range(NB)]
            sn = [self.sb(st, "rp_sn%d" % i, [128, 1024], F32) for i in range(NB)]
            tmp = [[self.sb(st, "rp_t%d_%d" % (i, j), [128, 1024], F32) for j in range(4)] for i in range(NB)]
            rb = [self.sb(st, "rp_rb%d" % i, [128, 2048], BF16) for i in range(NB)]
            kfb = [self.sb(st, "rp_kf%d" % i, [128, 3, 1024], BF16) for i in range(NB)]
            qkT = [self.sb(st, "rp_qkT%d" % i, [128, 2048], BF16) for i in range(NB)]
            tqk = [Tk() for _ in range(NB)]
            trv = [Tk() for _ in range(NB)]
            tcs = [Tk() for _ in range(NB)]
            ttmp = [[Tk() for _ in range(4)] for _ in range(NB)]
            trb = [Tk() for _ in range(NB)]
            tkf = [Tk() for _ in range(NB)]
            tqT = [Tk() for _ in range(NB)]
            dst = self.rqkT.rearrange("h d t -> d h t")
            rkd = self.RK.rearrange("i t c -> t i c")
            for n in range(NCH):
                i = n % NB
                rows = slice(n * 128, (n + 1) * 128)
                S.dma("sp", qk[i][:], self.Y_r[rows, 0:2048], writes=[tqk[i]])
                S.dma("sp", rv[i][:], self.Y_r[rows, 2048:3072], writes=[trv[i]])
                S.dma("sp", cs[i][:], self.cos_in[rows, :], writes=[tcs[i]])
                S.dma("sp", sn[i][:], self.sin_in[rows, :], writes=[tcs[i]])
                self.rope(S, qk[i][:], rb[i][:], cs[i][:], sn[i][:], 2048, tqk[i], tcs[i], trb[i], [t[:] for t in tmp[i]], ttmp[i])
                for h in range(8):
                    kh = rb[i][:, 1024 + h * 128:1024 + (h + 1) * 128]
                    S.op("dve", lambda e: e.tensor_scalar(out=kfb[i][:, 0, h * 128:(h + 1) * 128], in0=kh, scalar1=rdec[:, h:h + 1], scalar2=1.0, op0=ALU.mult, op1=ALU.mult),
                         reads=[trb[i], tcst], writes=[tkf[i]])
                    S.op("pool", lambda e: e.tensor_scalar(out=kfb[i][:, 1, h * 128:(h + 1) * 128], in0=kh, scalar1=rdec[:, 8 + h:9 + h], scalar2=1.0,
                                                           op0=ALU.mult, op1=ALU.mult), reads=[trb[i], tcst], writes=[tkf[i]])
                S.op("act", lambda e: e.activation(out=kfb[i][:, 2, :], in_=rv[i][:], func=AF.Copy), reads=[trv[i]], writes=[tkf[i]])
                S.dma("pool", rkd[rows, :, :], kfb[i][:], reads=[tkf[i]], writes=[Tk()])
                for h in range(16):
                    pb, tpb = self.bbanks[h // 8]
                    c0 = (h % 8) * 128
                    S.op("pe", lambda e: e.transpose(pb[:, c0:c0 + 128], rb[i][:, h * 128:(h + 1) * 128], self.idb[:]),
                         reads=[trb[i], self.t_c], writes=[tpb])
                S.op("act", lambda e: e.activation(out=qkT[i][:, 0:1024], in_=self.bbanks[0][0][:], func=AF.Copy),
                     reads=[self.bbanks[0][1]], writes=[tqT[i]])
                S.op("dve", lambda e: e.tensor_copy(out=qkT[i][:, 1024:2048], in_=self.bbanks[1][0][:]),
                     reads=[self.bbanks[1][1]], writes=[tqT[i]])
                S.dma("pool", dst[:, :, rows], qkT[i][:].rearrange("p (h t) -> p h t", t=128), reads=[tqT[i]], writes=[Tk()])
        S.barrier()
        with contextlib.ExitStack() as st:
            rdec = self.sb(st, "rs_rdec", [128, 32], F32)
            dsym = self.sb(st, "rs_dsym", [128, 8, 128], F32)
            gcol = self.sb(st, "rs_gcol", [128, 8], F32)
            tcst = Tk()
            S.dma("sp", rdec[:], self.rdec_in, writes=[tcst])
            S.dma("sp", dsym[:], self.dsym_in, writes=[tcst])
            b0 = l * 200
            S.op("dve", lambda e: e.tensor_tensor(out=gcol[:], in0=self.pp[:, b0 + 168:b0 + 176], in1=self.pp[:, b0 + 128 + 24:b0 + 128 + 32], op=ALU.mult),
                 reads=[self.t_pp], writes=[tcst])
            qT = self.sb(st, "rs_qT", [128, T], BF16)
            kT = self.sb(st, "rs_kT", [128, T], BF16)
            kv = self.sb(st, "rs_kv", [128, 3, NCH, 128], BF16)
            rg = self.sb(st, "rs_rg", [128, NCH, 128], F32)
            oacc = self.sb(st, "rs_oacc", [128, NCH, 128], F32)
            NG = min(8, NCH)
            sqb = [self.sb(st, "rs_sq%d" % i, [128, NG, 128], F32) for i in range(2)]
            ob = [self.sb(st, "rs_ob%d" % i, [128, NG, 128], BF16) for i in range(2)]
            ss = [self.sb(st, "rs_ss%d" % i, [128, NG], F32) for i in range(2)]
            tld, trg, toa = Tk(), Tk(), Tk()
            tsq, tob, tss = [Tk(), Tk()], [Tk(), Tk()], [Tk(), Tk()]
            R = [self.sb(st, "rs_R%d" % i, [128, 128], F32) for i in range(2)]
            Rb = [self.sb(st, "rs_Rb%d" % i, [128, 128], BF16) for i in range(2)]
            tR = [Tk(), Tk()]
            tRb = [Tk(), Tk()]
            pT = [self.sb(st, "rs_pT%d" % i, [128, 128], BF16) for i in range(2)]
            tpT = [Tk(), Tk()]
            mT = [self.sb(st, "rs_mT%d" % i, [128, 1024], BF16) for i in range(2)]
            tmT = [Tk(), Tk()]
            bS = [self.banks[0], self.banks[1]]
            bO = [self.banks[2], self.banks[3]]
            for h in range(8):
                S.dma("sp", qT[:], self.rqkT[h], writes=[tld])
                S.dma("sp", kT[:], self.rqkT[8 + h], writes=[tld])
                for i3 in range(3):
                    self.dma_chunks("sp", kv[:, i3, :, :], self.RK[i3, :, h * 128:(h + 1) * 128].rearrange("(n p) d -> p n d", p=128), NCH, tld)
                self.dma_chunks("sp", rg[:], self.Y_r[:, 3072 + h * 128:3072 + (h + 1) * 128].rearrange("(n p) d -> p n d", p=128), NCH, trg)
                for d in range(2):
                    S.op("pool", lambda e: e.memset(R[d][:], 0.0), writes=[tR[d]])
                    S.op("pool", lambda e: e.memset(Rb[d][:], 0.0), writes=[tRb[d]])
                for j in range(NCH):
                    cols = slice(j * 128, (j + 1) * 128)
                    bs_, tbs = bS[j % 2]
                    bo_, tbo = bO[j % 2]
                    S.op("pe", lambda e: e.matmul(bs_[:, 0:128], kT[:, cols], qT[:, cols], start=True, stop=True), reads=[tld], writes=[tbs])
                    p = j % 2
                    S.op("dve", lambda e: e.tensor_tensor(out=pT[p][:], in0=bs_[:, 0:128], in1=dsym[:, h, :], op=ALU.mult),
                         reads=[tbs, tcst], writes=[tpT[p]])
                    S.op("pe", lambda e: e.matmul(bo_[:, 0:128], pT[p][:], kv[:, 2, j, :], start=True, stop=True), reads=[tpT[p], tld], writes=[tbo])
                    S.op("pe", lambda e: e.matmul(bo_[:, 128:256], qT[:, cols], Rb[0][:], start=True, stop=True), reads=[tld, tRb[0]], writes=[tbo])
                    S.op("pe", lambda e: e.matmul(bo_[:, 256:384], kv[:, 0, j, :], kv[:, 2, j, :], start=True, stop=True), reads=[tld], writes=[tbo])
                    S.op("act", lambda e: e.activation(out=oacc[:, j, :], in_=bo_[:, 0:128], func=AF.Copy), reads=[tbo], writes=[toa])
                    S.op("dve", lambda e: e.scalar_tensor_tensor(out=oacc[:, j, :], in0=bo_[:, 128:256], scalar=rdec[:, 16 + h:17 + h],
                                                                in1=oacc[:, j, :], op0=ALU.mult, op1=ALU.add), reads=[tbo, tcst, toa], writes=[toa])
                    S.op("dve", lambda e: e.scalar_tensor_tensor(out=R[0][:], in0=R[0][:], scalar=gC[h], in1=bo_[:, 256:384],
                                                                op0=ALU.mult, op1=ALU.add), reads=[tbo], writes=[tR[0]])
                    S.op("dve", lambda e: e.tensor_copy(out=Rb[0][:], in_=R[0][:]), reads=[tR[0]], writes=[tRb[0]])
                for j in range(NCH - 1, -1, -1):
                    cols = slice(j * 128, (j + 1) * 128)
                    bo_, tbo = bO[j % 2]
                    S.op("pe", lambda e: e.matmul(bo_[:, 128:256], qT[:, cols], Rb[1][:], start=True, stop=True), reads=[tld, tRb[1]], writes=[tbo])
                    S.op("pe", lambda e: e.matmul(bo_[:, 256:384], kv[:, 1, j, :], kv[:, 2, j, :], start=True, stop=True), reads=[tld], writes=[tbo])
                    S.op("dve", lambda e: e.scalar_tensor_tensor(out=oacc[:, j, :], in0=bo_[:, 128:256], scalar=rdec[:, 24 + h:25 + h],
                                                                in1=oacc[:, j, :], op0=ALU.mult, op1=ALU.add), reads=[tbo, tcst, toa], writes=[toa])
                    S.op("dve", lambda e: e.scalar_tensor_tensor(out=R[1][:], in0=R[1][:], scalar=gC[h], in1=bo_[:, 256:384],
                                                                op0=ALU.mult, op1=ALU.add), reads=[tbo], writes=[tR[1]])
                    S.op("dve", lambda e: e.tensor_copy(out=Rb[1][:], in_=R[1][:]), reads=[tR[1]], writes=[tRb[1]])
                S.op("act", lambda e: e.activation(out=rg[:], in_=rg[:], func=AF.Silu), reads=[trg], writes=[trg])
                for j0 in range(0, NCH, NG):
                    gi = (j0 // NG) % 2
                    js = slice(j0, j0 + NG)
                    S.op("pool", lambda e: e.tensor_tensor(out=sqb[gi][:], in0=oacc[:, js, :], in1=oacc[:, js, :], op=ALU.mult), reads=[toa], writes=[tsq[gi]])
                    S.op("dve", lambda e: e.tensor_reduce(out=ss[gi][:], in_=sqb[gi][:], axis=AX.X, op=ALU.add), reads=[tsq[gi]], writes=[tss[gi]])
                    S.op("act", lambda e: e.activation(out=ss[gi][:], in_=ss[gi][:], func=AF.Sqrt, bias=self.epsln[:, 1:2], scale=1.0 / 128),
                         reads=[tss[gi], self.t_c], writes=[tss[gi]])
                    S.op("dve", lambda e: e.reciprocal(out=ss[gi][:], in_=ss[gi][:]), reads=[tss[gi]], writes=[tss[gi]])
                    S.op("dve", lambda e: e.tensor_tensor(out=sqb[gi][:], in0=oacc[:, js, :], in1=ss[gi][:].unsqueeze(2).to_broadcast([128, NG, 128]), op=ALU.mult),
                         reads=[toa, tss[gi]], writes=[tsq[gi]])
                    S.op("pool", lambda e: e.tensor_tensor(out=ob[gi][:], in0=sqb[gi][:], in1=rg[:, js, :], op=ALU.mult), reads=[tsq[gi], trg], writes=[tob[gi]])
                    pb, tpb = self.bbanks[gi]
                    for jj in range(NG):
                        S.op("pe", lambda e: e.transpose(pb[:, jj * 128:(jj + 1) * 128], ob[gi][:, jj, :], self.idb[:]),
                             reads=[tob[gi], self.t_c], writes=[tpb])
                    S.op("act", lambda e: e.activation(out=mT[gi][:, :NG * 128], in_=pb[:, :NG * 128], func=AF.Copy, scale=gcol[:, h:h + 1]),
                         reads=[tpb, tcst], writes=[tmT[gi]])
                    S.dma("pool", self.mergedT[3072 + h * 128:3072 + (h + 1) * 128, j0 * 128:(j0 + NG) * 128], mT[gi][:, :NG * 128],
                          reads=[tmT[gi]], writes=[Tk()])
        S.barrier()

    def dma_chunks(self, q, out3, in3, n, tk, step=8, reads=()):
        for a in range(0, n, step):
            b = min(n, a + step)
            self.S.dma(q, out3[:, a:b, :], in3[:, a:b, :], reads=list(reads), writes=[tk])

    def mix_hgrn(self, l):
        S = self.S
        T = self.T
        NCH = T // 128
        SEG = min(2048, T)
        NSEG = T // SEG
        NT = SEG // 128
        NBLK = SEG // 32
        PW = min(512, T)
        b0 = l * 200
        with contextlib.ExitStack() as st:
            mbd = self.sb(st, "hg_mbd", [128, 2, 128], F32)
            oml = self.sb(st, "hg_oml", [128, 8], F32)
            gcol = self.sb(st, "hg_gcol", [128, 8], F32)
            one = self.sb(st, "hg_one", [128, 1], F32)
            tcst = Tk()
            S.dma("sp", mbd[:], self.mbd_in, writes=[tcst])
            S.op("dve", lambda e: e.memset(one[:], 1.0), writes=[tcst])
            if l == 0:
                S.op("dve", lambda e: e.memset(oml[:], 1.0), writes=[tcst])
            else:
                S.op("dve", lambda e: e.tensor_tensor(out=oml[:], in0=self.pp[:, b0 + 176:b0 + 184], in1=self.pp[:, b0 + 184:b0 + 192], op=ALU.subtract),
                     reads=[self.t_pp], writes=[tcst])
                S.op("act", lambda e: e.activation(out=oml[:], in_=oml[:], func=AF.Sigmoid), reads=[tcst], writes=[tcst])
            S.op("dve", lambda e: e.tensor_tensor(out=gcol[:], in0=self.pp[:, b0 + 160:b0 + 168], in1=self.pp[:, b0 + 128 + 8:b0 + 128 + 16], op=ALU.mult),
                 reads=[self.t_pp], writes=[tcst])
            oT = self.sb(st, "hg_oT", [128, T], F32)
            vf = self.sb(st, "hg_vf", [128, T], F32)
            vb = self.sb(st, "hg_vb", [128, NCH, 128], BF16)
            toT, tvb, thg = Tk(), Tk(), Tk()
            z = self.sb(st, "hg_z", [128, SEG], F32)
            q = self.sb(st, "hg_q", [128, SEG], F32)
            kk = self.sb(st, "hg_kk", [128, SEG], F32)
            ba = self.sb(st, "hg_ba", [128, SEG], F32)
            bb = self.sb(st, "hg_bb", [128, SEG], F32)
            eb = self.sb(st, "hg_eb", [128, SEG], F32)
            enb = self.sb(st, "hg_enb", [128, SEG], F32)
            Qt = self.sb(st, "hg_Qt", [128, SEG], F32)
            Kt = self.sb(st, "hg_Kt", [128, SEG], F32)
            KpT = self.sb(st, "hg_KpT", [128, SEG], BF16)
            Kp = self.sb(st, "hg_Kp", [128, NT, 128], BF16)
            Kpz = self.sb(st, "hg_Kpz", [128, NT, 128], BF16)
            dec = self.sb(st, "hg_dec", [128, NBLK], F32)
            tz, tq, tkk, tba, tbb, teb, tenb, tQt, tKt, tKpT, tKp, tdec = (Tk() for _ in range(12))
            Sst = self.sb(st, "hg_S", [128, 128], F32)
            tS = Tk()
            PT = [self.sb(st, "hg_PT%d" % i, [128, 128], BF16) for i in range(2)]
            tPT = [Tk(), Tk()]
            sqp = [self.sb(st, "hg_sq%d" % i, [128, PW], BF16) for i in range(2)]
            rsp = [self.sb(st, "hg_rs%d" % i, [128, PW], F32) for i in range(2)]
            t1p = [self.sb(st, "hg_t1%d" % i, [128, PW], F32) for i in range(2)]
            mbp = [self.sb(st, "hg_mb%d" % i, [128, PW], BF16) for i in range(2)]
            tsqp, trsp, tt1p, tmbp = ([Tk(), Tk()] for _ in range(4))
            bA = [self.banks[0], self.banks[1]]
            bO = [self.banks[2], self.banks[3]]
            bR = [self.banks[4], self.banks[5]]
            nR = 0
            v3 = lambda t_: t_[:].rearrange("p (b c) -> p b c", c=32)
            for h in range(8):
                self.dma_chunks("sp", vf[:].rearrange("p (n d) -> p n d", d=128),
                                self.Y_hi[:, h * 128:(h + 1) * 128].rearrange("(n p) d -> p n d", p=128), NCH, thg)
                S.op("pool", lambda e: e.tensor_copy(out=vb[:], in_=vf[:].rearrange("p (n d) -> p n d", d=128)), reads=[thg], writes=[tvb])
                for d in range(2):
                    S.op("pool", lambda e: e.memset(Sst[:], 0.0), writes=[tS])
                    segs = range(NSEG) if d == 0 else range(NSEG - 1, -1, -1)
                    for sg_ in segs:
                        scol = slice(sg_ * SEG, (sg_ + 1) * SEG)
                        zr = 1024 * (1 + d) + h * 128
                        S.dma("sp", z[:], self.YT[zr:zr + 128, scol], writes=[tz])
                        S.dma("sp", q[:], self.YT[h * 128:(h + 1) * 128, scol], writes=[tq])
                        S.op("act", lambda e: e.activation(out=z[:], in_=z[:], func=AF.Sigmoid, scale=-1.0), reads=[tz], writes=[tz])
                        S.op("dve", lambda e: e.tensor_scalar(out=kk[:], in0=z[:], scalar1=oml[:, h:h + 1], scalar2=1.0, op0=ALU.mult, op1=ALU.mult),
                             reads=[tz, tcst], writes=[tkk])
                        S.op("act", lambda e: e.activation(out=ba[:], in_=kk[:], func=AF.Ln, scale=-1.0, bias=one[:, 0:1]),
                             reads=[tkk, tcst], writes=[tba])
                        cur, nxt, tcur, tnxt = ba, bb, tba, tbb
                        for sh in (1, 2, 4, 8, 16):
                            c3, n3 = v3(cur), v3(nxt)
                            if d == 0:
                                S.op("dve", lambda e: e.tensor_tensor(out=n3[:, :, sh:], in0=c3[:, :, sh:], in1=c3[:, :, :32 - sh], op=ALU.add),
                                     reads=[tcur], writes=[tnxt])
                                S.op("dve", lambda e: e.tensor_copy(out=n3[:, :, :sh], in_=c3[:, :, :sh]), reads=[tcur], writes=[tnxt])
                            else:
                                S.op("dve", lambda e: e.tensor_tensor(out=n3[:, :, :32 - sh], in0=c3[:, :, :32 - sh], in1=c3[:, :, sh:], op=ALU.add),
                                     reads=[tcur], writes=[tnxt])
                                S.op("dve", lambda e: e.tensor_copy(out=n3[:, :, 32 - sh:], in_=c3[:, :, 32 - sh:]), reads=[tcur], writes=[tnxt])
                            cur, nxt, tcur, tnxt = nxt, cur, tnxt, tcur
                        S.op("dve", lambda e: e.tensor_scalar(out=cur[:], in0=cur[:], scalar1=-80.0, scalar2=0.0, op0=ALU.max, op1=ALU.add),
                             reads=[tcur], writes=[tcur])
                        S.op("act", lambda e: e.activation(out=eb[:], in_=cur[:], func=AF.Exp), reads=[tcur], writes=[teb])
                        S.op("act", lambda e: e.activation(out=enb[:], in_=cur[:], func=AF.Exp, scale=-1.0), reads=[tcur], writes=[tenb])
                        S.op("dve", lambda e: e.tensor_copy(out=dec[:], in_=v3(eb)[:, :, (31 if d == 0 else 0)]), reads=[teb], writes=[tdec])
                        S.op("act", lambda e: e.activation(out=q[:], in_=q[:], func=AF.Silu), reads=[tq], writes=[tq])
                        S.op("dve", lambda e: e.scalar_tensor_tensor(out=Qt[:], in0=q[:], scalar=128.0 ** -0.5, in1=eb[:], op0=ALU.mult, op1=ALU.mult),
                             reads=[tq, teb], writes=[tQt])
                        S.op("pool", lambda e: e.tensor_tensor(out=Kt[:], in0=kk[:], in1=enb[:], op=ALU.mult), reads=[tkk, tenb], writes=[tKt])
                        S.op("dve", lambda e: e.tensor_tensor(out=v3(KpT), in0=v3(Kt), in1=dec[:].unsqueeze(2).to_broadcast([128, NBLK, 32]), op=ALU.mult),
                             reads=[tKt, tdec], writes=[tKpT])
                        for j0 in range(0, NT, 8):
                            pb, tpb = self.bbanks[(j0 // 8) % 2]
                            n8 = min(8, NT - j0)
                            for jj in range(n8):
                                jt = j0 + jj
                                S.op("pe", lambda e: e.transpose(pb[:, jj * 128:(jj + 1) * 128], KpT[:, jt * 128:(jt + 1) * 128], self.idb[:]),
                                     reads=[tKpT, self.t_c], writes=[tpb])
                            S.op("act", lambda e: e.activation(out=Kp[:, j0:j0 + n8, :], in_=pb[:, :n8 * 128].rearrange("p (n d) -> p n d", d=128), func=AF.Copy),
                                 reads=[tpb], writes=[tKp])
                            S.op("dve", lambda e: e.tensor_copy(out=Kpz[64:128, j0:j0 + n8, :], in_=pb[64:128, :n8 * 128].rearrange("p (n d) -> p n d", d=128)),
                                 reads=[tpb], writes=[tKp])
                            S.op("dve", lambda e: e.memset(Kpz[64:96, j0:j0 + n8, :], 0.0), writes=[tKp])
                        tiles = range(NT) if d == 0 else range(NT - 1, -1, -1)
                        for jt in tiles:
                            jg = sg_ * NT + jt
                            cols = slice(jt * 128, (jt + 1) * 128)
                            gcols = slice(jg * 128, (jg + 1) * 128)
                            ba_, tba_ = bA[jt % 2]
                            bo_, tbo_ = bO[jt % 2]
                            S.op("pe", lambda e: e.matmul(ba_[:, 0:128], Kt[:, cols], Qt[:, cols], start=True, stop=True), reads=[tKt, tQt], writes=[tba_])
                            p = jt % 2
                            S.op("dve", lambda e: e.tensor_tensor(out=PT[p][:], in0=ba_[:, 0:128], in1=mbd[:, d, :], op=ALU.mult),
                                 reads=[tba_, tcst], writes=[tPT[p]])
                            S.op("pe", lambda e: e.matmul(bo_[:, 0:128], vb[:, jg, :], PT[p][:], start=True, stop=True), reads=[tvb, tPT[p]], writes=[tbo_])
                            blks = range(4) if d == 0 else range(3, -1, -1)
                            for ib in blks:
                                c32 = slice(jt * 128 + ib * 32, jt * 128 + ib * 32 + 32)
                                S.op("pe", lambda e: e.matmul(bo_[:, 128 + ib * 32:128 + ib * 32 + 32], Sst[:], Qt[:, c32], start=True, stop=True),
                                     reads=[tS, tQt], writes=[tbo_])
                                br_, tbr_ = bR[nR % 2]
                                nR += 1
                                if ib < 3:
                                    S.op("pe", lambda e: e.matmul(br_[:, 0:128], Kp[ib * 32:(ib + 1) * 32, jt, :], vb[ib * 32:(ib + 1) * 32, jg, :], start=True, stop=True),
                                         reads=[tKp, tvb], writes=[tbr_])
                                else:
                                    S.op("pe", lambda e: e.matmul(br_[:, 0:128], Kpz[64:128, jt, :], vb[64:128, jg, :], start=True, stop=True),
                                         reads=[tKp, tvb], writes=[tbr_])
                                blk = jt * 4 + ib
                                S.op("dve", lambda e: e.scalar_tensor_tensor(out=Sst[:], in0=Sst[:], scalar=dec[:, blk:blk + 1], in1=br_[:, 0:128],
                                                                            op0=ALU.mult, op1=ALU.add), reads=[tbr_, tdec], writes=[tS])
                            if d == 0:
                                S.op("act", lambda e: e.activation(out=oT[:, gcols], in_=bo_[:, 0:128], func=AF.Copy), reads=[tbo_], writes=[toT])
                            else:
                                S.op("dve", lambda e: e.tensor_tensor(out=oT[:, gcols], in0=bo_[:, 0:128], in1=oT[:, gcols], op=ALU.add), reads=[tbo_], writes=[toT])
                            S.op("dve", lambda e: e.tensor_tensor(out=oT[:, gcols], in0=bo_[:, 128:256], in1=oT[:, gcols], op=ALU.add), reads=[tbo_], writes=[toT])
                S.dma("sp", vf[:], self.YT[3072 + h * 128:3072 + (h + 1) * 128, :], reads=[tvb], writes=[thg])
                S.op("act", lambda e: e.activation(out=vf[:], in_=vf[:], func=AF.Silu), reads=[thg], writes=[thg])
                for pc in range(T // PW):
                    i = pc % 2
                    pcs = slice(pc * PW, (pc + 1) * PW)
                    bk, tbk = bA[pc % 2]
                    S.op("pool", lambda e: e.tensor_tensor(out=sqp[i][:], in0=oT[:, pcs], in1=oT[:, pcs], op=ALU.mult), reads=[toT], writes=[tsqp[i]])
                    S.op("pe", lambda e: e.matmul(bk[:, :PW], self.onesb[:], sqp[i][:], start=True, stop=True), reads=[tsqp[i], self.t_c], writes=[tbk])
                    S.op("act", lambda e: e.activation(out=rsp[i][:], in_=bk[:, :PW], func=AF.Sqrt, bias=self.epsln[:, 1:2], scale=1.0 / 128),
                         reads=[tbk, self.t_c], writes=[trsp[i]])
                    S.op("dve", lambda e: e.reciprocal(out=rsp[i][:], in_=rsp[i][:]), reads=[trsp[i]], writes=[trsp[i]])
                    S.op("dve", lambda e: e.tensor_tensor(out=t1p[i][:], in0=oT[:, pcs], in1=rsp[i][:], op=ALU.mult), reads=[toT, trsp[i]], writes=[tt1p[i]])
                    S.op("pool", lambda e: e.tensor_tensor(out=t1p[i][:], in0=t1p[i][:], in1=vf[:, pcs], op=ALU.mult), reads=[thg], writes=[tt1p[i]])
                    S.op("act", lambda e: e.activation(out=mbp[i][:], in_=t1p[i][:], func=AF.Copy, scale=gcol[:, h:h + 1]), reads=[tt1p[i], tcst], writes=[tmbp[i]])
                    S.dma("pool", self.mergedT[1024 + h * 128:1024 + (h + 1) * 128, pcs], mbp[i][:], reads=[tmbp[i]], writes=[Tk()])
        S.barrier()


_CACHE = {}


def _consts(T):
    f64 = np.float64
    n_rows = T // 64
    rows = np.repeat(np.arange(n_rows, dtype=np.float32), 64)
    cols = np.tile(np.arange(64, dtype=np.float32), n_rows)
    inv_freq = (np.float32(10000.0) ** (-np.arange(32, dtype=np.float32) / np.float32(32))).astype(np.float32)
    ang = np.concatenate([rows[:, None] * inv_freq, cols[:, None] * inv_freq], axis=-1).astype(np.float32)
    cos16 = np.ascontiguousarray(np.tile(np.cos(ang).astype(np.float32), (1, 16)))
    sin16 = np.ascontiguousarray(np.tile(np.sin(ang).astype(np.float32), (1, 16)))
    lg = np.log1p(-np.exp2(-5.0 - np.arange(8, dtype=f64)))
    pos = np.arange(128, dtype=f64)
    sc = 128.0 ** -0.5
    rdec = np.zeros((128, 32), f64)
    for h in range(8):
        rdec[:, h] = np.exp((127.0 - pos) * lg[h]) * sc
        rdec[:, 8 + h] = np.exp(pos * lg[h]) * sc
        rdec[:, 16 + h] = np.exp((pos + 1.0) * lg[h])
        rdec[:, 24 + h] = np.exp((128.0 - pos) * lg[h])
    dsym = np.zeros((128, 8, 128), f64)
    ad = np.abs(pos[:, None] - pos[None, :])
    for h in range(8):
        dsym[:, h, :] = np.exp(ad * lg[h]) * sc
    blk = np.arange(128) // 32
    same = blk[:, None] == blk[None, :]
    s_i = np.arange(128)[:, None]
    t_i = np.arange(128)[None, :]
    mbd = np.zeros((128, 2, 128), np.float32)
    mbd[:, 0, :] = (same & (s_i <= t_i))
    mbd[:, 1, :] = (same & (s_i >= t_i))
    sel = np.zeros((8, 8, 128), np.float32)
    for e in range(8):
        sel[e, e, :] = 1.0
    return dict(cos16=cos16, sin16=sin16, rdec=rdec.astype(np.float32), dsymT=dsym.astype(np.float32), maskBD=mbd, sel=sel)


def _params(inp):
    f = lambda a: np.asarray(a, dtype=np.float32)
    pp = np.zeros((128, 400), np.float32)
    rp = np.zeros((2, 128, 4352), np.float32)
    for l in range(2):
        b = l * 200
        pp[:, b + 0:b + 32] = f(inp["ln1_g"])[l].reshape(32, 128).T
        pp[:, b + 32:b + 64] = f(inp["ln1_b"])[l].reshape(32, 128).T
        pp[:, b + 64:b + 96] = f(inp["ln2_g"])[l].reshape(32, 128).T
        pp[:, b + 96:b + 128] = f(inp["ln2_b"])[l].reshape(32, 128).T
        pp[:, b + 128:b + 160] = f(inp["merge_scale"])[l].reshape(32, 128).T
        pp[:, b + 160:b + 168] = f(inp["hgrn_out_norm"])[l].reshape(8, 128).T
        pp[:, b + 168:b + 176] = f(inp["ret_out_norm"])[l].reshape(8, 128).T
        pp[:, b + 176:b + 184] = f(inp["hgrn_lower_bound"])[0].reshape(8, 128).T
        pp[:, b + 184:b + 192] = f(inp["hgrn_lower_bound"])[1].reshape(8, 128).T
        row = np.concatenate([np.tile(f(inp["attn_q_norm"])[l], 8), np.tile(f(inp["attn_k_norm"])[l], 2),
                              f(inp["gmlp_v_norm_g"])[l], f(inp["gmlp_v_norm_b"])[l], f(inp["merge_scale"])[l][2048:3072]])
        rp[l] = np.broadcast_to(row[None, :], (128, 4352))
    wsT = np.ascontiguousarray(f(inp["gmlp_w_s"]).transpose(0, 3, 1, 2))
    bsT = np.ascontiguousarray(f(inp["gmlp_b_s"]).transpose(0, 2, 1))
    router = np.ascontiguousarray(f(inp["moe_router"])[0].reshape(32, 128, 8).transpose(1, 0, 2))
    return dict(
        pp=pp, rp=rp, wsT=wsT, bsT=bsT, router=router,
        w_in=f(inp["w_in"]), w_o=f(inp["w_o"]),
        ffn_g=f(inp["ffn_w_gate"])[0], ffn_u=f(inp["ffn_w_up"])[0], ffn_d=f(inp["ffn_w_down"])[0],
        moe_g=f(inp["moe_w_gate"])[0], moe_u=f(inp["moe_w_up"])[0],
        moe_d=f(inp["moe_w_down"])[0].reshape(NE * DFE, D),
    )


def kernel(**inputs):
    T = 8192
    if T not in _CACHE:
        _CACHE[T] = Prog(T)
    prog = _CACHE[T]
    shared = _params(inputs)
    shared.update(_consts(T))
    xp = np.asarray(inputs["x_prompt"], dtype=np.float32)
    xs = np.asarray(inputs["x_sample"], dtype=np.float32)
    seqs = [xp[0], xp[1], xs[0]]
    in_maps = []
    for c in range(NCORES):
        m = dict(shared)
        m["xT"] = np.ascontiguousarray(seqs[c].T)
        in_maps.append(m)
    res = run_bass_kernel_spmd(prog.nc, in_maps, core_ids=list(range(NCORES)))
    outs = [np.ascontiguousarray(np.asarray(res.results[c]["yT"], dtype=np.float32).T) for c in range(NCORES)]
    y_prompt = np.stack([outs[0], outs[1]], axis=0)
    y_sample = outs[2][None]
    return (y_prompt, y_sample)
```

```python
import contextlib
import numpy as np
import concourse.bass as bass
import concourse.mybir as mybir
from concourse.bass_utils import run_bass_kernel_spmd

F32 = mybir.dt.float32
BF16 = mybir.dt.bfloat16
AF = mybir.ActivationFunctionType
ALU = mybir.AluOpType
AX = mybir.AxisListType

D = 4096
DIN = 12800
DFF = 5632
NE = 8
DFE = 1024
HD = 128
OFF = dict(aq=0, ak=1024, av=1280, hq=1536, hff=2560, hfb=3584, hi=4608, hg=5632,
           gu=6656, gv=7680, rq=8704, rk=9728, rv=10752, rg=11776)
ALPHA = 4.0 ** 0.25
LN_EPS = 1e-5
NORM_EPS = 1e-6
NCORES = 3


class Tk:
    __slots__ = ("w", "r")

    def __init__(self):
        self.w = None
        self.r = {}


class Sched:
    NDS = 16

    def __init__(self, nc, es):
        self.nc = nc
        self.eng = {"pe": nc.tensor, "act": nc.scalar, "dve": nc.vector, "pool": nc.gpsimd, "sp": nc.sync}
        self.sem = {k: es.enter_context(nc.semaphore("s_" + k)) for k in self.eng}
        self.cnt = {k: 0 for k in self.eng}
        self.seen = {k: {} for k in self.eng}
        self.dsem = {}
        self.dval = {}
        self.dnext = {}
        for q in ("sp", "pool", "act"):
            self.dsem[q] = [es.enter_context(nc.semaphore("d_%s%d" % (q, i))) for i in range(self.NDS)]
            self.dval[q] = [0] * self.NDS
            self.dnext[q] = 0
        self.ninst = 0

    def _semobj(self, k):
        if isinstance(k, tuple):
            return self.dsem[k[0]][k[1]]
        return self.sem[k]

    def _wait(self, e, deps):
        eng = self.eng[e]
        seen = self.seen[e]
        for k, v in deps.items():
            if k == e and e in ("pe", "sp"):
                continue
            if seen.get(k, 0) >= v:
                continue
            eng.wait_ge(self._semobj(k), v)
            seen[k] = v

    @staticmethod
    def _deps(reads, writes):
        deps = {}
        for t in reads:
            if t.w is not None:
                k, v = t.w
                if deps.get(k, 0) < v:
                    deps[k] = v
        for t in writes:
            if t.w is not None:
                k, v = t.w
                if deps.get(k, 0) < v:
                    deps[k] = v
            for k, v in t.r.items():
                if deps.get(k, 0) < v:
                    deps[k] = v
        return deps

    @staticmethod
    def _mark(tok, reads, writes):
        k, v = tok
        for t in reads:
            t.r[k] = v
        for t in writes:
            t.w = tok
            t.r = {}

    def op(self, e, fn, reads=(), writes=()):
        self._wait(e, self._deps(reads, writes))
        ins = fn(self.eng[e])
        self.cnt[e] += 1
        ins.then_inc(self.sem[e], 1)
        self._mark((e, self.cnt[e]), reads, writes)
        self.ninst += 1
        return ins

    def dma(self, q, out, in_, reads=(), writes=()):
        deps = self._deps(reads, writes)
        i = self.dnext[q]
        self.dnext[q] = (i + 1) % self.NDS
        key = (q, i)
        if self.dval[q][i] > 0 and deps.get(key, 0) < self.dval[q][i]:
            deps[key] = self.dval[q][i]
        self._wait(q, deps)
        ins = self.eng[q].dma_start(out=out, in_=in_)
        self.dval[q][i] += 16
        ins.then_inc(self.dsem[q][i], 16)
        self._mark((key, self.dval[q][i]), reads, writes)
        self.ninst += 1
        return ins

    def barrier(self):
        deps = {k: self.cnt[k] for k in self.eng if self.cnt[k] > 0}
        for q in self.dsem:
            for i in range(self.NDS):
                if self.dval[q][i] > 0:
                    deps[(q, i)] = self.dval[q][i]
        for e in self.eng:
            self._wait(e, deps)


class Prog:
    def __init__(self, T, dbg=(), layers=2, phases=None):
        self.T = T
        self.dbg = set(dbg)
        self.layers = layers
        self.phases = phases
        self.nc = nc = bass.Bass("TRN2", target_bir_lowering=False)
        self.es = es = contextlib.ExitStack()
        self.S = Sched(nc, es)
        self.rr = 0
        self.build()
        es.close()

    def din(self, name, shape, dt=F32):
        return self.nc.dram_tensor(name, list(shape), dt, kind="ExternalInput").ap()

    def dscr(self, name, shape, dt, out=False):
        kind = "ExternalOutput" if (out or name in self.dbg) else "Internal"
        return self.nc.dram_tensor(name, list(shape), dt, kind=kind).ap()

    def sb(self, st, name, shape, dt):
        self.uid = getattr(self, "uid", 0) + 1
        return st.enter_context(self.nc.sbuf_tensor("%s_%d" % (name, self.uid), list(shape), dt))

    def any2(self):
        self.rr += 1
        return ("act", "dve")[self.rr % 2]

    def any3(self):
        self.rr += 1
        return ("act", "dve", "pool")[self.rr % 3]

    @staticmethod
    def copy(e, eng, out, in_):
        if e == "act":
            return eng.activation(out=out, in_=in_, func=AF.Copy)
        return eng.tensor_copy(out=out, in_=in_)

    def convert(self, src, R, C, dst_fn, piece=2048):
        S = self.S
        with contextlib.ExitStack() as st:
            NB = 3
            stg = [self.sb(st, "cv_f%d" % i, [128, piece], F32) for i in range(NB)]
            stb = [self.sb(st, "cv_b%d" % i, [128, piece], BF16) for i in range(NB)]
            tf = [Tk() for _ in range(NB)]
            tb = [Tk() for _ in range(NB)]
            i = 0
            for rt in range(R // 128):
                for c0 in range(0, C, piece):
                    n = min(piece, C - c0)
                    b = i % NB
                    i += 1
                    S.dma("sp", stg[b][:, :n], src[rt * 128:(rt + 1) * 128, c0:c0 + n], writes=[tf[b]])
                    e = self.any3()
                    S.op(e, lambda eng, b=b, n=n, e=e: self.copy(e, eng, stb[b][:, :n], stg[b][:, :n]),
                         reads=[tf[b]], writes=[tb[b]])
                    dst, view = dst_fn(rt, c0, n)
                    S.dma("pool", dst, view(stb[b][:, :n]), reads=[tb[b]], writes=[Tk()])
        S.barrier()

    def conv_blocked(self, src, K, N, Wb, CB, off=0, w=None, cb_base=0):
        w = w or CB

        def dst_fn(rt, c0, n):
            nb = n // w
            cb0 = cb_base + c0 // w
            dst = Wb[cb0:cb0 + nb, :, rt, off:off + w].rearrange("cb p c -> p cb c")
            return dst, (lambda v: v.rearrange("p (cb c) -> p cb c", c=w))
        piece = 2048 if N % 2048 == 0 or N > 2048 else N
        self.convert(src, K, N, dst_fn, piece=piece)

    def conv_plain(self, src, R, C, dst):
        def dst_fn(rt, c0, n):
            return dst[rt * 128:(rt + 1) * 128, c0:c0 + n], (lambda v: v)
        self.convert(src, R, C, dst_fn, piece=min(2048, C))

    def gemm(self, actT, KC, Wb, NCB, CB, form, epi, TT, banks, name):
        S = self.S
        T = self.T
        TT = min(TT, T)
        TS = min(512, TT)
        nb = len(banks)
        bi = 0
        with contextlib.ExitStack() as st:
            act = self.sb(st, name + "_act", [128, KC, TT], BF16)
            G = 8
            ngr = (KC + G - 1) // G
            t_act = [Tk() for _ in range(ngr)]
            wbuf = [self.sb(st, name + "_w%d" % i, [128, KC, CB], BF16) for i in range(2)]
            t_w = [Tk(), Tk()]
            aT = actT.rearrange("(kc p) t -> p kc t", p=128)
            wi = 0
            for st_i in range(T // TT):
                for g in range(ngr):
                    k0, k1 = g * G, min(KC, (g + 1) * G)
                    S.dma("sp", act[:, k0:k1, :], aT[:, k0:k1, st_i * TT:(st_i + 1) * TT], writes=[t_act[g]])
                for cb in range(NCB):
                    wb = wi % 2
                    wi += 1
                    S.dma("sp", wbuf[wb][:], Wb[cb], writes=[t_w[wb]])
                    if form == "A":
                        for ts in range(TT // 128):
                            bank, tb = banks[bi % nb]
                            bi += 1
                            for kc in range(KC):
                                S.op("pe", lambda e, kc=kc, ts=ts, wb=wb, bank=bank: e.matmul(
                                    bank[:, :CB], act[:, kc, ts * 128:(ts + 1) * 128], wbuf[wb][:, kc, :],
                                    start=(kc == 0), stop=(kc == KC - 1)),
                                    reads=[t_act[kc // G], t_w[wb]], writes=[tb])
                            epi((bank, tb), st_i * TT + ts * 128, cb)
                    else:
                        for ts in range(TT // TS):
                            subs = []
                            for sub in range(CB // 128):
                                bank, tb = banks[bi % nb]
                                bi += 1
                                for kc in range(KC):
                                    S.op("pe", lambda e, kc=kc, ts=ts, wb=wb, bank=bank, sub=sub: e.matmul(
                                        bank[:, :TS], wbuf[wb][:, kc, sub * 128:(sub + 1) * 128],
                                        act[:, kc, ts * TS:(ts + 1) * TS],
                                        start=(kc == 0), stop=(kc == KC - 1)),
                                        reads=[t_act[kc // G], t_w[wb]], writes=[tb])
                                subs.append((bank, tb))
                            epi(subs, st_i * TT + ts * TS, cb)
        S.barrier()


    def build(self):
        nc, S, T = self.nc, self.S, self.T
        es = self.es
        L = self.layers
        ph = self.phases
        NPP, NRP = 400, 4352
        self.xT = self.din("xT", [D, T])
        self.w_in = self.din("w_in", [2, D, DIN])
        self.w_o = self.din("w_o", [2, D, D])
        self.ffn_g = self.din("ffn_g", [D, DFF])
        self.ffn_u = self.din("ffn_u", [D, DFF])
        self.ffn_d = self.din("ffn_d", [DFF, D])
        self.moe_g = self.din("moe_g", [NE, D, DFE])
        self.moe_u = self.din("moe_u", [NE, D, DFE])
        self.moe_d = self.din("moe_d", [NE * DFE, D])
        self.router = self.din("router", [128, 32, NE])
        self.pp_in = self.din("pp", [128, NPP])
        self.rp_in = self.din("rp", [2, 128, NRP])
        self.wsT_in = self.din("wsT", [2, 128, 8, 128])
        self.bsT_in = self.din("bsT", [2, 128, 8])
        self.cos_in = self.din("cos16", [T, 1024])
        self.sin_in = self.din("sin16", [T, 1024])
        self.dsym_in = self.din("dsymT", [128, 8, 128])
        self.rdec_in = self.din("rdec", [128, 32])
        self.mbd_in = self.din("maskBD", [128, 2, 128])
        self.sel_in = self.din("sel", [8, 8, 128])
        self.yT = self.dscr("yT", [D, T], F32, out=True)
        self.xTb = self.dscr("xTb", [D, T], BF16)
        self.winA = [self.dscr("winA%d" % l, [17, 128, 32, 512], BF16) for l in range(2)]
        self.winB = [self.dscr("winB%d" % l, [16, 128, 32, 256], BF16) for l in range(2)]
        self.wob = [self.dscr("wob%d" % l, [16, 128, 32, 256], BF16) for l in range(2)]
        self.wgub = self.dscr("wgub", [44, 128, 32, 256], BF16)
        self.wdb = self.dscr("wdb", [16, 128, 44, 256], BF16)
        self.wmgub = self.dscr("wmgub", [64, 128, 32, 256], BF16)
        self.wmdb = self.dscr("wmdb", [16, 128, 64, 256], BF16)
        self.Y_a = self.dscr("Y_a", [T, 1536], F32)
        self.Y_hi = self.dscr("Y_hi", [T, 1024], F32)
        self.Y_g = self.dscr("Y_g", [T, 2048], F32)
        self.Y_r = self.dscr("Y_r", [T, 4096], F32)
        self.YT = self.dscr("YT", [D, T], F32)
        self.aqkT = self.dscr("aqkT", [10, 128, T], BF16)
        self.rqkT = self.dscr("rqkT", [16, 128, T], BF16)
        self.RK = self.dscr("RK", [3, T, 1024], BF16)
        self.mergedT = self.dscr("mergedT", [D, T], BF16)
        self.zT = self.dscr("zT", [D, T], F32)
        self.x1T = self.dscr("x1T", [D, T], F32)
        self.x1Tb = self.dscr("x1Tb", [D, T], BF16)
        self.x2T = self.dscr("x2T", [D, T], F32)
        self.x2Tb = self.dscr("x2Tb", [D, T], BF16)
        self.hT = self.dscr("hT", [NE * DFE, T], BF16)
        self.Grep = self.dscr("Grep", [NE, 128, T], F32)
        self.banks = []
        for i in range(6):
            p = es.enter_context(nc.psum_tensor("ps%d" % i, [128, 512], F32))
            self.banks.append((p, Tk()))
        self.bbanks = []
        for i in range(2):
            p = es.enter_context(nc.psum_tensor("pb%d" % i, [128, 1024], BF16))
            self.bbanks.append((p, Tk()))
        self.pp = self.sb(es, "pp", [128, NPP], F32)
        self.t_pp = Tk()
        self.idf = self.sb(es, "idf", [128, 128], F32)
        self.idb = self.sb(es, "idb", [128, 128], BF16)
        self.onesb = self.sb(es, "onesb", [128, 128], BF16)
        self.epsln = self.sb(es, "epsln", [128, 2], F32)
        self.t_c = Tk()
        S.dma("sp", self.pp[:], self.pp_in, writes=[self.t_pp])
        S.op("pool", lambda e: e.memset(self.idf[:], 0.0), writes=[self.t_c])
        S.op("pool", lambda e: e.affine_select(out=self.idf[:], in_=self.idf[:], pattern=[[-1, 128]],
                                               compare_op=ALU.not_equal, fill=1.0, base=0, channel_multiplier=1),
             writes=[self.t_c])
        S.op("pool", lambda e: e.tensor_copy(out=self.idb[:], in_=self.idf[:]), writes=[self.t_c])
        S.op("pool", lambda e: e.memset(self.onesb[:], 1.0), writes=[self.t_c])
        S.op("pool", lambda e: e.memset(self.epsln[:, 0:1], LN_EPS), writes=[self.t_c])
        S.op("pool", lambda e: e.memset(self.epsln[:, 1:2], NORM_EPS), writes=[self.t_c])
        S.barrier()

        def on(p):
            return ph is None or p in ph
        if on("conv"):
            self.conv_plain(self.xT, D, T, self.xTb)
            for l in range(L):
                w = self.w_in[l]
                self.conv_blocked(w[:, 0:1536], D, 1536, self.winA[l], 512, cb_base=0)
                self.conv_blocked(w[:, 4608:5632], D, 1024, self.winA[l], 512, cb_base=3)
                self.conv_blocked(w[:, 6656:12800], D, 6144, self.winA[l], 512, cb_base=5)
                self.conv_blocked(w[:, 1536:4608], D, 3072, self.winB[l], 256, cb_base=0)
                self.conv_blocked(w[:, 5632:6656], D, 1024, self.winB[l], 256, cb_base=12)
                self.conv_blocked(self.w_o[l], D, D, self.wob[l], 256)
            if on("ffn"):
                self.conv_blocked(self.ffn_g, D, DFF, self.wgub, 256, off=0, w=128)
                self.conv_blocked(self.ffn_u, D, DFF, self.wgub, 256, off=128, w=128)
                self.conv_blocked(self.ffn_d, DFF, D, self.wdb, 256)
            if L > 1 and on("moe"):
                for e_ in range(NE):
                    self.conv_blocked(self.moe_g[e_], D, DFE, self.wmgub, 256, off=0, w=128, cb_base=e_ * 8)
                    self.conv_blocked(self.moe_u[e_], D, DFE, self.wmgub, 256, off=128, w=128, cb_base=e_ * 8)
                self.conv_blocked(self.moe_d, NE * DFE, D, self.wmdb, 256)
        xF, xB = self.xT, self.xTb
        for l in range(L):
            if on("inproj"):
                self.in_proj(l, xB)
            if on("gmlp"):
                self.mix_gmlp(l)
            if on("attn"):
                self.mix_attn(l)
            if on("ret"):
                self.mix_ret(l)
            if on("hgrn"):
                self.mix_hgrn(l)
            if on("wo"):
                self.resid_gemm(self.mergedT, 32, self.wob[l], xF, 1024, "wo")
                self.ln_pass(self.zT, l * 200 + 0, l * 200 + 32, self.x1T, self.x1Tb)
            if l == 0:
                if on("ffn"):
                    self.ffn_up(self.x1Tb, self.wgub, DFF // 128, None)
                    self.resid_gemm(self.hT, DFF // 128, self.wdb, self.x1T, 1024, "fd")
            else:
                if on("moe"):
                    self.router_pass(self.x1T)
                    self.ffn_up(self.x1Tb, self.wmgub, NE * DFE // 128, self.Grep)
                    self.resid_gemm(self.hT, NE * DFE // 128, self.wmdb, self.x1T, 512, "md")
            if on("ffn") or on("moe"):
                last = (l == L - 1)
                self.ln_pass(self.zT, l * 200 + 64, l * 200 + 96, self.yT if last else self.x2T,
                             None if last else self.x2Tb)
            xF, xB = self.x2T, self.x2Tb
        S.barrier()

    def in_proj(self, l, actT):
        S = self.S
        colA = ([(self.Y_a, 512 * i) for i in range(3)] + [(self.Y_hi, 512 * i) for i in range(2)]
                + [(self.Y_g, 512 * i) for i in range(4)] + [(self.Y_r, 512 * i) for i in range(8)])
        with contextlib.ExitStack() as st:
            NSTG = 4
            stg = [self.sb(st, "p1_s%d" % i, [128, 512], F32) for i in range(NSTG)]
            ts = [Tk() for _ in range(NSTG)]
            cnt = [0]

            def epi(bt, tok0, cb):
                bank, tb = bt
                i = cnt[0] % NSTG
                cnt[0] += 1
                e = self.any2()
                S.op(e, lambda eng: self.copy(e, eng, stg[i][:], bank[:]), reads=[tb], writes=[ts[i]])
                yt_, yc_ = colA[cb]
                S.dma("pool", yt_[tok0:tok0 + 128, yc_:yc_ + 512], stg[i][:], reads=[ts[i]], writes=[Tk()])
            self.gemm(actT, 32, self.winA[l], 17, 512, "A", epi, 1024, self.banks[:4], "p1a")
        with contextlib.ExitStack() as st:
            NSTG = 4
            TS = min(512, self.T)
            stg = [self.sb(st, "p1b_s%d" % i, [128, TS], F32) for i in range(NSTG)]
            ts = [Tk() for _ in range(NSTG)]
            cnt = [0]

            def epi(subs, tok0, cb):
                for sub, (bank, tb) in enumerate(subs):
                    i = cnt[0] % NSTG
                    cnt[0] += 1
                    e = self.any2()
                    S.op(e, lambda eng, i=i, bank=bank, e=e: self.copy(e, eng, stg[i][:], bank[:, :TS]), reads=[tb], writes=[ts[i]])
                    r0 = (cb * 2 + sub) * 128
                    S.dma("pool", self.YT[r0:r0 + 128, tok0:tok0 + TS], stg[i][:], reads=[ts[i]], writes=[Tk()])
            self.gemm(actT, 32, self.winB[l], 16, 256, "B", epi, 1024, self.banks[:4], "p1b")

    def resid_gemm(self, actT, KC, Wb, xresT, TT, name):
        S = self.S
        TS = min(512, self.T)
        with contextlib.ExitStack() as st:
            NSTG = 3
            xr = [self.sb(st, name + "_x%d" % i, [128, TS], F32) for i in range(NSTG)]
            zt = [self.sb(st, name + "_z%d" % i, [128, TS], F32) for i in range(NSTG)]
            tx = [Tk() for _ in range(NSTG)]
            tz = [Tk() for _ in range(NSTG)]
            cnt = [0]

            def epi(subs, tok0, cb):
                for sub, (bank, tb) in enumerate(subs):
                    i = cnt[0] % NSTG
                    cnt[0] += 1
                    r0 = (cb * 2 + sub) * 128
                    S.dma("sp", xr[i][:], xresT[r0:r0 + 128, tok0:tok0 + TS], writes=[tx[i]])
                    S.op("dve", lambda e, i=i, bank=bank: e.scalar_tensor_tensor(
                        out=zt[i][:], in0=xr[i][:], scalar=ALPHA, in1=bank[:, :TS], op0=ALU.mult, op1=ALU.add),
                        reads=[tx[i], tb], writes=[tz[i]])
                    S.dma("pool", self.zT[r0:r0 + 128, tok0:tok0 + TS], zt[i][:], reads=[tz[i]], writes=[Tk()])
            self.gemm(actT, KC, Wb, 16, 256, "B", epi, TT, self.banks[:4], name)

    def ffn_up(self, actT, Wb, NCB, grep):
        S = self.S
        TS = min(512, self.T)
        with contextlib.ExitStack() as st:
            NSTG = 3
            sg = [self.sb(st, "fu_s%d" % i, [128, TS], F32) for i in range(NSTG)]
            h1 = [self.sb(st, "fu_h%d" % i, [128, TS], F32) for i in range(NSTG)]
            gr = [self.sb(st, "fu_g%d" % i, [128, TS], F32) for i in range(NSTG)]
            hb = [self.sb(st, "fu_b%d" % i, [128, TS], BF16) for i in range(NSTG)]
            tsg = [Tk() for _ in range(NSTG)]
            th1 = [Tk() for _ in range(NSTG)]
            tgr = [Tk() for _ in range(NSTG)]
            thb = [Tk() for _ in range(NSTG)]
            cnt = [0]

            def epi(subs, tok0, cb):
                (bg, tg), (bu, tu) = subs
                i = cnt[0] % NSTG
                cnt[0] += 1
                S.op("act", lambda e: e.activation(out=sg[i][:], in_=bg[:, :TS], func=AF.Silu), reads=[tg], writes=[tsg[i]])
                if grep is None:
                    S.op("dve", lambda e: e.tensor_tensor(out=hb[i][:], in0=sg[i][:], in1=bu[:, :TS], op=ALU.mult),
                         reads=[tsg[i], tu], writes=[thb[i]])
                else:
                    S.dma("sp", gr[i][:], grep[cb // 8, :, tok0:tok0 + TS], writes=[tgr[i]])
                    S.op("dve", lambda e: e.tensor_tensor(out=h1[i][:], in0=sg[i][:], in1=bu[:, :TS], op=ALU.mult),
                         reads=[tsg[i], tu], writes=[th1[i]])
                    S.op("pool", lambda e: e.tensor_tensor(out=hb[i][:], in0=h1[i][:], in1=gr[i][:], op=ALU.mult),
                         reads=[th1[i], tgr[i]], writes=[thb[i]])
                S.dma("pool", self.hT[cb * 128:(cb + 1) * 128, tok0:tok0 + TS], hb[i][:], reads=[thb[i]], writes=[Tk()])
            self.gemm(actT, 32, Wb, NCB, 256, "B", epi, 1024, self.banks[:4], "fu")

    def ln_pass(self, zT, gcol, bcol, outF, outB):
        S = self.S
        T = self.T
        TS = min(512, T)
        bs_, bq_ = self.banks[4], self.banks[5]
        zTr = zT.rearrange("(c p) t -> p c t", p=128)
        with contextlib.ExitStack() as st:
            z = [self.sb(st, "ln_z%d" % i, [128, 32, TS], F32) for i in range(2)]
            tz = [[Tk() for _ in range(4)] for _ in range(2)]
            NR = 3
            zb = [self.sb(st, "ln_zb%d" % i, [128, TS], BF16) for i in range(NR)]
            zq = [self.sb(st, "ln_zq%d" % i, [128, TS], BF16) for i in range(NR)]
            tzb = [Tk() for _ in range(NR)]
            tzq = [Tk() for _ in range(NR)]
            mean = self.sb(st, "ln_mean", [128, TS], F32)
            msq = self.sb(st, "ln_msq", [128, TS], F32)
            rstd = self.sb(st, "ln_rstd", [128, TS], F32)
            tst = Tk()
            t1 = [self.sb(st, "ln_t%d" % i, [128, TS], F32) for i in range(NR)]
            of = [self.sb(st, "ln_of%d" % i, [128, TS], F32) for i in range(NR)]
            ob = [self.sb(st, "ln_ob%d" % i, [128, TS], BF16) for i in range(NR)]
            tt1 = [Tk() for _ in range(NR)]
            tof = [Tk() for _ in range(NR)]
            tob = [Tk() for _ in range(NR)]
            k = 0
            for tt in range(T // TS):
                zi = tt % 2
                tok = slice(tt * TS, (tt + 1) * TS)
                for g in range(4):
                    S.dma("sp", z[zi][:, g * 8:(g + 1) * 8, :], zTr[:, g * 8:(g + 1) * 8, tok], writes=[tz[zi][g]])
                for c in range(32):
                    i = k % NR
                    k += 1
                    S.op("act", lambda e: e.activation(out=zb[i][:], in_=z[zi][:, c, :], func=AF.Copy),
                         reads=[tz[zi][c // 8]], writes=[tzb[i]])
                    S.op("act", lambda e: e.activation(out=zq[i][:], in_=z[zi][:, c, :], func=AF.Square),
                         reads=[tz[zi][c // 8]], writes=[tzq[i]])
                    S.op("pe", lambda e: e.matmul(bs_[0][:, :TS], self.onesb[:], zb[i][:], start=(c == 0), stop=(c == 31)),
                         reads=[tzb[i], self.t_c], writes=[bs_[1]])
                    S.op("pe", lambda e: e.matmul(bq_[0][:, :TS], self.onesb[:], zq[i][:], start=(c == 0), stop=(c == 31)),
                         reads=[tzq[i], self.t_c], writes=[bq_[1]])
                S.op("dve", lambda e: e.tensor_scalar(out=mean[:], in0=bs_[0][:, :TS], scalar1=1.0 / D, scalar2=1.0, op0=ALU.mult, op1=ALU.mult),
                     reads=[bs_[1]], writes=[tst])
                S.op("dve", lambda e: e.tensor_tensor(out=msq[:], in0=mean[:], in1=mean[:], op=ALU.mult), reads=[tst], writes=[tst])
                S.op("dve", lambda e: e.scalar_tensor_tensor(out=msq[:], in0=bq_[0][:, :TS], scalar=1.0 / D, in1=msq[:],
                                                            op0=ALU.mult, op1=ALU.subtract), reads=[bq_[1], tst], writes=[tst])
                S.op("act", lambda e: e.activation(out=rstd[:], in_=msq[:], func=AF.Sqrt, bias=self.epsln[:, 0:1], scale=1.0),
                     reads=[tst, self.t_c], writes=[tst])
                S.op("dve", lambda e: e.reciprocal(out=rstd[:], in_=rstd[:]), reads=[tst], writes=[tst])
                for c in range(32):
                    i = k % NR
                    k += 1
                    S.op("dve", lambda e: e.tensor_tensor(out=t1[i][:], in0=z[zi][:, c, :], in1=mean[:], op=ALU.subtract),
                         reads=[tz[zi][c // 8], tst], writes=[tt1[i]])
                    S.op("dve", lambda e: e.tensor_tensor(out=t1[i][:], in0=t1[i][:], in1=rstd[:], op=ALU.mult),
                         reads=[tst], writes=[tt1[i]])
                    S.op("act", lambda e: e.activation(out=of[i][:], in_=t1[i][:], func=AF.Identity,
                                                       bias=self.pp[:, bcol + c:bcol + c + 1], scale=self.pp[:, gcol + c:gcol + c + 1]),
                         reads=[tt1[i], self.t_pp], writes=[tof[i]])
                    S.dma("act", outF[c * 128:(c + 1) * 128, tok], of[i][:], reads=[tof[i]], writes=[Tk()])
                    if outB is not None:
                        S.op("pool", lambda e: e.tensor_copy(out=ob[i][:], in_=of[i][:]), reads=[tof[i]], writes=[tob[i]])
                        S.dma("act", outB[c * 128:(c + 1) * 128, tok], ob[i][:], reads=[tob[i]], writes=[Tk()])
        S.barrier()

    def router_pass(self, x1T):
        S = self.S
        T = self.T
        TS = min(512, T)
        xr_ = x1T.rearrange("(c p) t -> p c t", p=128)
        bl, bt, br = self.banks[0], self.banks[1], self.banks[2]
        with contextlib.ExitStack() as st:
            wr = self.sb(st, "rt_w", [128, 32, NE], F32)
            sel = self.sb(st, "rt_sel", [8, 8, 128], F32)
            tw = Tk()
            S.dma("sp", wr[:], self.router, writes=[tw])
            S.dma("sp", sel[:], self.sel_in, writes=[tw])
            xr = [self.sb(st, "rt_x%d" % i, [128, 32, 128], F32) for i in range(2)]
            tx = [Tk(), Tk()]
            sm = self.sb(st, "rt_sm", [128, 64], F32)
            tsm = Tk()
            gT = self.sb(st, "rt_gT", [8, TS], F32)
            tgT = Tk()
            gr = [self.sb(st, "rt_gr%d" % i, [128, TS], F32) for i in range(2)]
            tgr = [Tk(), Tk()]
            lg, eq1, l2, eq2, g1, gt = (sm[:, 0:8], sm[:, 8:16], sm[:, 16:24], sm[:, 24:32], sm[:, 32:40], sm[:, 40:48])
            m1, m2, dl, w1, w2 = (sm[:, 48:49], sm[:, 49:50], sm[:, 50:51], sm[:, 51:52], sm[:, 52:53])
            npg = TS // 128
            k = 0
            for n in range(T // 128):
                i = n % 2
                S.dma("sp", xr[i][:], xr_[:, :, n * 128:(n + 1) * 128], writes=[tx[i]])
                for c in range(32):
                    S.op("pe", lambda e: e.matmul(bl[0][:, 0:NE], xr[i][:, c, :], wr[:, c, :], start=(c == 0), stop=(c == 31)),
                         reads=[tx[i], tw], writes=[bl[1]])
                V = "dve"
                S.op(V, lambda e: e.tensor_copy(out=lg, in_=bl[0][:, 0:NE]), reads=[bl[1]], writes=[tsm])
                S.op(V, lambda e: e.tensor_reduce(out=m1, in_=lg, axis=AX.X, op=ALU.max), reads=[tsm], writes=[tsm])
                S.op(V, lambda e: e.tensor_scalar(out=eq1, in0=lg, scalar1=m1, scalar2=1.0, op0=ALU.is_equal, op1=ALU.mult), reads=[tsm], writes=[tsm])
                S.op(V, lambda e: e.scalar_tensor_tensor(out=l2, in0=eq1, scalar=-1e30, in1=lg, op0=ALU.mult, op1=ALU.add), reads=[tsm], writes=[tsm])
                S.op(V, lambda e: e.tensor_reduce(out=m2, in_=l2, axis=AX.X, op=ALU.max), reads=[tsm], writes=[tsm])
                S.op(V, lambda e: e.tensor_scalar(out=eq2, in0=l2, scalar1=m2, scalar2=1.0, op0=ALU.is_equal, op1=ALU.mult), reads=[tsm], writes=[tsm])
                S.op(V, lambda e: e.tensor_tensor(out=dl, in0=m1, in1=m2, op=ALU.subtract), reads=[tsm], writes=[tsm])
                S.op("act", lambda e: e.activation(out=w1, in_=dl, func=AF.Sigmoid), reads=[tsm], writes=[tsm])
                S.op("act", lambda e: e.activation(out=w2, in_=dl, func=AF.Sigmoid, scale=-1.0), reads=[tsm], writes=[tsm])
                S.op(V, lambda e: e.tensor_scalar(out=g1, in0=eq1, scalar1=w1, scalar2=1.0, op0=ALU.mult, op1=ALU.mult), reads=[tsm], writes=[tsm])
                S.op(V, lambda e: e.scalar_tensor_tensor(out=gt, in0=eq2, scalar=w2, in1=g1, op0=ALU.mult, op1=ALU.add), reads=[tsm], writes=[tsm])
                j = n % npg
                S.op("pe", lambda e: e.transpose(bt[0][0:8, j * 128:(j + 1) * 128], gt, self.idf[:]),
                     reads=[tsm, self.t_c], writes=[bt[1]])
                if j == npg - 1:
                    tok0 = (n - j) * 128
                    S.op("act", lambda e: e.activation(out=gT[:], in_=bt[0][0:8, :TS], func=AF.Copy), reads=[bt[1]], writes=[tgT])
                    for ex in range(NE):
                        S.op("pe", lambda e: e.matmul(br[0][:, :TS], sel[:, ex, :], gT[:], start=True, stop=True),
                             reads=[tgT, tw], writes=[br[1]])
                        b = k % 2
                        k += 1
                        en = self.any2()
                        S.op(en, lambda eng: self.copy(en, eng, gr[b][:], br[0][:, :TS]), reads=[br[1]], writes=[tgr[b]])
                        S.dma("pool", self.Grep[ex, :, tok0:tok0 + TS], gr[b][:], reads=[tgr[b]], writes=[Tk()])
        S.barrier()

    def rope(self, S, xin, rb, cs, sn, W, tin, tcs, tout, tmp, ttmp):
        xv = xin.rearrange("p (j two) -> p j two", two=2)
        ov = rb.rearrange("p (j two) -> p j two", two=2)
        x0, x1 = xv[:, :, 0], xv[:, :, 1]
        t1, t2, t3, t4 = tmp
        S.op("dve", lambda e: e.tensor_tensor(out=t1, in0=x0, in1=cs, op=ALU.mult), reads=[tin, tcs], writes=[ttmp[0]])
        S.op("pool", lambda e: e.tensor_tensor(out=t2, in0=x1, in1=sn, op=ALU.mult), reads=[tin, tcs], writes=[ttmp[1]])
        S.op("pool", lambda e: e.tensor_tensor(out=t3, in0=x0, in1=sn, op=ALU.mult), reads=[tin, tcs], writes=[ttmp[2]])
        S.op("dve", lambda e: e.tensor_tensor(out=t4, in0=x1, in1=cs, op=ALU.mult), reads=[tin, tcs], writes=[ttmp[3]])
        S.op("dve", lambda e: e.tensor_tensor(out=ov[:, :, 0], in0=t1, in1=t2, op=ALU.subtract),
             reads=[ttmp[0], ttmp[1]], writes=[tout])
        S.op("pool", lambda e: e.tensor_tensor(out=ov[:, :, 1], in0=t3, in1=t4, op=ALU.add),
             reads=[ttmp[2], ttmp[3], tout], writes=[tout])

    def mix_gmlp(self, l):
        S = self.S
        T = self.T
        bA, bB = self.banks[0], self.banks[1]
        with contextlib.ExitStack() as st:
            grep = self.sb(st, "gm_g", [128, 1024], F32)
            brep = self.sb(st, "gm_b", [128, 1024], F32)
            msrep = self.sb(st, "gm_ms", [128, 1024], F32)
            wsf = self.sb(st, "gm_wsf", [128, 8, 128], F32)
            wsb = self.sb(st, "gm_wsb", [128, 8, 128], BF16)
            bs = self.sb(st, "gm_bs", [128, 8], F32)
            tc_ = Tk()
            S.dma("sp", grep[:], self.rp_in[l, :, 1280:2304], writes=[tc_])
            S.dma("sp", brep[:], self.rp_in[l, :, 2304:3328], writes=[tc_])
            S.dma("sp", msrep[:], self.rp_in[l, :, 3328:4352], writes=[tc_])
            S.dma("sp", wsf[:], self.wsT_in[l], writes=[tc_])
            S.dma("sp", bs[:], self.bsT_in[l], writes=[tc_])
            S.op("dve", lambda e: e.tensor_copy(out=wsb[:], in_=wsf[:]), reads=[tc_], writes=[tc_])
            NB = 2
            gv = [self.sb(st, "gm_gv%d" % i, [128, 1024], F32) for i in range(NB)]
            gu = [self.sb(st, "gm_gu%d" % i, [128, 1024], F32) for i in range(NB)]
            a = [self.sb(st, "gm_a%d" % i, [128, 1024], F32) for i in range(NB)]
            sq = [self.sb(st, "gm_sq%d" % i, [128, 1024], F32) for i in range(NB)]
            vnb = [self.sb(st, "gm_vn%d" % i, [128, 1024], BF16) for i in range(NB)]
            oc = [self.sb(st, "gm_oc%d" % i, [128, 1024], F32) for i in range(NB)]
            ocb = [self.sb(st, "gm_ob%d" % i, [128, 1024], BF16) for i in range(NB)]
            mT = [self.sb(st, "gm_mT%d" % i, [128, 1024], BF16) for i in range(NB)]
            sm = [self.sb(st, "gm_sm%d" % i, [128, 8], F32) for i in range(NB)]
            tgv = [Tk() for _ in range(NB)]
            tgu = [Tk() for _ in range(NB)]
            ta = [Tk() for _ in range(NB)]
            tsq = [Tk() for _ in range(NB)]
            tvn = [Tk() for _ in range(NB)]
            toc = [Tk() for _ in range(NB)]
            tob = [Tk() for _ in range(NB)]
            tmT = [Tk() for _ in range(NB)]
            tsm = [Tk() for _ in range(NB)]
            mdst = self.mergedT[2048:3072, :].rearrange("(g p) t -> p g t", p=128)
            for n in range(T // 128):
                i = n % NB
                rows = slice(n * 128, (n + 1) * 128)
                S.dma("sp", gv[i][:], self.Y_g[rows, 1024:2048], writes=[tgv[i]])
                S.dma("sp", gu[i][:], self.Y_g[rows, 0:1024], writes=[tgu[i]])
                s1, nm, s2, rs = sm[i][:, 0:1], sm[i][:, 1:2], sm[i][:, 2:3], sm[i][:, 3:4]
                S.op("act", lambda e: e.activation(out=a[i][:], in_=gv[i][:], func=AF.Gelu), reads=[tgv[i]], writes=[ta[i]])
                S.op("dve", lambda e: e.tensor_reduce(out=s1, in_=a[i][:], axis=AX.X, op=ALU.add), reads=[ta[i]], writes=[tsm[i]])
                S.op("dve", lambda e: e.tensor_scalar(out=nm, in0=s1, scalar1=-1.0 / 1024, scalar2=1.0, op0=ALU.mult, op1=ALU.mult), reads=[tsm[i]], writes=[tsm[i]])
                S.op("dve", lambda e: e.tensor_scalar(out=a[i][:], in0=a[i][:], scalar1=nm, scalar2=0.0, op0=ALU.add, op1=ALU.add), reads=[tsm[i]], writes=[ta[i]])
                S.op("pool", lambda e: e.tensor_tensor(out=sq[i][:], in0=a[i][:], in1=a[i][:], op=ALU.mult), reads=[ta[i]], writes=[tsq[i]])
                S.op("dve", lambda e: e.tensor_reduce(out=s2, in_=sq[i][:], axis=AX.X, op=ALU.add), reads=[tsq[i]], writes=[tsm[i]])
                S.op("act", lambda e: e.activation(out=rs, in_=s2, func=AF.Sqrt, bias=self.epsln[:, 0:1], scale=1.0 / 1024),
                     reads=[tsm[i], self.t_c], writes=[tsm[i]])
                S.op("dve", lambda e: e.reciprocal(out=rs, in_=rs), reads=[tsm[i]], writes=[tsm[i]])
                S.op("dve", lambda e: e.scalar_tensor_tensor(out=sq[i][:], in0=a[i][:], scalar=rs, in1=grep[:], op0=ALU.mult, op1=ALU.mult),
                     reads=[ta[i], tsm[i], tc_], writes=[tsq[i]])
                S.op("pool", lambda e: e.tensor_tensor(out=vnb[i][:], in0=sq[i][:], in1=brep[:], op=ALU.add), reads=[tsq[i], tc_], writes=[tvn[i]])
                for g in range(8):
                    bank = bA if g < 4 else bB
                    c0 = (g % 4) * 128
                    S.op("pe", lambda e: e.matmul(bank[0][:, c0:c0 + 128], wsb[:, g, :], vnb[i][:, g * 128:(g + 1) * 128], start=True, stop=True),
                         reads=[tvn[i], tc_], writes=[bank[1]])
                S.op("act", lambda e: e.activation(out=gu[i][:], in_=gu[i][:], func=AF.Gelu), reads=[tgu[i]], writes=[tgu[i]])
                for g in range(8):
                    bank = bA if g < 4 else bB
                    c0 = (g % 4) * 128
                    S.op("dve", lambda e: e.scalar_tensor_tensor(out=oc[i][:, g * 128:(g + 1) * 128], in0=bank[0][:, c0:c0 + 128],
                                                                scalar=bs[:, g:g + 1], in1=gu[i][:, g * 128:(g + 1) * 128],
                                                                op0=ALU.add, op1=ALU.mult),
                         reads=[bank[1], tgu[i], tc_], writes=[toc[i]])
                S.op("pool", lambda e: e.tensor_tensor(out=ocb[i][:], in0=oc[i][:], in1=msrep[:], op=ALU.mult), reads=[toc[i], tc_], writes=[tob[i]])
                pb, tpb = self.bbanks[n % 2]
                for g in range(8):
                    S.op("pe", lambda e: e.transpose(pb[:, g * 128:(g + 1) * 128], ocb[i][:, g * 128:(g + 1) * 128], self.idb[:]),
                         reads=[tob[i], self.t_c], writes=[tpb])
                S.op("act", lambda e: e.activation(out=mT[i][:], in_=pb[:], func=AF.Copy), reads=[tpb], writes=[tmT[i]])
                S.dma("pool", mdst[:, :, rows], mT[i][:].rearrange("p (g t) -> p g t", t=128), reads=[tmT[i]], writes=[Tk()])
        S.barrier()

    def mix_attn(self, l):
        S = self.S
        T = self.T
        NCH = T // 128
        QT = min(512, T)
        with contextlib.ExitStack() as st:
            gain = self.sb(st, "at_gain", [128, 1280], F32)
            tcst = Tk()
            S.dma("sp", gain[:], self.rp_in[l, :, 0:1280], writes=[tcst])
            NB = 2
            qk = [self.sb(st, "at_qk%d" % i, [128, 1280], F32) for i in range(NB)]
            sq = [self.sb(st, "at_sq%d" % i, [128, 1280], F32) for i in range(NB)]
            cs = [self.sb(st, "at_cs%d" % i, [128, 640], F32) for i in range(NB)]
            sn = [self.sb(st, "at_sn%d" % i, [128, 640], F32) for i in range(NB)]
            tmp = [[self.sb(st, "at_t%d_%d" % (i, j), [128, 640], F32) for j in range(4)] for i in range(NB)]
            rb = [self.sb(st, "at_rb%d" % i, [128, 1280], BF16) for i in range(NB)]
            qkT = [self.sb(st, "at_qkT%d" % i, [128, 1280], BF16) for i in range(NB)]
            sm = [self.sb(st, "at_sm%d" % i, [128, 16], F32) for i in range(NB)]
            tqk = [Tk() for _ in range(NB)]
            tsq = [Tk() for _ in range(NB)]
            tcs = [Tk() for _ in range(NB)]
            ttmp = [[Tk() for _ in range(4)] for _ in range(NB)]
            trb = [Tk() for _ in range(NB)]
            tqT = [Tk() for _ in range(NB)]
            tsm = [Tk() for _ in range(NB)]
            dst = self.aqkT.rearrange("h d t -> d h t")
            for n in range(NCH):
                i = n % NB
                rows = slice(n * 128, (n + 1) * 128)
                S.dma("sp", qk[i][:], self.Y_a[rows, 0:1280], writes=[tqk[i]])
                S.dma("sp", cs[i][:], self.cos_in[rows, 0:640], writes=[tcs[i]])
                S.dma("sp", sn[i][:], self.sin_in[rows, 0:640], writes=[tcs[i]])
                S.op("pool", lambda e: e.tensor_tensor(out=sq[i][:], in0=qk[i][:], in1=qk[i][:], op=ALU.mult), reads=[tqk[i]], writes=[tsq[i]])
                ss = sm[i][:, 0:10]
                S.op("dve", lambda e: e.tensor_reduce(out=ss, in_=sq[i][:].rearrange("p (h d) -> p h d", d=128), axis=AX.X, op=ALU.add),
                     reads=[tsq[i]], writes=[tsm[i]])
                S.op("act", lambda e: e.activation(out=ss, in_=ss, func=AF.Sqrt, bias=self.epsln[:, 1:2], scale=1.0 / 128),
                     reads=[tsm[i], self.t_c], writes=[tsm[i]])
                S.op("dve", lambda e: e.reciprocal(out=ss, in_=ss), reads=[tsm[i]], writes=[tsm[i]])
                S.op("dve", lambda e: e.tensor_tensor(out=sq[i][:].rearrange("p (h d) -> p h d", d=128),
                                                     in0=qk[i][:].rearrange("p (h d) -> p h d", d=128),
                                                     in1=ss.unsqueeze(2).to_broadcast([128, 10, 128]), op=ALU.mult),
                     reads=[tqk[i], tsm[i]], writes=[tsq[i]])
                S.op("pool", lambda e: e.tensor_tensor(out=sq[i][:], in0=sq[i][:], in1=gain[:], op=ALU.mult), reads=[tcst], writes=[tsq[i]])
                self.rope(S, sq[i][:], rb[i][:], cs[i][:], sn[i][:], 1280, tsq[i], tcs[i], trb[i],
                          [t[:] for t in tmp[i]], ttmp[i])
                for h in range(10):
                    pb, tpb = self.bbanks[0] if h < 8 else self.bbanks[1]
                    c0 = (h % 8) * 128
                    S.op("pe", lambda e: e.transpose(pb[:, c0:c0 + 128], rb[i][:, h * 128:(h + 1) * 128], self.idb[:]),
                         reads=[trb[i], self.t_c], writes=[tpb])
                S.op("act", lambda e: e.activation(out=qkT[i][:, 0:1024], in_=self.bbanks[0][0][:], func=AF.Copy),
                     reads=[self.bbanks[0][1]], writes=[tqT[i]])
                S.op("dve", lambda e: e.tensor_copy(out=qkT[i][:, 1024:1280], in_=self.bbanks[1][0][:, 0:256]),
                     reads=[self.bbanks[1][1]], writes=[tqT[i]])
                S.dma("pool", dst[:, :, rows], qkT[i][:].rearrange("p (h t) -> p h t", t=128), reads=[tqT[i]], writes=[Tk()])
        S.barrier()
        scale = 128.0 ** -0.5
        with contextlib.ExitStack() as st:
            kT = self.sb(st, "ac_kT", [128, T], BF16)
            vf = self.sb(st, "ac_vf", [128, NCH, 128], F32)
            vb = self.sb(st, "ac_vb", [128, NCH, 128], BF16)
            tk_, tvf, tvb = Tk(), Tk(), Tk()
            qT = [self.sb(st, "ac_qT%d" % i, [128, QT], BF16) for i in range(2)]
            tq = [Tk(), Tk()]
            NP = 3
            pT = [self.sb(st, "ac_pT%d" % i, [128, QT], BF16) for i in range(NP)]
            tp = [Tk() for _ in range(NP)]
            rec = self.sb(st, "ac_rec", [128, QT], F32)
            of = self.sb(st, "ac_of", [128, QT], F32)
            ob = [self.sb(st, "ac_ob%d" % i, [128, QT], BF16) for i in range(2)]
            trec, tof = Tk(), Tk()
            tob = [Tk(), Tk()]
            sbank = [self.banks[0], self.banks[1]]
            accs = [(self.banks[2], self.banks[3]), (self.banks[4], self.banks[5])]
            it = 0
            pi = 0
            for g in range(2):
                S.dma("sp", kT[:], self.aqkT[8 + g], writes=[tk_])
                self.dma_chunks("sp", vf[:], self.Y_a[:, 1280 + g * 128:1280 + (g + 1) * 128].rearrange("(n p) d -> p n d", p=128), NCH, tvf)
                S.op("pool", lambda e: e.tensor_copy(out=vb[:], in_=vf[:]), reads=[tvf], writes=[tvb])
                for h in range(4 * g, 4 * g + 4):
                    for qt in range(T // QT):
                        qi = it % 2
                        oacc, dacc = accs[it % 2]
                        it += 1
                        S.dma("sp", qT[qi][:], self.aqkT[h, :, qt * QT:(qt + 1) * QT], writes=[tq[qi]])
                        def emit_s(kc_):
                            sbx, tsbx = sbank[kc_ % 2]
                            S.op("pe", lambda e: e.matmul(sbx[:, :QT], kT[:, kc_ * 128:(kc_ + 1) * 128], qT[qi][:], start=True, stop=True),
                                 reads=[tk_, tq[qi]], writes=[tsbx])
                        emit_s(0)
                        for kc in range(NCH):
                            sb_, tsb = sbank[kc % 2]
                            if kc + 1 < NCH:
                                emit_s(kc + 1)
                            p = pi % NP
                            pi += 1
                            S.op("act", lambda e: e.activation(out=pT[p][:], in_=sb_[:, :QT], func=AF.Exp, scale=scale),
                                 reads=[tsb], writes=[tp[p]])
                            S.op("pe", lambda e: e.matmul(oacc[0][:, :QT], vb[:, kc, :], pT[p][:], start=(kc == 0), stop=(kc == NCH - 1)),
                                 reads=[tvb, tp[p]], writes=[oacc[1]])
                            S.op("pe", lambda e: e.matmul(dacc[0][:, :QT], self.onesb[:], pT[p][:], start=(kc == 0), stop=(kc == NCH - 1)),
                                 reads=[tp[p], self.t_c], writes=[dacc[1]])
                        S.op("dve", lambda e: e.reciprocal(out=rec[:], in_=dacc[0][:, :QT]), reads=[dacc[1]], writes=[trec])
                        S.op("dve", lambda e: e.tensor_tensor(out=of[:], in0=oacc[0][:, :QT], in1=rec[:], op=ALU.mult),
                             reads=[oacc[1], trec], writes=[tof])
                        mc = l * 200 + 128 + h
                        S.op("pool", lambda e: e.tensor_scalar(out=ob[qi][:], in0=of[:], scalar1=self.pp[:, mc:mc + 1], scalar2=1.0,
                                                               op0=ALU.mult, op1=ALU.mult), reads=[tof, self.t_pp], writes=[tob[qi]])
                        S.dma("pool", self.mergedT[h * 128:(h + 1) * 128, qt * QT:(qt + 1) * QT], ob[qi][:], reads=[tob[qi]], writes=[Tk()])
        S.barrier()

    def mix_ret(self, l):
        S = self.S
        T = self.T
        NCH = T // 128
        gam = [1.0 - 2.0 ** (-5.0 - h) for h in range(8)]
        gC = [float(np.float64(g) ** 128) for g in gam]
        with contextlib.ExitStack() as st:
            rdec = self.sb(st, "rt_rdec", [128, 32], F32)
            tcst = Tk()
            S.dma("sp", rdec[:], self.rdec_in, writes=[tcst])
            NB = 2
            qk = [self.sb(st, "rp_qk%d" % i, [128, 2048], F32) for i in range(NB)]
            rv = [self.sb(st, "rp_rv%d" % i, [128, 1024], F32) for i in range(NB)]
            cs = [self.sb(st, "rp_cs%d" % i, [128, 1024], F32) for i in range(NB)]
            sn = [self.sb(st, "rp_sn%d" % i, [128, 1024], F32) for i in range(NB)]
            tmp = [[self.sb(st, "rp_t%d_%d" % (i, j), [128, 1024], F32) for j in range(4)] for i in range(NB)]
            rb = [self.sb(st, "rp_rb%d" % i, [128, 2048], BF16) for i in range(NB)]
            kfb = [self.sb(st, "rp_kf%d" % i, [128, 3, 1024], BF16) for i in range(NB)]
            qkT = [self.sb(st, "rp_qkT%d" % i, [128, 2048], BF16) for i in range(NB)]
            tqk = [Tk() for _ in range(NB)]
            trv = [Tk() for _ in range(NB)]
            tcs = [Tk() for _ in range(NB)]
            ttmp = [[Tk() for _ in range(4)] for _ in range(NB)]
            trb = [Tk() for _ in range(NB)]
            tkf = [Tk() for _ in range(NB)]
            tqT = [Tk() for _ in range(NB)]
            dst = self.rqkT.rearrange("h d t -> d h t")
            rkd = self.RK.rearrange("i t c -> t i c")
            for n in range(NCH):
                i = n % NB
                rows = slice(n * 128, (n + 1) * 128)
                S.dma("sp", qk[i][:], self.Y_r[rows, 0:2048], writes=[tqk[i]])
                S.dma("sp", rv[i][:], self.Y_r[rows, 2048:3072], writes=[trv[i]])
                S.dma("sp", cs[i][:], self.cos_in[rows, :], writes=[tcs[i]])
                S.dma("sp", sn[i][:], self.sin_in[rows, :], writes=[tcs[i]])
                self.rope(S, qk[i][:], rb[i][:], cs[i][:], sn[i][:], 2048, tqk[i], tcs[i], trb[i], [t[:] for t in tmp[i]], ttmp[i])
                for h in range(8):
                    kh = rb[i][:, 1024 + h * 128:1024 + (h + 1) * 128]
                    S.op("dve", lambda e: e.tensor_scalar(out=kfb[i][:, 0, h * 128:(h + 1) * 128], in0=kh, scalar1=rdec[:, h:h + 1], scalar2=1.0, op0=ALU.mult, op1=ALU.mult),
                         reads=[trb[i], tcst], writes=[tkf[i]])
                    S.op("pool", lambda e: e.tensor_scalar(out=kfb[i][:, 1, h * 128:(h + 1) * 128], in0=kh, scalar1=rdec[:, 8 + h:9 + h], scalar2=1.0,
                                                           op0=ALU.mult, op1=ALU.mult), reads=[trb[i], tcst], writes=[tkf[i]])
                S.op("act", lambda e: e.activation(out=kfb[i][:, 2, :], in_=rv[i][:], func=AF.Copy), reads=[trv[i]], writes=[tkf[i]])
                S.dma("pool", rkd[rows, :, :], kfb[i][:], reads=[tkf[i]], writes=[Tk()])
                for h in range(16):
                    pb, tpb = self.bbanks[h // 8]
                    c0 = (h % 8) * 128
                    S.op("pe", lambda e: e.transpose(pb[:, c0:c0 + 128], rb[i][:, h * 128:(h + 1) * 128], self.idb[:]),
                         reads=[trb[i], self.t_c], writes=[tpb])
                S.op("act", lambda e: e.activation(out=qkT[i][:, 0:1024], in_=self.bbanks[0][0][:], func=AF.Copy),
                     reads=[self.bbanks[0][1]], writes=[tqT[i]])
                S.op("dve", lambda e: e.tensor_copy(out=qkT[i][:, 1024:2048], in_=self.bbanks[1][0][:]),
                     reads=[self.bbanks[1][1]], writes=[tqT[i]])
                S.dma("pool", dst[:, :, rows], qkT[i][:].rearrange("p (h t) -> p h t", t=128), reads=[tqT[i]], writes=[Tk()])
        S.barrier()
        with contextlib.ExitStack() as st:
            rdec = self.sb(st, "rs_rdec", [128, 32], F32)
            dsym = self.sb(st, "rs_dsym", [128, 8, 128], F32)
            gcol = self.sb(st, "rs_gcol", [128, 8], F32)
            tcst = Tk()
            S.dma("sp", rdec[:], self.rdec_in, writes=[tcst])
            S.dma("sp", dsym[:], self.dsym_in, writes=[tcst])
            b0 = l * 200
            S.op("dve", lambda e: e.tensor_tensor(out=gcol[:], in0=self.pp[:, b0 + 168:b0 + 176], in1=self.pp[:, b0 + 128 + 24:b0 + 128 + 32], op=ALU.mult),
                 reads=[self.t_pp], writes=[tcst])
            qT = self.sb(st, "rs_qT", [128, T], BF16)
            kT = self.sb(st, "rs_kT", [128, T], BF16)
            kv = self.sb(st, "rs_kv", [128, 3, NCH, 128], BF16)
            rg = self.sb(st, "rs_rg", [128, NCH, 128], F32)
            oacc = self.sb(st, "rs_oacc", [128, NCH, 128], F32)
            NG = min(8, NCH)
            sqb = [self.sb(st, "rs_sq%d" % i, [128, NG, 128], F32) for i in range(2)]
            ob = [self.sb(st, "rs_ob%d" % i, [128, NG, 128], BF16) for i in range(2)]
            ss = [self.sb(st, "rs_ss%d" % i, [128, NG], F32) for i in range(2)]
            tld, trg, toa = Tk(), Tk(), Tk()
            tsq, tob, tss = [Tk(), Tk()], [Tk(), Tk()], [Tk(), Tk()]
            R = [self.sb(st, "rs_R%d" % i, [128, 128], F32) for i in range(2)]
            Rb = [self.sb(st, "rs_Rb%d" % i, [128, 128], BF16) for i in range(2)]
            tR = [Tk(), Tk()]
            tRb = [Tk(), Tk()]
            pT = [self.sb(st, "rs_pT%d" % i, [128, 128], BF16) for i in range(2)]
            tpT = [Tk(), Tk()]
            mT = [self.sb(st, "rs_mT%d" % i, [128, 1024], BF16) for i in range(2)]
            tmT = [Tk(), Tk()]
            bS = [self.banks[0], self.banks[1]]
            bO = [self.banks[2], self.banks[3]]
            for h in range(8):
                S.dma("sp", qT[:], self.rqkT[h], writes=[tld])
                S.dma("sp", kT[:], self.rqkT[8 + h], writes=[tld])
                for i3 in range(3):
                    self.dma_chunks("sp", kv[:, i3, :, :], self.RK[i3, :, h * 128:(h + 1) * 128].rearrange("(n p) d -> p n d", p=128), NCH, tld)
                self.dma_chunks("sp", rg[:], self.Y_r[:, 3072 + h * 128:3072 + (h + 1) * 128].rearrange("(n p) d -> p n d", p=128), NCH, trg)
                for d in range(2):
                    S.op("pool", lambda e: e.memset(R[d][:], 0.0), writes=[tR[d]])
                    S.op("pool", lambda e: e.memset(Rb[d][:], 0.0), writes=[tRb[d]])
                for j in range(NCH):
                    cols = slice(j * 128, (j + 1) * 128)
                    bs_, tbs = bS[j % 2]
                    bo_, tbo = bO[j % 2]
                    S.op("pe", lambda e: e.matmul(bs_[:, 0:128], kT[:, cols], qT[:, cols], start=True, stop=True), reads=[tld], writes=[tbs])
                    p = j % 2
                    S.op("dve", lambda e: e.tensor_tensor(out=pT[p][:], in0=bs_[:, 0:128], in1=dsym[:, h, :], op=ALU.mult),
                         reads=[tbs, tcst], writes=[tpT[p]])
                    S.op("pe", lambda e: e.matmul(bo_[:, 0:128], pT[p][:], kv[:, 2, j, :], start=True, stop=True), reads=[tpT[p], tld], writes=[tbo])
                    S.op("pe", lambda e: e.matmul(bo_[:, 128:256], qT[:, cols], Rb[0][:], start=True, stop=True), reads=[tld, tRb[0]], writes=[tbo])
                    S.op("pe", lambda e: e.matmul(bo_[:, 256:384], kv[:, 0, j, :], kv[:, 2, j, :], start=True, stop=True), reads=[tld], writes=[tbo])
                    S.op("act", lambda e: e.activation(out=oacc[:, j, :], in_=bo_[:, 0:128], func=AF.Copy), reads=[tbo], writes=[toa])
                    S.op("dve", lambda e: e.scalar_tensor_tensor(out=oacc[:, j, :], in0=bo_[:, 128:256], scalar=rdec[:, 16 + h:17 + h],
                                                                in1=oacc[:, j, :], op0=ALU.mult, op1=ALU.add), reads=[tbo, tcst, toa], writes=[toa])
                    S.op("dve", lambda e: e.scalar_tensor_tensor(out=R[0][:], in0=R[0][:], scalar=gC[h], in1=bo_[:, 256:384],
                                                                op0=ALU.mult, op1=ALU.add), reads=[tbo], writes=[tR[0]])
                    S.op("dve", lambda e: e.tensor_copy(out=Rb[0][:], in_=R[0][:]), reads=[tR[0]], writes=[tRb[0]])
                for j in range(NCH - 1, -1, -1):
                    cols = slice(j * 128, (j + 1) * 128)
                    bo_, tbo = bO[j % 2]
                    S.op("pe", lambda e: e.matmul(bo_[:, 128:256], qT[:, cols], Rb[1][:], start=True, stop=True), reads=[tld, tRb[1]], writes=[tbo])
                    S.op("pe", lambda e: e.matmul(bo_[:, 256:384], kv[:, 1, j, :], kv[:, 2, j, :], start=True, stop=True), reads=[tld], writes=[tbo])
                    S.op("dve", lambda e: e.scalar_tensor_tensor(out=oacc[:, j, :], in0=bo_[:, 128:256], scalar=rdec[:, 24 + h:25 + h],
                                                                in1=oacc[:, j, :], op0=ALU.mult, op1=ALU.add), reads=[tbo, tcst, toa], writes=[toa])
                    S.op("dve", lambda e: e.scalar_tensor_tensor(out=R[1][:], in0=R[1][:], scalar=gC[h], in1=bo_[:, 256:384],
                                                                op0=ALU.mult, op1=ALU.add), reads=[tbo], writes=[tR[1]])
                    S.op("dve", lambda e: e.tensor_copy(out=Rb[1][:], in_=R[1][:]), reads=[tR[1]], writes=[tRb[1]])
                S.op("act", lambda e: e.activation(out=rg[:], in_=rg[:], func=AF.Silu), reads=[trg], writes=[trg])
                for j0 in range(0, NCH, NG):
                    gi = (j0 // NG) % 2
                    js = slice(j0, j0 + NG)
                    S.op("pool", lambda e: e.tensor_tensor(out=sqb[gi][:], in0=oacc[:, js, :], in1=oacc[:, js, :], op=ALU.mult), reads=[toa], writes=[tsq[gi]])
                    S.op("dve", lambda e: e.tensor_reduce(out=ss[gi][:], in_=sqb[gi][:], axis=AX.X, op=ALU.add), reads=[tsq[gi]], writes=[tss[gi]])
                    S.op("act", lambda e: e.activation(out=ss[gi][:], in_=ss[gi][:], func=AF.Sqrt, bias=self.epsln[:, 1:2], scale=1.0 / 128),
                         reads=[tss[gi], self.t_c], writes=[tss[gi]])
                    S.op("dve", lambda e: e.reciprocal(out=ss[gi][:], in_=ss[gi][:]), reads=[tss[gi]], writes=[tss[gi]])
                    S.op("dve", lambda e: e.tensor_tensor(out=sqb[gi][:], in0=oacc[:, js, :], in1=ss[gi][:].unsqueeze(2).to_broadcast([128, NG, 128]), op=ALU.mult),
                         reads=[toa, tss[gi]], writes=[tsq[gi]])
                    S.op("pool", lambda e: e.tensor_tensor(out=ob[gi][:], in0=sqb[gi][:], in1=rg[:, js, :], op=ALU.mult), reads=[tsq[gi], trg], writes=[tob[gi]])
                    pb, tpb = self.bbanks[gi]
                    for jj in range(NG):
                        S.op("pe", lambda e: e.transpose(pb[:, jj * 128:(jj + 1) * 128], ob[gi][:, jj, :], self.idb[:]),
                             reads=[tob[gi], self.t_c], writes=[tpb])
                    S.op("act", lambda e: e.activation(out=mT[gi][:, :NG * 128], in_=pb[:, :NG * 128], func=AF.Copy, scale=gcol[:, h:h + 1]),
                         reads=[tpb, tcst], writes=[tmT[gi]])
                    S.dma("pool", self.mergedT[3072 + h * 128:3072 + (h + 1) * 128, j0 * 128:(j0 + NG) * 128], mT[gi][:, :NG * 128],
                          reads=[tmT[gi]], writes=[Tk()])
        S.barrier()

    def dma_chunks(self, q, out3, in3, n, tk, step=8, reads=()):
        for a in range(0, n, step):
            b = min(n, a + step)
            self.S.dma(q, out3[:, a:b, :], in3[:, a:b, :], reads=list(reads), writes=[tk])

    def mix_hgrn(self, l):
        S = self.S
        T = self.T
        NCH = T // 128
        SEG = min(2048, T)
        NSEG = T // SEG
        NT = SEG // 128
        NBLK = SEG // 32
        PW = min(512, T)
        b0 = l * 200
        with contextlib.ExitStack() as st:
            mbd = self.sb(st, "hg_mbd", [128, 2, 128], F32)
            oml = self.sb(st, "hg_oml", [128, 8], F32)
            gcol = self.sb(st, "hg_gcol", [128, 8], F32)
            one = self.sb(st, "hg_one", [128, 1], F32)
            tcst = Tk()
            S.dma("sp", mbd[:], self.mbd_in, writes=[tcst])
            S.op("dve", lambda e: e.memset(one[:], 1.0), writes=[tcst])
            if l == 0:
                S.op("dve", lambda e: e.memset(oml[:], 1.0), writes=[tcst])
            else:
                S.op("dve", lambda e: e.tensor_tensor(out=oml[:], in0=self.pp[:, b0 + 176:b0 + 184], in1=self.pp[:, b0 + 184:b0 + 192], op=ALU.subtract),
                     reads=[self.t_pp], writes=[tcst])
                S.op("act", lambda e: e.activation(out=oml[:], in_=oml[:], func=AF.Sigmoid), reads=[tcst], writes=[tcst])
            S.op("dve", lambda e: e.tensor_tensor(out=gcol[:], in0=self.pp[:, b0 + 160:b0 + 168], in1=self.pp[:, b0 + 128 + 8:b0 + 128 + 16], op=ALU.mult),
                 reads=[self.t_pp], writes=[tcst])
            oT = self.sb(st, "hg_oT", [128, T], F32)
            vf = self.sb(st, "hg_vf", [128, T], F32)
            vb = self.sb(st, "hg_vb", [128, NCH, 128], BF16)
            toT, tvb, thg = Tk(), Tk(), Tk()
            z = self.sb(st, "hg_z", [128, SEG], F32)
            q = self.sb(st, "hg_q", [128, SEG], F32)
            kk = self.sb(st, "hg_kk", [128, SEG], F32)
            ba = self.sb(st, "hg_ba", [128, SEG], F32)
            bb = self.sb(st, "hg_bb", [128, SEG], F32)
            eb = self.sb(st, "hg_eb", [128, SEG], F32)
            enb = self.sb(st, "hg_enb", [128, SEG], F32)
            Qt = self.sb(st, "hg_Qt", [128, SEG], F32)
            Kt = self.sb(st, "hg_Kt", [128, SEG], F32)
            KpT = self.sb(st, "hg_KpT", [128, SEG], BF16)
            Kp = self.sb(st, "hg_Kp", [128, NT, 128], BF16)
            Kpz = self.sb(st, "hg_Kpz", [128, NT, 128], BF16)
            dec = self.sb(st, "hg_dec", [128, NBLK], F32)
            tz, tq, tkk, tba, tbb, teb, tenb, tQt, tKt, tKpT, tKp, tdec = (Tk() for _ in range(12))
            Sst = self.sb(st, "hg_S", [128, 128], F32)
            tS = Tk()
            PT = [self.sb(st, "hg_PT%d" % i, [128, 128], BF16) for i in range(2)]
            tPT = [Tk(), Tk()]
            sqp = [self.sb(st, "hg_sq%d" % i, [128, PW], BF16) for i in range(2)]
            rsp = [self.sb(st, "hg_rs%d" % i, [128, PW], F32) for i in range(2)]
            t1p = [self.sb(st, "hg_t1%d" % i, [128, PW], F32) for i in range(2)]
            mbp = [self.sb(st, "hg_mb%d" % i, [128, PW], BF16) for i in range(2)]
            tsqp, trsp, tt1p, tmbp = ([Tk(), Tk()] for _ in range(4))
            bA = [self.banks[0], self.banks[1]]
            bO = [self.banks[2], self.banks[3]]
            bR = [self.banks[4], self.banks[5]]
            nR = 0
            v3 = lambda t_: t_[:].rearrange("p (b c) -> p b c", c=32)
            for h in range(8):
                self.dma_chunks("sp", vf[:].rearrange("p (n d) -> p n d", d=128),
                                self.Y_hi[:, h * 128:(h + 1) * 128].rearrange("(n p) d -> p n d", p=128), NCH, thg)
                S.op("pool", lambda e: e.tensor_copy(out=vb[:], in_=vf[:].rearrange("p (n d) -> p n d", d=128)), reads=[thg], writes=[tvb])
                for d in range(2):
                    S.op("pool", lambda e: e.memset(Sst[:], 0.0), writes=[tS])
                    segs = range(NSEG) if d == 0 else range(NSEG - 1, -1, -1)
                    for sg_ in segs:
                        scol = slice(sg_ * SEG, (sg_ + 1) * SEG)
                        zr = 1024 * (1 + d) + h * 128
                        S.dma("sp", z[:], self.YT[zr:zr + 128, scol], writes=[tz])
                        S.dma("sp", q[:], self.YT[h * 128:(h + 1) * 128, scol], writes=[tq])
                        S.op("act", lambda e: e.activation(out=z[:], in_=z[:], func=AF.Sigmoid, scale=-1.0), reads=[tz], writes=[tz])
                        S.op("dve", lambda e: e.tensor_scalar(out=kk[:], in0=z[:], scalar1=oml[:, h:h + 1], scalar2=1.0, op0=ALU.mult, op1=ALU.mult),
                             reads=[tz, tcst], writes=[tkk])
                        S.op("act", lambda e: e.activation(out=ba[:], in_=kk[:], func=AF.Ln, scale=-1.0, bias=one[:, 0:1]),
                             reads=[tkk, tcst], writes=[tba])
                        cur, nxt, tcur, tnxt = ba, bb, tba, tbb
                        for sh in (1, 2, 4, 8, 16):
                            c3, n3 = v3(cur), v3(nxt)
                            if d == 0:
                                S.op("dve", lambda e: e.tensor_tensor(out=n3[:, :, sh:], in0=c3[:, :, sh:], in1=c3[:, :, :32 - sh], op=ALU.add),
                                     reads=[tcur], writes=[tnxt])
                                S.op("dve", lambda e: e.tensor_copy(out=n3[:, :, :sh], in_=c3[:, :, :sh]), reads=[tcur], writes=[tnxt])
                            else:
                                S.op("dve", lambda e: e.tensor_tensor(out=n3[:, :, :32 - sh], in0=c3[:, :, :32 - sh], in1=c3[:, :, sh:], op=ALU.add),
                                     reads=[tcur], writes=[tnxt])
                                S.op("dve", lambda e: e.tensor_copy(out=n3[:, :, 32 - sh:], in_=c3[:, :, 32 - sh:]), reads=[tcur], writes=[tnxt])
                            cur, nxt, tcur, tnxt = nxt, cur, tnxt, tcur
                        S.op("dve", lambda e: e.tensor_scalar(out=cur[:], in0=cur[:], scalar1=-80.0, scalar2=0.0, op0=ALU.max, op1=ALU.add),
                             reads=[tcur], writes=[tcur])
                        S.op("act", lambda e: e.activation(out=eb[:], in_=cur[:], func=AF.Exp), reads=[tcur], writes=[teb])
                        S.op("act", lambda e: e.activation(out=enb[:], in_=cur[:], func=AF.Exp, scale=-1.0), reads=[tcur], writes=[tenb])
                        S.op("dve", lambda e: e.tensor_copy(out=dec[:], in_=v3(eb)[:, :, (31 if d == 0 else 0)]), reads=[teb], writes=[tdec])
                        S.op("act", lambda e: e.activation(out=q[:], in_=q[:], func=AF.Silu), reads=[tq], writes=[tq])
                        S.op("dve", lambda e: e.scalar_tensor_tensor(out=Qt[:], in0=q[:], scalar=128.0 ** -0.5, in1=eb[:], op0=ALU.mult, op1=ALU.mult),
                             reads=[tq, teb], writes=[tQt])
                        S.op("pool", lambda e: e.tensor_tensor(out=Kt[:], in0=kk[:], in1=enb[:], op=ALU.mult), reads=[tkk, tenb], writes=[tKt])
                        S.op("dve", lambda e: e.tensor_tensor(out=v3(KpT), in0=v3(Kt), in1=dec[:].unsqueeze(2).to_broadcast([128, NBLK, 32]), op=ALU.mult),
                             reads=[tKt, tdec], writes=[tKpT])
                        for j0 in range(0, NT, 8):
                            pb, tpb = self.bbanks[(j0 // 8) % 2]
                            n8 = min(8, NT - j0)
                            for jj in range(n8):
                                jt = j0 + jj
                                S.op("pe", lambda e: e.transpose(pb[:, jj * 128:(jj + 1) * 128], KpT[:, jt * 128:(jt + 1) * 128], self.idb[:]),
                                     reads=[tKpT, self.t_c], writes=[tpb])
                            S.op("act", lambda e: e.activation(out=Kp[:, j0:j0 + n8, :], in_=pb[:, :n8 * 128].rearrange("p (n d) -> p n d", d=128), func=AF.Copy),
                                 reads=[tpb], writes=[tKp])
                            S.op("dve", lambda e: e.tensor_copy(out=Kpz[64:128, j0:j0 + n8, :], in_=pb[64:128, :n8 * 128].rearrange("p (n d) -> p n d", d=128)),
                                 reads=[tpb], writes=[tKp])
                            S.op("dve", lambda e: e.memset(Kpz[64:96, j0:j0 + n8, :], 0.0), writes=[tKp])
                        tiles = range(NT) if d == 0 else range(NT - 1, -1, -1)
                        for jt in tiles:
                            jg = sg_ * NT + jt
                            cols = slice(jt * 128, (jt + 1) * 128)
                            gcols = slice(jg * 128, (jg + 1) * 128)
                            ba_, tba_ = bA[jt % 2]
                            bo_, tbo_ = bO[jt % 2]
                            S.op("pe", lambda e: e.matmul(ba_[:, 0:128], Kt[:, cols], Qt[:, cols], start=True, stop=True), reads=[tKt, tQt], writes=[tba_])
                            p = jt % 2
                            S.op("dve", lambda e: e.tensor_tensor(out=PT[p][:], in0=ba_[:, 0:128], in1=mbd[:, d, :], op=ALU.mult),
                                 reads=[tba_, tcst], writes=[tPT[p]])
                            S.op("pe", lambda e: e.matmul(bo_[:, 0:128], vb[:, jg, :], PT[p][:], start=True, stop=True), reads=[tvb, tPT[p]], writes=[tbo_])
                            blks = range(4) if d == 0 else range(3, -1, -1)
                            for ib in blks:
                                c32 = slice(jt * 128 + ib * 32, jt * 128 + ib * 32 + 32)
                                S.op("pe", lambda e: e.matmul(bo_[:, 128 + ib * 32:128 + ib * 32 + 32], Sst[:], Qt[:, c32], start=True, stop=True),
                                     reads=[tS, tQt], writes=[tbo_])
                                br_, tbr_ = bR[nR % 2]
                                nR += 1
                                if ib < 3:
                                    S.op("pe", lambda e: e.matmul(br_[:, 0:128], Kp[ib * 32:(ib + 1) * 32, jt, :], vb[ib * 32:(ib + 1) * 32, jg, :], start=True, stop=True),
                                         reads=[tKp, tvb], writes=[tbr_])
                                else:
                                    S.op("pe", lambda e: e.matmul(br_[:, 0:128], Kpz[64:128, jt, :], vb[64:128, jg, :], start=True, stop=True),
                                         reads=[tKp, tvb], writes=[tbr_])
                                blk = jt * 4 + ib
                                S.op("dve", lambda e: e.scalar_tensor_tensor(out=Sst[:], in0=Sst[:], scalar=dec[:, blk:blk + 1], in1=br_[:, 0:128],
                                                                            op0=ALU.mult, op1=ALU.add), reads=[tbr_, tdec], writes=[tS])
                            if d == 0:
                                S.op("act", lambda e: e.activation(out=oT[:, gcols], in_=bo_[:, 0:128], func=AF.Copy), reads=[tbo_], writes=[toT])
                            else:
                                S.op("dve", lambda e: e.tensor_tensor(out=oT[:, gcols], in0=bo_[:, 0:128], in1=oT[:, gcols], op=ALU.add), reads=[tbo_], writes=[toT])
                            S.op("dve", lambda e: e.tensor_tensor(out=oT[:, gcols], in0=bo_[:, 128:256], in1=oT[:, gcols], op=ALU.add), reads=[tbo_], writes=[toT])
                S.dma("sp", vf[:], self.YT[3072 + h * 128:3072 + (h + 1) * 128, :], reads=[tvb], writes=[thg])
                S.op("act", lambda e: e.activation(out=vf[:], in_=vf[:], func=AF.Silu), reads=[thg], writes=[thg])
                for pc in range(T // PW):
                    i = pc % 2
                    pcs = slice(pc * PW, (pc + 1) * PW)
                    bk, tbk = bA[pc % 2]
                    S.op("pool", lambda e: e.tensor_tensor(out=sqp[i][:], in0=oT[:, pcs], in1=oT[:, pcs], op=ALU.mult), reads=[toT], writes=[tsqp[i]])
                    S.op("pe", lambda e: e.matmul(bk[:, :PW], self.onesb[:], sqp[i][:], start=True, stop=True), reads=[tsqp[i], self.t_c], writes=[tbk])
                    S.op("act", lambda e: e.activation(out=rsp[i][:], in_=bk[:, :PW], func=AF.Sqrt, bias=self.epsln[:, 1:2], scale=1.0 / 128),
                         reads=[tbk, self.t_c], writes=[trsp[i]])
                    S.op("dve", lambda e: e.reciprocal(out=rsp[i][:], in_=rsp[i][:]), reads=[trsp[i]], writes=[trsp[i]])
                    S.op("dve", lambda e: e.tensor_tensor(out=t1p[i][:], in0=oT[:, pcs], in1=rsp[i][:], op=ALU.mult), reads=[toT, trsp[i]], writes=[tt1p[i]])
                    S.op("pool", lambda e: e.tensor_tensor(out=t1p[i][:], in0=t1p[i][:], in1=vf[:, pcs], op=ALU.mult), reads=[thg], writes=[tt1p[i]])
                    S.op("act", lambda e: e.activation(out=mbp[i][:], in_=t1p[i][:], func=AF.Copy, scale=gcol[:, h:h + 1]), reads=[tt1p[i], tcst], writes=[tmbp[i]])
                    S.dma("pool", self.mergedT[1024 + h * 128:1024 + (h + 1) * 128, pcs], mbp[i][:], reads=[tmbp[i]], writes=[Tk()])
        S.barrier()


_CACHE = {}


def _consts(T):
    f64 = np.float64
    n_rows = T // 64
    rows = np.repeat(np.arange(n_rows, dtype=np.float32), 64)
    cols = np.tile(np.arange(64, dtype=np.float32), n_rows)
    inv_freq = (np.float32(10000.0) ** (-np.arange(32, dtype=np.float32) / np.float32(32))).astype(np.float32)
    ang = np.concatenate([rows[:, None] * inv_freq, cols[:, None] * inv_freq], axis=-1).astype(np.float32)
    cos16 = np.ascontiguousarray(np.tile(np.cos(ang).astype(np.float32), (1, 16)))
    sin16 = np.ascontiguousarray(np.tile(np.sin(ang).astype(np.float32), (1, 16)))
    lg = np.log1p(-np.exp2(-5.0 - np.arange(8, dtype=f64)))
    pos = np.arange(128, dtype=f64)
    sc = 128.0 ** -0.5
    rdec = np.zeros((128, 32), f64)
    for h in range(8):
        rdec[:, h] = np.exp((127.0 - pos) * lg[h]) * sc
        rdec[:, 8 + h] = np.exp(pos * lg[h]) * sc
        rdec[:, 16 + h] = np.exp((pos + 1.0) * lg[h])
        rdec[:, 24 + h] = np.exp((128.0 - pos) * lg[h])
    dsym = np.zeros((128, 8, 128), f64)
    ad = np.abs(pos[:, None] - pos[None, :])
    for h in range(8):
        dsym[:, h, :] = np.exp(ad * lg[h]) * sc
    blk = np.arange(128) // 32
    same = blk[:, None] == blk[None, :]
    s_i = np.arange(128)[:, None]
    t_i = np.arange(128)[None, :]
    mbd = np.zeros((128, 2, 128), np.float32)
    mbd[:, 0, :] = (same & (s_i <= t_i))
    mbd[:, 1, :] = (same & (s_i >= t_i))
    sel = np.zeros((8, 8, 128), np.float32)
    for e in range(8):
        sel[e, e, :] = 1.0
    return dict(cos16=cos16, sin16=sin16, rdec=rdec.astype(np.float32), dsymT=dsym.astype(np.float32), maskBD=mbd, sel=sel)


def _params(inp):
    f = lambda a: np.asarray(a, dtype=np.float32)
    pp = np.zeros((128, 400), np.float32)
    rp = np.zeros((2, 128, 4352), np.float32)
    for l in range(2):
        b = l * 200
        pp[:, b + 0:b + 32] = f(inp["ln1_g"])[l].reshape(32, 128).T
        pp[:, b + 32:b + 64] = f(inp["ln1_b"])[l].reshape(32, 128).T
        pp[:, b + 64:b + 96] = f(inp["ln2_g"])[l].reshape(32, 128).T
        pp[:, b + 96:b + 128] = f(inp["ln2_b"])[l].reshape(32, 128).T
        pp[:, b + 128:b + 160] = f(inp["merge_scale"])[l].reshape(32, 128).T
        pp[:, b + 160:b + 168] = f(inp["hgrn_out_norm"])[l].reshape(8, 128).T
        pp[:, b + 168:b + 176] = f(inp["ret_out_norm"])[l].reshape(8, 128).T
        pp[:, b + 176:b + 184] = f(inp["hgrn_lower_bound"])[0].reshape(8, 128).T
        pp[:, b + 184:b + 192] = f(inp["hgrn_lower_bound"])[1].reshape(8, 128).T
        row = np.concatenate([np.tile(f(inp["attn_q_norm"])[l], 8), np.tile(f(inp["attn_k_norm"])[l], 2),
                              f(inp["gmlp_v_norm_g"])[l], f(inp["gmlp_v_norm_b"])[l], f(inp["merge_scale"])[l][2048:3072]])
        rp[l] = np.broadcast_to(row[None, :], (128, 4352))
    wsT = np.ascontiguousarray(f(inp["gmlp_w_s"]).transpose(0, 3, 1, 2))
    bsT = np.ascontiguousarray(f(inp["gmlp_b_s"]).transpose(0, 2, 1))
    router = np.ascontiguousarray(f(inp["moe_router"])[0].reshape(32, 128, 8).transpose(1, 0, 2))
    return dict(
        pp=pp, rp=rp, wsT=wsT, bsT=bsT, router=router,
        w_in=f(inp["w_in"]), w_o=f(inp["w_o"]),
        ffn_g=f(inp["ffn_w_gate"])[0], ffn_u=f(inp["ffn_w_up"])[0], ffn_d=f(inp["ffn_w_down"])[0],
        moe_g=f(inp["moe_w_gate"])[0], moe_u=f(inp["moe_w_up"])[0],
        moe_d=f(inp["moe_w_down"])[0].reshape(NE * DFE, D),
    )


def kernel(**inputs):
    T = 8192
    if T not in _CACHE:
        _CACHE[T] = Prog(T)
    prog = _CACHE[T]
    shared = _params(inputs)
    shared.update(_consts(T))
    xp = np.asarray(inputs["x_prompt"], dtype=np.float32)
    xs = np.asarray(inputs["x_sample"], dtype=np.float32)
    seqs = [xp[0], xp[1], xs[0]]
    in_maps = []
    for c in range(NCORES):
        m = dict(shared)
        m["xT"] = np.ascontiguousarray(seqs[c].T)
        in_maps.append(m)
    res = run_bass_kernel_spmd(prog.nc, in_maps, core_ids=list(range(NCORES)))
    outs = [np.ascontiguousarray(np.asarray(res.results[c]["yT"], dtype=np.float32).T) for c in range(NCORES)]
    y_prompt = np.stack([outs[0], outs[1]], axis=0)
    y_sample = outs[2][None]
    return (y_prompt, y_sample)
```

```python
import contextlib
import numpy as np
import concourse.bass as bass
import concourse.mybir as mybir
from concourse.bass_utils import run_bass_kernel_spmd

F32 = mybir.dt.float32
BF16 = mybir.dt.bfloat16
AF = mybir.ActivationFunctionType
ALU = mybir.AluOpType
AX = mybir.AxisListType

D = 4096
DIN = 12800
DFF = 5632
NE = 8
DFE = 1024
HD = 128
OFF = dict(aq=0, ak=1024, av=1280, hq=1536, hff=2560, hfb=3584, hi=4608, hg=5632,
           gu=6656, gv=7680, rq=8704, rk=9728, rv=10752, rg=11776)
ALPHA = 4.0 ** 0.25
LN_EPS = 1e-5
NORM_EPS = 1e-6
NCORES = 3


class Tk:
    __slots__ = ("w", "r")

    def __init__(self):
        self.w = None
        self.r = {}


class Sched:
    NDS = 16

    def __init__(self, nc, es):
        self.nc = nc
        self.eng = {"pe": nc.tensor, "act": nc.scalar, "dve": nc.vector, "pool": nc.gpsimd, "sp": nc.sync}
        self.sem = {k: es.enter_context(nc.semaphore("s_" + k)) for k in self.eng}
        self.cnt = {k: 0 for k in self.eng}
        self.seen = {k: {} for k in self.eng}
        self.dsem = {}
        self.dval = {}
        self.dnext = {}
        for q in ("sp", "pool", "act"):
            self.dsem[q] = [es.enter_context(nc.semaphore("d_%s%d" % (q, i))) for i in range(self.NDS)]
            self.dval[q] = [0] * self.NDS
            self.dnext[q] = 0
        self.ninst = 0

    def _semobj(self, k):
        if isinstance(k, tuple):
            return self.dsem[k[0]][k[1]]
        return self.sem[k]

    def _wait(self, e, deps):
        eng = self.eng[e]
        seen = self.seen[e]
        for k, v in deps.items():
            if k == e and e in ("pe", "sp"):
                continue
            if seen.get(k, 0) >= v:
                continue
            eng.wait_ge(self._semobj(k), v)
            seen[k] = v

    @staticmethod
    def _deps(reads, writes):
        deps = {}
        for t in reads:
            if t.w is not None:
                k, v = t.w
                if deps.get(k, 0) < v:
                    deps[k] = v
        for t in writes:
            if t.w is not None:
                k, v = t.w
                if deps.get(k, 0) < v:
                    deps[k] = v
            for k, v in t.r.items():
                if deps.get(k, 0) < v:
                    deps[k] = v
        return deps

    @staticmethod
    def _mark(tok, reads, writes):
        k, v = tok
        for t in reads:
            t.r[k] = v
        for t in writes:
            t.w = tok
            t.r = {}

    def op(self, e, fn, reads=(), writes=()):
        self._wait(e, self._deps(reads, writes))
        ins = fn(self.eng[e])
        self.cnt[e] += 1
        ins.then_inc(self.sem[e], 1)
        self._mark((e, self.cnt[e]), reads, writes)
        self.ninst += 1
        return ins

    def dma(self, q, out, in_, reads=(), writes=()):
        deps = self._deps(reads, writes)
        i = self.dnext[q]
        self.dnext[q] = (i + 1) % self.NDS
        key = (q, i)
        if self.dval[q][i] > 0 and deps.get(key, 0) < self.dval[q][i]:
            deps[key] = self.dval[q][i]
        self._wait(q, deps)
        ins = self.eng[q].dma_start(out=out, in_=in_)
        self.dval[q][i] += 16
        ins.then_inc(self.dsem[q][i], 16)
        self._mark((key, self.dval[q][i]), reads, writes)
        self.ninst += 1
        return ins

    def barrier(self):
        deps = {k: self.cnt[k] for k in self.eng if self.cnt[k] > 0}
        for q in self.dsem:
            for i in range(self.NDS):
                if self.dval[q][i] > 0:
                    deps[(q, i)] = self.dval[q][i]
        for e in self.eng:
            self._wait(e, deps)


class Prog:
    def __init__(self, T, dbg=(), layers=2, phases=None):
        self.T = T
        self.dbg = set(dbg)
        self.layers = layers
        self.phases = phases
        self.nc = nc = bass.Bass("TRN2", target_bir_lowering=False)
        self.es = es = contextlib.ExitStack()
        self.S = Sched(nc, es)
        self.rr = 0
        self.build()
        es.close()

    def din(self, name, shape, dt=F32):
        return self.nc.dram_tensor(name, list(shape), dt, kind="ExternalInput").ap()

    def dscr(self, name, shape, dt, out=False):
        kind = "ExternalOutput" if (out or name in self.dbg) else "Internal"
        return self.nc.dram_tensor(name, list(shape), dt, kind=kind).ap()

    def sb(self, st, name, shape, dt):
        self.uid = getattr(self, "uid", 0) + 1
        return st.enter_context(self.nc.sbuf_tensor("%s_%d" % (name, self.uid), list(shape), dt))

    def any2(self):
        self.rr += 1
        return ("act", "dve")[self.rr % 2]

    def any3(self):
        self.rr += 1
        return ("act", "dve", "pool")[self.rr % 3]

    @staticmethod
    def copy(e, eng, out, in_):
        if e == "act":
            return eng.activation(out=out, in_=in_, func=AF.Copy)
        return eng.tensor_copy(out=out, in_=in_)

    def convert(self, src, R, C, dst_fn, piece=2048):
        S = self.S
        with contextlib.ExitStack() as st:
            NB = 3
            stg = [self.sb(st, "cv_f%d" % i, [128, piece], F32) for i in range(NB)]
            stb = [self.sb(st, "cv_b%d" % i, [128, piece], BF16) for i in range(NB)]
            tf = [Tk() for _ in range(NB)]
            tb = [Tk() for _ in range(NB)]
            i = 0
            for rt in range(R // 128):
                for c0 in range(0, C, piece):
                    n = min(piece, C - c0)
                    b = i % NB
                    i += 1
                    S.dma("sp", stg[b][:, :n], src[rt * 128:(rt + 1) * 128, c0:c0 + n], writes=[tf[b]])
                    e = self.any3()
                    S.op(e, lambda eng, b=b, n=n, e=e: self.copy(e, eng, stb[b][:, :n], stg[b][:, :n]),
                         reads=[tf[b]], writes=[tb[b]])
                    dst, view = dst_fn(rt, c0, n)
                    S.dma("pool", dst, view(stb[b][:, :n]), reads=[tb[b]], writes=[Tk()])
        S.barrier()

    def conv_blocked(self, src, K, N, Wb, CB, off=0, w=None, cb_base=0):
        w = w or CB

        def dst_fn(rt, c0, n):
            nb = n // w
            cb0 = cb_base + c0 // w
            dst = Wb[cb0:cb0 + nb, :, rt, off:off + w].rearrange("cb p c -> p cb c")
            return dst, (lambda v: v.rearrange("p (cb c) -> p cb c", c=w))
        piece = 2048 if N % 2048 == 0 or N > 2048 else N
        self.convert(src, K, N, dst_fn, piece=piece)

    def conv_plain(self, src, R, C, dst):
        def dst_fn(rt, c0, n):
            return dst[rt * 128:(rt + 1) * 128, c0:c0 + n], (lambda v: v)
        self.convert(src, R, C, dst_fn, piece=min(2048, C))

    def gemm(self, actT, KC, Wb, NCB, CB, form, epi, TT, banks, name):
        S = self.S
        T = self.T
        TT = min(TT, T)
        TS = min(512, TT)
        nb = len(banks)
        bi = 0
        with contextlib.ExitStack() as st:
            act = self.sb(st, name + "_act", [128, KC, TT], BF16)
            G = 8
            ngr = (KC + G - 1) // G
            t_act = [Tk() for _ in range(ngr)]
            wbuf = [self.sb(st, name + "_w%d" % i, [128, KC, CB], BF16) for i in range(2)]
            t_w = [Tk(), Tk()]
            aT = actT.rearrange("(kc p) t -> p kc t", p=128)
            wi = 0
            for st_i in range(T // TT):
                for g in range(ngr):
                    k0, k1 = g * G, min(KC, (g + 1) * G)
                    S.dma("sp", act[:, k0:k1, :], aT[:, k0:k1, st_i * TT:(st_i + 1) * TT], writes=[t_act[g]])
                for cb in range(NCB):
                    wb = wi % 2
                    wi += 1
                    S.dma("sp", wbuf[wb][:], Wb[cb], writes=[t_w[wb]])
                    if form == "A":
                        for ts in range(TT // 128):
                            bank, tb = banks[bi % nb]
                            bi += 1
                            for kc in range(KC):
                                S.op("pe", lambda e, kc=kc, ts=ts, wb=wb, bank=bank: e.matmul(
                                    bank[:, :CB], act[:, kc, ts * 128:(ts + 1) * 128], wbuf[wb][:, kc, :],
                                    start=(kc == 0), stop=(kc == KC - 1)),
                                    reads=[t_act[kc // G], t_w[wb]], writes=[tb])
                            epi((bank, tb), st_i * TT + ts * 128, cb)
                    else:
                        for ts in range(TT // TS):
                            subs = []
                            for sub in range(CB // 128):
                                bank, tb = banks[bi % nb]
                                bi += 1
                                for kc in range(KC):
                                    S.op("pe", lambda e, kc=kc, ts=ts, wb=wb, bank=bank, sub=sub: e.matmul(
                                        bank[:, :TS], wbuf[wb][:, kc, sub * 128:(sub + 1) * 128],
                                        act[:, kc, ts * TS:(ts + 1) * TS],
                                        start=(kc == 0), stop=(kc == KC - 1)),
                                        reads=[t_act[kc // G], t_w[wb]], writes=[tb])
                                subs.append((bank, tb))
                            epi(subs, st_i * TT + ts * TS, cb)
        S.barrier()


    def build(self):
        nc, S, T = self.nc, self.S, self.T
        es = self.es
        L = self.layers
        ph = self.phases
        NPP, NRP = 400, 4352
        self.xT = self.din("xT", [D, T])
        self.w_in = self.din("w_in", [2, D, DIN])
        self.w_o = self.din("w_o", [2, D, D])
        self.ffn_g = self.din("ffn_g", [D, DFF])
        self.ffn_u = self.din("ffn_u", [D, DFF])
        self.ffn_d = self.din("ffn_d", [DFF, D])
        self.moe_g = self.din("moe_g", [NE, D, DFE])
        self.moe_u = self.din("moe_u", [NE, D, DFE])
        self.moe_d = self.din("moe_d", [NE * DFE, D])
        self.router = self.din("router", [128, 32, NE])
        self.pp_in = self.din("pp", [128, NPP])
        self.rp_in = self.din("rp", [2, 128, NRP])
        self.wsT_in = self.din("wsT", [2, 128, 8, 128])
        self.bsT_in = self.din("bsT", [2, 128, 8])
        self.cos_in = self.din("cos16", [T, 1024])
        self.sin_in = self.din("sin16", [T, 1024])
        self.dsym_in = self.din("dsymT", [128, 8, 128])
        self.rdec_in = self.din("rdec", [128, 32])
        self.mbd_in = self.din("maskBD", [128, 2, 128])
        self.sel_in = self.din("sel", [8, 8, 128])
        self.yT = self.dscr("yT", [D, T], F32, out=True)
        self.xTb = self.dscr("xTb", [D, T], BF16)
        self.winA = [self.dscr("winA%d" % l, [17, 128, 32, 512], BF16) for l in range(2)]
        self.winB = [self.dscr("winB%d" % l, [16, 128, 32, 256], BF16) for l in range(2)]
        self.wob = [self.dscr("wob%d" % l, [16, 128, 32, 256], BF16) for l in range(2)]
        self.wgub = self.dscr("wgub", [44, 128, 32, 256], BF16)
        self.wdb = self.dscr("wdb", [16, 128, 44, 256], BF16)
        self.wmgub = self.dscr("wmgub", [64, 128, 32, 256], BF16)
        self.wmdb = self.dscr("wmdb", [16, 128, 64, 256], BF16)
        self.Y_a = self.dscr("Y_a", [T, 1536], F32)
        self.Y_hi = self.dscr("Y_hi", [T, 1024], F32)
        self.Y_g = self.dscr("Y_g", [T, 2048], F32)
        self.Y_r = self.dscr("Y_r", [T, 4096], F32)
        self.YT = self.dscr("YT", [D, T], F32)
        self.aqkT = self.dscr("aqkT", [10, 128, T], BF16)
        self.rqkT = self.dscr("rqkT", [16, 128, T], BF16)
        self.RK = self.dscr("RK", [3, T, 1024], BF16)
        self.mergedT = self.dscr("mergedT", [D, T], BF16)
        self.zT = self.dscr("zT", [D, T], F32)
        self.x1T = self.dscr("x1T", [D, T], F32)
        self.x1Tb = self.dscr("x1Tb", [D, T], BF16)
        self.x2T = self.dscr("x2T", [D, T], F32)
        self.x2Tb = self.dscr("x2Tb", [D, T], BF16)
        self.hT = self.dscr("hT", [NE * DFE, T], BF16)
        self.Grep = self.dscr("Grep", [NE, 128, T], F32)
        self.banks = []
        for i in range(6):
            p = es.enter_context(nc.psum_tensor("ps%d" % i, [128, 512], F32))
            self.banks.append((p, Tk()))
        self.bbanks = []
        for i in range(2):
            p = es.enter_context(nc.psum_tensor("pb%d" % i, [128, 1024], BF16))
            self.bbanks.append((p, Tk()))
        self.pp = self.sb(es, "pp", [128, NPP], F32)
        self.t_pp = Tk()
        self.idf = self.sb(es, "idf", [128, 128], F32)
        self.idb = self.sb(es, "idb", [128, 128], BF16)
        self.onesb = self.sb(es, "onesb", [128, 128], BF16)
        self.epsln = self.sb(es, "epsln", [128, 2], F32)
        self.t_c = Tk()
        S.dma("sp", self.pp[:], self.pp_in, writes=[self.t_pp])
        S.op("pool", lambda e: e.memset(self.idf[:], 0.0), writes=[self.t_c])
        S.op("pool", lambda e: e.affine_select(out=self.idf[:], in_=self.idf[:], pattern=[[-1, 128]],
                                               compare_op=ALU.not_equal, fill=1.0, base=0, channel_multiplier=1),
             writes=[self.t_c])
        S.op("pool", lambda e: e.tensor_copy(out=self.idb[:], in_=self.idf[:]), writes=[self.t_c])
        S.op("pool", lambda e: e.memset(self.onesb[:], 1.0), writes=[self.t_c])
        S.op("pool", lambda e: e.memset(self.epsln[:, 0:1], LN_EPS), writes=[self.t_c])
        S.op("pool", lambda e: e.memset(self.epsln[:, 1:2], NORM_EPS), writes=[self.t_c])
        S.barrier()

        def on(p):
            return ph is None or p in ph
        if on("conv"):
            self.conv_plain(self.xT, D, T, self.xTb)
            for l in range(L):
                w = self.w_in[l]
                self.conv_blocked(w[:, 0:1536], D, 1536, self.winA[l], 512, cb_base=0)
                self.conv_blocked(w[:, 4608:5632], D, 1024, self.winA[l], 512, cb_base=3)
                self.conv_blocked(w[:, 6656:12800], D, 6144, self.winA[l], 512, cb_base=5)
                self.conv_blocked(w[:, 1536:4608], D, 3072, self.winB[l], 256, cb_base=0)
                self.conv_blocked(w[:, 5632:6656], D, 1024, self.winB[l], 256, cb_base=12)
                self.conv_blocked(self.w_o[l], D, D, self.wob[l], 256)
            if on("ffn"):
                self.conv_blocked(self.ffn_g, D, DFF, self.wgub, 256, off=0, w=128)
                self.conv_blocked(self.ffn_u, D, DFF, self.wgub, 256, off=128, w=128)
                self.conv_blocked(self.ffn_d, DFF, D, self.wdb, 256)
            if L > 1 and on("moe"):
                for e_ in range(NE):
                    self.conv_blocked(self.moe_g[e_], D, DFE, self.wmgub, 256, off=0, w=128, cb_base=e_ * 8)
                    self.conv_blocked(self.moe_u[e_], D, DFE, self.wmgub, 256, off=128, w=128, cb_base=e_ * 8)
                self.conv_blocked(self.moe_d, NE * DFE, D, self.wmdb, 256)
        xF, xB = self.xT, self.xTb
        for l in range(L):
            if on("inproj"):
                self.in_proj(l, xB)
            if on("gmlp"):
                self.mix_gmlp(l)
            if on("attn"):
                self.mix_attn(l)
            if on("ret"):
                self.mix_ret(l)
            if on("hgrn"):
                self.mix_hgrn(l)
            if on("wo"):
                self.resid_gemm(self.mergedT, 32, self.wob[l], xF, 1024, "wo")
                self.ln_pass(self.zT, l * 200 + 0, l * 200 + 32, self.x1T, self.x1Tb)
            if l == 0:
                if on("ffn"):
                    self.ffn_up(self.x1Tb, self.wgub, DFF // 128, None)
                    self.resid_gemm(self.hT, DFF // 128, self.wdb, self.x1T, 1024, "fd")
            else:
                if on("moe"):
                    self.router_pass(self.x1T)
                    self.ffn_up(self.x1Tb, self.wmgub, NE * DFE // 128, self.Grep)
                    self.resid_gemm(self.hT, NE * DFE // 128, self.wmdb, self.x1T, 512, "md")
            if on("ffn") or on("moe"):
                last = (l == L - 1)
                self.ln_pass(self.zT, l * 200 + 64, l * 200 + 96, self.yT if last else self.x2T,
                             None if last else self.x2Tb)
            xF, xB = self.x2T, self.x2Tb
        S.barrier()

    def in_proj(self, l, actT):
        S = self.S
        colA = ([(self.Y_a, 512 * i) for i in range(3)] + [(self.Y_hi, 512 * i) for i in range(2)]
                + [(self.Y_g, 512 * i) for i in range(4)] + [(self.Y_r, 512 * i) for i in range(8)])
        with contextlib.ExitStack() as st:
            NSTG = 4
            stg = [self.sb(st, "p1_s%d" % i, [128, 512], F32) for i in range(NSTG)]
            ts = [Tk() for _ in range(NSTG)]
            cnt = [0]

            def epi(bt, tok0, cb):
                bank, tb = bt
                i = cnt[0] % NSTG
                cnt[0] += 1
                e = self.any2()
                S.op(e, lambda eng: self.copy(e, eng, stg[i][:], bank[:]), reads=[tb], writes=[ts[i]])
                yt_, yc_ = colA[cb]
                S.dma("pool", yt_[tok0:tok0 + 128, yc_:yc_ + 512], stg[i][:], reads=[ts[i]], writes=[Tk()])
            self.gemm(actT, 32, self.winA[l], 17, 512, "A", epi, 1024, self.banks[:4], "p1a")
        with contextlib.ExitStack() as st:
            NSTG = 4
            TS = min(512, self.T)
            stg = [self.sb(st, "p1b_s%d" % i, [128, TS], F32) for i in range(NSTG)]
            ts = [Tk() for _ in range(NSTG)]
            cnt = [0]

            def epi(subs, tok0, cb):
                for sub, (bank, tb) in enumerate(subs):
                    i = cnt[0] % NSTG
                    cnt[0] += 1
                    e = self.any2()
                    S.op(e, lambda eng, i=i, bank=bank, e=e: self.copy(e, eng, stg[i][:], bank[:, :TS]), reads=[tb], writes=[ts[i]])
                    r0 = (cb * 2 + sub) * 128
                    S.dma("pool", self.YT[r0:r0 + 128, tok0:tok0 + TS], stg[i][:], reads=[ts[i]], writes=[Tk()])
            self.gemm(actT, 32, self.winB[l], 16, 256, "B", epi, 1024, self.banks[:4], "p1b")

    def resid_gemm(self, actT, KC, Wb, xresT, TT, name):
        S = self.S
        TS = min(512, self.T)
        with contextlib.ExitStack() as st:
            NSTG = 3
            xr = [self.sb(st, name + "_x%d" % i, [128, TS], F32) for i in range(NSTG)]
            zt = [self.sb(st, name + "_z%d" % i, [128, TS], F32) for i in range(NSTG)]
            tx = [Tk() for _ in range(NSTG)]
            tz = [Tk() for _ in range(NSTG)]
            cnt = [0]

            def epi(subs, tok0, cb):
                for sub, (bank, tb) in enumerate(subs):
                    i = cnt[0] % NSTG
                    cnt[0] += 1
                    r0 = (cb * 2 + sub) * 128
                    S.dma("sp", xr[i][:], xresT[r0:r0 + 128, tok0:tok0 + TS], writes=[tx[i]])
                    S.op("dve", lambda e, i=i, bank=bank: e.scalar_tensor_tensor(
                        out=zt[i][:], in0=xr[i][:], scalar=ALPHA, in1=bank[:, :TS], op0=ALU.mult, op1=ALU.add),
                        reads=[tx[i], tb], writes=[tz[i]])
                    S.dma("pool", self.zT[r0:r0 + 128, tok0:tok0 + TS], zt[i][:], reads=[tz[i]], writes=[Tk()])
            self.gemm(actT, KC, Wb, 16, 256, "B", epi, TT, self.banks[:4], name)

    def ffn_up(self, actT, Wb, NCB, grep):
        S = self.S
        TS = min(512, self.T)
        with contextlib.ExitStack() as st:
            NSTG = 3
            sg = [self.sb(st, "fu_s%d" % i, [128, TS], F32) for i in range(NSTG)]
            h1 = [self.sb(st, "fu_h%d" % i, [128, TS], F32) for i in range(NSTG)]
            gr = [self.sb(st, "fu_g%d" % i, [128, TS], F32) for i in range(NSTG)]
            hb = [self.sb(st, "fu_b%d" % i, [128, TS], BF16) for i in range(NSTG)]
            tsg = [Tk() for _ in range(NSTG)]
            th1 = [Tk() for _ in range(NSTG)]
            tgr = [Tk() for _ in range(NSTG)]
            thb = [Tk() for _ in range(NSTG)]
            cnt = [0]

            def epi(subs, tok0, cb):
                (bg, tg), (bu, tu) = subs
                i = cnt[0] % NSTG
                cnt[0] += 1
                S.op("act", lambda e: e.activation(out=sg[i][:], in_=bg[:, :TS], func=AF.Silu), reads=[tg], writes=[tsg[i]])
                if grep is None:
                    S.op("dve", lambda e: e.tensor_tensor(out=hb[i][:], in0=sg[i][:], in1=bu[:, :TS], op=ALU.mult),
                         reads=[tsg[i], tu], writes=[thb[i]])
                else:
                    S.dma("sp", gr[i][:], grep[cb // 8, :, tok0:tok0 + TS], writes=[tgr[i]])
                    S.op("dve", lambda e: e.tensor_tensor(out=h1[i][:], in0=sg[i][:], in1=bu[:, :TS], op=ALU.mult),
                         reads=[tsg[i], tu], writes=[th1[i]])
                    S.op("pool", lambda e: e.tensor_tensor(out=hb[i][:], in0=h1[i][:], in1=gr[i][:], op=ALU.mult),
                         reads=[th1[i], tgr[i]], writes=[thb[i]])
                S.dma("pool", self.hT[cb * 128:(cb + 1) * 128, tok0:tok0 + TS], hb[i][:], reads=[thb[i]], writes=[Tk()])
            self.gemm(actT, 32, Wb, NCB, 256, "B", epi, 1024, self.banks[:4], "fu")

    def ln_pass(self, zT, gcol, bcol, outF, outB):
        S = self.S
        T = self.T
        TS = min(512, T)
        bs_, bq_ = self.banks[4], self.banks[5]
        zTr = zT.rearrange("(c p) t -> p c t", p=128)
        with contextlib.ExitStack() as st:
            z = [self.sb(st, "ln_z%d" % i, [128, 32, TS], F32) for i in range(2)]
            tz = [[Tk() for _ in range(4)] for _ in range(2)]
            NR = 3
            zb = [self.sb(st, "ln_zb%d" % i, [128, TS], BF16) for i in range(NR)]
            zq = [self.sb(st, "ln_zq%d" % i, [128, TS], BF16) for i in range(NR)]
            tzb = [Tk() for _ in range(NR)]
            tzq = [Tk() for _ in range(NR)]
            mean = self.sb(st, "ln_mean", [128, TS], F32)
            msq = self.sb(st, "ln_msq", [128, TS], F32)
            rstd = self.sb(st, "ln_rstd", [128, TS], F32)
            tst = Tk()
            t1 = [self.sb(st, "ln_t%d" % i, [128, TS], F32) for i in range(NR)]
            of = [self.sb(st, "ln_of%d" % i, [128, TS], F32) for i in range(NR)]
            ob = [self.sb(st, "ln_ob%d" % i, [128, TS], BF16) for i in range(NR)]
            tt1 = [Tk() for _ in range(NR)]
            tof = [Tk() for _ in range(NR)]
            tob = [Tk() for _ in range(NR)]
            k = 0
            for tt in range(T // TS):
                zi = tt % 2
                tok = slice(tt * TS, (tt + 1) * TS)
                for g in range(4):
                    S.dma("sp", z[zi][:, g * 8:(g + 1) * 8, :], zTr[:, g * 8:(g + 1) * 8, tok], writes=[tz[zi][g]])
                for c in range(32):
                    i = k % NR
                    k += 1
                    S.op("act", lambda e: e.activation(out=zb[i][:], in_=z[zi][:, c, :], func=AF.Copy),
                         reads=[tz[zi][c // 8]], writes=[tzb[i]])
                    S.op("act", lambda e: e.activation(out=zq[i][:], in_=z[zi][:, c, :], func=AF.Square),
                         reads=[tz[zi][c // 8]], writes=[tzq[i]])
                    S.op("pe", lambda e: e.matmul(bs_[0][:, :TS], self.onesb[:], zb[i][:], start=(c == 0), stop=(c == 31)),
                         reads=[tzb[i], self.t_c], writes=[bs_[1]])
                    S.op("pe", lambda e: e.matmul(bq_[0][:, :TS], self.onesb[:], zq[i][:], start=(c == 0), stop=(c == 31)),
                         reads=[tzq[i], self.t_c], writes=[bq_[1]])
                S.op("dve", lambda e: e.tensor_scalar(out=mean[:], in0=bs_[0][:, :TS], scalar1=1.0 / D, scalar2=1.0, op0=ALU.mult, op1=ALU.mult),
                     reads=[bs_[1]], writes=[tst])
                S.op("dve", lambda e: e.tensor_tensor(out=msq[:], in0=mean[:], in1=mean[:], op=ALU.mult), reads=[tst], writes=[tst])
                S.op("dve", lambda e: e.scalar_tensor_tensor(out=msq[:], in0=bq_[0][:, :TS], scalar=1.0 / D, in1=msq[:],
                                                            op0=ALU.mult, op1=ALU.subtract), reads=[bq_[1], tst], writes=[tst])
                S.op("act", lambda e: e.activation(out=rstd[:], in_=msq[:], func=AF.Sqrt, bias=self.epsln[:, 0:1], scale=1.0),
                     reads=[tst, self.t_c], writes=[tst])
                S.op("dve", lambda e: e.reciprocal(out=rstd[:], in_=rstd[:]), reads=[tst], writes=[tst])
                for c in range(32):
                    i = k % NR
                    k += 1
                    S.op("dve", lambda e: e.tensor_tensor(out=t1[i][:], in0=z[zi][:, c, :], in1=mean[:], op=ALU.subtract),
                         reads=[tz[zi][c // 8], tst], writes=[tt1[i]])
                    S.op("dve", lambda e: e.tensor_tensor(out=t1[i][:], in0=t1[i][:], in1=rstd[:], op=ALU.mult),
                         reads=[tst], writes=[tt1[i]])
                    S.op("act", lambda e: e.activation(out=of[i][:], in_=t1[i][:], func=AF.Identity,
                                                       bias=self.pp[:, bcol + c:bcol + c + 1], scale=self.pp[:, gcol + c:gcol + c + 1]),
                         reads=[tt1[i], self.t_pp], writes=[tof[i]])
                    S.dma("act", outF[c * 128:(c + 1) * 128, tok], of[i][:], reads=[tof[i]], writes=[Tk()])
                    if outB is not None:
                        S.op("pool", lambda e: e.tensor_copy(out=ob[i][:], in_=of[i][:]), reads=[tof[i]], writes=[tob[i]])
                        S.dma("act", outB[c * 128:(c + 1) * 128, tok], ob[i][:], reads=[tob[i]], writes=[Tk()])
        S.barrier()

    def router_pass(self, x1T):
        S = self.S
        T = self.T
        TS = min(512, T)
        xr_ = x1T.rearrange("(c p) t -> p c t", p=128)
        bl, bt, br = self.banks[0], self.banks[1], self.banks[2]
        with contextlib.ExitStack() as st:
            wr = self.sb(st, "rt_w", [128, 32, NE], F32)
            sel = self.sb(st, "rt_sel", [8, 8, 128], F32)
            tw = Tk()
            S.dma("sp", wr[:], self.router, writes=[tw])
            S.dma("sp", sel[:], self.sel_in, writes=[tw])
            xr = [self.sb(st, "rt_x%d" % i, [128, 32, 128], F32) for i in range(2)]
            tx = [Tk(), Tk()]
            sm = self.sb(st, "rt_sm", [128, 64], F32)
            tsm = Tk()
            gT = self.sb(st, "rt_gT", [8, TS], F32)
            tgT = Tk()
            gr = [self.sb(st, "rt_gr%d" % i, [128, TS], F32) for i in range(2)]
            tgr = [Tk(), Tk()]
            lg, eq1, l2, eq2, g1, gt = (sm[:, 0:8], sm[:, 8:16], sm[:, 16:24], sm[:, 24:32], sm[:, 32:40], sm[:, 40:48])
            m1, m2, dl, w1, w2 = (sm[:, 48:49], sm[:, 49:50], sm[:, 50:51], sm[:, 51:52], sm[:, 52:53])
            npg = TS // 128
            k = 0
            for n in range(T // 128):
                i = n % 2
                S.dma("sp", xr[i][:], xr_[:, :, n * 128:(n + 1) * 128], writes=[tx[i]])
                for c in range(32):
                    S.op("pe", lambda e: e.matmul(bl[0][:, 0:NE], xr[i][:, c, :], wr[:, c, :], start=(c == 0), stop=(c == 31)),
                         reads=[tx[i], tw], writes=[bl[1]])
                V = "dve"
                S.op(V, lambda e: e.tensor_copy(out=lg, in_=bl[0][:, 0:NE]), reads=[bl[1]], writes=[tsm])
                S.op(V, lambda e: e.tensor_reduce(out=m1, in_=lg, axis=AX.X, op=ALU.max), reads=[tsm], writes=[tsm])
                S.op(V, lambda e: e.tensor_scalar(out=eq1, in0=lg, scalar1=m1, scalar2=1.0, op0=ALU.is_equal, op1=ALU.mult), reads=[tsm], writes=[tsm])
                S.op(V, lambda e: e.scalar_tensor_tensor(out=l2, in0=eq1, scalar=-1e30, in1=lg, op0=ALU.mult, op1=ALU.add), reads=[tsm], writes=[tsm])
                S.op(V, lambda e: e.tensor_reduce(out=m2, in_=l2, axis=AX.X, op=ALU.max), reads=[tsm], writes=[tsm])
                S.op(V, lambda e: e.tensor_scalar(out=eq2, in0=l2, scalar1=m2, scalar2=1.0, op0=ALU.is_equal, op1=ALU.mult), reads=[tsm], writes=[tsm])
                S.op(V, lambda e: e.tensor_tensor(out=dl, in0=m1, in1=m2, op=ALU.subtract), reads=[tsm], writes=[tsm])
                S.op("act", lambda e: e.activation(out=w1, in_=dl, func=AF.Sigmoid), reads=[tsm], writes=[tsm])
                S.op("act", lambda e: e.activation(out=w2, in_=dl, func=AF.Sigmoid, scale=-1.0), reads=[tsm], writes=[tsm])
                S.op(V, lambda e: e.tensor_scalar(out=g1, in0=eq1, scalar1=w1, scalar2=1.0, op0=ALU.mult, op1=ALU.mult), reads=[tsm], writes=[tsm])
                S.op(V, lambda e: e.scalar_tensor_tensor(out=gt, in0=eq2, scalar=w2, in1=g1, op0=ALU.mult, op1=ALU.add), reads=[tsm], writes=[tsm])
                j = n % npg
                S.op("pe", lambda e: e.transpose(bt[0][0:8, j * 128:(j + 1) * 128], gt, self.idf[:]),
                     reads=[tsm, self.t_c], writes=[bt[1]])
                if j == npg - 1:
                    tok0 = (n - j) * 128
                    S.op("act", lambda e: e.activation(out=gT[:], in_=bt[0][0:8, :TS], func=AF.Copy), reads=[bt[1]], writes=[tgT])
                    for ex in range(NE):
                        S.op("pe", lambda e: e.matmul(br[0][:, :TS], sel[:, ex, :], gT[:], start=True, stop=True),
                             reads=[tgT, tw], writes=[br[1]])
                        b = k % 2
                        k += 1
                        en = self.any2()
                        S.op(en, lambda eng: self.copy(en, eng, gr[b][:], br[0][:, :TS]), reads=[br[1]], writes=[tgr[b]])
                        S.dma("pool", self.Grep[ex, :, tok0:tok0 + TS], gr[b][:], reads=[tgr[b]], writes=[Tk()])
        S.barrier()

    def rope(self, S, xin, rb, cs, sn, W, tin, tcs, tout, tmp, ttmp):
        xv = xin.rearrange("p (j two) -> p j two", two=2)
        ov = rb.rearrange("p (j two) -> p j two", two=2)
        x0, x1 = xv[:, :, 0], xv[:, :, 1]
        t1, t2, t3, t4 = tmp
        S.op("dve", lambda e: e.tensor_tensor(out=t1, in0=x0, in1=cs, op=ALU.mult), reads=[tin, tcs], writes=[ttmp[0]])
        S.op("pool", lambda e: e.tensor_tensor(out=t2, in0=x1, in1=sn, op=ALU.mult), reads=[tin, tcs], writes=[ttmp[1]])
        S.op("pool", lambda e: e.tensor_tensor(out=t3, in0=x0, in1=sn, op=ALU.mult), reads=[tin, tcs], writes=[ttmp[2]])
        S.op("dve", lambda e: e.tensor_tensor(out=t4, in0=x1, in1=cs, op=ALU.mult), reads=[tin, tcs], writes=[ttmp[3]])
        S.op("dve", lambda e: e.tensor_tensor(out=ov[:, :, 0], in0=t1, in1=t2, op=ALU.subtract),
             reads=[ttmp[0], ttmp[1]], writes=[tout])
        S.op("pool", lambda e: e.tensor_tensor(out=ov[:, :, 1], in0=t3, in1=t4, op=ALU.add),
             reads=[ttmp[2], ttmp[3], tout], writes=[tout])

    def mix_gmlp(self, l):
        S = self.S
        T = self.T
        bA, bB = self.banks[0], self.banks[1]
        with contextlib.ExitStack() as st:
            grep = self.sb(st, "gm_g", [128, 1024], F32)
            brep = self.sb(st, "gm_b", [128, 1024], F32)
            msrep = self.sb(st, "gm_ms", [128, 1024], F32)
            wsf = self.sb(st, "gm_wsf", [128, 8, 128], F32)
            wsb = self.sb(st, "gm_wsb", [128, 8, 128], BF16)
            bs = self.sb(st, "gm_bs", [128, 8], F32)
            tc_ = Tk()
            S.dma("sp", grep[:], self.rp_in[l, :, 1280:2304], writes=[tc_])
            S.dma("sp", brep[:], self.rp_in[l, :, 2304:3328], writes=[tc_])
            S.dma("sp", msrep[:], self.rp_in[l, :, 3328:4352], writes=[tc_])
            S.dma("sp", wsf[:], self.wsT_in[l], writes=[tc_])
            S.dma("sp", bs[:], self.bsT_in[l], writes=[tc_])
            S.op("dve", lambda e: e.tensor_copy(out=wsb[:], in_=wsf[:]), reads=[tc_], writes=[tc_])
            NB = 2
            gv = [self.sb(st, "gm_gv%d" % i, [128, 1024], F32) for i in range(NB)]
            gu = [self.sb(st, "gm_gu%d" % i, [128, 1024], F32) for i in range(NB)]
            a = [self.sb(st, "gm_a%d" % i, [128, 1024], F32) for i in range(NB)]
            sq = [self.sb(st, "gm_sq%d" % i, [128, 1024], F32) for i in range(NB)]
            vnb = [self.sb(st, "gm_vn%d" % i, [128, 1024], BF16) for i in range(NB)]
            oc = [self.sb(st, "gm_oc%d" % i, [128, 1024], F32) for i in range(NB)]
            ocb = [self.sb(st, "gm_ob%d" % i, [128, 1024], BF16) for i in range(NB)]
            mT = [self.sb(st, "gm_mT%d" % i, [128, 1024], BF16) for i in range(NB)]
            sm = [self.sb(st, "gm_sm%d" % i, [128, 8], F32) for i in range(NB)]
            tgv = [Tk() for _ in range(NB)]
            tgu = [Tk() for _ in range(NB)]
            ta = [Tk() for _ in range(NB)]
            tsq = [Tk() for _ in range(NB)]
            tvn = [Tk() for _ in range(NB)]
            toc = [Tk() for _ in range(NB)]
            tob = [Tk() for _ in range(NB)]
            tmT = [Tk() for _ in range(NB)]
            tsm = [Tk() for _ in range(NB)]
            mdst = self.mergedT[2048:3072, :].rearrange("(g p) t -> p g t", p=128)
            for n in range(T // 128):
                i = n % NB
                rows = slice(n * 128, (n + 1) * 128)
                S.dma("sp", gv[i][:], self.Y_g[rows, 1024:2048], writes=[tgv[i]])
                S.dma("sp", gu[i][:], self.Y_g[rows, 0:1024], writes=[tgu[i]])
                s1, nm, s2, rs = sm[i][:, 0:1], sm[i][:, 1:2], sm[i][:, 2:3], sm[i][:, 3:4]
                S.op("act", lambda e: e.activation(out=a[i][:], in_=gv[i][:], func=AF.Gelu), reads=[tgv[i]], writes=[ta[i]])
                S.op("dve", lambda e: e.tensor_reduce(out=s1, in_=a[i][:], axis=AX.X, op=ALU.add), reads=[ta[i]], writes=[tsm[i]])
                S.op("dve", lambda e: e.tensor_scalar(out=nm, in0=s1, scalar1=-1.0 / 1024, scalar2=1.0, op0=ALU.mult, op1=ALU.mult), reads=[tsm[i]], writes=[tsm[i]])
                S.op("dve", lambda e: e.tensor_scalar(out=a[i][:], in0=a[i][:], scalar1=nm, scalar2=0.0, op0=ALU.add, op1=ALU.add), reads=[tsm[i]], writes=[ta[i]])
                S.op("pool", lambda e: e.tensor_tensor(out=sq[i][:], in0=a[i][:], in1=a[i][:], op=ALU.mult), reads=[ta[i]], writes=[tsq[i]])
                S.op("dve", lambda e: e.tensor_reduce(out=s2, in_=sq[i][:], axis=AX.X, op=ALU.add), reads=[tsq[i]], writes=[tsm[i]])
                S.op("act", lambda e: e.activation(out=rs, in_=s2, func=AF.Sqrt, bias=self.epsln[:, 0:1], scale=1.0 / 1024),
                     reads=[tsm[i], self.t_c], writes=[tsm[i]])
                S.op("dve", lambda e: e.reciprocal(out=rs, in_=rs), reads=[tsm[i]], writes=[tsm[i]])
                S.op("dve", lambda e: e.scalar_tensor_tensor(out=sq[i][:], in0=a[i][:], scalar=rs, in1=grep[:], op0=ALU.mult, op1=ALU.mult),
                     reads=[ta[i], tsm[i], tc_], writes=[tsq[i]])
                S.op("pool", lambda e: e.tensor_tensor(out=vnb[i][:], in0=sq[i][:], in1=brep[:], op=ALU.add), reads=[tsq[i], tc_], writes=[tvn[i]])
                for g in range(8):
                    bank = bA if g < 4 else bB
                    c0 = (g % 4) * 128
                    S.op("pe", lambda e: e.matmul(bank[0][:, c0:c0 + 128], wsb[:, g, :], vnb[i][:, g * 128:(g + 1) * 128], start=True, stop=True),
                         reads=[tvn[i], tc_], writes=[bank[1]])
                S.op("act", lambda e: e.activation(out=gu[i][:], in_=gu[i][:], func=AF.Gelu), reads=[tgu[i]], writes=[tgu[i]])
                for g in range(8):
                    bank = bA if g < 4 else bB
                    c0 = (g % 4) * 128
                    S.op("dve", lambda e: e.scalar_tensor_tensor(out=oc[i][:, g * 128:(g + 1) * 128], in0=bank[0][:, c0:c0 + 128],
                                                                scalar=bs[:, g:g + 1], in1=gu[i][:, g * 128:(g + 1) * 128],
                                                                op0=ALU.add, op1=ALU.mult),
                         reads=[bank[1], tgu[i], tc_], writes=[toc[i]])
                S.op("pool", lambda e: e.tensor_tensor(out=ocb[i][:], in0=oc[i][:], in1=msrep[:], op=ALU.mult), reads=[toc[i], tc_], writes=[tob[i]])
                pb, tpb = self.bbanks[n % 2]
                for g in range(8):
                    S.op("pe", lambda e: e.transpose(pb[:, g * 128:(g + 1) * 128], ocb[i][:, g * 128:(g + 1) * 128], self.idb[:]),
                         reads=[tob[i], self.t_c], writes=[tpb])
                S.op("act", lambda e: e.activation(out=mT[i][:], in_=pb[:], func=AF.Copy), reads=[tpb], writes=[tmT[i]])
                S.dma("pool", mdst[:, :, rows], mT[i][:].rearrange("p (g t) -> p g t", t=128), reads=[tmT[i]], writes=[Tk()])
        S.barrier()

    def mix_attn(self, l):
        S = self.S
        T = self.T
        NCH = T // 128
        QT = min(512, T)
        with contextlib.ExitStack() as st:
            gain = self.sb(st, "at_gain", [128, 1280], F32)
            tcst = Tk()
            S.dma("sp", gain[:], self.rp_in[l, :, 0:1280], writes=[tcst])
            NB = 2
            qk = [self.sb(st, "at_qk%d" % i, [128, 1280], F32) for i in range(NB)]
            sq = [self.sb(st, "at_sq%d" % i, [128, 1280], F32) for i in range(NB)]
            cs = [self.sb(st, "at_cs%d" % i, [128, 640], F32) for i in range(NB)]
            sn = [self.sb(st, "at_sn%d" % i, [128, 640], F32) for i in range(NB)]
            tmp = [[self.sb(st, "at_t%d_%d" % (i, j), [128, 640], F32) for j in range(4)] for i in range(NB)]
            rb = [self.sb(st, "at_rb%d" % i, [128, 1280], BF16) for i in range(NB)]
            qkT = [self.sb(st, "at_qkT%d" % i, [128, 1280], BF16) for i in range(NB)]
            sm = [self.sb(st, "at_sm%d" % i, [128, 16], F32) for i in range(NB)]
            tqk = [Tk() for _ in range(NB)]
            tsq = [Tk() for _ in range(NB)]
            tcs = [Tk() for _ in range(NB)]
            ttmp = [[Tk() for _ in range(4)] for _ in range(NB)]
            trb = [Tk() for _ in range(NB)]
            tqT = [Tk() for _ in range(NB)]
            tsm = [Tk() for _ in range(NB)]
            dst = self.aqkT.rearrange("h d t -> d h t")
            for n in range(NCH):
                i = n % NB
                rows = slice(n * 128, (n + 1) * 128)
                S.dma("sp", qk[i][:], self.Y_a[rows, 0:1280], writes=[tqk[i]])
                S.dma("sp", cs[i][:], self.cos_in[rows, 0:640], writes=[tcs[i]])
                S.dma("sp", sn[i][:], self.sin_in[rows, 0:640], writes=[tcs[i]])
                S.op("pool", lambda e: e.tensor_tensor(out=sq[i][:], in0=qk[i][:], in1=qk[i][:], op=ALU.mult), reads=[tqk[i]], writes=[tsq[i]])
                ss = sm[i][:, 0:10]
                S.op("dve", lambda e: e.tensor_reduce(out=ss, in_=sq[i][:].rearrange("p (h d) -> p h d", d=128), axis=AX.X, op=ALU.add),
                     reads=[tsq[i]], writes=[tsm[i]])
                S.op("act", lambda e: e.activation(out=ss, in_=ss, func=AF.Sqrt, bias=self.epsln[:, 1:2], scale=1.0 / 128),
                     reads=[tsm[i], self.t_c], writes=[tsm[i]])
                S.op("dve", lambda e: e.reciprocal(out=ss, in_=ss), reads=[tsm[i]], writes=[tsm[i]])
                S.op("dve", lambda e: e.tensor_tensor(out=sq[i][:].rearrange("p (h d) -> p h d", d=128),
                                                     in0=qk[i][:].rearrange("p (h d) -> p h d", d=128),
                                                     in1=ss.unsqueeze(2).to_broadcast([128, 10, 128]), op=ALU.mult),
                     reads=[tqk[i], tsm[i]], writes=[tsq[i]])
                S.op("pool", lambda e: e.tensor_tensor(out=sq[i][:], in0=sq[i][:], in1=gain[:], op=ALU.mult), reads=[tcst], writes=[tsq[i]])
                self.rope(S, sq[i][:], rb[i][:], cs[i][:], sn[i][:], 1280, tsq[i], tcs[i], trb[i],
                          [t[:] for t in tmp[i]], ttmp[i])
                for h in range(10):
                    pb, tpb = self.bbanks[0] if h < 8 else self.bbanks[1]
                    c0 = (h % 8) * 128
                    S.op("pe", lambda e: e.transpose(pb[:, c0:c0 + 128], rb[i][:, h * 128:(h + 1) * 128], self.idb[:]),
                         reads=[trb[i], self.t_c], writes=[tpb])
                S.op("act", lambda e: e.activation(out=qkT[i][:, 0:1024], in_=self.bbanks[0][0][:], func=AF.Copy),
                     reads=[self.bbanks[0][1]], writes=[tqT[i]])
                S.op("dve", lambda e: e.tensor_copy(out=qkT[i][:, 1024:1280], in_=self.bbanks[1][0][:, 0:256]),
                     reads=[self.bbanks[1][1]], writes=[tqT[i]])
                S.dma("pool", dst[:, :, rows], qkT[i][:].rearrange("p (h t) -> p h t", t=128), reads=[tqT[i]], writes=[Tk()])
        S.barrier()
        scale = 128.0 ** -0.5
        with contextlib.ExitStack() as st:
            kT = self.sb(st, "ac_kT", [128, T], BF16)
            vf = self.sb(st, "ac_vf", [128, NCH, 128], F32)
            vb = self.sb(st, "ac_vb", [128, NCH, 128], BF16)
            tk_, tvf, tvb = Tk(), Tk(), Tk()
            qT = [self.sb(st, "ac_qT%d" % i, [128, QT], BF16) for i in range(2)]
            tq = [Tk(), Tk()]
            NP = 3
            pT = [self.sb(st, "ac_pT%d" % i, [128, QT], BF16) for i in range(NP)]
            tp = [Tk() for _ in range(NP)]
            rec = self.sb(st, "ac_rec", [128, QT], F32)
            of = self.sb(st, "ac_of", [128, QT], F32)
            ob = [self.sb(st, "ac_ob%d" % i, [128, QT], BF16) for i in range(2)]
            trec, tof = Tk(), Tk()
            tob = [Tk(), Tk()]
            sbank = [self.banks[0], self.banks[1]]
            accs = [(self.banks[2], self.banks[3]), (self.banks[4], self.banks[5])]
            it = 0
            pi = 0
            for g in range(2):
                S.dma("sp", kT[:], self.aqkT[8 + g], writes=[tk_])
                self.dma_chunks("sp", vf[:], self.Y_a[:, 1280 + g * 128:1280 + (g + 1) * 128].rearrange("(n p) d -> p n d", p=128), NCH, tvf)
                S.op("pool", lambda e: e.tensor_copy(out=vb[:], in_=vf[:]), reads=[tvf], writes=[tvb])
                for h in range(4 * g, 4 * g + 4):
                    for qt in range(T // QT):
                        qi = it % 2
                        oacc, dacc = accs[it % 2]
                        it += 1
                        S.dma("sp", qT[qi][:], self.aqkT[h, :, qt * QT:(qt + 1) * QT], writes=[tq[qi]])
                        def emit_s(kc_):
                            sbx, tsbx = sbank[kc_ % 2]
                            S.op("pe", lambda e: e.matmul(sbx[:, :QT], kT[:, kc_ * 128:(kc_ + 1) * 128], qT[qi][:], start=True, stop=True),
                                 reads=[tk_, tq[qi]], writes=[tsbx])
                        emit_s(0)
                        for kc in range(NCH):
                            sb_, tsb = sbank[kc % 2]
                            if kc + 1 < NCH:
                                emit_s(kc + 1)
                            p = pi % NP
                            pi += 1
                            S.op("act", lambda e: e.activation(out=pT[p][:], in_=sb_[:, :QT], func=AF.Exp, scale=scale),
                                 reads=[tsb], writes=[tp[p]])
                            S.op("pe", lambda e: e.matmul(oacc[0][:, :QT], vb[:, kc, :], pT[p][:], start=(kc == 0), stop=(kc == NCH - 1)),
                                 reads=[tvb, tp[p]], writes=[oacc[1]])
                            S.op("pe", lambda e: e.matmul(dacc[0][:, :QT], self.onesb[:], pT[p][:], start=(kc == 0), stop=(kc == NCH - 1)),
                                 reads=[tp[p], self.t_c], writes=[dacc[1]])
                        S.op("dve", lambda e: e.reciprocal(out=rec[:], in_=dacc[0][:, :QT]), reads=[dacc[1]], writes=[trec])
                        S.op("dve", lambda e: e.tensor_tensor(out=of[:], in0=oacc[0][:, :QT], in1=rec[:], op=ALU.mult),
                             reads=[oacc[1], trec], writes=[tof])
                        mc = l * 200 + 128 + h
                        S.op("pool", lambda e: e.tensor_scalar(out=ob[qi][:], in0=of[:], scalar1=self.pp[:, mc:mc + 1], scalar2=1.0,
                                                               op0=ALU.mult, op1=ALU.mult), reads=[tof, self.t_pp], writes=[tob[qi]])
                        S.dma("pool", self.mergedT[h * 128:(h + 1) * 128, qt * QT:(qt + 1) * QT], ob[qi][:], reads=[tob[qi]], writes=[Tk()])
        S.barrier()

    def mix_ret(self, l):
        S = self.S
        T = self.T
        NCH = T // 128
        gam = [1.0 - 2.0 ** (-5.0 - h) for h in range(8)]
        gC = [float(np.float64(g) ** 128) for g in gam]
        with contextlib.ExitStack() as st:
            rdec = self.sb(st, "rt_rdec", [128, 32], F32)
            tcst = Tk()
            S.dma("sp", rdec[:], self.rdec_in, writes=[tcst])
            NB = 2
            qk = [self.sb(st, "rp_qk%d" % i, [128, 2048], F32) for i in range(NB)]
            rv = [self.sb(st, "rp_rv%d" % i, [128, 1024], F32) for i in range(NB)]
            cs = [self.sb(st, "rp_cs%d" % i, [128, 1024], F32) for i in range(NB)]
            sn = [self.sb(st, "rp_sn%d" % i, [128, 1024], F32) for i in range(NB)]
            tmp = [[self.sb(st, "rp_t%d_%d" % (i, j), [128, 1024], F32) for j in range(4)] for i in range(NB)]
            rb = [self.sb(st, "rp_rb%d" % i, [128, 2048], BF16) for i in range(NB)]
            kfb = [self.sb(st, "rp_kf%d" % i, [128, 3, 1024], BF16) for i in range(NB)]
            qkT = [self.sb(st, "rp_qkT%d" % i, [128, 2048], BF16) for i in range(NB)]
            tqk = [Tk() for _ in range(NB)]
            trv = [Tk() for _ in range(NB)]
            tcs = [Tk() for _ in range(NB)]
            ttmp = [[Tk() for _ in range(4)] for _ in range(NB)]
            trb = [Tk() for _ in range(NB)]
            tkf = [Tk() for _ in range(NB)]
            tqT = [Tk() for _ in range(NB)]
            dst = self.rqkT.rearrange("h d t -> d h t")
            rkd = self.RK.rearrange("i t c -> t i c")
            for n in range(NCH):
                i = n % NB
                rows = slice(n * 128, (n + 1) * 128)
                S.dma("sp", qk[i][:], self.Y_r[rows, 0:2048], writes=[tqk[i]])
                S.dma("sp", rv[i][:], self.Y_r[rows, 2048:3072], writes=[trv[i]])
                S.dma("sp", cs[i][:], self.cos_in[rows, :], writes=[tcs[i]])
                S.dma("sp", sn[i][:], self.sin_in[rows, :], writes=[tcs[i]])
                self.rope(S, qk[i][:], rb[i][:], cs[i][:], sn[i][:], 2048, tqk[i], tcs[i], trb[i], [t[:] for t in tmp[i]], ttmp[i])
                for h in range(8):
                    kh = rb[i][:, 1024 + h * 128:1024 + (h + 1) * 128]
                    S.op("dve", lambda e: e.tensor_scalar(out=kfb[i][:, 0, h * 128:(h + 1) * 128], in0=kh, scalar1=rdec[:, h:h + 1], scalar2=1.0, op0=ALU.mult, op1=ALU.mult),
                         reads=[trb[i], tcst], writes=[tkf[i]])
                    S.op("pool", lambda e: e.tensor_scalar(out=kfb[i][:, 1, h * 128:(h + 1) * 128], in0=kh, scalar1=rdec[:, 8 + h:9 + h], scalar2=1.0,
                                                           op0=ALU.mult, op1=ALU.mult), reads=[trb[i], tcst], writes=[tkf[i]])
                S.op("act", lambda e: e.activation(out=kfb[i][:, 2, :], in_=rv[i][:], func=AF.Copy), reads=[trv[i]], writes=[tkf[i]])
                S.dma("pool", rkd[rows, :, :], kfb[i][:], reads=[tkf[i]], writes=[Tk()])
                for h in range(16):
                    pb, tpb = self.bbanks[h // 8]
                    c0 = (h % 8) * 128
                    S.op("pe", lambda e: e.transpose(pb[:, c0:c0 + 128], rb[i][:, h * 128:(h + 1) * 128], self.idb[:]),
                         reads=[trb[i], self.t_c], writes=[tpb])
                S.op("act", lambda e: e.activation(out=qkT[i][:, 0:1024], in_=self.bbanks[0][0][:], func=AF.Copy),
                     reads=[self.bbanks[0][1]], writes=[tqT[i]])
                S.op("dve", lambda e: e.tensor_copy(out=qkT[i][:, 1024:2048], in_=self.bbanks[1][0][:]),
                     reads=[self.bbanks[1][1]], writes=[tqT[i]])
                S.dma("pool", dst[:, :, rows], qkT[i][:].rearrange("p (h t) -> p h t", t=128), reads=[tqT[i]], writes=[Tk()])
        S.barrier()
        with contextlib.ExitStack() as st:
            rdec = self.sb(st, "rs_rdec", [128, 32], F32)
            dsym = self.sb(st, "rs_dsym", [128, 8, 128], F32)
            gcol = self.sb(st, "rs_gcol", [128, 8], F32)
            tcst = Tk()
            S.dma("sp", rdec[:], self.rdec_in, writes=[tcst])
            S.dma("sp", dsym[:], self.dsym_in, writes=[tcst])
            b0 = l * 200
            S.op("dve", lambda e: e.tensor_tensor(out=gcol[:], in0=self.pp[:, b0 + 168:b0 + 176], in1=self.pp[:, b0 + 128 + 24:b0 + 128 + 32], op=ALU.mult),
                 reads=[self.t_pp], writes=[tcst])
            qT = self.sb(st, "rs_qT", [128, T], BF16)
            kT = self.sb(st, "rs_kT", [128, T], BF16)
            kv = self.sb(st, "rs_kv", [128, 3, NCH, 128], BF16)
            rg = self.sb(st, "rs_rg", [128, NCH, 128], F32)
            oacc = self.sb(st, "rs_oacc", [128, NCH, 128], F32)
            NG = min(8, NCH)
            sqb = [self.sb(st, "rs_sq%d" % i, [128, NG, 128], F32) for i in range(2)]
            ob = [self.sb(st, "rs_ob%d" % i, [128, NG, 128], BF16) for i in range(2)]
            ss = [self.sb(st, "rs_ss%d" % i, [128, NG], F32) for i in range(2)]
            tld, trg, toa = Tk(), Tk(), Tk()
            tsq, tob, tss = [Tk(), Tk()], [Tk(), Tk()], [Tk(), Tk()]
            R = [self.sb(st, "rs_R%d" % i, [128, 128], F32) for i in range(2)]
            NSR = 8
            Rbr = self.sb(st, "rs_Rbr", [128, NSR, 128], BF16)
            tR = [Tk(), Tk()]
            tRbr = [Tk() for _ in range(NSR)]
            pT = [self.sb(st, "rs_pT%d" % i, [128, 128], BF16) for i in range(2)]
            tpT = [Tk(), Tk()]
            mT = [self.sb(st, "rs_mT%d" % i, [128, 1024], BF16) for i in range(2)]
            tmT = [Tk(), Tk()]
            bS = [self.banks[0], self.banks[1]]
            bO = [self.banks[2], self.banks[3]]
            bR = [self.banks[4], self.banks[5]]
            for h in range(8):
                S.dma("sp", qT[:], self.rqkT[h], writes=[tld])
                S.dma("sp", kT[:], self.rqkT[8 + h], writes=[tld])
                for i3 in range(3):
                    self.dma_chunks("sp", kv[:, i3, :, :], self.RK[i3, :, h * 128:(h + 1) * 128].rearrange("(n p) d -> p n d", p=128), NCH, tld)
                self.dma_chunks("sp", rg[:], self.Y_r[:, 3072 + h * 128:3072 + (h + 1) * 128].rearrange("(n p) d -> p n d", p=128), NCH, trg)
                S.op("pool", lambda e: e.memset(R[0][:], 0.0), writes=[tR[0]])
                S.op("pool", lambda e: e.memset(Rbr[:, 0, :], 0.0), writes=[tRbr[0]])

                def a1(j):
                    cols = slice(j * 128, (j + 1) * 128)
                    bs_, tbs = bS[j % 2]
                    br_, tbr = bR[j % 2]
                    S.op("pe", lambda e: e.matmul(bs_[:, 0:128], kT[:, cols], qT[:, cols], start=True, stop=True), reads=[tld], writes=[tbs])
                    S.op("pe", lambda e: e.matmul(br_[:, 0:128], kv[:, 0, j, :], kv[:, 2, j, :], start=True, stop=True), reads=[tld], writes=[tbr])
                    p = j % 2
                    S.op("dve", lambda e: e.tensor_tensor(out=pT[p][:], in0=bs_[:, 0:128], in1=dsym[:, h, :], op=ALU.mult),
                         reads=[tbs, tcst], writes=[tpT[p]])
                    S.op("dve", lambda e: e.scalar_tensor_tensor(out=R[0][:], in0=R[0][:], scalar=gC[h], in1=br_[:, 0:128],
                                                                op0=ALU.mult, op1=ALU.add), reads=[tbr], writes=[tR[0]])
                    s1 = (j + 1) % NSR
                    S.op("dve", lambda e: e.tensor_copy(out=Rbr[:, s1, :], in_=R[0][:]), reads=[tR[0]], writes=[tRbr[s1]])

                def a2(j):
                    cols = slice(j * 128, (j + 1) * 128)
                    bo_, tbo = bO[j % 2]
                    p = j % 2
                    s0 = j % NSR
                    S.op("pe", lambda e: e.matmul(bo_[:, 0:128], pT[p][:], kv[:, 2, j, :], start=True, stop=True), reads=[tpT[p], tld], writes=[tbo])
                    S.op("pe", lambda e: e.matmul(bo_[:, 128:256], qT[:, cols], Rbr[:, s0, :], start=True, stop=True), reads=[tld, tRbr[s0]], writes=[tbo])
                    S.op("act", lambda e: e.activation(out=oacc[:, j, :], in_=bo_[:, 0:128], func=AF.Copy), reads=[tbo], writes=[toa])
                    S.op("dve", lambda e: e.scalar_tensor_tensor(out=oacc[:, j, :], in0=bo_[:, 128:256], scalar=rdec[:, 16 + h:17 + h],
                                                                in1=oacc[:, j, :], op0=ALU.mult, op1=ALU.add), reads=[tbo, tcst, toa], writes=[toa])
                for j in range(NCH):
                    a1(j)
                    a2(j)
                S.op("pool", lambda e: e.memset(R[1][:], 0.0), writes=[tR[1]])
                S.op("pool", lambda e: e.memset(Rbr[:, 0, :], 0.0), writes=[tRbr[0]])

                def d1(n):
                    j = NCH - 1 - n
                    br_, tbr = bR[n % 2]
                    S.op("pe", lambda e: e.matmul(br_[:, 0:128], kv[:, 1, j, :], kv[:, 2, j, :], start=True, stop=True), reads=[tld], writes=[tbr])
                    S.op("dve", lambda e: e.scalar_tensor_tensor(out=R[1][:], in0=R[1][:], scalar=gC[h], in1=br_[:, 0:128],
                                                                op0=ALU.mult, op1=ALU.add), reads=[tbr], writes=[tR[1]])
                    s1 = (n + 1) % NSR
                    S.op("dve", lambda e: e.tensor_copy(out=Rbr[:, s1, :], in_=R[1][:]), reads=[tR[1]], writes=[tRbr[s1]])

                def d2(n):
                    j = NCH - 1 - n
                    cols = slice(j * 128, (j + 1) * 128)
                    bo_, tbo = bO[n % 2]
                    s0 = n % NSR
                    S.op("pe", lambda e: e.matmul(bo_[:, 128:256], qT[:, cols], Rbr[:, s0, :], start=True, stop=True), reads=[tld, tRbr[s0]], writes=[tbo])
                    S.op("dve", lambda e: e.scalar_tensor_tensor(out=oacc[:, j, :], in0=bo_[:, 128:256], scalar=rdec[:, 24 + h:25 + h],
                                                                in1=oacc[:, j, :], op0=ALU.mult, op1=ALU.add), reads=[tbo, tcst, toa], writes=[toa])
                for n in range(NCH):
                    d1(n)
                    d2(n)
                S.op("act", lambda e: e.activation(out=rg[:], in_=rg[:], func=AF.Silu), reads=[trg], writes=[trg])
                for j0 in range(0, NCH, NG):
                    gi = (j0 // NG) % 2
                    js = slice(j0, j0 + NG)
                    S.op("pool", lambda e: e.tensor_tensor(out=sqb[gi][:], in0=oacc[:, js, :], in1=oacc[:, js, :], op=ALU.mult), reads=[toa], writes=[tsq[gi]])
                    S.op("dve", lambda e: e.tensor_reduce(out=ss[gi][:], in_=sqb[gi][:], axis=AX.X, op=ALU.add), reads=[tsq[gi]], writes=[tss[gi]])
                    S.op("act", lambda e: e.activation(out=ss[gi][:], in_=ss[gi][:], func=AF.Sqrt, bias=self.epsln[:, 1:2], scale=1.0 / 128),
                         reads=[tss[gi], self.t_c], writes=[tss[gi]])
                    S.op("dve", lambda e: e.reciprocal(out=ss[gi][:], in_=ss[gi][:]), reads=[tss[gi]], writes=[tss[gi]])
                    S.op("dve", lambda e: e.tensor_tensor(out=sqb[gi][:], in0=oacc[:, js, :], in1=ss[gi][:].unsqueeze(2).to_broadcast([128, NG, 128]), op=ALU.mult),
                         reads=[toa, tss[gi]], writes=[tsq[gi]])
                    S.op("pool", lambda e: e.tensor_tensor(out=ob[gi][:], in0=sqb[gi][:], in1=rg[:, js, :], op=ALU.mult), reads=[tsq[gi], trg], writes=[tob[gi]])
                    pb, tpb = self.bbanks[gi]
                    for jj in range(NG):
                        S.op("pe", lambda e: e.transpose(pb[:, jj * 128:(jj + 1) * 128], ob[gi][:, jj, :], self.idb[:]),
                             reads=[tob[gi], self.t_c], writes=[tpb])
                    S.op("act", lambda e: e.activation(out=mT[gi][:, :NG * 128], in_=pb[:, :NG * 128], func=AF.Copy, scale=gcol[:, h:h + 1]),
                         reads=[tpb, tcst], writes=[tmT[gi]])
                    S.dma("pool", self.mergedT[3072 + h * 128:3072 + (h + 1) * 128, j0 * 128:(j0 + NG) * 128], mT[gi][:, :NG * 128],
                          reads=[tmT[gi]], writes=[Tk()])
        S.barrier()

    def dma_chunks(self, q, out3, in3, n, tk, step=8, reads=()):
        for a in range(0, n, step):
            b = min(n, a + step)
            self.S.dma(q, out3[:, a:b, :], in3[:, a:b, :], reads=list(reads), writes=[tk])

    def mix_hgrn(self, l):
        S = self.S
        T = self.T
        NCH = T // 128
        SEG = min(2048, T)
        NSEG = T // SEG
        NT = SEG // 128
        BK = 64
        NBPT = 128 // BK
        NBLK = SEG // BK
        PW = min(512, T)
        b0 = l * 200
        with contextlib.ExitStack() as st:
            mbd = self.sb(st, "hg_mbd", [128, 2, 128], F32)
            oml = self.sb(st, "hg_oml", [128, 8], F32)
            gcol = self.sb(st, "hg_gcol", [128, 8], F32)
            one = self.sb(st, "hg_one", [128, 1], F32)
            tcst = Tk()
            S.dma("sp", mbd[:], self.mbd_in, writes=[tcst])
            S.op("dve", lambda e: e.memset(one[:], 1.0), writes=[tcst])
            if l == 0:
                S.op("dve", lambda e: e.memset(oml[:], 1.0), writes=[tcst])
            else:
                S.op("dve", lambda e: e.tensor_tensor(out=oml[:], in0=self.pp[:, b0 + 176:b0 + 184], in1=self.pp[:, b0 + 184:b0 + 192], op=ALU.subtract),
                     reads=[self.t_pp], writes=[tcst])
                S.op("act", lambda e: e.activation(out=oml[:], in_=oml[:], func=AF.Sigmoid), reads=[tcst], writes=[tcst])
            S.op("dve", lambda e: e.tensor_tensor(out=gcol[:], in0=self.pp[:, b0 + 160:b0 + 168], in1=self.pp[:, b0 + 128 + 8:b0 + 128 + 16], op=ALU.mult),
                 reads=[self.t_pp], writes=[tcst])
            oT = self.sb(st, "hg_oT", [128, T], F32)
            vf = self.sb(st, "hg_vf", [128, T], F32)
            vb = self.sb(st, "hg_vb", [128, NCH, 128], BF16)
            toT, tvb, thg = Tk(), Tk(), Tk()
            z = self.sb(st, "hg_z", [128, SEG], F32)
            q = self.sb(st, "hg_q", [128, SEG], F32)
            kk = self.sb(st, "hg_kk", [128, SEG], F32)
            ba = self.sb(st, "hg_ba", [128, SEG], F32)
            bb = self.sb(st, "hg_bb", [128, SEG], F32)
            eb = self.sb(st, "hg_eb", [128, SEG], F32)
            enb = self.sb(st, "hg_enb", [128, SEG], F32)
            Qt = self.sb(st, "hg_Qt", [128, SEG], F32)
            Kt = self.sb(st, "hg_Kt", [128, SEG], F32)
            KpT = self.sb(st, "hg_KpT", [128, SEG], BF16)
            Kp = self.sb(st, "hg_Kp", [128, NT, 128], BF16)
            Kpz = self.sb(st, "hg_Kpz", [128, NT, 128], BF16)
            dec = self.sb(st, "hg_dec", [128, NBLK], F32)
            tz, tq, tkk, tba, tbb, teb, tenb, tQt, tKt, tKpT, tKp, tdec = (Tk() for _ in range(12))
            Sst = self.sb(st, "hg_S", [128, 128], F32)
            tS = Tk()
            PT = [self.sb(st, "hg_PT%d" % i, [128, 128], BF16) for i in range(2)]
            tPT = [Tk(), Tk()]
            sqp = [self.sb(st, "hg_sq%d" % i, [128, PW], BF16) for i in range(2)]
            rsp = [self.sb(st, "hg_rs%d" % i, [128, PW], F32) for i in range(2)]
            t1p = [self.sb(st, "hg_t1%d" % i, [128, PW], F32) for i in range(2)]
            mbp = [self.sb(st, "hg_mb%d" % i, [128, PW], BF16) for i in range(2)]
            tsqp, trsp, tt1p, tmbp = ([Tk(), Tk()] for _ in range(4))
            bA = [self.banks[0], self.banks[1]]
            bO = [self.banks[2], self.banks[3]]
            bR = [self.banks[4], self.banks[5]]
            nR = 0
            v3 = lambda t_: t_[:].rearrange("p (b c) -> p b c", c=BK)
            for h in range(8):
                self.dma_chunks("sp", vf[:].rearrange("p (n d) -> p n d", d=128),
                                self.Y_hi[:, h * 128:(h + 1) * 128].rearrange("(n p) d -> p n d", p=128), NCH, thg)
                S.op("pool", lambda e: e.tensor_copy(out=vb[:], in_=vf[:].rearrange("p (n d) -> p n d", d=128)), reads=[thg], writes=[tvb])
                for d in range(2):
                    S.op("pool", lambda e: e.memset(Sst[:], 0.0), writes=[tS])
                    segs = range(NSEG) if d == 0 else range(NSEG - 1, -1, -1)
                    for sg_ in segs:
                        scol = slice(sg_ * SEG, (sg_ + 1) * SEG)
                        zr = 1024 * (1 + d) + h * 128
                        S.dma("sp", z[:], self.YT[zr:zr + 128, scol], writes=[tz])
                        S.dma("sp", q[:], self.YT[h * 128:(h + 1) * 128, scol], writes=[tq])
                        S.op("act", lambda e: e.activation(out=z[:], in_=z[:], func=AF.Sigmoid, scale=-1.0), reads=[tz], writes=[tz])
                        S.op("dve", lambda e: e.tensor_scalar(out=kk[:], in0=z[:], scalar1=oml[:, h:h + 1], scalar2=1.0, op0=ALU.mult, op1=ALU.mult),
                             reads=[tz, tcst], writes=[tkk])
                        S.op("act", lambda e: e.activation(out=ba[:], in_=kk[:], func=AF.Ln, scale=-1.0, bias=one[:, 0:1]),
                             reads=[tkk, tcst], writes=[tba])
                        cur, nxt, tcur, tnxt = ba, bb, tba, tbb
                        for sh in [1 << k_ for k_ in range(6) if (1 << k_) < BK]:
                            c3, n3 = v3(cur), v3(nxt)
                            if d == 0:
                                S.op("dve", lambda e: e.tensor_tensor(out=n3[:, :, sh:], in0=c3[:, :, sh:], in1=c3[:, :, :BK - sh], op=ALU.add),
                                     reads=[tcur], writes=[tnxt])
                                S.op("dve", lambda e: e.tensor_copy(out=n3[:, :, :sh], in_=c3[:, :, :sh]), reads=[tcur], writes=[tnxt])
                            else:
                                S.op("dve", lambda e: e.tensor_tensor(out=n3[:, :, :BK - sh], in0=c3[:, :, :BK - sh], in1=c3[:, :, sh:], op=ALU.add),
                                     reads=[tcur], writes=[tnxt])
                                S.op("dve", lambda e: e.tensor_copy(out=n3[:, :, BK - sh:], in_=c3[:, :, BK - sh:]), reads=[tcur], writes=[tnxt])
                            cur, nxt, tcur, tnxt = nxt, cur, tnxt, tcur
                        S.op("dve", lambda e: e.tensor_scalar(out=cur[:], in0=cur[:], scalar1=-80.0, scalar2=0.0, op0=ALU.max, op1=ALU.add),
                             reads=[tcur], writes=[tcur])
                        S.op("act", lambda e: e.activation(out=eb[:], in_=cur[:], func=AF.Exp), reads=[tcur], writes=[teb])
                        S.op("act", lambda e: e.activation(out=enb[:], in_=cur[:], func=AF.Exp, scale=-1.0), reads=[tcur], writes=[tenb])
                        S.op("dve", lambda e: e.tensor_copy(out=dec[:], in_=v3(eb)[:, :, (BK - 1 if d == 0 else 0)]), reads=[teb], writes=[tdec])
                        S.op("act", lambda e: e.activation(out=q[:], in_=q[:], func=AF.Silu), reads=[tq], writes=[tq])
                        S.op("dve", lambda e: e.scalar_tensor_tensor(out=Qt[:], in0=q[:], scalar=128.0 ** -0.5, in1=eb[:], op0=ALU.mult, op1=ALU.mult),
                             reads=[tq, teb], writes=[tQt])
                        S.op("pool", lambda e: e.tensor_tensor(out=Kt[:], in0=kk[:], in1=enb[:], op=ALU.mult), reads=[tkk, tenb], writes=[tKt])
                        S.op("dve", lambda e: e.tensor_tensor(out=v3(KpT), in0=v3(Kt), in1=dec[:].unsqueeze(2).to_broadcast([128, NBLK, BK]), op=ALU.mult),
                             reads=[tKt, tdec], writes=[tKpT])
                        for j0 in range(0, NT, 8):
                            pb, tpb = self.bbanks[(j0 // 8) % 2]
                            n8 = min(8, NT - j0)
                            for jj in range(n8):
                                jt = j0 + jj
                                S.op("pe", lambda e: e.transpose(pb[:, jj * 128:(jj + 1) * 128], KpT[:, jt * 128:(jt + 1) * 128], self.idb[:]),
                                     reads=[tKpT, self.t_c], writes=[tpb])
                            S.op("act", lambda e: e.activation(out=Kp[:, j0:j0 + n8, :], in_=pb[:, :n8 * 128].rearrange("p (n d) -> p n d", d=128), func=AF.Copy),
                                 reads=[tpb], writes=[tKp])
                        tiles = range(NT) if d == 0 else range(NT - 1, -1, -1)
                        for jt in tiles:
                            jg = sg_ * NT + jt
                            cols = slice(jt * 128, (jt + 1) * 128)
                            gcols = slice(jg * 128, (jg + 1) * 128)
                            ba_, tba_ = bA[jt % 2]
                            bo_, tbo_ = bO[jt % 2]
                            S.op("pe", lambda e: e.matmul(ba_[:, 0:128], Kt[:, cols], Qt[:, cols], start=True, stop=True), reads=[tKt, tQt], writes=[tba_])
                            p = jt % 2
                            S.op("dve", lambda e: e.tensor_tensor(out=PT[p][:], in0=ba_[:, 0:128], in1=mbd[:, d, :], op=ALU.mult),
                                 reads=[tba_, tcst], writes=[tPT[p]])
                            S.op("pe", lambda e: e.matmul(bo_[:, 0:128], vb[:, jg, :], PT[p][:], start=True, stop=True), reads=[tvb, tPT[p]], writes=[tbo_])
                            blks = range(NBPT) if d == 0 else range(NBPT - 1, -1, -1)
                            for ib in blks:
                                c32 = slice(jt * 128 + ib * BK, jt * 128 + (ib + 1) * BK)
                                S.op("pe", lambda e: e.matmul(bo_[:, 128 + ib * BK:128 + (ib + 1) * BK], Sst[:], Qt[:, c32], start=True, stop=True),
                                     reads=[tS, tQt], writes=[tbo_])
                                br_, tbr_ = bR[nR % 2]
                                nR += 1
                                S.op("pe", lambda e: e.matmul(br_[:, 0:128], Kp[ib * BK:(ib + 1) * BK, jt, :], vb[ib * BK:(ib + 1) * BK, jg, :], start=True, stop=True),
                                     reads=[tKp, tvb], writes=[tbr_])
                                blk = jt * NBPT + ib
                                S.op("dve", lambda e: e.scalar_tensor_tensor(out=Sst[:], in0=Sst[:], scalar=dec[:, blk:blk + 1], in1=br_[:, 0:128],
                                                                            op0=ALU.mult, op1=ALU.add), reads=[tbr_, tdec], writes=[tS])
                            if d == 0:
                                S.op("act", lambda e: e.activation(out=oT[:, gcols], in_=bo_[:, 0:128], func=AF.Copy), reads=[tbo_], writes=[toT])
                            else:
                                S.op("dve", lambda e: e.tensor_tensor(out=oT[:, gcols], in0=bo_[:, 0:128], in1=oT[:, gcols], op=ALU.add), reads=[tbo_], writes=[toT])
                            S.op("dve", lambda e: e.tensor_tensor(out=oT[:, gcols], in0=bo_[:, 128:256], in1=oT[:, gcols], op=ALU.add), reads=[tbo_], writes=[toT])
                S.dma("sp", vf[:], self.YT[3072 + h * 128:3072 + (h + 1) * 128, :], reads=[tvb], writes=[thg])
                S.op("act", lambda e: e.activation(out=vf[:], in_=vf[:], func=AF.Silu), reads=[thg], writes=[thg])
                for pc in range(T // PW):
                    i = pc % 2
                    pcs = slice(pc * PW, (pc + 1) * PW)
                    bk, tbk = bA[pc % 2]
                    S.op("pool", lambda e: e.tensor_tensor(out=sqp[i][:], in0=oT[:, pcs], in1=oT[:, pcs], op=ALU.mult), reads=[toT], writes=[tsqp[i]])
                    S.op("pe", lambda e: e.matmul(bk[:, :PW], self.onesb[:], sqp[i][:], start=True, stop=True), reads=[tsqp[i], self.t_c], writes=[tbk])
                    S.op("act", lambda e: e.activation(out=rsp[i][:], in_=bk[:, :PW], func=AF.Sqrt, bias=self.epsln[:, 1:2], scale=1.0 / 128),
                         reads=[tbk, self.t_c], writes=[trsp[i]])
                    S.op("dve", lambda e: e.reciprocal(out=rsp[i][:], in_=rsp[i][:]), reads=[trsp[i]], writes=[trsp[i]])
                    S.op("dve", lambda e: e.tensor_tensor(out=t1p[i][:], in0=oT[:, pcs], in1=rsp[i][:], op=ALU.mult), reads=[toT, trsp[i]], writes=[tt1p[i]])
                    S.op("pool", lambda e: e.tensor_tensor(out=t1p[i][:], in0=t1p[i][:], in1=vf[:, pcs], op=ALU.mult), reads=[thg], writes=[tt1p[i]])
                    S.op("act", lambda e: e.activation(out=mbp[i][:], in_=t1p[i][:], func=AF.Copy, scale=gcol[:, h:h + 1]), reads=[tt1p[i], tcst], writes=[tmbp[i]])
                    S.dma("pool", self.mergedT[1024 + h * 128:1024 + (h + 1) * 128, pcs], mbp[i][:], reads=[tmbp[i]], writes=[Tk()])
        S.barrier()


_CACHE = {}


def _consts(T):
    f64 = np.float64
    n_rows = T // 64
    rows = np.repeat(np.arange(n_rows, dtype=np.float32), 64)
    cols = np.tile(np.arange(64, dtype=np.float32), n_rows)
    inv_freq = (np.float32(10000.0) ** (-np.arange(32, dtype=np.float32) / np.float32(32))).astype(np.float32)
    ang = np.concatenate([rows[:, None] * inv_freq, cols[:, None] * inv_freq], axis=-1).astype(np.float32)
    cos16 = np.ascontiguousarray(np.tile(np.cos(ang).astype(np.float32), (1, 16)))
    sin16 = np.ascontiguousarray(np.tile(np.sin(ang).astype(np.float32), (1, 16)))
    lg = np.log1p(-np.exp2(-5.0 - np.arange(8, dtype=f64)))
    pos = np.arange(128, dtype=f64)
    sc = 128.0 ** -0.5
    rdec = np.zeros((128, 32), f64)
    for h in range(8):
        rdec[:, h] = np.exp((127.0 - pos) * lg[h]) * sc
        rdec[:, 8 + h] = np.exp(pos * lg[h]) * sc
        rdec[:, 16 + h] = np.exp((pos + 1.0) * lg[h])
        rdec[:, 24 + h] = np.exp((128.0 - pos) * lg[h])
    dsym = np.zeros((128, 8, 128), f64)
    ad = np.abs(pos[:, None] - pos[None, :])
    for h in range(8):
        dsym[:, h, :] = np.exp(ad * lg[h]) * sc
    blk = np.arange(128) // 64
    same = blk[:, None] == blk[None, :]
    s_i = np.arange(128)[:, None]
    t_i = np.arange(128)[None, :]
    mbd = np.zeros((128, 2, 128), np.float32)
    mbd[:, 0, :] = (same & (s_i <= t_i))
    mbd[:, 1, :] = (same & (s_i >= t_i))
    sel = np.zeros((8, 8, 128), np.float32)
    for e in range(8):
        sel[e, e, :] = 1.0
    return dict(cos16=cos16, sin16=sin16, rdec=rdec.astype(np.float32), dsymT=dsym.astype(np.float32), maskBD=mbd, sel=sel)


def _params(inp):
    f = lambda a: np.asarray(a, dtype=np.float32)
    pp = np.zeros((128, 400), np.float32)
    rp = np.zeros((2, 128, 4352), np.float32)
    for l in range(2):
        b = l * 200
        pp[:, b + 0:b + 32] = f(inp["ln1_g"])[l].reshape(32, 128).T
        pp[:, b + 32:b + 64] = f(inp["ln1_b"])[l].reshape(32, 128).T
        pp[:, b + 64:b + 96] = f(inp["ln2_g"])[l].reshape(32, 128).T
        pp[:, b + 96:b + 128] = f(inp["ln2_b"])[l].reshape(32, 128).T
        pp[:, b + 128:b + 160] = f(inp["merge_scale"])[l].reshape(32, 128).T
        pp[:, b + 160:b + 168] = f(inp["hgrn_out_norm"])[l].reshape(8, 128).T
        pp[:, b + 168:b + 176] = f(inp["ret_out_norm"])[l].reshape(8, 128).T
        pp[:, b + 176:b + 184] = f(inp["hgrn_lower_bound"])[0].reshape(8, 128).T
        pp[:, b + 184:b + 192] = f(inp["hgrn_lower_bound"])[1].reshape(8, 128).T
        row = np.concatenate([np.tile(f(inp["attn_q_norm"])[l], 8), np.tile(f(inp["attn_k_norm"])[l], 2),
                              f(inp["gmlp_v_norm_g"])[l], f(inp["gmlp_v_norm_b"])[l], f(inp["merge_scale"])[l][2048:3072]])
        rp[l] = np.broadcast_to(row[None, :], (128, 4352))
    wsT = np.ascontiguousarray(f(inp["gmlp_w_s"]).transpose(0, 3, 1, 2))
    bsT = np.ascontiguousarray(f(inp["gmlp_b_s"]).transpose(0, 2, 1))
    router = np.ascontiguousarray(f(inp["moe_router"])[0].reshape(32, 128, 8).transpose(1, 0, 2))
    return dict(
        pp=pp, rp=rp, wsT=wsT, bsT=bsT, router=router,
        w_in=f(inp["w_in"]), w_o=f(inp["w_o"]),
        ffn_g=f(inp["ffn_w_gate"])[0], ffn_u=f(inp["ffn_w_up"])[0], ffn_d=f(inp["ffn_w_down"])[0],
        moe_g=f(inp["moe_w_gate"])[0], moe_u=f(inp["moe_w_up"])[0],
        moe_d=f(inp["moe_w_down"])[0].reshape(NE * DFE, D),
    )


def kernel(**inputs):
    T = 8192
    if T not in _CACHE:
        _CACHE[T] = Prog(T)
    prog = _CACHE[T]
    shared = _params(inputs)
    shared.update(_consts(T))
    xp = np.asarray(inputs["x_prompt"], dtype=np.float32)
    xs = np.asarray(inputs["x_sample"], dtype=np.float32)
    seqs = [xp[0], xp[1], xs[0]]
    in_maps = []
    for c in range(NCORES):
        m = dict(shared)
        m["xT"] = np.ascontiguousarray(seqs[c].T)
        in_maps.append(m)
    res = run_bass_kernel_spmd(prog.nc, in_maps, core_ids=list(range(NCORES)))
    outs = [np.ascontiguousarray(np.asarray(res.results[c]["yT"], dtype=np.float32).T) for c in range(NCORES)]
    y_prompt = np.stack([outs[0], outs[1]], axis=0)
    y_sample = outs[2][None]
    return (y_prompt, y_sample)
```

```python
import contextlib
import numpy as np
import concourse.bass as bass
import concourse.mybir as mybir
from concourse.bass_utils import run_bass_kernel_spmd

F32 = mybir.dt.float32
BF16 = mybir.dt.bfloat16
AF = mybir.ActivationFunctionType
ALU = mybir.AluOpType
AX = mybir.AxisListType

D = 4096
DIN = 12800
DFF = 5632
NE = 8
DFE = 1024
HD = 128
OFF = dict(aq=0, ak=1024, av=1280, hq=1536, hff=2560, hfb=3584, hi=4608, hg=5632,
           gu=6656, gv=7680, rq=8704, rk=9728, rv=10752, rg=11776)
ALPHA = 4.0 ** 0.25
LN_EPS = 1e-5
NORM_EPS = 1e-6
NCORES = 3


class Tk:
    __slots__ = ("w", "r")

    def __init__(self):
        self.w = None
        self.r = {}


class Sched:
    NDS = 16

    def __init__(self, nc, es):
        self.nc = nc
        self.eng = {"pe": nc.tensor, "act": nc.scalar, "dve": nc.vector, "pool": nc.gpsimd, "sp": nc.sync}
        self.sem = {k: es.enter_context(nc.semaphore("s_" + k)) for k in self.eng}
        self.cnt = {k: 0 for k in self.eng}
        self.seen = {k: {} for k in self.eng}
        self.dsem = {}
        self.dval = {}
        self.dnext = {}
        for q in ("sp", "pool", "act"):
            self.dsem[q] = [es.enter_context(nc.semaphore("d_%s%d" % (q, i))) for i in range(self.NDS)]
            self.dval[q] = [0] * self.NDS
            self.dnext[q] = 0
        self.ninst = 0

    def _semobj(self, k):
        if isinstance(k, tuple):
            return self.dsem[k[0]][k[1]]
        return self.sem[k]

    def _wait(self, e, deps):
        eng = self.eng[e]
        seen = self.seen[e]
        for k, v in deps.items():
            if k == e and e in ("pe", "sp"):
                continue
            if seen.get(k, 0) >= v:
                continue
            eng.wait_ge(self._semobj(k), v)
            seen[k] = v

    @staticmethod
    def _deps(reads, writes):
        deps = {}
        for t in reads:
            if t.w is not None:
                k, v = t.w
                if deps.get(k, 0) < v:
                    deps[k] = v
        for t in writes:
            if t.w is not None:
                k, v = t.w
                if deps.get(k, 0) < v:
                    deps[k] = v
            for k, v in t.r.items():
                if deps.get(k, 0) < v:
                    deps[k] = v
        return deps

    @staticmethod
    def _mark(tok, reads, writes):
        k, v = tok
        for t in reads:
            t.r[k] = v
        for t in writes:
            t.w = tok
            t.r = {}

    def op(self, e, fn, reads=(), writes=()):
        self._wait(e, self._deps(reads, writes))
        ins = fn(self.eng[e])
        self.cnt[e] += 1
        ins.then_inc(self.sem[e], 1)
        self._mark((e, self.cnt[e]), reads, writes)
        self.ninst += 1
        return ins

    def dma(self, q, out, in_, reads=(), writes=()):
        deps = self._deps(reads, writes)
        i = self.dnext[q]
        self.dnext[q] = (i + 1) % self.NDS
        key = (q, i)
        if self.dval[q][i] > 0 and deps.get(key, 0) < self.dval[q][i]:
            deps[key] = self.dval[q][i]
        self._wait(q, deps)
        ins = self.eng[q].dma_start(out=out, in_=in_)
        self.dval[q][i] += 16
        ins.then_inc(self.dsem[q][i], 16)
        self._mark((key, self.dval[q][i]), reads, writes)
        self.ninst += 1
        return ins

    def barrier(self):
        deps = {k: self.cnt[k] for k in self.eng if self.cnt[k] > 0}
        for q in self.dsem:
            for i in range(self.NDS):
                if self.dval[q][i] > 0:
                    deps[(q, i)] = self.dval[q][i]
        for e in self.eng:
            self._wait(e, deps)


class Prog:
    def __init__(self, T, dbg=(), layers=2, phases=None):
        self.T = T
        self.dbg = set(dbg)
        self.layers = layers
        self.phases = phases
        self.nc = nc = bass.Bass("TRN2", target_bir_lowering=False)
        self.es = es = contextlib.ExitStack()
        self.S = Sched(nc, es)
        self.rr = 0
        self.build()
        es.close()

    def din(self, name, shape, dt=F32):
        return self.nc.dram_tensor(name, list(shape), dt, kind="ExternalInput").ap()

    def dscr(self, name, shape, dt, out=False):
        kind = "ExternalOutput" if (out or name in self.dbg) else "Internal"
        return self.nc.dram_tensor(name, list(shape), dt, kind=kind).ap()

    def sb(self, st, name, shape, dt):
        self.uid = getattr(self, "uid", 0) + 1
        return st.enter_context(self.nc.sbuf_tensor("%s_%d" % (name, self.uid), list(shape), dt))

    def any2(self):
        self.rr += 1
        return ("act", "dve")[self.rr % 2]

    def any3(self):
        self.rr += 1
        return ("act", "dve", "pool")[self.rr % 3]

    @staticmethod
    def copy(e, eng, out, in_):
        if e == "act":
            return eng.activation(out=out, in_=in_, func=AF.Copy)
        return eng.tensor_copy(out=out, in_=in_)

    def convert(self, src, R, C, dst_fn, piece=2048):
        S = self.S
        with contextlib.ExitStack() as st:
            NB = 3
            stg = [self.sb(st, "cv_f%d" % i, [128, piece], F32) for i in range(NB)]
            stb = [self.sb(st, "cv_b%d" % i, [128, piece], BF16) for i in range(NB)]
            tf = [Tk() for _ in range(NB)]
            tb = [Tk() for _ in range(NB)]
            i = 0
            for rt in range(R // 128):
                for c0 in range(0, C, piece):
                    n = min(piece, C - c0)
                    b = i % NB
                    i += 1
                    S.dma("sp", stg[b][:, :n], src[rt * 128:(rt + 1) * 128, c0:c0 + n], writes=[tf[b]])
                    e = self.any3()
                    S.op(e, lambda eng, b=b, n=n, e=e: self.copy(e, eng, stb[b][:, :n], stg[b][:, :n]),
                         reads=[tf[b]], writes=[tb[b]])
                    dst, view = dst_fn(rt, c0, n)
                    S.dma("pool", dst, view(stb[b][:, :n]), reads=[tb[b]], writes=[Tk()])
        S.barrier()

    def conv_blocked(self, src, K, N, Wb, CB, off=0, w=None, cb_base=0):
        w = w or CB

        def dst_fn(rt, c0, n):
            nb = n // w
            cb0 = cb_base + c0 // w
            dst = Wb[cb0:cb0 + nb, :, rt, off:off + w].rearrange("cb p c -> p cb c")
            return dst, (lambda v: v.rearrange("p (cb c) -> p cb c", c=w))
        piece = 2048 if N % 2048 == 0 or N > 2048 else N
        self.convert(src, K, N, dst_fn, piece=piece)

    def conv_plain(self, src, R, C, dst):
        def dst_fn(rt, c0, n):
            return dst[rt * 128:(rt + 1) * 128, c0:c0 + n], (lambda v: v)
        self.convert(src, R, C, dst_fn, piece=min(2048, C))

    def gemm(self, actT, KC, Wb, NCB, CB, form, epi, TT, banks, name):
        S = self.S
        T = self.T
        TT = min(TT, T)
        TS = min(512, TT)
        nb = len(banks)
        bi = 0
        with contextlib.ExitStack() as st:
            act = self.sb(st, name + "_act", [128, KC, TT], BF16)
            G = 8
            ngr = (KC + G - 1) // G
            t_act = [Tk() for _ in range(ngr)]
            wbuf = [self.sb(st, name + "_w%d" % i, [128, KC, CB], BF16) for i in range(2)]
            t_w = [Tk(), Tk()]
            aT = actT.rearrange("(kc p) t -> p kc t", p=128)
            wi = 0
            for st_i in range(T // TT):
                for g in range(ngr):
                    k0, k1 = g * G, min(KC, (g + 1) * G)
                    S.dma("sp", act[:, k0:k1, :], aT[:, k0:k1, st_i * TT:(st_i + 1) * TT], writes=[t_act[g]])
                for cb in range(NCB):
                    wb = wi % 2
                    wi += 1
                    S.dma("sp", wbuf[wb][:], Wb[cb], writes=[t_w[wb]])
                    if form == "A":
                        for ts in range(TT // 128):
                            bank, tb = banks[bi % nb]
                            bi += 1
                            for kc in range(KC):
                                S.op("pe", lambda e, kc=kc, ts=ts, wb=wb, bank=bank: e.matmul(
                                    bank[:, :CB], act[:, kc, ts * 128:(ts + 1) * 128], wbuf[wb][:, kc, :],
                                    start=(kc == 0), stop=(kc == KC - 1)),
                                    reads=[t_act[kc // G], t_w[wb]], writes=[tb])
                            epi((bank, tb), st_i * TT + ts * 128, cb)
                    else:
                        for ts in range(TT // TS):
                            subs = []
                            for sub in range(CB // 128):
                                bank, tb = banks[bi % nb]
                                bi += 1
                                for kc in range(KC):
                                    S.op("pe", lambda e, kc=kc, ts=ts, wb=wb, bank=bank, sub=sub: e.matmul(
                                        bank[:, :TS], wbuf[wb][:, kc, sub * 128:(sub + 1) * 128],
                                        act[:, kc, ts * TS:(ts + 1) * TS],
                                        start=(kc == 0), stop=(kc == KC - 1)),
                                        reads=[t_act[kc // G], t_w[wb]], writes=[tb])
                                subs.append((bank, tb))
                            epi(subs, st_i * TT + ts * TS, cb)
        S.barrier()


    def build(self):
        nc, S, T = self.nc, self.S, self.T
        es = self.es
        L = self.layers
        ph = self.phases
        NPP, NRP = 400, 4352
        self.xT = self.din("xT", [D, T])
        self.w_in = self.din("w_in", [2, D, DIN])
        self.w_o = self.din("w_o", [2, D, D])
        self.ffn_g = self.din("ffn_g", [D, DFF])
        self.ffn_u = self.din("ffn_u", [D, DFF])
        self.ffn_d = self.din("ffn_d", [DFF, D])
        self.moe_g = self.din("moe_g", [NE, D, DFE])
        self.moe_u = self.din("moe_u", [NE, D, DFE])
        self.moe_d = self.din("moe_d", [NE * DFE, D])
        self.router = self.din("router", [128, 32, NE])
        self.pp_in = self.din("pp", [128, NPP])
        self.rp_in = self.din("rp", [2, 128, NRP])
        self.wsT_in = self.din("wsT", [2, 128, 8, 128])
        self.bsT_in = self.din("bsT", [2, 128, 8])
        self.cos_in = self.din("cos16", [T, 1024])
        self.sin_in = self.din("sin16", [T, 1024])
        self.dsym_in = self.din("dsymT", [128, 8, 128])
        self.rdec_in = self.din("rdec", [128, 32])
        self.mbd_in = self.din("maskBD", [128, 2, 128])
        self.sel_in = self.din("sel", [8, 8, 128])
        self.yT = self.dscr("yT", [D, T], F32, out=True)
        self.xTb = self.dscr("xTb", [D, T], BF16)
        self.winA = [self.dscr("winA%d" % l, [17, 128, 32, 512], BF16) for l in range(2)]
        self.winB = [self.dscr("winB%d" % l, [16, 128, 32, 256], BF16) for l in range(2)]
        self.wob = [self.dscr("wob%d" % l, [16, 128, 32, 256], BF16) for l in range(2)]
        self.wgub = self.dscr("wgub", [44, 128, 32, 256], BF16)
        self.wdb = self.dscr("wdb", [16, 128, 44, 256], BF16)
        self.wmgub = self.dscr("wmgub", [64, 128, 32, 256], BF16)
        self.wmdb = self.dscr("wmdb", [16, 128, 64, 256], BF16)
        self.Y_a = self.dscr("Y_a", [T, 1536], F32)
        self.Y_hi = self.dscr("Y_hi", [T, 1024], F32)
        self.Y_g = self.dscr("Y_g", [T, 2048], F32)
        self.Y_r = self.dscr("Y_r", [T, 4096], F32)
        self.YT = self.dscr("YT", [D, T], F32)
        self.aqkT = self.dscr("aqkT", [10, 128, T], BF16)
        self.rqkT = self.dscr("rqkT", [16, 128, T], BF16)
        self.RK = self.dscr("RK", [3, T, 1024], BF16)
        self.mergedT = self.dscr("mergedT", [D, T], BF16)
        self.zT = self.dscr("zT", [D, T], F32)
        self.x1T = self.dscr("x1T", [D, T], F32)
        self.x1Tb = self.dscr("x1Tb", [D, T], BF16)
        self.x2T = self.dscr("x2T", [D, T], F32)
        self.x2Tb = self.dscr("x2Tb", [D, T], BF16)
        self.hT = self.dscr("hT", [NE * DFE, T], BF16)
        self.Grep = self.dscr("Grep", [NE, 128, T], F32)
        self.banks = []
        for i in range(6):
            p = es.enter_context(nc.psum_tensor("ps%d" % i, [128, 512], F32))
            self.banks.append((p, Tk()))
        self.bbanks = []
        for i in range(2):
            p = es.enter_context(nc.psum_tensor("pb%d" % i, [128, 1024], BF16))
            self.bbanks.append((p, Tk()))
        self.pp = self.sb(es, "pp", [128, NPP], F32)
        self.t_pp = Tk()
        self.idf = self.sb(es, "idf", [128, 128], F32)
        self.idb = self.sb(es, "idb", [128, 128], BF16)
        self.onesb = self.sb(es, "onesb", [128, 128], BF16)
        self.epsln = self.sb(es, "epsln", [128, 2], F32)
        self.t_c = Tk()
        S.dma("sp", self.pp[:], self.pp_in, writes=[self.t_pp])
        S.op("pool", lambda e: e.memset(self.idf[:], 0.0), writes=[self.t_c])
        S.op("pool", lambda e: e.affine_select(out=self.idf[:], in_=self.idf[:], pattern=[[-1, 128]],
                                               compare_op=ALU.not_equal, fill=1.0, base=0, channel_multiplier=1),
             writes=[self.t_c])
        S.op("pool", lambda e: e.tensor_copy(out=self.idb[:], in_=self.idf[:]), writes=[self.t_c])
        S.op("pool", lambda e: e.memset(self.onesb[:], 1.0), writes=[self.t_c])
        S.op("pool", lambda e: e.memset(self.epsln[:, 0:1], LN_EPS), writes=[self.t_c])
        S.op("pool", lambda e: e.memset(self.epsln[:, 1:2], NORM_EPS), writes=[self.t_c])
        S.barrier()

        def on(p):
            return ph is None or p in ph
        if on("conv"):
            self.conv_plain(self.xT, D, T, self.xTb)
            for l in range(L):
                w = self.w_in[l]
                self.conv_blocked(w[:, 0:1536], D, 1536, self.winA[l], 512, cb_base=0)
                self.conv_blocked(w[:, 4608:5632], D, 1024, self.winA[l], 512, cb_base=3)
                self.conv_blocked(w[:, 6656:12800], D, 6144, self.winA[l], 512, cb_base=5)
                self.conv_blocked(w[:, 1536:4608], D, 3072, self.winB[l], 256, cb_base=0)
                self.conv_blocked(w[:, 5632:6656], D, 1024, self.winB[l], 256, cb_base=12)
                self.conv_blocked(self.w_o[l], D, D, self.wob[l], 256)
            if on("ffn"):
                self.conv_blocked(self.ffn_g, D, DFF, self.wgub, 256, off=0, w=128)
                self.conv_blocked(self.ffn_u, D, DFF, self.wgub, 256, off=128, w=128)
                self.conv_blocked(self.ffn_d, DFF, D, self.wdb, 256)
            if L > 1 and on("moe"):
                for e_ in range(NE):
                    self.conv_blocked(self.moe_g[e_], D, DFE, self.wmgub, 256, off=0, w=128, cb_base=e_ * 8)
                    self.conv_blocked(self.moe_u[e_], D, DFE, self.wmgub, 256, off=128, w=128, cb_base=e_ * 8)
                self.conv_blocked(self.moe_d, NE * DFE, D, self.wmdb, 256)
        xF, xB = self.xT, self.xTb
        for l in range(L):
            if on("inproj"):
                self.in_proj(l, xB)
            if on("gmlp"):
                self.mix_gmlp(l)
            if on("attn"):
                self.mix_attn(l)
            if on("ret"):
                self.mix_ret(l)
            if on("hgrn"):
                self.mix_hgrn(l)
            if on("wo"):
                self.resid_gemm(self.mergedT, 32, self.wob[l], xF, 1024, "wo")
                self.ln_pass(self.zT, l * 200 + 0, l * 200 + 32, self.x1T, self.x1Tb)
            if l == 0:
                if on("ffn"):
                    self.ffn_up(self.x1Tb, self.wgub, DFF // 128, None)
                    self.resid_gemm(self.hT, DFF // 128, self.wdb, self.x1T, 1024, "fd")
            else:
                if on("moe"):
                    self.router_pass(self.x1T)
                    self.ffn_up(self.x1Tb, self.wmgub, NE * DFE // 128, self.Grep)
                    self.resid_gemm(self.hT, NE * DFE // 128, self.wmdb, self.x1T, 512, "md")
            if on("ffn") or on("moe"):
                last = (l == L - 1)
                self.ln_pass(self.zT, l * 200 + 64, l * 200 + 96, self.yT if last else self.x2T,
                             None if last else self.x2Tb)
            xF, xB = self.x2T, self.x2Tb
        S.barrier()

    def in_proj(self, l, actT):
        S = self.S
        colA = ([(self.Y_a, 512 * i) for i in range(3)] + [(self.Y_hi, 512 * i) for i in range(2)]
                + [(self.Y_g, 512 * i) for i in range(4)] + [(self.Y_r, 512 * i) for i in range(8)])
        with contextlib.ExitStack() as st:
            NSTG = 4
            stg = [self.sb(st, "p1_s%d" % i, [128, 512], F32) for i in range(NSTG)]
            ts = [Tk() for _ in range(NSTG)]
            cnt = [0]

            def epi(bt, tok0, cb):
                bank, tb = bt
                i = cnt[0] % NSTG
                cnt[0] += 1
                e = self.any2()
                S.op(e, lambda eng: self.copy(e, eng, stg[i][:], bank[:]), reads=[tb], writes=[ts[i]])
                yt_, yc_ = colA[cb]
                S.dma("pool", yt_[tok0:tok0 + 128, yc_:yc_ + 512], stg[i][:], reads=[ts[i]], writes=[Tk()])
            self.gemm(actT, 32, self.winA[l], 17, 512, "A", epi, 1024, self.banks[:4], "p1a")
        with contextlib.ExitStack() as st:
            NSTG = 4
            TS = min(512, self.T)
            stg = [self.sb(st, "p1b_s%d" % i, [128, TS], F32) for i in range(NSTG)]
            ts = [Tk() for _ in range(NSTG)]
            cnt = [0]

            def epi(subs, tok0, cb):
                for sub, (bank, tb) in enumerate(subs):
                    i = cnt[0] % NSTG
                    cnt[0] += 1
                    e = self.any2()
                    S.op(e, lambda eng, i=i, bank=bank, e=e: self.copy(e, eng, stg[i][:], bank[:, :TS]), reads=[tb], writes=[ts[i]])
                    r0 = (cb * 2 + sub) * 128
                    S.dma("pool", self.YT[r0:r0 + 128, tok0:tok0 + TS], stg[i][:], reads=[ts[i]], writes=[Tk()])
            self.gemm(actT, 32, self.winB[l], 16, 256, "B", epi, 1024, self.banks[:4], "p1b")

    def resid_gemm(self, actT, KC, Wb, xresT, TT, name):
        S = self.S
        TS = min(512, self.T)
        with contextlib.ExitStack() as st:
            NSTG = 3
            xr = [self.sb(st, name + "_x%d" % i, [128, TS], F32) for i in range(NSTG)]
            zt = [self.sb(st, name + "_z%d" % i, [128, TS], F32) for i in range(NSTG)]
            tx = [Tk() for _ in range(NSTG)]
            tz = [Tk() for _ in range(NSTG)]
            cnt = [0]

            def epi(subs, tok0, cb):
                for sub, (bank, tb) in enumerate(subs):
                    i = cnt[0] % NSTG
                    cnt[0] += 1
                    r0 = (cb * 2 + sub) * 128
                    S.dma("sp", xr[i][:], xresT[r0:r0 + 128, tok0:tok0 + TS], writes=[tx[i]])
                    S.op("dve", lambda e, i=i, bank=bank: e.scalar_tensor_tensor(
                        out=zt[i][:], in0=xr[i][:], scalar=ALPHA, in1=bank[:, :TS], op0=ALU.mult, op1=ALU.add),
                        reads=[tx[i], tb], writes=[tz[i]])
                    S.dma("pool", self.zT[r0:r0 + 128, tok0:tok0 + TS], zt[i][:], reads=[tz[i]], writes=[Tk()])
            self.gemm(actT, KC, Wb, 16, 256, "B", epi, TT, self.banks[:4], name)

    def ffn_up(self, actT, Wb, NCB, grep):
        S = self.S
        TS = min(512, self.T)
        with contextlib.ExitStack() as st:
            NSTG = 3
            sg = [self.sb(st, "fu_s%d" % i, [128, TS], F32) for i in range(NSTG)]
            h1 = [self.sb(st, "fu_h%d" % i, [128, TS], F32) for i in range(NSTG)]
            gr = [self.sb(st, "fu_g%d" % i, [128, TS], F32) for i in range(NSTG)]
            hb = [self.sb(st, "fu_b%d" % i, [128, TS], BF16) for i in range(NSTG)]
            tsg = [Tk() for _ in range(NSTG)]
            th1 = [Tk() for _ in range(NSTG)]
            tgr = [Tk() for _ in range(NSTG)]
            thb = [Tk() for _ in range(NSTG)]
            cnt = [0]

            def epi(subs, tok0, cb):
                (bg, tg), (bu, tu) = subs
                i = cnt[0] % NSTG
                cnt[0] += 1
                S.op("act", lambda e: e.activation(out=sg[i][:], in_=bg[:, :TS], func=AF.Silu), reads=[tg], writes=[tsg[i]])
                if grep is None:
                    S.op("dve", lambda e: e.tensor_tensor(out=hb[i][:], in0=sg[i][:], in1=bu[:, :TS], op=ALU.mult),
                         reads=[tsg[i], tu], writes=[thb[i]])
                else:
                    S.dma("sp", gr[i][:], grep[cb // 8, :, tok0:tok0 + TS], writes=[tgr[i]])
                    S.op("dve", lambda e: e.tensor_tensor(out=h1[i][:], in0=sg[i][:], in1=bu[:, :TS], op=ALU.mult),
                         reads=[tsg[i], tu], writes=[th1[i]])
                    S.op("pool", lambda e: e.tensor_tensor(out=hb[i][:], in0=h1[i][:], in1=gr[i][:], op=ALU.mult),
                         reads=[th1[i], tgr[i]], writes=[thb[i]])
                S.dma("pool", self.hT[cb * 128:(cb + 1) * 128, tok0:tok0 + TS], hb[i][:], reads=[thb[i]], writes=[Tk()])
            self.gemm(actT, 32, Wb, NCB, 256, "B", epi, 1024, self.banks[:4], "fu")

    def ln_pass(self, zT, gcol, bcol, outF, outB):
        S = self.S
        T = self.T
        TS = min(512, T)
        bs_, bq_ = self.banks[4], self.banks[5]
        zTr = zT.rearrange("(c p) t -> p c t", p=128)
        with contextlib.ExitStack() as st:
            z = [self.sb(st, "ln_z%d" % i, [128, 32, TS], F32) for i in range(2)]
            tz = [[Tk() for _ in range(4)] for _ in range(2)]
            NR = 3
            zb = [self.sb(st, "ln_zb%d" % i, [128, TS], BF16) for i in range(NR)]
            zq = [self.sb(st, "ln_zq%d" % i, [128, TS], BF16) for i in range(NR)]
            tzb = [Tk() for _ in range(NR)]
            tzq = [Tk() for _ in range(NR)]
            mean = self.sb(st, "ln_mean", [128, TS], F32)
            msq = self.sb(st, "ln_msq", [128, TS], F32)
            rstd = self.sb(st, "ln_rstd", [128, TS], F32)
            tst = Tk()
            t1 = [self.sb(st, "ln_t%d" % i, [128, TS], F32) for i in range(NR)]
            of = [self.sb(st, "ln_of%d" % i, [128, TS], F32) for i in range(NR)]
            ob = [self.sb(st, "ln_ob%d" % i, [128, TS], BF16) for i in range(NR)]
            tt1 = [Tk() for _ in range(NR)]
            tof = [Tk() for _ in range(NR)]
            tob = [Tk() for _ in range(NR)]
            k = 0
            for tt in range(T // TS):
                zi = tt % 2
                tok = slice(tt * TS, (tt + 1) * TS)
                for g in range(4):
                    S.dma("sp", z[zi][:, g * 8:(g + 1) * 8, :], zTr[:, g * 8:(g + 1) * 8, tok], writes=[tz[zi][g]])
                for c in range(32):
                    i = k % NR
                    k += 1
                    S.op("act", lambda e: e.activation(out=zb[i][:], in_=z[zi][:, c, :], func=AF.Copy),
                         reads=[tz[zi][c // 8]], writes=[tzb[i]])
                    S.op("act", lambda e: e.activation(out=zq[i][:], in_=z[zi][:, c, :], func=AF.Square),
                         reads=[tz[zi][c // 8]], writes=[tzq[i]])
                    S.op("pe", lambda e: e.matmul(bs_[0][:, :TS], self.onesb[:], zb[i][:], start=(c == 0), stop=(c == 31)),
                         reads=[tzb[i], self.t_c], writes=[bs_[1]])
                    S.op("pe", lambda e: e.matmul(bq_[0][:, :TS], self.onesb[:], zq[i][:], start=(c == 0), stop=(c == 31)),
                         reads=[tzq[i], self.t_c], writes=[bq_[1]])
                S.op("dve", lambda e: e.tensor_scalar(out=mean[:], in0=bs_[0][:, :TS], scalar1=1.0 / D, scalar2=1.0, op0=ALU.mult, op1=ALU.mult),
                     reads=[bs_[1]], writes=[tst])
                S.op("dve", lambda e: e.tensor_tensor(out=msq[:], in0=mean[:], in1=mean[:], op=ALU.mult), reads=[tst], writes=[tst])
                S.op("dve", lambda e: e.scalar_tensor_tensor(out=msq[:], in0=bq_[0][:, :TS], scalar=1.0 / D, in1=msq[:],
                                                            op0=ALU.mult, op1=ALU.subtract), reads=[bq_[1], tst], writes=[tst])
                S.op("act", lambda e: e.activation(out=rstd[:], in_=msq[:], func=AF.Sqrt, bias=self.epsln[:, 0:1], scale=1.0),
                     reads=[tst, self.t_c], writes=[tst])
                S.op("dve", lambda e: e.reciprocal(out=rstd[:], in_=rstd[:]), reads=[tst], writes=[tst])
                for c in range(32):
                    i = k % NR
                    k += 1
                    S.op("dve", lambda e: e.tensor_tensor(out=t1[i][:], in0=z[zi][:, c, :], in1=mean[:], op=ALU.subtract),
                         reads=[tz[zi][c // 8], tst], writes=[tt1[i]])
                    S.op("dve", lambda e: e.tensor_tensor(out=t1[i][:], in0=t1[i][:], in1=rstd[:], op=ALU.mult),
                         reads=[tst], writes=[tt1[i]])
                    S.op("act", lambda e: e.activation(out=of[i][:], in_=t1[i][:], func=AF.Identity,
                                                       bias=self.pp[:, bcol + c:bcol + c + 1], scale=self.pp[:, gcol + c:gcol + c + 1]),
                         reads=[tt1[i], self.t_pp], writes=[tof[i]])
                    S.dma("act", outF[c * 128:(c + 1) * 128, tok], of[i][:], reads=[tof[i]], writes=[Tk()])
                    if outB is not None:
                        S.op("pool", lambda e: e.tensor_copy(out=ob[i][:], in_=of[i][:]), reads=[tof[i]], writes=[tob[i]])
                        S.dma("act", outB[c * 128:(c + 1) * 128, tok], ob[i][:], reads=[tob[i]], writes=[Tk()])
        S.barrier()

    def router_pass(self, x1T):
        S = self.S
        T = self.T
        TS = min(512, T)
        xr_ = x1T.rearrange("(c p) t -> p c t", p=128)
        bl, bt, br = self.banks[0], self.banks[1], self.banks[2]
        with contextlib.ExitStack() as st:
            wr = self.sb(st, "rt_w", [128, 32, NE], F32)
            sel = self.sb(st, "rt_sel", [8, 8, 128], F32)
            tw = Tk()
            S.dma("sp", wr[:], self.router, writes=[tw])
            S.dma("sp", sel[:], self.sel_in, writes=[tw])
            xr = [self.sb(st, "rt_x%d" % i, [128, 32, 128], F32) for i in range(2)]
            tx = [Tk(), Tk()]
            sm = self.sb(st, "rt_sm", [128, 64], F32)
            tsm = Tk()
            gT = self.sb(st, "rt_gT", [8, TS], F32)
            tgT = Tk()
            gr = [self.sb(st, "rt_gr%d" % i, [128, TS], F32) for i in range(2)]
            tgr = [Tk(), Tk()]
            lg, eq1, l2, eq2, g1, gt = (sm[:, 0:8], sm[:, 8:16], sm[:, 16:24], sm[:, 24:32], sm[:, 32:40], sm[:, 40:48])
            m1, m2, dl, w1, w2 = (sm[:, 48:49], sm[:, 49:50], sm[:, 50:51], sm[:, 51:52], sm[:, 52:53])
            npg = TS // 128
            k = 0
            for n in range(T // 128):
                i = n % 2
                S.dma("sp", xr[i][:], xr_[:, :, n * 128:(n + 1) * 128], writes=[tx[i]])
                for c in range(32):
                    S.op("pe", lambda e: e.matmul(bl[0][:, 0:NE], xr[i][:, c, :], wr[:, c, :], start=(c == 0), stop=(c == 31)),
                         reads=[tx[i], tw], writes=[bl[1]])
                V = "dve"
                S.op(V, lambda e: e.tensor_copy(out=lg, in_=bl[0][:, 0:NE]), reads=[bl[1]], writes=[tsm])
                S.op(V, lambda e: e.tensor_reduce(out=m1, in_=lg, axis=AX.X, op=ALU.max), reads=[tsm], writes=[tsm])
                S.op(V, lambda e: e.tensor_scalar(out=eq1, in0=lg, scalar1=m1, scalar2=1.0, op0=ALU.is_equal, op1=ALU.mult), reads=[tsm], writes=[tsm])
                S.op(V, lambda e: e.scalar_tensor_tensor(out=l2, in0=eq1, scalar=-1e30, in1=lg, op0=ALU.mult, op1=ALU.add), reads=[tsm], writes=[tsm])
                S.op(V, lambda e: e.tensor_reduce(out=m2, in_=l2, axis=AX.X, op=ALU.max), reads=[tsm], writes=[tsm])
                S.op(V, lambda e: e.tensor_scalar(out=eq2, in0=l2, scalar1=m2, scalar2=1.0, op0=ALU.is_equal, op1=ALU.mult), reads=[tsm], writes=[tsm])
                S.op(V, lambda e: e.tensor_tensor(out=dl, in0=m1, in1=m2, op=ALU.subtract), reads=[tsm], writes=[tsm])
                S.op("act", lambda e: e.activation(out=w1, in_=dl, func=AF.Sigmoid), reads=[tsm], writes=[tsm])
                S.op("act", lambda e: e.activation(out=w2, in_=dl, func=AF.Sigmoid, scale=-1.0), reads=[tsm], writes=[tsm])
                S.op(V, lambda e: e.tensor_scalar(out=g1, in0=eq1, scalar1=w1, scalar2=1.0, op0=ALU.mult, op1=ALU.mult), reads=[tsm], writes=[tsm])
                S.op(V, lambda e: e.scalar_tensor_tensor(out=gt, in0=eq2, scalar=w2, in1=g1, op0=ALU.mult, op1=ALU.add), reads=[tsm], writes=[tsm])
                j = n % npg
                S.op("pe", lambda e: e.transpose(bt[0][0:8, j * 128:(j + 1) * 128], gt, self.idf[:]),
                     reads=[tsm, self.t_c], writes=[bt[1]])
                if j == npg - 1:
                    tok0 = (n - j) * 128
                    S.op("act", lambda e: e.activation(out=gT[:], in_=bt[0][0:8, :TS], func=AF.Copy), reads=[bt[1]], writes=[tgT])
                    for ex in range(NE):
                        S.op("pe", lambda e: e.matmul(br[0][:, :TS], sel[:, ex, :], gT[:], start=True, stop=True),
                             reads=[tgT, tw], writes=[br[1]])
                        b = k % 2
                        k += 1
                        en = self.any2()
                        S.op(en, lambda eng: self.copy(en, eng, gr[b][:], br[0][:, :TS]), reads=[br[1]], writes=[tgr[b]])
                        S.dma("pool", self.Grep[ex, :, tok0:tok0 + TS], gr[b][:], reads=[tgr[b]], writes=[Tk()])
        S.barrier()

    def rope(self, S, xin, rb, cs, sn, W, tin, tcs, tout, tmp, ttmp):
        xv = xin.rearrange("p (j two) -> p j two", two=2)
        ov = rb.rearrange("p (j two) -> p j two", two=2)
        x0, x1 = xv[:, :, 0], xv[:, :, 1]
        t1, t2, t3, t4 = tmp
        S.op("dve", lambda e: e.tensor_tensor(out=t1, in0=x0, in1=cs, op=ALU.mult), reads=[tin, tcs], writes=[ttmp[0]])
        S.op("pool", lambda e: e.tensor_tensor(out=t2, in0=x1, in1=sn, op=ALU.mult), reads=[tin, tcs], writes=[ttmp[1]])
        S.op("pool", lambda e: e.tensor_tensor(out=t3, in0=x0, in1=sn, op=ALU.mult), reads=[tin, tcs], writes=[ttmp[2]])
        S.op("dve", lambda e: e.tensor_tensor(out=t4, in0=x1, in1=cs, op=ALU.mult), reads=[tin, tcs], writes=[ttmp[3]])
        S.op("dve", lambda e: e.tensor_tensor(out=ov[:, :, 0], in0=t1, in1=t2, op=ALU.subtract),
             reads=[ttmp[0], ttmp[1]], writes=[tout])
        S.op("pool", lambda e: e.tensor_tensor(out=ov[:, :, 1], in0=t3, in1=t4, op=ALU.add),
             reads=[ttmp[2], ttmp[3], tout], writes=[tout])

    def mix_gmlp(self, l):
        S = self.S
        T = self.T
        bA, bB = self.banks[0], self.banks[1]
        with contextlib.ExitStack() as st:
            grep = self.sb(st, "gm_g", [128, 1024], F32)
            brep = self.sb(st, "gm_b", [128, 1024], F32)
            msrep = self.sb(st, "gm_ms", [128, 1024], F32)
            wsf = self.sb(st, "gm_wsf", [128, 8, 128], F32)
            wsb = self.sb(st, "gm_wsb", [128, 8, 128], BF16)
            bs = self.sb(st, "gm_bs", [128, 8], F32)
            tc_ = Tk()
            S.dma("sp", grep[:], self.rp_in[l, :, 1280:2304], writes=[tc_])
            S.dma("sp", brep[:], self.rp_in[l, :, 2304:3328], writes=[tc_])
            S.dma("sp", msrep[:], self.rp_in[l, :, 3328:4352], writes=[tc_])
            S.dma("sp", wsf[:], self.wsT_in[l], writes=[tc_])
            S.dma("sp", bs[:], self.bsT_in[l], writes=[tc_])
            S.op("dve", lambda e: e.tensor_copy(out=wsb[:], in_=wsf[:]), reads=[tc_], writes=[tc_])
            NB = 2
            gv = [self.sb(st, "gm_gv%d" % i, [128, 1024], F32) for i in range(NB)]
            gu = [self.sb(st, "gm_gu%d" % i, [128, 1024], F32) for i in range(NB)]
            a = [self.sb(st, "gm_a%d" % i, [128, 1024], F32) for i in range(NB)]
            sq = [self.sb(st, "gm_sq%d" % i, [128, 1024], F32) for i in range(NB)]
            vnb = [self.sb(st, "gm_vn%d" % i, [128, 1024], BF16) for i in range(NB)]
            oc = [self.sb(st, "gm_oc%d" % i, [128, 1024], F32) for i in range(NB)]
            ocb = [self.sb(st, "gm_ob%d" % i, [128, 1024], BF16) for i in range(NB)]
            mT = [self.sb(st, "gm_mT%d" % i, [128, 1024], BF16) for i in range(NB)]
            sm = [self.sb(st, "gm_sm%d" % i, [128, 8], F32) for i in range(NB)]
            tgv = [Tk() for _ in range(NB)]
            tgu = [Tk() for _ in range(NB)]
            ta = [Tk() for _ in range(NB)]
            tsq = [Tk() for _ in range(NB)]
            tvn = [Tk() for _ in range(NB)]
            toc = [Tk() for _ in range(NB)]
            tob = [Tk() for _ in range(NB)]
            tmT = [Tk() for _ in range(NB)]
            tsm = [Tk() for _ in range(NB)]
            mdst = self.mergedT[2048:3072, :].rearrange("(g p) t -> p g t", p=128)
            for n in range(T // 128):
                i = n % NB
                rows = slice(n * 128, (n + 1) * 128)
                S.dma("sp", gv[i][:], self.Y_g[rows, 1024:2048], writes=[tgv[i]])
                S.dma("sp", gu[i][:], self.Y_g[rows, 0:1024], writes=[tgu[i]])
                s1, nm, s2, rs = sm[i][:, 0:1], sm[i][:, 1:2], sm[i][:, 2:3], sm[i][:, 3:4]
                S.op("act", lambda e: e.activation(out=a[i][:], in_=gv[i][:], func=AF.Gelu), reads=[tgv[i]], writes=[ta[i]])
                S.op("dve", lambda e: e.tensor_reduce(out=s1, in_=a[i][:], axis=AX.X, op=ALU.add), reads=[ta[i]], writes=[tsm[i]])
                S.op("dve", lambda e: e.tensor_scalar(out=nm, in0=s1, scalar1=-1.0 / 1024, scalar2=1.0, op0=ALU.mult, op1=ALU.mult), reads=[tsm[i]], writes=[tsm[i]])
                S.op("dve", lambda e: e.tensor_scalar(out=a[i][:], in0=a[i][:], scalar1=nm, scalar2=0.0, op0=ALU.add, op1=ALU.add), reads=[tsm[i]], writes=[ta[i]])
                S.op("pool", lambda e: e.tensor_tensor(out=sq[i][:], in0=a[i][:], in1=a[i][:], op=ALU.mult), reads=[ta[i]], writes=[tsq[i]])
                S.op("dve", lambda e: e.tensor_reduce(out=s2, in_=sq[i][:], axis=AX.X, op=ALU.add), reads=[tsq[i]], writes=[tsm[i]])
                S.op("act", lambda e: e.activation(out=rs, in_=s2, func=AF.Sqrt, bias=self.epsln[:, 0:1], scale=1.0 / 1024),
                     reads=[tsm[i], self.t_c], writes=[tsm[i]])
                S.op("dve", lambda e: e.reciprocal(out=rs, in_=rs), reads=[tsm[i]], writes=[tsm[i]])
                S.op("dve", lambda e: e.scalar_tensor_tensor(out=sq[i][:], in0=a[i][:], scalar=rs, in1=grep[:], op0=ALU.mult, op1=ALU.mult),
                     reads=[ta[i], tsm[i], tc_], writes=[tsq[i]])
                S.op("pool", lambda e: e.tensor_tensor(out=vnb[i][:], in0=sq[i][:], in1=brep[:], op=ALU.add), reads=[tsq[i], tc_], writes=[tvn[i]])
                for g in range(8):
                    bank = bA if g < 4 else bB
                    c0 = (g % 4) * 128
                    S.op("pe", lambda e: e.matmul(bank[0][:, c0:c0 + 128], wsb[:, g, :], vnb[i][:, g * 128:(g + 1) * 128], start=True, stop=True),
                         reads=[tvn[i], tc_], writes=[bank[1]])
                S.op("act", lambda e: e.activation(out=gu[i][:], in_=gu[i][:], func=AF.Gelu), reads=[tgu[i]], writes=[tgu[i]])
                for g in range(8):
                    bank = bA if g < 4 else bB
                    c0 = (g % 4) * 128
                    S.op("dve", lambda e: e.scalar_tensor_tensor(out=oc[i][:, g * 128:(g + 1) * 128], in0=bank[0][:, c0:c0 + 128],
                                                                scalar=bs[:, g:g + 1], in1=gu[i][:, g * 128:(g + 1) * 128],
                                                                op0=ALU.add, op1=ALU.mult),
                         reads=[bank[1], tgu[i], tc_], writes=[toc[i]])
                S.op("pool", lambda e: e.tensor_tensor(out=ocb[i][:], in0=oc[i][:], in1=msrep[:], op=ALU.mult), reads=[toc[i], tc_], writes=[tob[i]])
                pb, tpb = self.bbanks[n % 2]
                for g in range(8):
                    S.op("pe", lambda e: e.transpose(pb[:, g * 128:(g + 1) * 128], ocb[i][:, g * 128:(g + 1) * 128], self.idb[:]),
                         reads=[tob[i], self.t_c], writes=[tpb])
                S.op("act", lambda e: e.activation(out=mT[i][:], in_=pb[:], func=AF.Copy), reads=[tpb], writes=[tmT[i]])
                S.dma("pool", mdst[:, :, rows], mT[i][:].rearrange("p (g t) -> p g t", t=128), reads=[tmT[i]], writes=[Tk()])
        S.barrier()

    def mix_attn(self, l):
        S = self.S
        T = self.T
        NCH = T // 128
        QT = min(512, T)
        with contextlib.ExitStack() as st:
            gain = self.sb(st, "at_gain", [128, 1280], F32)
            tcst = Tk()
            S.dma("sp", gain[:], self.rp_in[l, :, 0:1280], writes=[tcst])
            NB = 2
            qk = [self.sb(st, "at_qk%d" % i, [128, 1280], F32) for i in range(NB)]
            sq = [self.sb(st, "at_sq%d" % i, [128, 1280], F32) for i in range(NB)]
            cs = [self.sb(st, "at_cs%d" % i, [128, 640], F32) for i in range(NB)]
            sn = [self.sb(st, "at_sn%d" % i, [128, 640], F32) for i in range(NB)]
            tmp = [[self.sb(st, "at_t%d_%d" % (i, j), [128, 640], F32) for j in range(4)] for i in range(NB)]
            rb = [self.sb(st, "at_rb%d" % i, [128, 1280], BF16) for i in range(NB)]
            qkT = [self.sb(st, "at_qkT%d" % i, [128, 1280], BF16) for i in range(NB)]
            sm = [self.sb(st, "at_sm%d" % i, [128, 16], F32) for i in range(NB)]
            tqk = [Tk() for _ in range(NB)]
            tsq = [Tk() for _ in range(NB)]
            tcs = [Tk() for _ in range(NB)]
            ttmp = [[Tk() for _ in range(4)] for _ in range(NB)]
            trb = [Tk() for _ in range(NB)]
            tqT = [Tk() for _ in range(NB)]
            tsm = [Tk() for _ in range(NB)]
            dst = self.aqkT.rearrange("h d t -> d h t")
            for n in range(NCH):
                i = n % NB
                rows = slice(n * 128, (n + 1) * 128)
                S.dma("sp", qk[i][:], self.Y_a[rows, 0:1280], writes=[tqk[i]])
                S.dma("sp", cs[i][:], self.cos_in[rows, 0:640], writes=[tcs[i]])
                S.dma("sp", sn[i][:], self.sin_in[rows, 0:640], writes=[tcs[i]])
                S.op("pool", lambda e: e.tensor_tensor(out=sq[i][:], in0=qk[i][:], in1=qk[i][:], op=ALU.mult), reads=[tqk[i]], writes=[tsq[i]])
                ss = sm[i][:, 0:10]
                S.op("dve", lambda e: e.tensor_reduce(out=ss, in_=sq[i][:].rearrange("p (h d) -> p h d", d=128), axis=AX.X, op=ALU.add),
                     reads=[tsq[i]], writes=[tsm[i]])
                S.op("act", lambda e: e.activation(out=ss, in_=ss, func=AF.Sqrt, bias=self.epsln[:, 1:2], scale=1.0 / 128),
                     reads=[tsm[i], self.t_c], writes=[tsm[i]])
                S.op("dve", lambda e: e.reciprocal(out=ss, in_=ss), reads=[tsm[i]], writes=[tsm[i]])
                S.op("dve", lambda e: e.tensor_tensor(out=sq[i][:].rearrange("p (h d) -> p h d", d=128),
                                                     in0=qk[i][:].rearrange("p (h d) -> p h d", d=128),
                                                     in1=ss.unsqueeze(2).to_broadcast([128, 10, 128]), op=ALU.mult),
                     reads=[tqk[i], tsm[i]], writes=[tsq[i]])
                S.op("pool", lambda e: e.tensor_tensor(out=sq[i][:], in0=sq[i][:], in1=gain[:], op=ALU.mult), reads=[tcst], writes=[tsq[i]])
                self.rope(S, sq[i][:], rb[i][:], cs[i][:], sn[i][:], 1280, tsq[i], tcs[i], trb[i],
                          [t[:] for t in tmp[i]], ttmp[i])
                for h in range(10):
                    pb, tpb = self.bbanks[0] if h < 8 else self.bbanks[1]
                    c0 = (h % 8) * 128
                    S.op("pe", lambda e: e.transpose(pb[:, c0:c0 + 128], rb[i][:, h * 128:(h + 1) * 128], self.idb[:]),
                         reads=[trb[i], self.t_c], writes=[tpb])
                S.op("act", lambda e: e.activation(out=qkT[i][:, 0:1024], in_=self.bbanks[0][0][:], func=AF.Copy),
                     reads=[self.bbanks[0][1]], writes=[tqT[i]])
                S.op("dve", lambda e: e.tensor_copy(out=qkT[i][:, 1024:1280], in_=self.bbanks[1][0][:, 0:256]),
                     reads=[self.bbanks[1][1]], writes=[tqT[i]])
                S.dma("pool", dst[:, :, rows], qkT[i][:].rearrange("p (h t) -> p h t", t=128), reads=[tqT[i]], writes=[Tk()])
        S.barrier()
        scale = 128.0 ** -0.5
        with contextlib.ExitStack() as st:
            kT = self.sb(st, "ac_kT", [128, T], BF16)
            vf = self.sb(st, "ac_vf", [128, NCH, 128], F32)
            vb = self.sb(st, "ac_vb", [128, NCH, 128], BF16)
            tk_, tvf, tvb = Tk(), Tk(), Tk()
            qT = [self.sb(st, "ac_qT%d" % i, [128, QT], BF16) for i in range(2)]
            tq = [Tk(), Tk()]
            NP = 3
            pT = [self.sb(st, "ac_pT%d" % i, [128, QT], BF16) for i in range(NP)]
            tp = [Tk() for _ in range(NP)]
            rec = self.sb(st, "ac_rec", [128, QT], F32)
            of = self.sb(st, "ac_of", [128, QT], F32)
            ob = [self.sb(st, "ac_ob%d" % i, [128, QT], BF16) for i in range(2)]
            trec, tof = Tk(), Tk()
            tob = [Tk(), Tk()]
            sbank = [self.banks[0], self.banks[1]]
            accs = [(self.banks[2], self.banks[3]), (self.banks[4], self.banks[5])]
            it = 0
            pi = 0
            for g in range(2):
                S.dma("sp", kT[:], self.aqkT[8 + g], writes=[tk_])
                self.dma_chunks("sp", vf[:], self.Y_a[:, 1280 + g * 128:1280 + (g + 1) * 128].rearrange("(n p) d -> p n d", p=128), NCH, tvf)
                S.op("pool", lambda e: e.tensor_copy(out=vb[:], in_=vf[:]), reads=[tvf], writes=[tvb])
                for h in range(4 * g, 4 * g + 4):
                    for qt in range(T // QT):
                        qi = it % 2
                        oacc, dacc = accs[it % 2]
                        it += 1
                        S.dma("sp", qT[qi][:], self.aqkT[h, :, qt * QT:(qt + 1) * QT], writes=[tq[qi]])
                        def emit_s(kc_):
                            sbx, tsbx = sbank[kc_ % 2]
                            S.op("pe", lambda e: e.matmul(sbx[:, :QT], kT[:, kc_ * 128:(kc_ + 1) * 128], qT[qi][:], start=True, stop=True),
                                 reads=[tk_, tq[qi]], writes=[tsbx])
                        emit_s(0)
                        for kc in range(NCH):
                            sb_, tsb = sbank[kc % 2]
                            if kc + 1 < NCH:
                                emit_s(kc + 1)
                            p = pi % NP
                            pi += 1
                            S.op("act", lambda e: e.activation(out=pT[p][:], in_=sb_[:, :QT], func=AF.Exp, scale=scale),
                                 reads=[tsb], writes=[tp[p]])
                            S.op("pe", lambda e: e.matmul(oacc[0][:, :QT], vb[:, kc, :], pT[p][:], start=(kc == 0), stop=(kc == NCH - 1)),
                                 reads=[tvb, tp[p]], writes=[oacc[1]])
                            S.op("pe", lambda e: e.matmul(dacc[0][:, :QT], self.onesb[:], pT[p][:], start=(kc == 0), stop=(kc == NCH - 1)),
                                 reads=[tp[p], self.t_c], writes=[dacc[1]])
                        S.op("dve", lambda e: e.reciprocal(out=rec[:], in_=dacc[0][:, :QT]), reads=[dacc[1]], writes=[trec])
                        S.op("dve", lambda e: e.tensor_tensor(out=of[:], in0=oacc[0][:, :QT], in1=rec[:], op=ALU.mult),
                             reads=[oacc[1], trec], writes=[tof])
                        mc = l * 200 + 128 + h
                        S.op("pool", lambda e: e.tensor_scalar(out=ob[qi][:], in0=of[:], scalar1=self.pp[:, mc:mc + 1], scalar2=1.0,
                                                               op0=ALU.mult, op1=ALU.mult), reads=[tof, self.t_pp], writes=[tob[qi]])
                        S.dma("pool", self.mergedT[h * 128:(h + 1) * 128, qt * QT:(qt + 1) * QT], ob[qi][:], reads=[tob[qi]], writes=[Tk()])
        S.barrier()

    def mix_ret(self, l):
        S = self.S
        T = self.T
        NCH = T // 128
        gam = [1.0 - 2.0 ** (-5.0 - h) for h in range(8)]
        gC = [float(np.float64(g) ** 128) for g in gam]
        with contextlib.ExitStack() as st:
            rdec = self.sb(st, "rt_rdec", [128, 32], F32)
            tcst = Tk()
            S.dma("sp", rdec[:], self.rdec_in, writes=[tcst])
            NB = 2
            qk = [self.sb(st, "rp_qk%d" % i, [128, 2048], F32) for i in range(NB)]
            rv = [self.sb(st, "rp_rv%d" % i, [128, 1024], F32) for i in range(NB)]
            cs = [self.sb(st, "rp_cs%d" % i, [128, 1024], F32) for i in range(NB)]
            sn = [self.sb(st, "rp_sn%d" % i, [128, 1024], F32) for i in range(NB)]
            tmp = [[self.sb(st, "rp_t%d_%d" % (i, j), [128, 1024], F32) for j in range(4)] for i in range(NB)]
            rb = [self.sb(st, "rp_rb%d" % i, [128, 2048], BF16) for i in range(NB)]
            kfb = [self.sb(st, "rp_kf%d" % i, [128, 3, 1024], BF16) for i in range(NB)]
            qkT = [self.sb(st, "rp_qkT%d" % i, [128, 2048], BF16) for i in range(NB)]
            tqk = [Tk() for _ in range(NB)]
            trv = [Tk() for _ in range(NB)]
            tcs = [Tk() for _ in range(NB)]
            ttmp = [[Tk() for _ in range(4)] for _ in range(NB)]
            trb = [Tk() for _ in range(NB)]
            tkf = [Tk() for _ in range(NB)]
            tqT = [Tk() for _ in range(NB)]
            dst = self.rqkT.rearrange("h d t -> d h t")
            rkd = self.RK.rearrange("i t c -> t i c")
            for n in range(NCH):
                i = n % NB
                rows = slice(n * 128, (n + 1) * 128)
                S.dma("sp", qk[i][:], self.Y_r[rows, 0:2048], writes=[tqk[i]])
                S.dma("sp", rv[i][:], self.Y_r[rows, 2048:3072], writes=[trv[i]])
                S.dma("sp", cs[i][:], self.cos_in[rows, :], writes=[tcs[i]])
                S.dma("sp", sn[i][:], self.sin_in[rows, :], writes=[tcs[i]])
                self.rope(S, qk[i][:], rb[i][:], cs[i][:], sn[i][:], 2048, tqk[i], tcs[i], trb[i], [t[:] for t in tmp[i]], ttmp[i])
                for h in range(8):
                    kh = rb[i][:, 1024 + h * 128:1024 + (h + 1) * 128]
                    S.op("dve", lambda e: e.tensor_scalar(out=kfb[i][:, 0, h * 128:(h + 1) * 128], in0=kh, scalar1=rdec[:, h:h + 1], scalar2=1.0, op0=ALU.mult, op1=ALU.mult),
                         reads=[trb[i], tcst], writes=[tkf[i]])
                    S.op("pool", lambda e: e.tensor_scalar(out=kfb[i][:, 1, h * 128:(h + 1) * 128], in0=kh, scalar1=rdec[:, 8 + h:9 + h], scalar2=1.0,
                                                           op0=ALU.mult, op1=ALU.mult), reads=[trb[i], tcst], writes=[tkf[i]])
                S.op("act", lambda e: e.activation(out=kfb[i][:, 2, :], in_=rv[i][:], func=AF.Copy), reads=[trv[i]], writes=[tkf[i]])
                S.dma("pool", rkd[rows, :, :], kfb[i][:], reads=[tkf[i]], writes=[Tk()])
                for h in range(16):
                    pb, tpb = self.bbanks[h // 8]
                    c0 = (h % 8) * 128
                    S.op("pe", lambda e: e.transpose(pb[:, c0:c0 + 128], rb[i][:, h * 128:(h + 1) * 128], self.idb[:]),
                         reads=[trb[i], self.t_c], writes=[tpb])
                S.op("act", lambda e: e.activation(out=qkT[i][:, 0:1024], in_=self.bbanks[0][0][:], func=AF.Copy),
                     reads=[self.bbanks[0][1]], writes=[tqT[i]])
                S.op("dve", lambda e: e.tensor_copy(out=qkT[i][:, 1024:2048], in_=self.bbanks[1][0][:]),
                     reads=[self.bbanks[1][1]], writes=[tqT[i]])
                S.dma("pool", dst[:, :, rows], qkT[i][:].rearrange("p (h t) -> p h t", t=128), reads=[tqT[i]], writes=[Tk()])
        S.barrier()
        with contextlib.ExitStack() as st:
            rdec = self.sb(st, "rs_rdec", [128, 32], F32)
            dsym = self.sb(st, "rs_dsym", [128, 8, 128], F32)
            gcol = self.sb(st, "rs_gcol", [128, 8], F32)
            tcst = Tk()
            S.dma("sp", rdec[:], self.rdec_in, writes=[tcst])
            S.dma("sp", dsym[:], self.dsym_in, writes=[tcst])
            b0 = l * 200
            S.op("dve", lambda e: e.tensor_tensor(out=gcol[:], in0=self.pp[:, b0 + 168:b0 + 176], in1=self.pp[:, b0 + 128 + 24:b0 + 128 + 32], op=ALU.mult),
                 reads=[self.t_pp], writes=[tcst])
            qT = self.sb(st, "rs_qT", [128, T], BF16)
            kT = self.sb(st, "rs_kT", [128, T], BF16)
            kv = self.sb(st, "rs_kv", [128, 3, NCH, 128], BF16)
            rg = self.sb(st, "rs_rg", [128, NCH, 128], F32)
            oacc = self.sb(st, "rs_oacc", [128, NCH, 128], F32)
            NG = min(8, NCH)
            sqb = [self.sb(st, "rs_sq%d" % i, [128, NG, 128], F32) for i in range(2)]
            ob = [self.sb(st, "rs_ob%d" % i, [128, NG, 128], BF16) for i in range(2)]
            ss = [self.sb(st, "rs_ss%d" % i, [128, NG], F32) for i in range(2)]
            tld, trg, toa = Tk(), Tk(), Tk()
            tsq, tob, tss = [Tk(), Tk()], [Tk(), Tk()], [Tk(), Tk()]
            R = [self.sb(st, "rs_R%d" % i, [128, 128], F32) for i in range(2)]
            NSR = 8
            Rbr = self.sb(st, "rs_Rbr", [128, NSR, 128], BF16)
            tR = [Tk(), Tk()]
            tRbr = [Tk() for _ in range(NSR)]
            pT = [self.sb(st, "rs_pT%d" % i, [128, 128], BF16) for i in range(2)]
            tpT = [Tk(), Tk()]
            mT = [self.sb(st, "rs_mT%d" % i, [128, 1024], BF16) for i in range(2)]
            tmT = [Tk(), Tk()]
            bS = [self.banks[0], self.banks[1]]
            bO = [self.banks[2], self.banks[3]]
            bR = [self.banks[4], self.banks[5]]
            for h in range(8):
                S.dma("sp", qT[:], self.rqkT[h], writes=[tld])
                S.dma("sp", kT[:], self.rqkT[8 + h], writes=[tld])
                for i3 in range(3):
                    self.dma_chunks("sp", kv[:, i3, :, :], self.RK[i3, :, h * 128:(h + 1) * 128].rearrange("(n p) d -> p n d", p=128), NCH, tld)
                self.dma_chunks("sp", rg[:], self.Y_r[:, 3072 + h * 128:3072 + (h + 1) * 128].rearrange("(n p) d -> p n d", p=128), NCH, trg)
                S.op("pool", lambda e: e.memset(R[0][:], 0.0), writes=[tR[0]])
                S.op("pool", lambda e: e.memset(Rbr[:, 0, :], 0.0), writes=[tRbr[0]])

                def a1(j):
                    cols = slice(j * 128, (j + 1) * 128)
                    bs_, tbs = bS[j % 2]
                    br_, tbr = bR[j % 2]
                    S.op("pe", lambda e: e.matmul(bs_[:, 0:128], kT[:, cols], qT[:, cols], start=True, stop=True), reads=[tld], writes=[tbs])
                    S.op("pe", lambda e: e.matmul(br_[:, 0:128], kv[:, 0, j, :], kv[:, 2, j, :], start=True, stop=True), reads=[tld], writes=[tbr])
                    p = j % 2
                    S.op("dve", lambda e: e.tensor_tensor(out=pT[p][:], in0=bs_[:, 0:128], in1=dsym[:, h, :], op=ALU.mult),
                         reads=[tbs, tcst], writes=[tpT[p]])
                    S.op("dve", lambda e: e.scalar_tensor_tensor(out=R[0][:], in0=R[0][:], scalar=gC[h], in1=br_[:, 0:128],
                                                                op0=ALU.mult, op1=ALU.add), reads=[tbr], writes=[tR[0]])
                    s1 = (j + 1) % NSR
                    S.op("dve", lambda e: e.tensor_copy(out=Rbr[:, s1, :], in_=R[0][:]), reads=[tR[0]], writes=[tRbr[s1]])

                def a2(j):
                    cols = slice(j * 128, (j + 1) * 128)
                    bo_, tbo = bO[j % 2]
                    p = j % 2
                    s0 = j % NSR
                    S.op("pe", lambda e: e.matmul(bo_[:, 0:128], pT[p][:], kv[:, 2, j, :], start=True, stop=True), reads=[tpT[p], tld], writes=[tbo])
                    S.op("pe", lambda e: e.matmul(bo_[:, 128:256], qT[:, cols], Rbr[:, s0, :], start=True, stop=True), reads=[tld, tRbr[s0]], writes=[tbo])
                    S.op("act", lambda e: e.activation(out=oacc[:, j, :], in_=bo_[:, 0:128], func=AF.Copy), reads=[tbo], writes=[toa])
                    S.op("dve", lambda e: e.scalar_tensor_tensor(out=oacc[:, j, :], in0=bo_[:, 128:256], scalar=rdec[:, 16 + h:17 + h],
                                                                in1=oacc[:, j, :], op0=ALU.mult, op1=ALU.add), reads=[tbo, tcst, toa], writes=[toa])
                a1(0)
                for j in range(NCH):
                    if j + 1 < NCH:
                        a1(j + 1)
                    a2(j)
                S.op("pool", lambda e: e.memset(R[1][:], 0.0), writes=[tR[1]])
                S.op("pool", lambda e: e.memset(Rbr[:, 0, :], 0.0), writes=[tRbr[0]])

                def d1(n):
                    j = NCH - 1 - n
                    br_, tbr = bR[n % 2]
                    S.op("pe", lambda e: e.matmul(br_[:, 0:128], kv[:, 1, j, :], kv[:, 2, j, :], start=True, stop=True), reads=[tld], writes=[tbr])
                    S.op("dve", lambda e: e.scalar_tensor_tensor(out=R[1][:], in0=R[1][:], scalar=gC[h], in1=br_[:, 0:128],
                                                                op0=ALU.mult, op1=ALU.add), reads=[tbr], writes=[tR[1]])
                    s1 = (n + 1) % NSR
                    S.op("dve", lambda e: e.tensor_copy(out=Rbr[:, s1, :], in_=R[1][:]), reads=[tR[1]], writes=[tRbr[s1]])

                def d2(n):
                    j = NCH - 1 - n
                    cols = slice(j * 128, (j + 1) * 128)
                    bo_, tbo = bO[n % 2]
                    s0 = n % NSR
                    S.op("pe", lambda e: e.matmul(bo_[:, 128:256], qT[:, cols], Rbr[:, s0, :], start=True, stop=True), reads=[tld, tRbr[s0]], writes=[tbo])
                    S.op("dve", lambda e: e.scalar_tensor_tensor(out=oacc[:, j, :], in0=bo_[:, 128:256], scalar=rdec[:, 24 + h:25 + h],
                                                                in1=oacc[:, j, :], op0=ALU.mult, op1=ALU.add), reads=[tbo, tcst, toa], writes=[toa])
                d1(0)
                for n in range(NCH):
                    if n + 1 < NCH:
                        d1(n + 1)
                    d2(n)
                S.op("act", lambda e: e.activation(out=rg[:], in_=rg[:], func=AF.Silu), reads=[trg], writes=[trg])
                for j0 in range(0, NCH, NG):
                    gi = (j0 // NG) % 2
                    js = slice(j0, j0 + NG)
                    S.op("pool", lambda e: e.tensor_tensor(out=sqb[gi][:], in0=oacc[:, js, :], in1=oacc[:, js, :], op=ALU.mult), reads=[toa], writes=[tsq[gi]])
                    S.op("dve", lambda e: e.tensor_reduce(out=ss[gi][:], in_=sqb[gi][:], axis=AX.X, op=ALU.add), reads=[tsq[gi]], writes=[tss[gi]])
                    S.op("act", lambda e: e.activation(out=ss[gi][:], in_=ss[gi][:], func=AF.Sqrt, bias=self.epsln[:, 1:2], scale=1.0 / 128),
                         reads=[tss[gi], self.t_c], writes=[tss[gi]])
                    S.op("dve", lambda e: e.reciprocal(out=ss[gi][:], in_=ss[gi][:]), reads=[tss[gi]], writes=[tss[gi]])
                    S.op("dve", lambda e: e.tensor_tensor(out=sqb[gi][:], in0=oacc[:, js, :], in1=ss[gi][:].unsqueeze(2).to_broadcast([128, NG, 128]), op=ALU.mult),
                         reads=[toa, tss[gi]], writes=[tsq[gi]])
                    S.op("pool", lambda e: e.tensor_tensor(out=ob[gi][:], in0=sqb[gi][:], in1=rg[:, js, :], op=ALU.mult), reads=[tsq[gi], trg], writes=[tob[gi]])
                    pb, tpb = self.bbanks[gi]
                    for jj in range(NG):
                        S.op("pe", lambda e: e.transpose(pb[:, jj * 128:(jj + 1) * 128], ob[gi][:, jj, :], self.idb[:]),
                             reads=[tob[gi], self.t_c], writes=[tpb])
                    S.op("act", lambda e: e.activation(out=mT[gi][:, :NG * 128], in_=pb[:, :NG * 128], func=AF.Copy, scale=gcol[:, h:h + 1]),
                         reads=[tpb, tcst], writes=[tmT[gi]])
                    S.dma("pool", self.mergedT[3072 + h * 128:3072 + (h + 1) * 128, j0 * 128:(j0 + NG) * 128], mT[gi][:, :NG * 128],
                          reads=[tmT[gi]], writes=[Tk()])
        S.barrier()

    def dma_chunks(self, q, out3, in3, n, tk, step=8, reads=()):
        for a in range(0, n, step):
            b = min(n, a + step)
            self.S.dma(q, out3[:, a:b, :], in3[:, a:b, :], reads=list(reads), writes=[tk])

    def mix_hgrn(self, l):
        S = self.S
        T = self.T
        NCH = T // 128
        SEG = min(2048, T)
        NSEG = T // SEG
        NT = SEG // 128
        BK = 64
        NBPT = 128 // BK
        NBLK = SEG // BK
        PW = min(512, T)
        b0 = l * 200
        with contextlib.ExitStack() as st:
            mbd = self.sb(st, "hg_mbd", [128, 2, 128], F32)
            oml = self.sb(st, "hg_oml", [128, 8], F32)
            gcol = self.sb(st, "hg_gcol", [128, 8], F32)
            one = self.sb(st, "hg_one", [128, 1], F32)
            tcst = Tk()
            S.dma("sp", mbd[:], self.mbd_in, writes=[tcst])
            S.op("dve", lambda e: e.memset(one[:], 1.0), writes=[tcst])
            if l == 0:
                S.op("dve", lambda e: e.memset(oml[:], 1.0), writes=[tcst])
            else:
                S.op("dve", lambda e: e.tensor_tensor(out=oml[:], in0=self.pp[:, b0 + 176:b0 + 184], in1=self.pp[:, b0 + 184:b0 + 192], op=ALU.subtract),
                     reads=[self.t_pp], writes=[tcst])
                S.op("act", lambda e: e.activation(out=oml[:], in_=oml[:], func=AF.Sigmoid), reads=[tcst], writes=[tcst])
            S.op("dve", lambda e: e.tensor_tensor(out=gcol[:], in0=self.pp[:, b0 + 160:b0 + 168], in1=self.pp[:, b0 + 128 + 8:b0 + 128 + 16], op=ALU.mult),
                 reads=[self.t_pp], writes=[tcst])
            oT = self.sb(st, "hg_oT", [128, T], F32)
            vf = self.sb(st, "hg_vf", [128, T], F32)
            vb = self.sb(st, "hg_vb", [128, NCH, 128], BF16)
            toT, tvb, thg = Tk(), Tk(), Tk()
            z = self.sb(st, "hg_z", [128, SEG], F32)
            q = self.sb(st, "hg_q", [128, SEG], F32)
            kk = self.sb(st, "hg_kk", [128, SEG], F32)
            ba = self.sb(st, "hg_ba", [128, SEG], F32)
            bb = self.sb(st, "hg_bb", [128, SEG], F32)
            eb = self.sb(st, "hg_eb", [128, SEG], F32)
            enb = self.sb(st, "hg_enb", [128, SEG], F32)
            Qt = self.sb(st, "hg_Qt", [128, SEG], F32)
            Kt = self.sb(st, "hg_Kt", [128, SEG], F32)
            KpT = self.sb(st, "hg_KpT", [128, SEG], BF16)
            Kp = self.sb(st, "hg_Kp", [128, NT, 128], BF16)
            Kpz = self.sb(st, "hg_Kpz", [128, NT, 128], BF16)
            dec = self.sb(st, "hg_dec", [128, NBLK], F32)
            tz, tq, tkk, tba, tbb, teb, tenb, tQt, tKt, tKpT, tKp, tdec = (Tk() for _ in range(12))
            Sst = self.sb(st, "hg_S", [128, 128], F32)
            tS = Tk()
            PT = [self.sb(st, "hg_PT%d" % i, [128, 128], BF16) for i in range(2)]
            tPT = [Tk(), Tk()]
            sqp = [self.sb(st, "hg_sq%d" % i, [128, PW], BF16) for i in range(2)]
            rsp = [self.sb(st, "hg_rs%d" % i, [128, PW], F32) for i in range(2)]
            t1p = [self.sb(st, "hg_t1%d" % i, [128, PW], F32) for i in range(2)]
            mbp = [self.sb(st, "hg_mb%d" % i, [128, PW], BF16) for i in range(2)]
            tsqp, trsp, tt1p, tmbp = ([Tk(), Tk()] for _ in range(4))
            bA = [self.banks[0], self.banks[1]]
            bO = [self.banks[2], self.banks[3]]
            bR = [self.banks[4], self.banks[5]]
            nR = 0
            v3 = lambda t_: t_[:].rearrange("p (b c) -> p b c", c=BK)
            for h in range(8):
                self.dma_chunks("sp", vf[:].rearrange("p (n d) -> p n d", d=128),
                                self.Y_hi[:, h * 128:(h + 1) * 128].rearrange("(n p) d -> p n d", p=128), NCH, thg)
                S.op("pool", lambda e: e.tensor_copy(out=vb[:], in_=vf[:].rearrange("p (n d) -> p n d", d=128)), reads=[thg], writes=[tvb])
                for d in range(2):
                    S.op("pool", lambda e: e.memset(Sst[:], 0.0), writes=[tS])
                    segs = range(NSEG) if d == 0 else range(NSEG - 1, -1, -1)
                    for sg_ in segs:
                        scol = slice(sg_ * SEG, (sg_ + 1) * SEG)
                        zr = 1024 * (1 + d) + h * 128
                        S.dma("sp", z[:], self.YT[zr:zr + 128, scol], writes=[tz])
                        S.dma("sp", q[:], self.YT[h * 128:(h + 1) * 128, scol], writes=[tq])
                        S.op("act", lambda e: e.activation(out=z[:], in_=z[:], func=AF.Sigmoid, scale=-1.0), reads=[tz], writes=[tz])
                        S.op("dve", lambda e: e.tensor_scalar(out=kk[:], in0=z[:], scalar1=oml[:, h:h + 1], scalar2=1.0, op0=ALU.mult, op1=ALU.mult),
                             reads=[tz, tcst], writes=[tkk])
                        S.op("act", lambda e: e.activation(out=ba[:], in_=kk[:], func=AF.Ln, scale=-1.0, bias=one[:, 0:1]),
                             reads=[tkk, tcst], writes=[tba])
                        cur, nxt, tcur, tnxt = ba, bb, tba, tbb
                        for sh in [1 << k_ for k_ in range(6) if (1 << k_) < BK]:
                            c3, n3 = v3(cur), v3(nxt)
                            if d == 0:
                                S.op("dve", lambda e: e.tensor_tensor(out=n3[:, :, sh:], in0=c3[:, :, sh:], in1=c3[:, :, :BK - sh], op=ALU.add),
                                     reads=[tcur], writes=[tnxt])
                                S.op("dve", lambda e: e.tensor_copy(out=n3[:, :, :sh], in_=c3[:, :, :sh]), reads=[tcur], writes=[tnxt])
                            else:
                                S.op("dve", lambda e: e.tensor_tensor(out=n3[:, :, :BK - sh], in0=c3[:, :, :BK - sh], in1=c3[:, :, sh:], op=ALU.add),
                                     reads=[tcur], writes=[tnxt])
                                S.op("dve", lambda e: e.tensor_copy(out=n3[:, :, BK - sh:], in_=c3[:, :, BK - sh:]), reads=[tcur], writes=[tnxt])
                            cur, nxt, tcur, tnxt = nxt, cur, tnxt, tcur
                        S.op("dve", lambda e: e.tensor_scalar(out=cur[:], in0=cur[:], scalar1=-80.0, scalar2=0.0, op0=ALU.max, op1=ALU.add),
                             reads=[tcur], writes=[tcur])
                        S.op("act", lambda e: e.activation(out=eb[:], in_=cur[:], func=AF.Exp), reads=[tcur], writes=[teb])
                        S.op("act", lambda e: e.activation(out=enb[:], in_=cur[:], func=AF.Exp, scale=-1.0), reads=[tcur], writes=[tenb])
                        S.op("dve", lambda e: e.tensor_copy(out=dec[:], in_=v3(eb)[:, :, (BK - 1 if d == 0 else 0)]), reads=[teb], writes=[tdec])
                        S.op("act", lambda e: e.activation(out=q[:], in_=q[:], func=AF.Silu), reads=[tq], writes=[tq])
                        S.op("dve", lambda e: e.scalar_tensor_tensor(out=Qt[:], in0=q[:], scalar=128.0 ** -0.5, in1=eb[:], op0=ALU.mult, op1=ALU.mult),
                             reads=[tq, teb], writes=[tQt])
                        S.op("pool", lambda e: e.tensor_tensor(out=Kt[:], in0=kk[:], in1=enb[:], op=ALU.mult), reads=[tkk, tenb], writes=[tKt])
                        S.op("dve", lambda e: e.tensor_tensor(out=v3(KpT), in0=v3(Kt), in1=dec[:].unsqueeze(2).to_broadcast([128, NBLK, BK]), op=ALU.mult),
                             reads=[tKt, tdec], writes=[tKpT])
                        for j0 in range(0, NT, 8):
                            pb, tpb = self.bbanks[(j0 // 8) % 2]
                            n8 = min(8, NT - j0)
                            for jj in range(n8):
                                jt = j0 + jj
                                S.op("pe", lambda e: e.transpose(pb[:, jj * 128:(jj + 1) * 128], KpT[:, jt * 128:(jt + 1) * 128], self.idb[:]),
                                     reads=[tKpT, self.t_c], writes=[tpb])
                            S.op("act", lambda e: e.activation(out=Kp[:, j0:j0 + n8, :], in_=pb[:, :n8 * 128].rearrange("p (n d) -> p n d", d=128), func=AF.Copy),
                                 reads=[tpb], writes=[tKp])
                        tiles = range(NT) if d == 0 else range(NT - 1, -1, -1)
                        for jt in tiles:
                            jg = sg_ * NT + jt
                            cols = slice(jt * 128, (jt + 1) * 128)
                            gcols = slice(jg * 128, (jg + 1) * 128)
                            ba_, tba_ = bA[jt % 2]
                            bo_, tbo_ = bO[jt % 2]
                            S.op("pe", lambda e: e.matmul(ba_[:, 0:128], Kt[:, cols], Qt[:, cols], start=True, stop=True), reads=[tKt, tQt], writes=[tba_])
                            p = jt % 2
                            S.op("dve", lambda e: e.tensor_tensor(out=PT[p][:], in0=ba_[:, 0:128], in1=mbd[:, d, :], op=ALU.mult),
                                 reads=[tba_, tcst], writes=[tPT[p]])
                            S.op("pe", lambda e: e.matmul(bo_[:, 0:128], vb[:, jg, :], PT[p][:], start=True, stop=True), reads=[tvb, tPT[p]], writes=[tbo_])
                            blks = range(NBPT) if d == 0 else range(NBPT - 1, -1, -1)
                            for ib in blks:
                                c32 = slice(jt * 128 + ib * BK, jt * 128 + (ib + 1) * BK)
                                S.op("pe", lambda e: e.matmul(bo_[:, 128 + ib * BK:128 + (ib + 1) * BK], Sst[:], Qt[:, c32], start=True, stop=True),
                                     reads=[tS, tQt], writes=[tbo_])
                                br_, tbr_ = bR[nR % 2]
                                nR += 1
                                S.op("pe", lambda e: e.matmul(br_[:, 0:128], Kp[ib * BK:(ib + 1) * BK, jt, :], vb[ib * BK:(ib + 1) * BK, jg, :], start=True, stop=True),
                                     reads=[tKp, tvb], writes=[tbr_])
                                blk = jt * NBPT + ib
                                S.op("dve", lambda e: e.scalar_tensor_tensor(out=Sst[:], in0=Sst[:], scalar=dec[:, blk:blk + 1], in1=br_[:, 0:128],
                                                                            op0=ALU.mult, op1=ALU.add), reads=[tbr_, tdec], writes=[tS])
                            if d == 0:
                                S.op("act", lambda e: e.activation(out=oT[:, gcols], in_=bo_[:, 0:128], func=AF.Copy), reads=[tbo_], writes=[toT])
                            else:
                                S.op("dve", lambda e: e.tensor_tensor(out=oT[:, gcols], in0=bo_[:, 0:128], in1=oT[:, gcols], op=ALU.add), reads=[tbo_], writes=[toT])
                            S.op("dve", lambda e: e.tensor_tensor(out=oT[:, gcols], in0=bo_[:, 128:256], in1=oT[:, gcols], op=ALU.add), reads=[tbo_], writes=[toT])
                S.dma("sp", vf[:], self.YT[3072 + h * 128:3072 + (h + 1) * 128, :], reads=[tvb], writes=[thg])
                S.op("act", lambda e: e.activation(out=vf[:], in_=vf[:], func=AF.Silu), reads=[thg], writes=[thg])
                for pc in range(T // PW):
                    i = pc % 2
                    pcs = slice(pc * PW, (pc + 1) * PW)
                    bk, tbk = bA[pc % 2]
                    S.op("pool", lambda e: e.tensor_tensor(out=sqp[i][:], in0=oT[:, pcs], in1=oT[:, pcs], op=ALU.mult), reads=[toT], writes=[tsqp[i]])
                    S.op("pe", lambda e: e.matmul(bk[:, :PW], self.onesb[:], sqp[i][:], start=True, stop=True), reads=[tsqp[i], self.t_c], writes=[tbk])
                    S.op("act", lambda e: e.activation(out=rsp[i][:], in_=bk[:, :PW], func=AF.Sqrt, bias=self.epsln[:, 1:2], scale=1.0 / 128),
                         reads=[tbk, self.t_c], writes=[trsp[i]])
                    S.op("dve", lambda e: e.reciprocal(out=rsp[i][:], in_=rsp[i][:]), reads=[trsp[i]], writes=[trsp[i]])
                    S.op("dve", lambda e: e.tensor_tensor(out=t1p[i][:], in0=oT[:, pcs], in1=rsp[i][:], op=ALU.mult), reads=[toT, trsp[i]], writes=[tt1p[i]])
                    S.op("pool", lambda e: e.tensor_tensor(out=t1p[i][:], in0=t1p[i][:], in1=vf[:, pcs], op=ALU.mult), reads=[thg], writes=[tt1p[i]])
                    S.op("act", lambda e: e.activation(out=mbp[i][:], in_=t1p[i][:], func=AF.Copy, scale=gcol[:, h:h + 1]), reads=[tt1p[i], tcst], writes=[tmbp[i]])
                    S.dma("pool", self.mergedT[1024 + h * 128:1024 + (h + 1) * 128, pcs], mbp[i][:], reads=[tmbp[i]], writes=[Tk()])
        S.barrier()


_CACHE = {}


def _consts(T):
    f64 = np.float64
    n_rows = T // 64
    rows = np.repeat(np.arange(n_rows, dtype=np.float32), 64)
    cols = np.tile(np.arange(64, dtype=np.float32), n_rows)
    inv_freq = (np.float32(10000.0) ** (-np.arange(32, dtype=np.float32) / np.float32(32))).astype(np.float32)
    ang = np.concatenate([rows[:, None] * inv_freq, cols[:, None] * inv_freq], axis=-1).astype(np.float32)
    cos16 = np.ascontiguousarray(np.tile(np.cos(ang).astype(np.float32), (1, 16)))
    sin16 = np.ascontiguousarray(np.tile(np.sin(ang).astype(np.float32), (1, 16)))
    lg = np.log1p(-np.exp2(-5.0 - np.arange(8, dtype=f64)))
    pos = np.arange(128, dtype=f64)
    sc = 128.0 ** -0.5
    rdec = np.zeros((128, 32), f64)
    for h in range(8):
        rdec[:, h] = np.exp((127.0 - pos) * lg[h]) * sc
        rdec[:, 8 + h] = np.exp(pos * lg[h]) * sc
        rdec[:, 16 + h] = np.exp((pos + 1.0) * lg[h])
        rdec[:, 24 + h] = np.exp((128.0 - pos) * lg[h])
    dsym = np.zeros((128, 8, 128), f64)
    ad = np.abs(pos[:, None] - pos[None, :])
    for h in range(8):
        dsym[:, h, :] = np.exp(ad * lg[h]) * sc
    blk = np.arange(128) // 64
    same = blk[:, None] == blk[None, :]
    s_i = np.arange(128)[:, None]
    t_i = np.arange(128)[None, :]
    mbd = np.zeros((128, 2, 128), np.float32)
    mbd[:, 0, :] = (same & (s_i <= t_i))
    mbd[:, 1, :] = (same & (s_i >= t_i))
    sel = np.zeros((8, 8, 128), np.float32)
    for e in range(8):
        sel[e, e, :] = 1.0
    return dict(cos16=cos16, sin16=sin16, rdec=rdec.astype(np.float32), dsymT=dsym.astype(np.float32), maskBD=mbd, sel=sel)


def _params(inp):
    f = lambda a: np.asarray(a, dtype=np.float32)
    pp = np.zeros((128, 400), np.float32)
    rp = np.zeros((2, 128, 4352), np.float32)
    for l in range(2):
        b = l * 200
        pp[:, b + 0:b + 32] = f(inp["ln1_g"])[l].reshape(32, 128).T
        pp[:, b + 32:b + 64] = f(inp["ln1_b"])[l].reshape(32, 128).T
        pp[:, b + 64:b + 96] = f(inp["ln2_g"])[l].reshape(32, 128).T
        pp[:, b + 96:b + 128] = f(inp["ln2_b"])[l].reshape(32, 128).T
        pp[:, b + 128:b + 160] = f(inp["merge_scale"])[l].reshape(32, 128).T
        pp[:, b + 160:b + 168] = f(inp["hgrn_out_norm"])[l].reshape(8, 128).T
        pp[:, b + 168:b + 176] = f(inp["ret_out_norm"])[l].reshape(8, 128).T
        pp[:, b + 176:b + 184] = f(inp["hgrn_lower_bound"])[0].reshape(8, 128).T
        pp[:, b + 184:b + 192] = f(inp["hgrn_lower_bound"])[1].reshape(8, 128).T
        row = np.concatenate([np.tile(f(inp["attn_q_norm"])[l], 8), np.tile(f(inp["attn_k_norm"])[l], 2),
                              f(inp["gmlp_v_norm_g"])[l], f(inp["gmlp_v_norm_b"])[l], f(inp["merge_scale"])[l][2048:3072]])
        rp[l] = np.broadcast_to(row[None, :], (128, 4352))
    wsT = np.ascontiguousarray(f(inp["gmlp_w_s"]).transpose(0, 3, 1, 2))
    bsT = np.ascontiguousarray(f(inp["gmlp_b_s"]).transpose(0, 2, 1))
    router = np.ascontiguousarray(f(inp["moe_router"])[0].reshape(32, 128, 8).transpose(1, 0, 2))
    return dict(
        pp=pp, rp=rp, wsT=wsT, bsT=bsT, router=router,
        w_in=f(inp["w_in"]), w_o=f(inp["w_o"]),
        ffn_g=f(inp["ffn_w_gate"])[0], ffn_u=f(inp["ffn_w_up"])[0], ffn_d=f(inp["ffn_w_down"])[0],
        moe_g=f(inp["moe_w_gate"])[0], moe_u=f(inp["moe_w_up"])[0],
        moe_d=f(inp["moe_w_down"])[0].reshape(NE * DFE, D),
    )


def kernel(**inputs):
    T = 8192
    if T not in _CACHE:
        _CACHE[T] = Prog(T)
    prog = _CACHE[T]
    shared = _params(inputs)
    shared.update(_consts(T))
    xp = np.asarray(inputs["x_prompt"], dtype=np.float32)
    xs = np.asarray(inputs["x_sample"], dtype=np.float32)
    seqs = [xp[0], xp[1], xs[0]]
    in_maps = []
    for c in range(NCORES):
        m = dict(shared)
        m["xT"] = np.ascontiguousarray(seqs[c].T)
        in_maps.append(m)
    res = run_bass_kernel_spmd(prog.nc, in_maps, core_ids=list(range(NCORES)))
    outs = [np.ascontiguousarray(np.asarray(res.results[c]["yT"], dtype=np.float32).T) for c in range(NCORES)]
    y_prompt = np.stack([outs[0], outs[1]], axis=0)
    y_sample = outs[2][None]
    return (y_prompt, y_sample)
```

```python
import contextlib
import numpy as np
import concourse.bass as bass
import concourse.mybir as mybir
from concourse.bass_utils import run_bass_kernel_spmd

F32 = mybir.dt.float32
BF16 = mybir.dt.bfloat16
AF = mybir.ActivationFunctionType
ALU = mybir.AluOpType
AX = mybir.AxisListType

D = 4096
DIN = 12800
DFF = 5632
NE = 8
DFE = 1024
HD = 128
OFF = dict(aq=0, ak=1024, av=1280, hq=1536, hff=2560, hfb=3584, hi=4608, hg=5632,
           gu=6656, gv=7680, rq=8704, rk=9728, rv=10752, rg=11776)
ALPHA = 4.0 ** 0.25
LN_EPS = 1e-5
NORM_EPS = 1e-6
NCORES = 3


class Tk:
    __slots__ = ("w", "r")

    def __init__(self):
        self.w = None
        self.r = {}


class Sched:
    NDS = 16

    def __init__(self, nc, es):
        self.nc = nc
        self.eng = {"pe": nc.tensor, "act": nc.scalar, "dve": nc.vector, "pool": nc.gpsimd, "sp": nc.sync}
        self.sem = {k: es.enter_context(nc.semaphore("s_" + k)) for k in self.eng}
        self.cnt = {k: 0 for k in self.eng}
        self.seen = {k: {} for k in self.eng}
        self.dsem = {}
        self.dval = {}
        self.dnext = {}
        for q in ("sp", "pool", "act"):
            self.dsem[q] = [es.enter_context(nc.semaphore("d_%s%d" % (q, i))) for i in range(self.NDS)]
            self.dval[q] = [0] * self.NDS
            self.dnext[q] = 0
        self.ninst = 0

    def _semobj(self, k):
        if isinstance(k, tuple):
            return self.dsem[k[0]][k[1]]
        return self.sem[k]

    def _wait(self, e, deps):
        eng = self.eng[e]
        seen = self.seen[e]
        for k, v in deps.items():
            if k == e and e in ("pe", "sp"):
                continue
            if seen.get(k, 0) >= v:
                continue
            eng.wait_ge(self._semobj(k), v)
            seen[k] = v

    @staticmethod
    def _deps(reads, writes):
        deps = {}
        for t in reads:
            if t.w is not None:
                k, v = t.w
                if deps.get(k, 0) < v:
                    deps[k] = v
        for t in writes:
            if t.w is not None:
                k, v = t.w
                if deps.get(k, 0) < v:
                    deps[k] = v
            for k, v in t.r.items():
                if deps.get(k, 0) < v:
                    deps[k] = v
        return deps

    @staticmethod
    def _mark(tok, reads, writes):
        k, v = tok
        for t in reads:
            t.r[k] = v
        for t in writes:
            t.w = tok
            t.r = {}

    def op(self, e, fn, reads=(), writes=()):
        self._wait(e, self._deps(reads, writes))
        ins = fn(self.eng[e])
        self.cnt[e] += 1
        ins.then_inc(self.sem[e], 1)
        self._mark((e, self.cnt[e]), reads, writes)
        self.ninst += 1
        return ins

    def dma(self, q, out, in_, reads=(), writes=()):
        deps = self._deps(reads, writes)
        i = self.dnext[q]
        self.dnext[q] = (i + 1) % self.NDS
        key = (q, i)
        if self.dval[q][i] > 0 and deps.get(key, 0) < self.dval[q][i]:
            deps[key] = self.dval[q][i]
        self._wait(q, deps)
        ins = self.eng[q].dma_start(out=out, in_=in_)
        self.dval[q][i] += 16
        ins.then_inc(self.dsem[q][i], 16)
        self._mark((key, self.dval[q][i]), reads, writes)
        self.ninst += 1
        return ins

    def barrier(self):
        deps = {k: self.cnt[k] for k in self.eng if self.cnt[k] > 0}
        for q in self.dsem:
            for i in range(self.NDS):
                if self.dval[q][i] > 0:
                    deps[(q, i)] = self.dval[q][i]
        for e in self.eng:
            self._wait(e, deps)


class Prog:
    def __init__(self, T, dbg=(), layers=2, phases=None):
        self.T = T
        self.dbg = set(dbg)
        self.layers = layers
        self.phases = phases
        self.nc = nc = bass.Bass("TRN2", target_bir_lowering=False)
        self.es = es = contextlib.ExitStack()
        self.S = Sched(nc, es)
        self.rr = 0
        self.build()
        es.close()

    def din(self, name, shape, dt=F32):
        return self.nc.dram_tensor(name, list(shape), dt, kind="ExternalInput").ap()

    def dscr(self, name, shape, dt, out=False):
        kind = "ExternalOutput" if (out or name in self.dbg) else "Internal"
        return self.nc.dram_tensor(name, list(shape), dt, kind=kind).ap()

    def sb(self, st, name, shape, dt):
        self.uid = getattr(self, "uid", 0) + 1
        return st.enter_context(self.nc.sbuf_tensor("%s_%d" % (name, self.uid), list(shape), dt))

    def any2(self):
        self.rr += 1
        return ("act", "dve")[self.rr % 2]

    def any3(self):
        self.rr += 1
        return ("act", "dve", "pool")[self.rr % 3]

    @staticmethod
    def copy(e, eng, out, in_):
        if e == "act":
            return eng.activation(out=out, in_=in_, func=AF.Copy)
        return eng.tensor_copy(out=out, in_=in_)

    def convert(self, src, R, C, dst_fn, piece=2048):
        S = self.S
        with contextlib.ExitStack() as st:
            NB = 3
            stg = [self.sb(st, "cv_f%d" % i, [128, piece], F32) for i in range(NB)]
            stb = [self.sb(st, "cv_b%d" % i, [128, piece], BF16) for i in range(NB)]
            tf = [Tk() for _ in range(NB)]
            tb = [Tk() for _ in range(NB)]
            i = 0
            for rt in range(R // 128):
                for c0 in range(0, C, piece):
                    n = min(piece, C - c0)
                    b = i % NB
                    i += 1
                    S.dma("sp", stg[b][:, :n], src[rt * 128:(rt + 1) * 128, c0:c0 + n], writes=[tf[b]])
                    e = self.any3()
                    S.op(e, lambda eng, b=b, n=n, e=e: self.copy(e, eng, stb[b][:, :n], stg[b][:, :n]),
                         reads=[tf[b]], writes=[tb[b]])
                    dst, view = dst_fn(rt, c0, n)
                    S.dma("pool", dst, view(stb[b][:, :n]), reads=[tb[b]], writes=[Tk()])
        S.barrier()

    def conv_blocked(self, src, K, N, Wb, CB, off=0, w=None, cb_base=0):
        w = w or CB

        def dst_fn(rt, c0, n):
            nb = n // w
            cb0 = cb_base + c0 // w
            dst = Wb[cb0:cb0 + nb, :, rt, off:off + w].rearrange("cb p c -> p cb c")
            return dst, (lambda v: v.rearrange("p (cb c) -> p cb c", c=w))
        piece = 2048 if N % 2048 == 0 or N > 2048 else N
        self.convert(src, K, N, dst_fn, piece=piece)

    def conv_plain(self, src, R, C, dst):
        def dst_fn(rt, c0, n):
            return dst[rt * 128:(rt + 1) * 128, c0:c0 + n], (lambda v: v)
        self.convert(src, R, C, dst_fn, piece=min(2048, C))

    def gemm(self, actT, KC, Wb, NCB, CB, form, epi, TT, banks, name):
        S = self.S
        T = self.T
        TT = min(TT, T)
        TS = min(512, TT)
        nb = len(banks)
        bi = 0
        with contextlib.ExitStack() as st:
            act = self.sb(st, name + "_act", [128, KC, TT], BF16)
            G = 8
            ngr = (KC + G - 1) // G
            t_act = [Tk() for _ in range(ngr)]
            wbuf = [self.sb(st, name + "_w%d" % i, [128, KC, CB], BF16) for i in range(2)]
            t_w = [Tk(), Tk()]
            aT = actT.rearrange("(kc p) t -> p kc t", p=128)
            wi = 0
            for st_i in range(T // TT):
                for g in range(ngr):
                    k0, k1 = g * G, min(KC, (g + 1) * G)
                    S.dma("sp", act[:, k0:k1, :], aT[:, k0:k1, st_i * TT:(st_i + 1) * TT], writes=[t_act[g]])
                for cb in range(NCB):
                    wb = wi % 2
                    wi += 1
                    S.dma("sp", wbuf[wb][:], Wb[cb], writes=[t_w[wb]])
                    if form == "A":
                        for ts in range(TT // 128):
                            bank, tb = banks[bi % nb]
                            bi += 1
                            for kc in range(KC):
                                S.op("pe", lambda e, kc=kc, ts=ts, wb=wb, bank=bank: e.matmul(
                                    bank[:, :CB], act[:, kc, ts * 128:(ts + 1) * 128], wbuf[wb][:, kc, :],
                                    start=(kc == 0), stop=(kc == KC - 1)),
                                    reads=[t_act[kc // G], t_w[wb]], writes=[tb])
                            epi((bank, tb), st_i * TT + ts * 128, cb)
                    else:
                        for ts in range(TT // TS):
                            subs = []
                            for sub in range(CB // 128):
                                bank, tb = banks[bi % nb]
                                bi += 1
                                for kc in range(KC):
                                    S.op("pe", lambda e, kc=kc, ts=ts, wb=wb, bank=bank, sub=sub: e.matmul(
                                        bank[:, :TS], wbuf[wb][:, kc, sub * 128:(sub + 1) * 128],
                                        act[:, kc, ts * TS:(ts + 1) * TS],
                                        start=(kc == 0), stop=(kc == KC - 1)),
                                        reads=[t_act[kc // G], t_w[wb]], writes=[tb])
                                subs.append((bank, tb))
                            epi(subs, st_i * TT + ts * TS, cb)
        S.barrier()


    def build(self):
        nc, S, T = self.nc, self.S, self.T
        es = self.es
        L = self.layers
        ph = self.phases
        NPP, NRP = 400, 4352
        self.xT = self.din("xT", [D, T])
        self.w_in = self.din("w_in", [2, D, DIN])
        self.w_o = self.din("w_o", [2, D, D])
        self.ffn_g = self.din("ffn_g", [D, DFF])
        self.ffn_u = self.din("ffn_u", [D, DFF])
        self.ffn_d = self.din("ffn_d", [DFF, D])
        self.moe_g = self.din("moe_g", [NE, D, DFE])
        self.moe_u = self.din("moe_u", [NE, D, DFE])
        self.moe_d = self.din("moe_d", [NE * DFE, D])
        self.router = self.din("router", [128, 32, NE])
        self.pp_in = self.din("pp", [128, NPP])
        self.rp_in = self.din("rp", [2, 128, NRP])
        self.wsT_in = self.din("wsT", [2, 128, 8, 128])
        self.bsT_in = self.din("bsT", [2, 128, 8])
        self.cos_in = self.din("cos16", [T, 1024])
        self.sin_in = self.din("sin16", [T, 1024])
        self.dsym_in = self.din("dsymT", [128, 8, 128])
        self.rdec_in = self.din("rdec", [128, 32])
        self.mbd_in = self.din("maskBD", [128, 2, 128])
        self.sel_in = self.din("sel", [8, 8, 128])
        self.yT = self.dscr("yT", [D, T], F32, out=True)
        self.xTb = self.dscr("xTb", [D, T], BF16)
        self.winA = [self.dscr("winA%d" % l, [17, 128, 32, 512], BF16) for l in range(2)]
        self.winB = [self.dscr("winB%d" % l, [16, 128, 32, 256], BF16) for l in range(2)]
        self.wob = [self.dscr("wob%d" % l, [16, 128, 32, 256], BF16) for l in range(2)]
        self.wgub = self.dscr("wgub", [44, 128, 32, 256], BF16)
        self.wdb = self.dscr("wdb", [16, 128, 44, 256], BF16)
        self.wmgub = self.dscr("wmgub", [64, 128, 32, 256], BF16)
        self.wmdb = self.dscr("wmdb", [16, 128, 64, 256], BF16)
        self.Y_a = self.dscr("Y_a", [T, 1536], F32)
        self.Y_hi = self.dscr("Y_hi", [T, 1024], F32)
        self.Y_g = self.dscr("Y_g", [T, 2048], F32)
        self.Y_r = self.dscr("Y_r", [T, 4096], F32)
        self.YT = self.dscr("YT", [D, T], F32)
        self.aqkT = self.dscr("aqkT", [10, 128, T], BF16)
        self.rqkT = self.dscr("rqkT", [16, 128, T], BF16)
        self.RK = self.dscr("RK", [3, T, 1024], BF16)
        self.mergedT = self.dscr("mergedT", [D, T], BF16)
        self.zT = self.dscr("zT", [D, T], F32)
        self.x1T = self.dscr("x1T", [D, T], F32)
        self.x1Tb = self.dscr("x1Tb", [D, T], BF16)
        self.x2T = self.dscr("x2T", [D, T], F32)
        self.x2Tb = self.dscr("x2Tb", [D, T], BF16)
        self.hT = self.dscr("hT", [NE * DFE, T], BF16)
        self.Grep = self.dscr("Grep", [NE, 128, T], F32)
        self.banks = []
        for i in range(6):
            p = es.enter_context(nc.psum_tensor("ps%d" % i, [128, 512], F32))
            self.banks.append((p, Tk()))
        self.bbanks = []
        for i in range(2):
            p = es.enter_context(nc.psum_tensor("pb%d" % i, [128, 1024], BF16))
            self.bbanks.append((p, Tk()))
        self.pp = self.sb(es, "pp", [128, NPP], F32)
        self.t_pp = Tk()
        self.idf = self.sb(es, "idf", [128, 128], F32)
        self.idb = self.sb(es, "idb", [128, 128], BF16)
        self.onesb = self.sb(es, "onesb", [128, 128], BF16)
        self.epsln = self.sb(es, "epsln", [128, 2], F32)
        self.t_c = Tk()
        S.dma("sp", self.pp[:], self.pp_in, writes=[self.t_pp])
        S.op("pool", lambda e: e.memset(self.idf[:], 0.0), writes=[self.t_c])
        S.op("pool", lambda e: e.affine_select(out=self.idf[:], in_=self.idf[:], pattern=[[-1, 128]],
                                               compare_op=ALU.not_equal, fill=1.0, base=0, channel_multiplier=1),
             writes=[self.t_c])
        S.op("pool", lambda e: e.tensor_copy(out=self.idb[:], in_=self.idf[:]), writes=[self.t_c])
        S.op("pool", lambda e: e.memset(self.onesb[:], 1.0), writes=[self.t_c])
        S.op("pool", lambda e: e.memset(self.epsln[:, 0:1], LN_EPS), writes=[self.t_c])
        S.op("pool", lambda e: e.memset(self.epsln[:, 1:2], NORM_EPS), writes=[self.t_c])
        S.barrier()

        def on(p):
            return ph is None or p in ph
        if on("conv"):
            self.conv_plain(self.xT, D, T, self.xTb)
            for l in range(L):
                w = self.w_in[l]
                self.conv_blocked(w[:, 0:1536], D, 1536, self.winA[l], 512, cb_base=0)
                self.conv_blocked(w[:, 4608:5632], D, 1024, self.winA[l], 512, cb_base=3)
                self.conv_blocked(w[:, 6656:12800], D, 6144, self.winA[l], 512, cb_base=5)
                self.conv_blocked(w[:, 1536:4608], D, 3072, self.winB[l], 256, cb_base=0)
                self.conv_blocked(w[:, 5632:6656], D, 1024, self.winB[l], 256, cb_base=12)
                self.conv_blocked(self.w_o[l], D, D, self.wob[l], 256)
            if on("ffn"):
                self.conv_blocked(self.ffn_g, D, DFF, self.wgub, 256, off=0, w=128)
                self.conv_blocked(self.ffn_u, D, DFF, self.wgub, 256, off=128, w=128)
                self.conv_blocked(self.ffn_d, DFF, D, self.wdb, 256)
            if L > 1 and on("moe"):
                for e_ in range(NE):
                    self.conv_blocked(self.moe_g[e_], D, DFE, self.wmgub, 256, off=0, w=128, cb_base=e_ * 8)
                    self.conv_blocked(self.moe_u[e_], D, DFE, self.wmgub, 256, off=128, w=128, cb_base=e_ * 8)
                self.conv_blocked(self.moe_d, NE * DFE, D, self.wmdb, 256)
        xF, xB = self.xT, self.xTb
        for l in range(L):
            if on("inproj"):
                self.in_proj(l, xB)
            if on("gmlp"):
                self.mix_gmlp(l)
            if on("attn"):
                self.mix_attn(l)
            if on("ret"):
                self.mix_ret(l)
            if on("hgrn"):
                self.mix_hgrn(l)
            if on("wo"):
                self.resid_gemm(self.mergedT, 32, self.wob[l], xF, 1024, "wo")
                self.ln_pass(self.zT, l * 200 + 0, l * 200 + 32, self.x1T, self.x1Tb)
            if l == 0:
                if on("ffn"):
                    self.ffn_up(self.x1Tb, self.wgub, DFF // 128, None)
                    self.resid_gemm(self.hT, DFF // 128, self.wdb, self.x1T, 1024, "fd")
            else:
                if on("moe"):
                    self.router_pass(self.x1T)
                    self.ffn_up(self.x1Tb, self.wmgub, NE * DFE // 128, self.Grep)
                    self.resid_gemm(self.hT, NE * DFE // 128, self.wmdb, self.x1T, 512, "md")
            if on("ffn") or on("moe"):
                last = (l == L - 1)
                self.ln_pass(self.zT, l * 200 + 64, l * 200 + 96, self.yT if last else self.x2T,
                             None if last else self.x2Tb)
            xF, xB = self.x2T, self.x2Tb
        S.barrier()

    def in_proj(self, l, actT):
        S = self.S
        colA = ([(self.Y_a, 512 * i) for i in range(3)] + [(self.Y_hi, 512 * i) for i in range(2)]
                + [(self.Y_g, 512 * i) for i in range(4)] + [(self.Y_r, 512 * i) for i in range(8)])
        with contextlib.ExitStack() as st:
            NSTG = 4
            stg = [self.sb(st, "p1_s%d" % i, [128, 512], F32) for i in range(NSTG)]
            ts = [Tk() for _ in range(NSTG)]
            cnt = [0]

            def epi(bt, tok0, cb):
                bank, tb = bt
                i = cnt[0] % NSTG
                cnt[0] += 1
                e = self.any2()
                S.op(e, lambda eng: self.copy(e, eng, stg[i][:], bank[:]), reads=[tb], writes=[ts[i]])
                yt_, yc_ = colA[cb]
                S.dma("pool", yt_[tok0:tok0 + 128, yc_:yc_ + 512], stg[i][:], reads=[ts[i]], writes=[Tk()])
            self.gemm(actT, 32, self.winA[l], 17, 512, "A", epi, 1024, self.banks[:4], "p1a")
        with contextlib.ExitStack() as st:
            NSTG = 4
            TS = min(512, self.T)
            stg = [self.sb(st, "p1b_s%d" % i, [128, TS], F32) for i in range(NSTG)]
            ts = [Tk() for _ in range(NSTG)]
            cnt = [0]

            def epi(subs, tok0, cb):
                for sub, (bank, tb) in enumerate(subs):
                    i = cnt[0] % NSTG
                    cnt[0] += 1
                    e = self.any2()
                    S.op(e, lambda eng, i=i, bank=bank, e=e: self.copy(e, eng, stg[i][:], bank[:, :TS]), reads=[tb], writes=[ts[i]])
                    r0 = (cb * 2 + sub) * 128
                    S.dma("pool", self.YT[r0:r0 + 128, tok0:tok0 + TS], stg[i][:], reads=[ts[i]], writes=[Tk()])
            self.gemm(actT, 32, self.winB[l], 16, 256, "B", epi, 1024, self.banks[:4], "p1b")

    def resid_gemm(self, actT, KC, Wb, xresT, TT, name):
        S = self.S
        TS = min(512, self.T)
        with contextlib.ExitStack() as st:
            NSTG = 3
            xr = [self.sb(st, name + "_x%d" % i, [128, TS], F32) for i in range(NSTG)]
            zt = [self.sb(st, name + "_z%d" % i, [128, TS], F32) for i in range(NSTG)]
            tx = [Tk() for _ in range(NSTG)]
            tz = [Tk() for _ in range(NSTG)]
            cnt = [0]

            def epi(subs, tok0, cb):
                for sub, (bank, tb) in enumerate(subs):
                    i = cnt[0] % NSTG
                    cnt[0] += 1
                    r0 = (cb * 2 + sub) * 128
                    S.dma("sp", xr[i][:], xresT[r0:r0 + 128, tok0:tok0 + TS], writes=[tx[i]])
                    S.op("dve", lambda e, i=i, bank=bank: e.scalar_tensor_tensor(
                        out=zt[i][:], in0=xr[i][:], scalar=ALPHA, in1=bank[:, :TS], op0=ALU.mult, op1=ALU.add),
                        reads=[tx[i], tb], writes=[tz[i]])
                    S.dma("pool", self.zT[r0:r0 + 128, tok0:tok0 + TS], zt[i][:], reads=[tz[i]], writes=[Tk()])
            self.gemm(actT, KC, Wb, 16, 256, "B", epi, TT, self.banks[:4], name)

    def ffn_up(self, actT, Wb, NCB, grep):
        S = self.S
        TS = min(512, self.T)
        with contextlib.ExitStack() as st:
            NSTG = 3
            sg = [self.sb(st, "fu_s%d" % i, [128, TS], F32) for i in range(NSTG)]
            h1 = [self.sb(st, "fu_h%d" % i, [128, TS], F32) for i in range(NSTG)]
            gr = [self.sb(st, "fu_g%d" % i, [128, TS], F32) for i in range(NSTG)]
            hb = [self.sb(st, "fu_b%d" % i, [128, TS], BF16) for i in range(NSTG)]
            tsg = [Tk() for _ in range(NSTG)]
            th1 = [Tk() for _ in range(NSTG)]
            tgr = [Tk() for _ in range(NSTG)]
            thb = [Tk() for _ in range(NSTG)]
            cnt = [0]

            def epi(subs, tok0, cb):
                (bg, tg), (bu, tu) = subs
                i = cnt[0] % NSTG
                cnt[0] += 1
                S.op("act", lambda e: e.activation(out=sg[i][:], in_=bg[:, :TS], func=AF.Silu), reads=[tg], writes=[tsg[i]])
                if grep is None:
                    S.op("dve", lambda e: e.tensor_tensor(out=hb[i][:], in0=sg[i][:], in1=bu[:, :TS], op=ALU.mult),
                         reads=[tsg[i], tu], writes=[thb[i]])
                else:
                    S.dma("sp", gr[i][:], grep[cb // 8, :, tok0:tok0 + TS], writes=[tgr[i]])
                    S.op("dve", lambda e: e.tensor_tensor(out=h1[i][:], in0=sg[i][:], in1=bu[:, :TS], op=ALU.mult),
                         reads=[tsg[i], tu], writes=[th1[i]])
                    S.op("pool", lambda e: e.tensor_tensor(out=hb[i][:], in0=h1[i][:], in1=gr[i][:], op=ALU.mult),
                         reads=[th1[i], tgr[i]], writes=[thb[i]])
                S.dma("pool", self.hT[cb * 128:(cb + 1) * 128, tok0:tok0 + TS], hb[i][:], reads=[thb[i]], writes=[Tk()])
            self.gemm(actT, 32, Wb, NCB, 256, "B", epi, 1024, self.banks[:4], "fu")

    def ln_pass(self, zT, gcol, bcol, outF, outB):
        S = self.S
        T = self.T
        TS = min(512, T)
        bs_, bq_ = self.banks[4], self.banks[5]
        zTr = zT.rearrange("(c p) t -> p c t", p=128)
        with contextlib.ExitStack() as st:
            z = [self.sb(st, "ln_z%d" % i, [128, 32, TS], F32) for i in range(2)]
            tz = [[Tk() for _ in range(4)] for _ in range(2)]
            NR = 3
            zb = [self.sb(st, "ln_zb%d" % i, [128, TS], BF16) for i in range(NR)]
            zq = [self.sb(st, "ln_zq%d" % i, [128, TS], BF16) for i in range(NR)]
            tzb = [Tk() for _ in range(NR)]
            tzq = [Tk() for _ in range(NR)]
            mean = self.sb(st, "ln_mean", [128, TS], F32)
            msq = self.sb(st, "ln_msq", [128, TS], F32)
            rstd = self.sb(st, "ln_rstd", [128, TS], F32)
            tst = Tk()
            t1 = [self.sb(st, "ln_t%d" % i, [128, TS], F32) for i in range(NR)]
            of = [self.sb(st, "ln_of%d" % i, [128, TS], F32) for i in range(NR)]
            ob = [self.sb(st, "ln_ob%d" % i, [128, TS], BF16) for i in range(NR)]
            tt1 = [Tk() for _ in range(NR)]
            tof = [Tk() for _ in range(NR)]
            tob = [Tk() for _ in range(NR)]
            k = 0
            for tt in range(T // TS):
                zi = tt % 2
                tok = slice(tt * TS, (tt + 1) * TS)
                for g in range(4):
                    S.dma("sp", z[zi][:, g * 8:(g + 1) * 8, :], zTr[:, g * 8:(g + 1) * 8, tok], writes=[tz[zi][g]])
                for c in range(32):
                    i = k % NR
                    k += 1
                    S.op("act", lambda e: e.activation(out=zb[i][:], in_=z[zi][:, c, :], func=AF.Copy),
                         reads=[tz[zi][c // 8]], writes=[tzb[i]])
                    S.op("act", lambda e: e.activation(out=zq[i][:], in_=z[zi][:, c, :], func=AF.Square),
                         reads=[tz[zi][c // 8]], writes=[tzq[i]])
                    S.op("pe", lambda e: e.matmul(bs_[0][:, :TS], self.onesb[:], zb[i][:], start=(c == 0), stop=(c == 31)),
                         reads=[tzb[i], self.t_c], writes=[bs_[1]])
                    S.op("pe", lambda e: e.matmul(bq_[0][:, :TS], self.onesb[:], zq[i][:], start=(c == 0), stop=(c == 31)),
                         reads=[tzq[i], self.t_c], writes=[bq_[1]])
                S.op("dve", lambda e: e.tensor_scalar(out=mean[:], in0=bs_[0][:, :TS], scalar1=1.0 / D, scalar2=1.0, op0=ALU.mult, op1=ALU.mult),
                     reads=[bs_[1]], writes=[tst])
                S.op("dve", lambda e: e.tensor_tensor(out=msq[:], in0=mean[:], in1=mean[:], op=ALU.mult), reads=[tst], writes=[tst])
                S.op("dve", lambda e: e.scalar_tensor_tensor(out=msq[:], in0=bq_[0][:, :TS], scalar=1.0 / D, in1=msq[:],
                                                            op0=ALU.mult, op1=ALU.subtract), reads=[bq_[1], tst], writes=[tst])
                S.op("act", lambda e: e.activation(out=rstd[:], in_=msq[:], func=AF.Sqrt, bias=self.epsln[:, 0:1], scale=1.0),
                     reads=[tst, self.t_c], writes=[tst])
                S.op("dve", lambda e: e.reciprocal(out=rstd[:], in_=rstd[:]), reads=[tst], writes=[tst])
                for c in range(32):
                    i = k % NR
                    k += 1
                    S.op("dve", lambda e: e.tensor_tensor(out=t1[i][:], in0=z[zi][:, c, :], in1=mean[:], op=ALU.subtract),
                         reads=[tz[zi][c // 8], tst], writes=[tt1[i]])
                    S.op("dve", lambda e: e.tensor_tensor(out=t1[i][:], in0=t1[i][:], in1=rstd[:], op=ALU.mult),
                         reads=[tst], writes=[tt1[i]])
                    S.op("act", lambda e: e.activation(out=of[i][:], in_=t1[i][:], func=AF.Identity,
                                                       bias=self.pp[:, bcol + c:bcol + c + 1], scale=self.pp[:, gcol + c:gcol + c + 1]),
                         reads=[tt1[i], self.t_pp], writes=[tof[i]])
                    S.dma("act", outF[c * 128:(c + 1) * 128, tok], of[i][:], reads=[tof[i]], writes=[Tk()])
                    if outB is not None:
                        S.op("pool", lambda e: e.tensor_copy(out=ob[i][:], in_=of[i][:]), reads=[tof[i]], writes=[tob[i]])
                        S.dma("act", outB[c * 128:(c + 1) * 128, tok], ob[i][:], reads=[tob[i]], writes=[Tk()])
        S.barrier()

    def router_pass(self, x1T):
        S = self.S
        T = self.T
        TS = min(512, T)
        xr_ = x1T.rearrange("(c p) t -> p c t", p=128)
        bl, bt, br = self.banks[0], self.banks[1], self.banks[2]
        with contextlib.ExitStack() as st:
            wr = self.sb(st, "rt_w", [128, 32, NE], F32)
            sel = self.sb(st, "rt_sel", [8, 8, 128], F32)
            tw = Tk()
            S.dma("sp", wr[:], self.router, writes=[tw])
            S.dma("sp", sel[:], self.sel_in, writes=[tw])
            xr = [self.sb(st, "rt_x%d" % i, [128, 32, 128], F32) for i in range(2)]
            tx = [Tk(), Tk()]
            sm = self.sb(st, "rt_sm", [128, 64], F32)
            tsm = Tk()
            gT = self.sb(st, "rt_gT", [8, TS], F32)
            tgT = Tk()
            gr = [self.sb(st, "rt_gr%d" % i, [128, TS], F32) for i in range(2)]
            tgr = [Tk(), Tk()]
            lg, eq1, l2, eq2, g1, gt = (sm[:, 0:8], sm[:, 8:16], sm[:, 16:24], sm[:, 24:32], sm[:, 32:40], sm[:, 40:48])
            m1, m2, dl, w1, w2 = (sm[:, 48:49], sm[:, 49:50], sm[:, 50:51], sm[:, 51:52], sm[:, 52:53])
            npg = TS // 128
            k = 0
            for n in range(T // 128):
                i = n % 2
                S.dma("sp", xr[i][:], xr_[:, :, n * 128:(n + 1) * 128], writes=[tx[i]])
                for c in range(32):
                    S.op("pe", lambda e: e.matmul(bl[0][:, 0:NE], xr[i][:, c, :], wr[:, c, :], start=(c == 0), stop=(c == 31)),
                         reads=[tx[i], tw], writes=[bl[1]])
                V = "dve"
                S.op(V, lambda e: e.tensor_copy(out=lg, in_=bl[0][:, 0:NE]), reads=[bl[1]], writes=[tsm])
                S.op(V, lambda e: e.tensor_reduce(out=m1, in_=lg, axis=AX.X, op=ALU.max), reads=[tsm], writes=[tsm])
                S.op(V, lambda e: e.tensor_scalar(out=eq1, in0=lg, scalar1=m1, scalar2=1.0, op0=ALU.is_equal, op1=ALU.mult), reads=[tsm], writes=[tsm])
                S.op(V, lambda e: e.scalar_tensor_tensor(out=l2, in0=eq1, scalar=-1e30, in1=lg, op0=ALU.mult, op1=ALU.add), reads=[tsm], writes=[tsm])
                S.op(V, lambda e: e.tensor_reduce(out=m2, in_=l2, axis=AX.X, op=ALU.max), reads=[tsm], writes=[tsm])
                S.op(V, lambda e: e.tensor_scalar(out=eq2, in0=l2, scalar1=m2, scalar2=1.0, op0=ALU.is_equal, op1=ALU.mult), reads=[tsm], writes=[tsm])
                S.op(V, lambda e: e.tensor_tensor(out=dl, in0=m1, in1=m2, op=ALU.subtract), reads=[tsm], writes=[tsm])
                S.op("act", lambda e: e.activation(out=w1, in_=dl, func=AF.Sigmoid), reads=[tsm], writes=[tsm])
                S.op("act", lambda e: e.activation(out=w2, in_=dl, func=AF.Sigmoid, scale=-1.0), reads=[tsm], writes=[tsm])
                S.op(V, lambda e: e.tensor_scalar(out=g1, in0=eq1, scalar1=w1, scalar2=1.0, op0=ALU.mult, op1=ALU.mult), reads=[tsm], writes=[tsm])
                S.op(V, lambda e: e.scalar_tensor_tensor(out=gt, in0=eq2, scalar=w2, in1=g1, op0=ALU.mult, op1=ALU.add), reads=[tsm], writes=[tsm])
                j = n % npg
                S.op("pe", lambda e: e.transpose(bt[0][0:8, j * 128:(j + 1) * 128], gt, self.idf[:]),
                     reads=[tsm, self.t_c], writes=[bt[1]])
                if j == npg - 1:
                    tok0 = (n - j) * 128
                    S.op("act", lambda e: e.activation(out=gT[:], in_=bt[0][0:8, :TS], func=AF.Copy), reads=[bt[1]], writes=[tgT])
                    for ex in range(NE):
                        S.op("pe", lambda e: e.matmul(br[0][:, :TS], sel[:, ex, :], gT[:], start=True, stop=True),
                             reads=[tgT, tw], writes=[br[1]])
                        b = k % 2
                        k += 1
                        en = self.any2()
                        S.op(en, lambda eng: self.copy(en, eng, gr[b][:], br[0][:, :TS]), reads=[br[1]], writes=[tgr[b]])
                        S.dma("pool", self.Grep[ex, :, tok0:tok0 + TS], gr[b][:], reads=[tgr[b]], writes=[Tk()])
        S.barrier()

    def rope(self, S, xin, rb, cs, sn, W, tin, tcs, tout, tmp, ttmp):
        xv = xin.rearrange("p (j two) -> p j two", two=2)
        ov = rb.rearrange("p (j two) -> p j two", two=2)
        x0, x1 = xv[:, :, 0], xv[:, :, 1]
        t1, t2, t3, t4 = tmp
        S.op("dve", lambda e: e.tensor_tensor(out=t1, in0=x0, in1=cs, op=ALU.mult), reads=[tin, tcs], writes=[ttmp[0]])
        S.op("pool", lambda e: e.tensor_tensor(out=t2, in0=x1, in1=sn, op=ALU.mult), reads=[tin, tcs], writes=[ttmp[1]])
        S.op("pool", lambda e: e.tensor_tensor(out=t3, in0=x0, in1=sn, op=ALU.mult), reads=[tin, tcs], writes=[ttmp[2]])
        S.op("dve", lambda e: e.tensor_tensor(out=t4, in0=x1, in1=cs, op=ALU.mult), reads=[tin, tcs], writes=[ttmp[3]])
        S.op("dve", lambda e: e.tensor_tensor(out=ov[:, :, 0], in0=t1, in1=t2, op=ALU.subtract),
             reads=[ttmp[0], ttmp[1]], writes=[tout])
        S.op("pool", lambda e: e.tensor_tensor(out=ov[:, :, 1], in0=t3, in1=t4, op=ALU.add),
             reads=[ttmp[2], ttmp[3], tout], writes=[tout])

    def mix_gmlp(self, l):
        S = self.S
        T = self.T
        bA, bB = self.banks[0], self.banks[1]
        with contextlib.ExitStack() as st:
            grep = self.sb(st, "gm_g", [128, 1024], F32)
            brep = self.sb(st, "gm_b", [128, 1024], F32)
            msrep = self.sb(st, "gm_ms", [128, 1024], F32)
            wsf = self.sb(st, "gm_wsf", [128, 8, 128], F32)
            wsb = self.sb(st, "gm_wsb", [128, 8, 128], BF16)
            bs = self.sb(st, "gm_bs", [128, 8], F32)
            tc_ = Tk()
            S.dma("sp", grep[:], self.rp_in[l, :, 1280:2304], writes=[tc_])
            S.dma("sp", brep[:], self.rp_in[l, :, 2304:3328], writes=[tc_])
            S.dma("sp", msrep[:], self.rp_in[l, :, 3328:4352], writes=[tc_])
            S.dma("sp", wsf[:], self.wsT_in[l], writes=[tc_])
            S.dma("sp", bs[:], self.bsT_in[l], writes=[tc_])
            S.op("dve", lambda e: e.tensor_copy(out=wsb[:], in_=wsf[:]), reads=[tc_], writes=[tc_])
            NB = 2
            gv = [self.sb(st, "gm_gv%d" % i, [128, 1024], F32) for i in range(NB)]
            gu = [self.sb(st, "gm_gu%d" % i, [128, 1024], F32) for i in range(NB)]
            a = [self.sb(st, "gm_a%d" % i, [128, 1024], F32) for i in range(NB)]
            sq = [self.sb(st, "gm_sq%d" % i, [128, 1024], F32) for i in range(NB)]
            vnb = [self.sb(st, "gm_vn%d" % i, [128, 1024], BF16) for i in range(NB)]
            oc = [self.sb(st, "gm_oc%d" % i, [128, 1024], F32) for i in range(NB)]
            ocb = [self.sb(st, "gm_ob%d" % i, [128, 1024], BF16) for i in range(NB)]
            mT = [self.sb(st, "gm_mT%d" % i, [128, 1024], BF16) for i in range(NB)]
            sm = [self.sb(st, "gm_sm%d" % i, [128, 8], F32) for i in range(NB)]
            tgv = [Tk() for _ in range(NB)]
            tgu = [Tk() for _ in range(NB)]
            ta = [Tk() for _ in range(NB)]
            tsq = [Tk() for _ in range(NB)]
            tvn = [Tk() for _ in range(NB)]
            toc = [Tk() for _ in range(NB)]
            tob = [Tk() for _ in range(NB)]
            tmT = [Tk() for _ in range(NB)]
            tsm = [Tk() for _ in range(NB)]
            mdst = self.mergedT[2048:3072, :].rearrange("(g p) t -> p g t", p=128)
            for n in range(T // 128):
                i = n % NB
                rows = slice(n * 128, (n + 1) * 128)
                S.dma("sp", gv[i][:], self.Y_g[rows, 1024:2048], writes=[tgv[i]])
                S.dma("sp", gu[i][:], self.Y_g[rows, 0:1024], writes=[tgu[i]])
                s1, nm, s2, rs = sm[i][:, 0:1], sm[i][:, 1:2], sm[i][:, 2:3], sm[i][:, 3:4]
                S.op("act", lambda e: e.activation(out=a[i][:], in_=gv[i][:], func=AF.Gelu), reads=[tgv[i]], writes=[ta[i]])
                S.op("dve", lambda e: e.tensor_reduce(out=s1, in_=a[i][:], axis=AX.X, op=ALU.add), reads=[ta[i]], writes=[tsm[i]])
                S.op("dve", lambda e: e.tensor_scalar(out=nm, in0=s1, scalar1=-1.0 / 1024, scalar2=1.0, op0=ALU.mult, op1=ALU.mult), reads=[tsm[i]], writes=[tsm[i]])
                S.op("dve", lambda e: e.tensor_scalar(out=a[i][:], in0=a[i][:], scalar1=nm, scalar2=0.0, op0=ALU.add, op1=ALU.add), reads=[tsm[i]], writes=[ta[i]])
                S.op("pool", lambda e: e.tensor_tensor(out=sq[i][:], in0=a[i][:], in1=a[i][:], op=ALU.mult), reads=[ta[i]], writes=[tsq[i]])
                S.op("dve", lambda e: e.tensor_reduce(out=s2, in_=sq[i][:], axis=AX.X, op=ALU.add), reads=[tsq[i]], writes=[tsm[i]])
                S.op("act", lambda e: e.activation(out=rs, in_=s2, func=AF.Sqrt, bias=self.epsln[:, 0:1], scale=1.0 / 1024),
                     reads=[tsm[i], self.t_c], writes=[tsm[i]])
                S.op("dve", lambda e: e.reciprocal(out=rs, in_=rs), reads=[tsm[i]], writes=[tsm[i]])
                S.op("dve", lambda e: e.scalar_tensor_tensor(out=sq[i][:], in0=a[i][:], scalar=rs, in1=grep[:], op0=ALU.mult, op1=ALU.mult),
                     reads=[ta[i], tsm[i], tc_], writes=[tsq[i]])
                S.op("pool", lambda e: e.tensor_tensor(out=vnb[i][:], in0=sq[i][:], in1=brep[:], op=ALU.add), reads=[tsq[i], tc_], writes=[tvn[i]])
                for g in range(8):
                    bank = bA if g < 4 else bB
                    c0 = (g % 4) * 128
                    S.op("pe", lambda e: e.matmul(bank[0][:, c0:c0 + 128], wsb[:, g, :], vnb[i][:, g * 128:(g + 1) * 128], start=True, stop=True),
                         reads=[tvn[i], tc_], writes=[bank[1]])
                S.op("act", lambda e: e.activation(out=gu[i][:], in_=gu[i][:], func=AF.Gelu), reads=[tgu[i]], writes=[tgu[i]])
                for g in range(8):
                    bank = bA if g < 4 else bB
                    c0 = (g % 4) * 128
                    S.op("dve", lambda e: e.scalar_tensor_tensor(out=oc[i][:, g * 128:(g + 1) * 128], in0=bank[0][:, c0:c0 + 128],
                                                                scalar=bs[:, g:g + 1], in1=gu[i][:, g * 128:(g + 1) * 128],
                                                                op0=ALU.add, op1=ALU.mult),
                         reads=[bank[1], tgu[i], tc_], writes=[toc[i]])
                S.op("pool", lambda e: e.tensor_tensor(out=ocb[i][:], in0=oc[i][:], in1=msrep[:], op=ALU.mult), reads=[toc[i], tc_], writes=[tob[i]])
                pb, tpb = self.bbanks[n % 2]
                for g in range(8):
                    S.op("pe", lambda e: e.transpose(pb[:, g * 128:(g + 1) * 128], ocb[i][:, g * 128:(g + 1) * 128], self.idb[:]),
                         reads=[tob[i], self.t_c], writes=[tpb])
                S.op("act", lambda e: e.activation(out=mT[i][:], in_=pb[:], func=AF.Copy), reads=[tpb], writes=[tmT[i]])
                S.dma("pool", mdst[:, :, rows], mT[i][:].rearrange("p (g t) -> p g t", t=128), reads=[tmT[i]], writes=[Tk()])
        S.barrier()

    def mix_attn(self, l):
        S = self.S
        T = self.T
        NCH = T // 128
        QT = min(512, T)
        with contextlib.ExitStack() as st:
            gain = self.sb(st, "at_gain", [128, 1280], F32)
            tcst = Tk()
            S.dma("sp", gain[:], self.rp_in[l, :, 0:1280], writes=[tcst])
            NB = 2
            qk = [self.sb(st, "at_qk%d" % i, [128, 1280], F32) for i in range(NB)]
            sq = [self.sb(st, "at_sq%d" % i, [128, 1280], F32) for i in range(NB)]
            cs = [self.sb(st, "at_cs%d" % i, [128, 640], F32) for i in range(NB)]
            sn = [self.sb(st, "at_sn%d" % i, [128, 640], F32) for i in range(NB)]
            tmp = [[self.sb(st, "at_t%d_%d" % (i, j), [128, 640], F32) for j in range(4)] for i in range(NB)]
            rb = [self.sb(st, "at_rb%d" % i, [128, 1280], BF16) for i in range(NB)]
            qkT = [self.sb(st, "at_qkT%d" % i, [128, 1280], BF16) for i in range(NB)]
            sm = [self.sb(st, "at_sm%d" % i, [128, 16], F32) for i in range(NB)]
            tqk = [Tk() for _ in range(NB)]
            tsq = [Tk() for _ in range(NB)]
            tcs = [Tk() for _ in range(NB)]
            ttmp = [[Tk() for _ in range(4)] for _ in range(NB)]
            trb = [Tk() for _ in range(NB)]
            tqT = [Tk() for _ in range(NB)]
            tsm = [Tk() for _ in range(NB)]
            dst = self.aqkT.rearrange("h d t -> d h t")
            for n in range(NCH):
                i = n % NB
                rows = slice(n * 128, (n + 1) * 128)
                S.dma("sp", qk[i][:], self.Y_a[rows, 0:1280], writes=[tqk[i]])
                S.dma("sp", cs[i][:], self.cos_in[rows, 0:640], writes=[tcs[i]])
                S.dma("sp", sn[i][:], self.sin_in[rows, 0:640], writes=[tcs[i]])
                S.op("pool", lambda e: e.tensor_tensor(out=sq[i][:], in0=qk[i][:], in1=qk[i][:], op=ALU.mult), reads=[tqk[i]], writes=[tsq[i]])
                ss = sm[i][:, 0:10]
                S.op("dve", lambda e: e.tensor_reduce(out=ss, in_=sq[i][:].rearrange("p (h d) -> p h d", d=128), axis=AX.X, op=ALU.add),
                     reads=[tsq[i]], writes=[tsm[i]])
                S.op("act", lambda e: e.activation(out=ss, in_=ss, func=AF.Sqrt, bias=self.epsln[:, 1:2], scale=1.0 / 128),
                     reads=[tsm[i], self.t_c], writes=[tsm[i]])
                S.op("dve", lambda e: e.reciprocal(out=ss, in_=ss), reads=[tsm[i]], writes=[tsm[i]])
                S.op("dve", lambda e: e.tensor_tensor(out=sq[i][:].rearrange("p (h d) -> p h d", d=128),
                                                     in0=qk[i][:].rearrange("p (h d) -> p h d", d=128),
                                                     in1=ss.unsqueeze(2).to_broadcast([128, 10, 128]), op=ALU.mult),
                     reads=[tqk[i], tsm[i]], writes=[tsq[i]])
                S.op("pool", lambda e: e.tensor_tensor(out=sq[i][:], in0=sq[i][:], in1=gain[:], op=ALU.mult), reads=[tcst], writes=[tsq[i]])
                self.rope(S, sq[i][:], rb[i][:], cs[i][:], sn[i][:], 1280, tsq[i], tcs[i], trb[i],
                          [t[:] for t in tmp[i]], ttmp[i])
                for h in range(10):
                    pb, tpb = self.bbanks[0] if h < 8 else self.bbanks[1]
                    c0 = (h % 8) * 128
                    S.op("pe", lambda e: e.transpose(pb[:, c0:c0 + 128], rb[i][:, h * 128:(h + 1) * 128], self.idb[:]),
                         reads=[trb[i], self.t_c], writes=[tpb])
                S.op("act", lambda e: e.activation(out=qkT[i][:, 0:1024], in_=self.bbanks[0][0][:], func=AF.Copy),
                     reads=[self.bbanks[0][1]], writes=[tqT[i]])
                S.op("dve", lambda e: e.tensor_copy(out=qkT[i][:, 1024:1280], in_=self.bbanks[1][0][:, 0:256]),
                     reads=[self.bbanks[1][1]], writes=[tqT[i]])
                S.dma("pool", dst[:, :, rows], qkT[i][:].rearrange("p (h t) -> p h t", t=128), reads=[tqT[i]], writes=[Tk()])
        S.barrier()
        scale = 128.0 ** -0.5
        with contextlib.ExitStack() as st:
            kT = self.sb(st, "ac_kT", [128, T], BF16)
            vf = self.sb(st, "ac_vf", [128, NCH, 128], F32)
            vb = self.sb(st, "ac_vb", [128, NCH, 128], BF16)
            tk_, tvf, tvb = Tk(), Tk(), Tk()
            qT = [self.sb(st, "ac_qT%d" % i, [128, QT], BF16) for i in range(2)]
            tq = [Tk(), Tk()]
            NP = 3
            pT = [self.sb(st, "ac_pT%d" % i, [128, QT], BF16) for i in range(NP)]
            tp = [Tk() for _ in range(NP)]
            rec = self.sb(st, "ac_rec", [128, QT], F32)
            of = self.sb(st, "ac_of", [128, QT], F32)
            ob = [self.sb(st, "ac_ob%d" % i, [128, QT], BF16) for i in range(2)]
            trec, tof = Tk(), Tk()
            tob = [Tk(), Tk()]
            sbank = [self.banks[0], self.banks[1]]
            accs = [(self.banks[2], self.banks[3]), (self.banks[4], self.banks[5])]
            it = 0
            pi = 0
            for g in range(2):
                S.dma("sp", kT[:], self.aqkT[8 + g], writes=[tk_])
                self.dma_chunks("sp", vf[:], self.Y_a[:, 1280 + g * 128:1280 + (g + 1) * 128].rearrange("(n p) d -> p n d", p=128), NCH, tvf)
                S.op("pool", lambda e: e.tensor_copy(out=vb[:], in_=vf[:]), reads=[tvf], writes=[tvb])
                for h in range(4 * g, 4 * g + 4):
                    for qt in range(T // QT):
                        qi = it % 2
                        oacc, dacc = accs[it % 2]
                        it += 1
                        S.dma("sp", qT[qi][:], self.aqkT[h, :, qt * QT:(qt + 1) * QT], writes=[tq[qi]])
                        def emit_s(kc_):
                            sbx, tsbx = sbank[kc_ % 2]
                            S.op("pe", lambda e: e.matmul(sbx[:, :QT], kT[:, kc_ * 128:(kc_ + 1) * 128], qT[qi][:], start=True, stop=True),
                                 reads=[tk_, tq[qi]], writes=[tsbx])
                        emit_s(0)
                        for kc in range(NCH):
                            sb_, tsb = sbank[kc % 2]
                            if kc + 1 < NCH:
                                emit_s(kc + 1)
                            p = pi % NP
                            pi += 1
                            S.op("act", lambda e: e.activation(out=pT[p][:], in_=sb_[:, :QT], func=AF.Exp, scale=scale),
                                 reads=[tsb], writes=[tp[p]])
                            S.op("pe", lambda e: e.matmul(oacc[0][:, :QT], vb[:, kc, :], pT[p][:], start=(kc == 0), stop=(kc == NCH - 1)),
                                 reads=[tvb, tp[p]], writes=[oacc[1]])
                            S.op("pe", lambda e: e.matmul(dacc[0][:, :QT], self.onesb[:], pT[p][:], start=(kc == 0), stop=(kc == NCH - 1)),
                                 reads=[tp[p], self.t_c], writes=[dacc[1]])
                        S.op("dve", lambda e: e.reciprocal(out=rec[:], in_=dacc[0][:, :QT]), reads=[dacc[1]], writes=[trec])
                        S.op("dve", lambda e: e.tensor_tensor(out=of[:], in0=oacc[0][:, :QT], in1=rec[:], op=ALU.mult),
                             reads=[oacc[1], trec], writes=[tof])
                        mc = l * 200 + 128 + h
                        S.op("pool", lambda e: e.tensor_scalar(out=ob[qi][:], in0=of[:], scalar1=self.pp[:, mc:mc + 1], scalar2=1.0,
                                                               op0=ALU.mult, op1=ALU.mult), reads=[tof, self.t_pp], writes=[tob[qi]])
                        S.dma("pool", self.mergedT[h * 128:(h + 1) * 128, qt * QT:(qt + 1) * QT], ob[qi][:], reads=[tob[qi]], writes=[Tk()])
        S.barrier()

    def mix_ret(self, l):
        S = self.S
        T = self.T
        NCH = T // 128
        gam = [1.0 - 2.0 ** (-5.0 - h) for h in range(8)]
        gC = [float(np.float64(g) ** 128) for g in gam]
        with contextlib.ExitStack() as st:
            rdec = self.sb(st, "rt_rdec", [128, 32], F32)
            tcst = Tk()
            S.dma("sp", rdec[:], self.rdec_in, writes=[tcst])
            NB = 2
            qk = [self.sb(st, "rp_qk%d" % i, [128, 2048], F32) for i in range(NB)]
            rv = [self.sb(st, "rp_rv%d" % i, [128, 1024], F32) for i in range(NB)]
            cs = [self.sb(st, "rp_cs%d" % i, [128, 1024], F32) for i in range(NB)]
            sn = [self.sb(st, "rp_sn%d" % i, [128, 1024], F32) for i in range(NB)]
            tmp = [[self.sb(st, "rp_t%d_%d" % (i, j), [128, 1024], F32) for j in range(4)] for i in range(NB)]
            rb = [self.sb(st, "rp_rb%d" % i, [128, 2048], BF16) for i in range(NB)]
            kfb = [self.sb(st, "rp_kf%d" % i, [128, 3, 1024], BF16) for i in range(NB)]
            qkT = [self.sb(st, "rp_qkT%d" % i, [128, 2048], BF16) for i in range(NB)]
            tqk = [Tk() for _ in range(NB)]
            trv = [Tk() for _ in range(NB)]
            tcs = [Tk() for _ in range(NB)]
            ttmp = [[Tk() for _ in range(4)] for _ in range(NB)]
            trb = [Tk() for _ in range(NB)]
            tkf = [Tk() for _ in range(NB)]
            tqT = [Tk() for _ in range(NB)]
            dst = self.rqkT.rearrange("h d t -> d h t")
            rkd = self.RK.rearrange("i t c -> t i c")
            for n in range(NCH):
                i = n % NB
                rows = slice(n * 128, (n + 1) * 128)
                S.dma("sp", qk[i][:], self.Y_r[rows, 0:2048], writes=[tqk[i]])
                S.dma("sp", rv[i][:], self.Y_r[rows, 2048:3072], writes=[trv[i]])
                S.dma("sp", cs[i][:], self.cos_in[rows, :], writes=[tcs[i]])
                S.dma("sp", sn[i][:], self.sin_in[rows, :], writes=[tcs[i]])
                self.rope(S, qk[i][:], rb[i][:], cs[i][:], sn[i][:], 2048, tqk[i], tcs[i], trb[i], [t[:] for t in tmp[i]], ttmp[i])
                for h in range(8):
                    kh = rb[i][:, 1024 + h * 128:1024 + (h + 1) * 128]
                    S.op("dve", lambda e: e.tensor_scalar(out=kfb[i][:, 0, h * 128:(h + 1) * 128], in0=kh, scalar1=rdec[:, h:h + 1], scalar2=1.0, op0=ALU.mult, op1=ALU.mult),
                         reads=[trb[i], tcst], writes=[tkf[i]])
                    S.op("pool", lambda e: e.tensor_scalar(out=kfb[i][:, 1, h * 128:(h + 1) * 128], in0=kh, scalar1=rdec[:, 8 + h:9 + h], scalar2=1.0,
                                                           op0=ALU.mult, op1=ALU.mult), reads=[trb[i], tcst], writes=[tkf[i]])
                S.op("act", lambda e: e.activation(out=kfb[i][:, 2, :], in_=rv[i][:], func=AF.Copy), reads=[trv[i]], writes=[tkf[i]])
                S.dma("pool", rkd[rows, :, :], kfb[i][:], reads=[tkf[i]], writes=[Tk()])
                for h in range(16):
                    pb, tpb = self.bbanks[h // 8]
                    c0 = (h % 8) * 128
                    S.op("pe", lambda e: e.transpose(pb[:, c0:c0 + 128], rb[i][:, h * 128:(h + 1) * 128], self.idb[:]),
                         reads=[trb[i], self.t_c], writes=[tpb])
                S.op("act", lambda e: e.activation(out=qkT[i][:, 0:1024], in_=self.bbanks[0][0][:], func=AF.Copy),
                     reads=[self.bbanks[0][1]], writes=[tqT[i]])
                S.op("dve", lambda e: e.tensor_copy(out=qkT[i][:, 1024:2048], in_=self.bbanks[1][0][:]),
                     reads=[self.bbanks[1][1]], writes=[tqT[i]])
                S.dma("pool", dst[:, :, rows], qkT[i][:].rearrange("p (h t) -> p h t", t=128), reads=[tqT[i]], writes=[Tk()])
        S.barrier()
        with contextlib.ExitStack() as st:
            rdec = self.sb(st, "rs_rdec", [128, 32], F32)
            dsym = self.sb(st, "rs_dsym", [128, 8, 128], F32)
            gcol = self.sb(st, "rs_gcol", [128, 8], F32)
            tcst = Tk()
            S.dma("sp", rdec[:], self.rdec_in, writes=[tcst])
            S.dma("sp", dsym[:], self.dsym_in, writes=[tcst])
            b0 = l * 200
            S.op("dve", lambda e: e.tensor_tensor(out=gcol[:], in0=self.pp[:, b0 + 168:b0 + 176], in1=self.pp[:, b0 + 128 + 24:b0 + 128 + 32], op=ALU.mult),
                 reads=[self.t_pp], writes=[tcst])
            qT = self.sb(st, "rs_qT", [128, T], BF16)
            kT = self.sb(st, "rs_kT", [128, T], BF16)
            kv = self.sb(st, "rs_kv", [128, 3, NCH, 128], BF16)
            rg = self.sb(st, "rs_rg", [128, NCH, 128], F32)
            oacc = self.sb(st, "rs_oacc", [128, NCH, 128], F32)
            NG = min(8, NCH)
            sqb = [self.sb(st, "rs_sq%d" % i, [128, NG, 128], F32) for i in range(2)]
            ob = [self.sb(st, "rs_ob%d" % i, [128, NG, 128], BF16) for i in range(2)]
            ss = [self.sb(st, "rs_ss%d" % i, [128, NG], F32) for i in range(2)]
            tld, trg, toa = Tk(), Tk(), Tk()
            tsq, tob, tss = [Tk(), Tk()], [Tk(), Tk()], [Tk(), Tk()]
            R = [self.sb(st, "rs_R%d" % i, [128, 128], F32) for i in range(2)]
            NSR = 8
            Rbr = self.sb(st, "rs_Rbr", [128, NSR, 128], BF16)
            tR = [Tk(), Tk()]
            tRbr = [Tk() for _ in range(NSR)]
            pT = [self.sb(st, "rs_pT%d" % i, [128, 128], BF16) for i in range(2)]
            tpT = [Tk(), Tk()]
            mT = [self.sb(st, "rs_mT%d" % i, [128, 1024], BF16) for i in range(2)]
            tmT = [Tk(), Tk()]
            bS = [self.banks[0], self.banks[1]]
            bO = [self.banks[2], self.banks[3]]
            bR = [self.banks[4], self.banks[5]]
            for h in range(8):
                S.dma("sp", qT[:], self.rqkT[h], writes=[tld])
                S.dma("sp", kT[:], self.rqkT[8 + h], writes=[tld])
                for i3 in range(3):
                    self.dma_chunks("sp", kv[:, i3, :, :], self.RK[i3, :, h * 128:(h + 1) * 128].rearrange("(n p) d -> p n d", p=128), NCH, tld)
                self.dma_chunks("sp", rg[:], self.Y_r[:, 3072 + h * 128:3072 + (h + 1) * 128].rearrange("(n p) d -> p n d", p=128), NCH, trg)
                S.op("pool", lambda e: e.memset(R[0][:], 0.0), writes=[tR[0]])
                S.op("pool", lambda e: e.memset(Rbr[:, 0, :], 0.0), writes=[tRbr[0]])

                def a1(j):
                    cols = slice(j * 128, (j + 1) * 128)
                    bs_, tbs = bS[j % 2]
                    br_, tbr = bR[j % 2]
                    S.op("pe", lambda e: e.matmul(bs_[:, 0:128], kT[:, cols], qT[:, cols], start=True, stop=True), reads=[tld], writes=[tbs])
                    S.op("pe", lambda e: e.matmul(br_[:, 0:128], kv[:, 0, j, :], kv[:, 2, j, :], start=True, stop=True), reads=[tld], writes=[tbr])
                    p = j % 2
                    S.op("dve", lambda e: e.tensor_tensor(out=pT[p][:], in0=bs_[:, 0:128], in1=dsym[:, h, :], op=ALU.mult),
                         reads=[tbs, tcst], writes=[tpT[p]])
                    S.op("dve", lambda e: e.scalar_tensor_tensor(out=R[0][:], in0=R[0][:], scalar=gC[h], in1=br_[:, 0:128],
                                                                op0=ALU.mult, op1=ALU.add), reads=[tbr], writes=[tR[0]])
                    s1 = (j + 1) % NSR
                    S.op("dve", lambda e: e.tensor_copy(out=Rbr[:, s1, :], in_=R[0][:]), reads=[tR[0]], writes=[tRbr[s1]])

                def a2(j):
                    cols = slice(j * 128, (j + 1) * 128)
                    bo_, tbo = bO[j % 2]
                    p = j % 2
                    s0 = j % NSR
                    S.op("pe", lambda e: e.matmul(bo_[:, 0:128], pT[p][:], kv[:, 2, j, :], start=True, stop=True), reads=[tpT[p], tld], writes=[tbo])
                    S.op("pe", lambda e: e.matmul(bo_[:, 128:256], qT[:, cols], Rbr[:, s0, :], start=True, stop=True), reads=[tld, tRbr[s0]], writes=[tbo])
                    S.op("act", lambda e: e.activation(out=oacc[:, j, :], in_=bo_[:, 0:128], func=AF.Copy), reads=[tbo], writes=[toa])
                    S.op("dve", lambda e: e.scalar_tensor_tensor(out=oacc[:, j, :], in0=bo_[:, 128:256], scalar=rdec[:, 16 + h:17 + h],
                                                                in1=oacc[:, j, :], op0=ALU.mult, op1=ALU.add), reads=[tbo, tcst, toa], writes=[toa])
                a1(0)
                for j in range(NCH):
                    if j + 1 < NCH:
                        a1(j + 1)
                    a2(j)
                S.op("pool", lambda e: e.memset(R[1][:], 0.0), writes=[tR[1]])
                S.op("pool", lambda e: e.memset(Rbr[:, 0, :], 0.0), writes=[tRbr[0]])

                def d1(n):
                    j = NCH - 1 - n
                    br_, tbr = bR[n % 2]
                    S.op("pe", lambda e: e.matmul(br_[:, 0:128], kv[:, 1, j, :], kv[:, 2, j, :], start=True, stop=True), reads=[tld], writes=[tbr])
                    S.op("dve", lambda e: e.scalar_tensor_tensor(out=R[1][:], in0=R[1][:], scalar=gC[h], in1=br_[:, 0:128],
                                                                op0=ALU.mult, op1=ALU.add), reads=[tbr], writes=[tR[1]])
                    s1 = (n + 1) % NSR
                    S.op("dve", lambda e: e.tensor_copy(out=Rbr[:, s1, :], in_=R[1][:]), reads=[tR[1]], writes=[tRbr[s1]])

                def d2(n):
                    j = NCH - 1 - n
                    cols = slice(j * 128, (j + 1) * 128)
                    bo_, tbo = bO[n % 2]
                    s0 = n % NSR
                    S.op("pe", lambda e: e.matmul(bo_[:, 128:256], qT[:, cols], Rbr[:, s0, :], start=True, stop=True), reads=[tld, tRbr[s0]], writes=[tbo])
                    S.op("dve", lambda e: e.scalar_tensor_tensor(out=oacc[:, j, :], in0=bo_[:, 128:256], scalar=rdec[:, 24 + h:25 + h],
                                                                in1=oacc[:, j, :], op0=ALU.mult, op1=ALU.add), reads=[tbo, tcst, toa], writes=[toa])
                d1(0)
                for n in range(NCH):
                    if n + 1 < NCH:
                        d1(n + 1)
                    d2(n)
                S.op("act", lambda e: e.activation(out=rg[:], in_=rg[:], func=AF.Silu), reads=[trg], writes=[trg])
                for j0 in range(0, NCH, NG):
                    gi = (j0 // NG) % 2
                    js = slice(j0, j0 + NG)
                    S.op("pool", lambda e: e.tensor_tensor(out=sqb[gi][:], in0=oacc[:, js, :], in1=oacc[:, js, :], op=ALU.mult), reads=[toa], writes=[tsq[gi]])
                    S.op("dve", lambda e: e.tensor_reduce(out=ss[gi][:], in_=sqb[gi][:], axis=AX.X, op=ALU.add), reads=[tsq[gi]], writes=[tss[gi]])
                    S.op("act", lambda e: e.activation(out=ss[gi][:], in_=ss[gi][:], func=AF.Sqrt, bias=self.epsln[:, 1:2], scale=1.0 / 128),
                         reads=[tss[gi], self.t_c], writes=[tss[gi]])
                    S.op("dve", lambda e: e.reciprocal(out=ss[gi][:], in_=ss[gi][:]), reads=[tss[gi]], writes=[tss[gi]])
                    S.op("dve", lambda e: e.tensor_tensor(out=sqb[gi][:], in0=oacc[:, js, :], in1=ss[gi][:].unsqueeze(2).to_broadcast([128, NG, 128]), op=ALU.mult),
                         reads=[toa, tss[gi]], writes=[tsq[gi]])
                    S.op("pool", lambda e: e.tensor_tensor(out=ob[gi][:], in0=sqb[gi][:], in1=rg[:, js, :], op=ALU.mult), reads=[tsq[gi], trg], writes=[tob[gi]])
                    pb, tpb = self.bbanks[gi]
                    for jj in range(NG):
                        S.op("pe", lambda e: e.transpose(pb[:, jj * 128:(jj + 1) * 128], ob[gi][:, jj, :], self.idb[:]),
                             reads=[tob[gi], self.t_c], writes=[tpb])
                    S.op("act", lambda e: e.activation(out=mT[gi][:, :NG * 128], in_=pb[:, :NG * 128], func=AF.Copy, scale=gcol[:, h:h + 1]),
                         reads=[tpb, tcst], writes=[tmT[gi]])
                    S.dma("pool", self.mergedT[3072 + h * 128:3072 + (h + 1) * 128, j0 * 128:(j0 + NG) * 128], mT[gi][:, :NG * 128],
                          reads=[tmT[gi]], writes=[Tk()])
        S.barrier()

    def dma_chunks(self, q, out3, in3, n, tk, step=8, reads=()):
        for a in range(0, n, step):
            b = min(n, a + step)
            self.S.dma(q, out3[:, a:b, :], in3[:, a:b, :], reads=list(reads), writes=[tk])

    def mix_hgrn(self, l):
        S = self.S
        T = self.T
        NCH = T // 128
        SEG = min(2048, T)
        NSEG = T // SEG
        NT = SEG // 128
        BK = 64
        NBPT = 128 // BK
        NBLK = SEG // BK
        PW = min(512, T)
        b0 = l * 200
        with contextlib.ExitStack() as st:
            mbd = self.sb(st, "hg_mbd", [128, 2, 128], F32)
            oml = self.sb(st, "hg_oml", [128, 8], F32)
            gcol = self.sb(st, "hg_gcol", [128, 8], F32)
            one = self.sb(st, "hg_one", [128, 1], F32)
            tcst = Tk()
            S.dma("sp", mbd[:], self.mbd_in, writes=[tcst])
            S.op("dve", lambda e: e.memset(one[:], 1.0), writes=[tcst])
            if l == 0:
                S.op("dve", lambda e: e.memset(oml[:], 1.0), writes=[tcst])
            else:
                S.op("dve", lambda e: e.tensor_tensor(out=oml[:], in0=self.pp[:, b0 + 176:b0 + 184], in1=self.pp[:, b0 + 184:b0 + 192], op=ALU.subtract),
                     reads=[self.t_pp], writes=[tcst])
                S.op("act", lambda e: e.activation(out=oml[:], in_=oml[:], func=AF.Sigmoid), reads=[tcst], writes=[tcst])
            S.op("dve", lambda e: e.tensor_tensor(out=gcol[:], in0=self.pp[:, b0 + 160:b0 + 168], in1=self.pp[:, b0 + 128 + 8:b0 + 128 + 16], op=ALU.mult),
                 reads=[self.t_pp], writes=[tcst])
            oT = self.sb(st, "hg_oT", [128, T], F32)
            vf = self.sb(st, "hg_vf", [128, T], F32)
            vb = self.sb(st, "hg_vb", [128, NCH, 128], BF16)
            toT, tvb, thg = Tk(), Tk(), Tk()
            z = self.sb(st, "hg_z", [128, SEG], F32)
            q = self.sb(st, "hg_q", [128, SEG], F32)
            kk = self.sb(st, "hg_kk", [128, SEG], F32)
            ba = self.sb(st, "hg_ba", [128, SEG], F32)
            bb = self.sb(st, "hg_bb", [128, SEG], F32)
            eb = self.sb(st, "hg_eb", [128, SEG], F32)
            enb = self.sb(st, "hg_enb", [128, SEG], F32)
            Qt = self.sb(st, "hg_Qt", [128, SEG], F32)
            Kt = self.sb(st, "hg_Kt", [128, SEG], F32)
            KpT = self.sb(st, "hg_KpT", [128, SEG], BF16)
            Kp = self.sb(st, "hg_Kp", [128, NT, 128], BF16)
            Kpz = self.sb(st, "hg_Kpz", [128, NT, 128], BF16)
            dec = self.sb(st, "hg_dec", [128, NBLK], F32)
            tz, tq, tkk, tba, tbb, teb, tenb, tQt, tKt, tKpT, tKp, tdec = (Tk() for _ in range(12))
            NS = 8
            Sr = [self.sb(st, "hg_Sr%d" % i, [128, 128], F32) for i in range(NS)]
            tSr = [Tk() for _ in range(NS)]
            nblk = [0]
            PT = [self.sb(st, "hg_PT%d" % i, [128, 128], BF16) for i in range(2)]
            tPT = [Tk(), Tk()]
            sqp = [self.sb(st, "hg_sq%d" % i, [128, PW], BF16) for i in range(2)]
            rsp = [self.sb(st, "hg_rs%d" % i, [128, PW], F32) for i in range(2)]
            t1p = [self.sb(st, "hg_t1%d" % i, [128, PW], F32) for i in range(2)]
            mbp = [self.sb(st, "hg_mb%d" % i, [128, PW], BF16) for i in range(2)]
            tsqp, trsp, tt1p, tmbp = ([Tk(), Tk()] for _ in range(4))
            bA = [self.banks[0], self.banks[1]]
            bO = [self.banks[2], self.banks[3]]
            bR = [self.banks[4], self.banks[5]]
            nR = 0
            v3 = lambda t_: t_[:].rearrange("p (b c) -> p b c", c=BK)
            for h in range(8):
                self.dma_chunks("sp", vf[:].rearrange("p (n d) -> p n d", d=128),
                                self.Y_hi[:, h * 128:(h + 1) * 128].rearrange("(n p) d -> p n d", p=128), NCH, thg)
                S.op("pool", lambda e: e.tensor_copy(out=vb[:], in_=vf[:].rearrange("p (n d) -> p n d", d=128)), reads=[thg], writes=[tvb])
                for d in range(2):
                    nblk[0] = 0
                    S.op("pool", lambda e: e.memset(Sr[0][:], 0.0), writes=[tSr[0]])
                    segs = range(NSEG) if d == 0 else range(NSEG - 1, -1, -1)
                    for sg_ in segs:
                        scol = slice(sg_ * SEG, (sg_ + 1) * SEG)
                        zr = 1024 * (1 + d) + h * 128
                        S.dma("sp", z[:], self.YT[zr:zr + 128, scol], writes=[tz])
                        S.dma("sp", q[:], self.YT[h * 128:(h + 1) * 128, scol], writes=[tq])
                        S.op("act", lambda e: e.activation(out=z[:], in_=z[:], func=AF.Sigmoid, scale=-1.0), reads=[tz], writes=[tz])
                        S.op("dve", lambda e: e.tensor_scalar(out=kk[:], in0=z[:], scalar1=oml[:, h:h + 1], scalar2=1.0, op0=ALU.mult, op1=ALU.mult),
                             reads=[tz, tcst], writes=[tkk])
                        S.op("act", lambda e: e.activation(out=ba[:], in_=kk[:], func=AF.Ln, scale=-1.0, bias=one[:, 0:1]),
                             reads=[tkk, tcst], writes=[tba])
                        cur, nxt, tcur, tnxt = ba, bb, tba, tbb
                        for sh in [1 << k_ for k_ in range(6) if (1 << k_) < BK]:
                            c3, n3 = v3(cur), v3(nxt)
                            if d == 0:
                                S.op("dve", lambda e: e.tensor_tensor(out=n3[:, :, sh:], in0=c3[:, :, sh:], in1=c3[:, :, :BK - sh], op=ALU.add),
                                     reads=[tcur], writes=[tnxt])
                                S.op("dve", lambda e: e.tensor_copy(out=n3[:, :, :sh], in_=c3[:, :, :sh]), reads=[tcur], writes=[tnxt])
                            else:
                                S.op("dve", lambda e: e.tensor_tensor(out=n3[:, :, :BK - sh], in0=c3[:, :, :BK - sh], in1=c3[:, :, sh:], op=ALU.add),
                                     reads=[tcur], writes=[tnxt])
                                S.op("dve", lambda e: e.tensor_copy(out=n3[:, :, BK - sh:], in_=c3[:, :, BK - sh:]), reads=[tcur], writes=[tnxt])
                            cur, nxt, tcur, tnxt = nxt, cur, tnxt, tcur
                        S.op("dve", lambda e: e.tensor_scalar(out=cur[:], in0=cur[:], scalar1=-80.0, scalar2=0.0, op0=ALU.max, op1=ALU.add),
                             reads=[tcur], writes=[tcur])
                        S.op("act", lambda e: e.activation(out=eb[:], in_=cur[:], func=AF.Exp), reads=[tcur], writes=[teb])
                        S.op("act", lambda e: e.activation(out=enb[:], in_=cur[:], func=AF.Exp, scale=-1.0), reads=[tcur], writes=[tenb])
                        S.op("dve", lambda e: e.tensor_copy(out=dec[:], in_=v3(eb)[:, :, (BK - 1 if d == 0 else 0)]), reads=[teb], writes=[tdec])
                        S.op("act", lambda e: e.activation(out=q[:], in_=q[:], func=AF.Silu), reads=[tq], writes=[tq])
                        S.op("dve", lambda e: e.scalar_tensor_tensor(out=Qt[:], in0=q[:], scalar=128.0 ** -0.5, in1=eb[:], op0=ALU.mult, op1=ALU.mult),
                             reads=[tq, teb], writes=[tQt])
                        S.op("pool", lambda e: e.tensor_tensor(out=Kt[:], in0=kk[:], in1=enb[:], op=ALU.mult), reads=[tkk, tenb], writes=[tKt])
                        S.op("dve", lambda e: e.tensor_tensor(out=v3(KpT), in0=v3(Kt), in1=dec[:].unsqueeze(2).to_broadcast([128, NBLK, BK]), op=ALU.mult),
                             reads=[tKt, tdec], writes=[tKpT])
                        for j0 in range(0, NT, 8):
                            pb, tpb = self.bbanks[(j0 // 8) % 2]
                            n8 = min(8, NT - j0)
                            for jj in range(n8):
                                jt = j0 + jj
                                S.op("pe", lambda e: e.transpose(pb[:, jj * 128:(jj + 1) * 128], KpT[:, jt * 128:(jt + 1) * 128], self.idb[:]),
                                     reads=[tKpT, self.t_c], writes=[tpb])
                            S.op("act", lambda e: e.activation(out=Kp[:, j0:j0 + n8, :], in_=pb[:, :n8 * 128].rearrange("p (n d) -> p n d", d=128), func=AF.Copy),
                                 reads=[tpb], writes=[tKp])
                        tl = list(range(NT)) if d == 0 else list(range(NT - 1, -1, -1))
                        blks = list(range(NBPT)) if d == 0 else list(range(NBPT - 1, -1, -1))
                        slots = {}

                        def stage1(jt):
                            jg = sg_ * NT + jt
                            cols = slice(jt * 128, (jt + 1) * 128)
                            ba_, tba_ = bA[jt % 2]
                            rc = (jt % 2) * 128
                            S.op("pe", lambda e: e.matmul(ba_[:, 0:128], Kt[:, cols], Qt[:, cols], start=True, stop=True), reads=[tKt, tQt], writes=[tba_])
                            for ib in blks:
                                br_, tbr_ = bR[ib % 2]
                                S.op("pe", lambda e: e.matmul(br_[:, rc:rc + 128], Kp[ib * BK:(ib + 1) * BK, jt, :], vb[ib * BK:(ib + 1) * BK, jg, :], start=True, stop=True),
                                     reads=[tKp, tvb], writes=[tbr_])
                            p = jt % 2
                            S.op("dve", lambda e: e.tensor_tensor(out=PT[p][:], in0=ba_[:, 0:128], in1=mbd[:, d, :], op=ALU.mult),
                                 reads=[tba_, tcst], writes=[tPT[p]])
                            for ib in blks:
                                br_, tbr_ = bR[ib % 2]
                                n = nblk[0]
                                nblk[0] += 1
                                s0, s1 = n % NS, (n + 1) % NS
                                slots[(jt, ib)] = s0
                                blk = jt * NBPT + ib
                                S.op("dve", lambda e: e.scalar_tensor_tensor(out=Sr[s1][:], in0=Sr[s0][:], scalar=dec[:, blk:blk + 1],
                                                                            in1=br_[:, rc:rc + 128], op0=ALU.mult, op1=ALU.add),
                                     reads=[tSr[s0], tbr_, tdec], writes=[tSr[s1]])

                        def stage2(jt):
                            jg = sg_ * NT + jt
                            gcols = slice(jg * 128, (jg + 1) * 128)
                            bo_, tbo_ = bO[jt % 2]
                            p = jt % 2
                            S.op("pe", lambda e: e.matmul(bo_[:, 0:128], vb[:, jg, :], PT[p][:], start=True, stop=True), reads=[tvb, tPT[p]], writes=[tbo_])
                            for ib in blks:
                                c32 = slice(jt * 128 + ib * BK, jt * 128 + (ib + 1) * BK)
                                s0 = slots[(jt, ib)]
                                S.op("pe", lambda e: e.matmul(bo_[:, 128 + ib * BK:128 + (ib + 1) * BK], Sr[s0][:], Qt[:, c32], start=True, stop=True),
                                     reads=[tSr[s0], tQt], writes=[tbo_])
                            if d == 0:
                                S.op("act", lambda e: e.activation(out=oT[:, gcols], in_=bo_[:, 0:128], func=AF.Copy), reads=[tbo_], writes=[toT])
                            else:
                                S.op("dve", lambda e: e.tensor_tensor(out=oT[:, gcols], in0=bo_[:, 0:128], in1=oT[:, gcols], op=ALU.add), reads=[tbo_], writes=[toT])
                            S.op("dve", lambda e: e.tensor_tensor(out=oT[:, gcols], in0=bo_[:, 128:256], in1=oT[:, gcols], op=ALU.add), reads=[tbo_], writes=[toT])

                        stage1(tl[0])
                        for ti in range(NT):
                            if ti + 1 < NT:
                                stage1(tl[ti + 1])
                            stage2(tl[ti])
                S.dma("sp", vf[:], self.YT[3072 + h * 128:3072 + (h + 1) * 128, :], reads=[tvb], writes=[thg])
                S.op("act", lambda e: e.activation(out=vf[:], in_=vf[:], func=AF.Silu), reads=[thg], writes=[thg])
                for pc in range(T // PW):
                    i = pc % 2
                    pcs = slice(pc * PW, (pc + 1) * PW)
                    bk, tbk = bA[pc % 2]
                    S.op("pool", lambda e: e.tensor_tensor(out=sqp[i][:], in0=oT[:, pcs], in1=oT[:, pcs], op=ALU.mult), reads=[toT], writes=[tsqp[i]])
                    S.op("pe", lambda e: e.matmul(bk[:, :PW], self.onesb[:], sqp[i][:], start=True, stop=True), reads=[tsqp[i], self.t_c], writes=[tbk])
                    S.op("act", lambda e: e.activation(out=rsp[i][:], in_=bk[:, :PW], func=AF.Sqrt, bias=self.epsln[:, 1:2], scale=1.0 / 128),
                         reads=[tbk, self.t_c], writes=[trsp[i]])
                    S.op("dve", lambda e: e.reciprocal(out=rsp[i][:], in_=rsp[i][:]), reads=[trsp[i]], writes=[trsp[i]])
                    S.op("dve", lambda e: e.tensor_tensor(out=t1p[i][:], in0=oT[:, pcs], in1=rsp[i][:], op=ALU.mult), reads=[toT, trsp[i]], writes=[tt1p[i]])
                    S.op("pool", lambda e: e.tensor_tensor(out=t1p[i][:], in0=t1p[i][:], in1=vf[:, pcs], op=ALU.mult), reads=[thg], writes=[tt1p[i]])
                    S.op("act", lambda e: e.activation(out=mbp[i][:], in_=t1p[i][:], func=AF.Copy, scale=gcol[:, h:h + 1]), reads=[tt1p[i], tcst], writes=[tmbp[i]])
                    S.dma("pool", self.mergedT[1024 + h * 128:1024 + (h + 1) * 128, pcs], mbp[i][:], reads=[tmbp[i]], writes=[Tk()])
        S.barrier()


_CACHE = {}


def _consts(T):
    f64 = np.float64
    n_rows = T // 64
    rows = np.repeat(np.arange(n_rows, dtype=np.float32), 64)
    cols = np.tile(np.arange(64, dtype=np.float32), n_rows)
    inv_freq = (np.float32(10000.0) ** (-np.arange(32, dtype=np.float32) / np.float32(32))).astype(np.float32)
    ang = np.concatenate([rows[:, None] * inv_freq, cols[:, None] * inv_freq], axis=-1).astype(np.float32)
    cos16 = np.ascontiguousarray(np.tile(np.cos(ang).astype(np.float32), (1, 16)))
    sin16 = np.ascontiguousarray(np.tile(np.sin(ang).astype(np.float32), (1, 16)))
    lg = np.log1p(-np.exp2(-5.0 - np.arange(8, dtype=f64)))
    pos = np.arange(128, dtype=f64)
    sc = 128.0 ** -0.5
    rdec = np.zeros((128, 32), f64)
    for h in range(8):
        rdec[:, h] = np.exp((127.0 - pos) * lg[h]) * sc
        rdec[:, 8 + h] = np.exp(pos * lg[h]) * sc
        rdec[:, 16 + h] = np.exp((pos + 1.0) * lg[h])
        rdec[:, 24 + h] = np.exp((128.0 - pos) * lg[h])
    dsym = np.zeros((128, 8, 128), f64)
    ad = np.abs(pos[:, None] - pos[None, :])
    for h in range(8):
        dsym[:, h, :] = np.exp(ad * lg[h]) * sc
    blk = np.arange(128) // 64
    same = blk[:, None] == blk[None, :]
    s_i = np.arange(128)[:, None]
    t_i = np.arange(128)[None, :]
    mbd = np.zeros((128, 2, 128), np.float32)
    mbd[:, 0, :] = (same & (s_i <= t_i))
    mbd[:, 1, :] = (same & (s_i >= t_i))
    sel = np.zeros((8, 8, 128), np.float32)
    for e in range(8):
        sel[e, e, :] = 1.0
    return dict(cos16=cos16, sin16=sin16, rdec=rdec.astype(np.float32), dsymT=dsym.astype(np.float32), maskBD=mbd, sel=sel)


def _params(inp):
    f = lambda a: np.asarray(a, dtype=np.float32)
    pp = np.zeros((128, 400), np.float32)
    rp = np.zeros((2, 128, 4352), np.float32)
    for l in range(2):
        b = l * 200
        pp[:, b + 0:b + 32] = f(inp["ln1_g"])[l].reshape(32, 128).T
        pp[:, b + 32:b + 64] = f(inp["ln1_b"])[l].reshape(32, 128).T
        pp[:, b + 64:b + 96] = f(inp["ln2_g"])[l].reshape(32, 128).T
        pp[:, b + 96:b + 128] = f(inp["ln2_b"])[l].reshape(32, 128).T
        pp[:, b + 128:b + 160] = f(inp["merge_scale"])[l].reshape(32, 128).T
        pp[:, b + 160:b + 168] = f(inp["hgrn_out_norm"])[l].reshape(8, 128).T
        pp[:, b + 168:b + 176] = f(inp["ret_out_norm"])[l].reshape(8, 128).T
        pp[:, b + 176:b + 184] = f(inp["hgrn_lower_bound"])[0].reshape(8, 128).T
        pp[:, b + 184:b + 192] = f(inp["hgrn_lower_bound"])[1].reshape(8, 128).T
        row = np.concatenate([np.tile(f(inp["attn_q_norm"])[l], 8), np.tile(f(inp["attn_k_norm"])[l], 2),
                              f(inp["gmlp_v_norm_g"])[l], f(inp["gmlp_v_norm_b"])[l], f(inp["merge_scale"])[l][2048:3072]])
        rp[l] = np.broadcast_to(row[None, :], (128, 4352))
    wsT = np.ascontiguousarray(f(inp["gmlp_w_s"]).transpose(0, 3, 1, 2))
    bsT = np.ascontiguousarray(f(inp["gmlp_b_s"]).transpose(0, 2, 1))
    router = np.ascontiguousarray(f(inp["moe_router"])[0].reshape(32, 128, 8).transpose(1, 0, 2))
    return dict(
        pp=pp, rp=rp, wsT=wsT, bsT=bsT, router=router,
        w_in=f(inp["w_in"]), w_o=f(inp["w_o"]),
        ffn_g=f(inp["ffn_w_gate"])[0], ffn_u=f(inp["ffn_w_up"])[0], ffn_d=f(inp["ffn_w_down"])[0],
        moe_g=f(inp["moe_w_gate"])[0], moe_u=f(inp["moe_w_up"])[0],
        moe_d=f(inp["moe_w_down"])[0].reshape(NE * DFE, D),
    )


def kernel(**inputs):
    T = 8192
    if T not in _CACHE:
        _CACHE[T] = Prog(T)
    prog = _CACHE[T]
    shared = _params(inputs)
    shared.update(_consts(T))
    xp = np.asarray(inputs["x_prompt"], dtype=np.float32)
    xs = np.asarray(inputs["x_sample"], dtype=np.float32)
    seqs = [xp[0], xp[1], xs[0]]
    in_maps = []
    for c in range(NCORES):
        m = dict(shared)
        m["xT"] = np.ascontiguousarray(seqs[c].T)
        in_maps.append(m)
    res = run_bass_kernel_spmd(prog.nc, in_maps, core_ids=list(range(NCORES)))
    outs = [np.ascontiguousarray(np.asarray(res.results[c]["yT"], dtype=np.float32).T) for c in range(NCORES)]
    y_prompt = np.stack([outs[0], outs[1]], axis=0)
    y_sample = outs[2][None]
    return (y_prompt, y_sample)
```
